# Optimizing a Trainium2 kernel written in Bass

```python
import jax, jax.numpy as jnp
from jax import lax
import numpy as np

D_MODEL = 1024
BATCH = 8
SEQ = 4096
DEPTH = 2

GRID_W = 64
CTX_LEN = 256
EPS = 1e-6
N_Q_HEADS = 8
N_KV_HEADS = 2
HEAD_DIM = 64
Q_PER_KV = N_Q_HEADS // N_KV_HEADS
ATTN_WIDTH = N_Q_HEADS * HEAD_DIM
KV_WIDTH = N_KV_HEADS * HEAD_DIM
Q_BLOCK = 128
ROPE_THETA = 10000.0
POOL_WINDOWS = (2, 4, 8, 16)
POOL_GROUP = 128
POOL_WIDTH = POOL_GROUP * len(POOL_WINDOWS)
SG_GROUPS = 4
SG_GROUP_WIDTH = 128
SG_CHUNK = 128
SG_WIDTH = SG_GROUPS * SG_GROUP_WIDTH
CONV_WIDTH = 512
CONV_K = 3
EVEN_IN = ATTN_WIDTH + 2 * KV_WIDTH + POOL_WIDTH
ODD_IN = 2 * SG_WIDTH + 3 * CONV_WIDTH
MIX_WIDTH = ATTN_WIDTH + POOL_WIDTH
N_GROUPS = 4
EXPERTS_PER_GROUP = 4
N_EXPERTS = N_GROUPS * EXPERTS_PER_GROUP
TOP_K = 2
D_EXPERT = 256

kernel_name = 'hybrid_diffusion_attn_pool_sgmlp_conv_hmoe'


def rms_norm(x, gain):
    x32 = x.astype(jnp.float32)
    y = x32 * lax.rsqrt(jnp.mean(x32 * x32, axis=-1, keepdims=True) + EPS)
    return (y * gain.astype(jnp.float32)).astype(x.dtype)


def modulate(x, gain, shift, scale):
    return rms_norm(x, gain) * (1 + scale) + shift


def adaln(cond, mod_w, mod_b):
    m = jax.nn.silu(cond) @ mod_w + mod_b
    return jnp.split(m, 6, axis=-1)


def axial_angles(n_rows):
    row = jnp.repeat(jnp.arange(n_rows, dtype=jnp.float32), GRID_W)
    col = jnp.tile(jnp.arange(GRID_W, dtype=jnp.float32), n_rows)
    half = HEAD_DIM // 2
    inv = ROPE_THETA ** (-jnp.arange(0, half, 2, dtype=jnp.float32) / half)
    return row[:, None] * inv, col[:, None] * inv


def rope_1d(x, ang):
    cos = jnp.cos(ang)[None, :, None, :]
    sin = jnp.sin(ang)[None, :, None, :]
    x1, x2 = jnp.split(x, 2, axis=-1)
    return jnp.concatenate([x1 * cos - x2 * sin, x2 * cos + x1 * sin], axis=-1)


def rope_2d(x, row_ang, col_ang):
    x32 = x.astype(jnp.float32)
    xr, xc = jnp.split(x32, 2, axis=-1)
    return jnp.concatenate([rope_1d(xr, row_ang), rope_1d(xc, col_ang)], axis=-1).astype(x.dtype)


def gqa_attend(q, k, v):
    s = jnp.einsum('bqkgd,bskd->bkgqs', q, k, preferred_element_type=jnp.float32) * (HEAD_DIM ** -0.5)
    p = jax.nn.softmax(s, axis=-1).astype(v.dtype)
    return jnp.einsum('bkgqs,bskd->bqkgd', p, v)


def multiscale_pool(p, pool_w, pool_scale):
    bsz, length, _ = p.shape
    p32 = p.astype(jnp.float32)
    cs = jnp.concatenate([jnp.zeros_like(p32[:, :1]), jnp.cumsum(p32, axis=1)], axis=1)
    t = jnp.arange(length)
    diffs = []
    for g, w in enumerate(POOL_WINDOWS):
        lo = jnp.clip(t - w // 2, 0, length)
        hi = jnp.clip(t + w - w // 2, 0, length)
        sl = slice(g * POOL_GROUP, (g + 1) * POOL_GROUP)
        csg = cs[..., sl]
        mean = (csg[:, hi] - csg[:, lo]) / (hi - lo).astype(jnp.float32)[None, :, None]
        diffs.append(mean - p32[..., sl])
    d = jnp.stack(diffs, axis=2).astype(p.dtype)
    y = jnp.einsum('blgc,gcd->blgd', d, pool_w).reshape(bsz, length, POOL_WIDTH)
    return y * pool_scale


def even_mixer(h, hc, w_in, q_gain, k_gain, pool_w, pool_scale, w_out, row_ang, col_ang, need_ctx_out):
    bsz, seq, _ = h.shape
    n_ctx = hc.shape[1]
    q, k, v, p = jnp.split(h @ w_in, [ATTN_WIDTH, ATTN_WIDTH + KV_WIDTH, ATTN_WIDTH + 2 * KV_WIDTH], axis=-1)
    q = rope_2d(rms_norm(q.reshape(bsz, seq, N_Q_HEADS, HEAD_DIM), q_gain), row_ang, col_ang)
    k = rope_2d(rms_norm(k.reshape(bsz, seq, N_KV_HEADS, HEAD_DIM), k_gain), row_ang, col_ang)
    v = v.reshape(bsz, seq, N_KV_HEADS, HEAD_DIM)
    if need_ctx_out:
        qc, kc, vc, pc = jnp.split(hc @ w_in, [ATTN_WIDTH, ATTN_WIDTH + KV_WIDTH, ATTN_WIDTH + 2 * KV_WIDTH], axis=-1)
    else:
        kc, vc = jnp.split(hc @ w_in[:, ATTN_WIDTH:ATTN_WIDTH + 2 * KV_WIDTH], [KV_WIDTH], axis=-1)
    kc = rms_norm(kc.reshape(bsz, n_ctx, N_KV_HEADS, HEAD_DIM), k_gain)
    vc = vc.reshape(bsz, n_ctx, N_KV_HEADS, HEAD_DIM)
    k_all = jnp.concatenate([kc, k], axis=1)
    v_all = jnp.concatenate([vc, v], axis=1)
    n_blk = seq // Q_BLOCK
    qb = q.reshape(bsz, n_blk, Q_BLOCK, N_KV_HEADS, Q_PER_KV, HEAD_DIM).transpose(1, 0, 2, 3, 4, 5)
    o = lax.map(lambda qq: gqa_attend(qq, k_all, v_all), qb)
    o = o.transpose(1, 0, 2, 3, 4, 5).reshape(bsz, seq, ATTN_WIDTH)
    y = jnp.concatenate([o, multiscale_pool(p, pool_w, pool_scale)], axis=-1) @ w_out
    if need_ctx_out:
        qc = rms_norm(qc.reshape(bsz, n_ctx, N_Q_HEADS, HEAD_DIM), q_gain).reshape(bsz, n_ctx, N_KV_HEADS, Q_PER_KV, HEAD_DIM)
        oc = gqa_attend(qc, kc, vc).reshape(bsz, n_ctx, ATTN_WIDTH)
        yc = jnp.concatenate([oc, multiscale_pool(pc, pool_w, pool_scale)], axis=-1) @ w_out
    else:
        yc = None
    return y, yc


def odd_mixer(h, w_in, sg_gain, sg_w, sg_b, conv_w, w_out):
    bsz, length, _ = h.shape
    u, v, hx, bg, cg = jnp.split(h @ w_in, [SG_WIDTH, 2 * SG_WIDTH, 2 * SG_WIDTH + CONV_WIDTH, 2 * SG_WIDTH + 2 * CONV_WIDTH], axis=-1)
    v = rms_norm(v.reshape(bsz, length // SG_CHUNK, SG_CHUNK, SG_GROUPS, SG_GROUP_WIDTH), sg_gain)
    s = jnp.einsum('gpq,bnqgc->bnpgc', sg_w, v) + sg_b.T[:, :, None]
    y_c = u * s.reshape(bsz, length, SG_WIDTH)
    z = cg * hx
    zc = lax.conv_general_dilated(z, conv_w, window_strides=(1,), padding=((CONV_K // 2, CONV_K // 2),),
                                  dimension_numbers=('NWC', 'WIO', 'NWC'), feature_group_count=CONV_WIDTH)
    y_d = bg * zc
    return jnp.concatenate([y_c, y_d], axis=-1) @ w_out


def hier_moe(h, rg_w, rg_b, re_w, re_b, w_gate, w_up, w_down):
    shape = h.shape
    t = h.reshape(-1, shape[-1])
    t32 = t.astype(jnp.float32)
    n_tok = t.shape[0]
    g_prob = jax.nn.softmax(t32 @ rg_w.astype(jnp.float32) + rg_b.astype(jnp.float32), axis=-1)
    g_p, g_idx = lax.top_k(g_prob, 1)
    e_logits = (t32 @ re_w.astype(jnp.float32) + re_b.astype(jnp.float32)).reshape(n_tok, N_GROUPS, EXPERTS_PER_GROUP)
    e_sel = jnp.take_along_axis(e_logits, g_idx[:, :, None], axis=1)[:, 0]
    top_p, top_i = lax.top_k(jax.nn.softmax(e_sel, axis=-1), TOP_K)
    top_p = top_p / jnp.sum(top_p, axis=-1, keepdims=True)
    weights = g_p * top_p
    expert_id = g_idx * EXPERTS_PER_GROUP + top_i
    comb = jnp.sum(jax.nn.one_hot(expert_id, N_EXPERTS, dtype=jnp.float32) * weights[..., None], axis=1)

    def step(acc, xs):
        wg, wu, wd, cw = xs
        y = (jax.nn.silu(t @ wg) * (t @ wu)) @ wd
        return acc + y.astype(jnp.float32) * cw[:, None], None

    acc, _ = lax.scan(step, jnp.zeros(t.shape, jnp.float32), (w_gate, w_up, w_down, comb.T))
    return acc.astype(h.dtype).reshape(shape)


def setup_inputs(seed: int = 0) -> dict:
    key = jax.random.key(seed)
    ks = jax.random.split(key, 32)
    n_even = (DEPTH + 1) // 2
    n_odd = DEPTH // 2
    f32 = jnp.float32

    def nrm(k, shape, scale):
        return jax.random.normal(k, shape, f32) * scale

    def gain(k, shape, noise=0.05):
        return 1.0 + nrm(k, shape, noise)

    return {
        'x': nrm(ks[0], (BATCH, SEQ, D_MODEL), 1.0),
        'c': nrm(ks[1], (BATCH, D_MODEL), 1.0),
        'ctx': nrm(ks[2], (BATCH, CTX_LEN, D_MODEL), 1.0),
        'c_ctx': nrm(ks[3], (D_MODEL,), 1.0),
        'mod_w': nrm(ks[4], (DEPTH, D_MODEL, 6 * D_MODEL), 0.5 * D_MODEL ** -0.5),
        'mod_b': nrm(ks[5], (DEPTH, 6 * D_MODEL), 0.02),
        'norm1_g': gain(ks[6], (DEPTH, D_MODEL)),
        'norm2_g': gain(ks[7], (DEPTH, D_MODEL)),
        'even_w_in': nrm(ks[8], (n_even, D_MODEL, EVEN_IN), D_MODEL ** -0.5),
        'q_gain': gain(ks[9], (n_even, HEAD_DIM)),
        'k_gain': gain(ks[10], (n_even, HEAD_DIM)),
        'pool_w': nrm(ks[11], (n_even, len(POOL_WINDOWS), POOL_GROUP, POOL_GROUP), POOL_GROUP ** -0.5),
        'pool_scale': gain(ks[12], (n_even, POOL_WIDTH), 0.1),
        'even_w_out': nrm(ks[13], (n_even, MIX_WIDTH, D_MODEL), MIX_WIDTH ** -0.5),
        'odd_w_in': nrm(ks[14], (n_odd, D_MODEL, ODD_IN), D_MODEL ** -0.5),
        'sg_gain': gain(ks[15], (n_odd, SG_GROUPS, SG_GROUP_WIDTH)),
        'sg_w': nrm(ks[16], (n_odd, SG_GROUPS, SG_CHUNK, SG_CHUNK), SG_CHUNK ** -0.5),
        'sg_b': gain(ks[17], (n_odd, SG_GROUPS, SG_CHUNK), 0.1),
        'conv_w': nrm(ks[18], (n_odd, CONV_K, 1, CONV_WIDTH), CONV_K ** -0.5),
        'odd_w_out': nrm(ks[19], (n_odd, MIX_WIDTH, D_MODEL), MIX_WIDTH ** -0.5),
        'router_g_w': nrm(ks[20], (DEPTH, D_MODEL, N_GROUPS), D_MODEL ** -0.5),
        'router_g_b': nrm(ks[21], (DEPTH, N_GROUPS), 0.01),
        'router_e_w': nrm(ks[22], (DEPTH, D_MODEL, N_EXPERTS), D_MODEL ** -0.5),
        'router_e_b': nrm(ks[23], (DEPTH, N_EXPERTS), 0.01),
        'w_gate': nrm(ks[24], (DEPTH, N_EXPERTS, D_MODEL, D_EXPERT), D_MODEL ** -0.5),
        'w_up': nrm(ks[25], (DEPTH, N_EXPERTS, D_MODEL, D_EXPERT), D_MODEL ** -0.5),
        'w_down': nrm(ks[26], (DEPTH, N_EXPERTS, D_EXPERT, D_MODEL), D_EXPERT ** -0.5),
    }


def reference(x, c, ctx, c_ctx, mod_w, mod_b, norm1_g, norm2_g, even_w_in, q_gain, k_gain, pool_w, pool_scale,
              even_w_out, odd_w_in, sg_gain, sg_w, sg_b, conv_w, odd_w_out, router_g_w, router_g_b,
              router_e_w, router_e_b, w_gate, w_up, w_down):
    seq = x.shape[1]
    ROWS = seq // GRID_W
    row_ang, col_ang = axial_angles(ROWS)
    last_even = 2 * ((DEPTH - 1) // 2)
    h_lat, h_ctx = x, ctx
    for i in range(DEPTH):
        j = i // 2
        need_ctx_in = i <= last_even
        need_ctx_out = i < last_even
        sh1, sc1, g1, sh2, sc2, g2 = [m[:, None, :] for m in adaln(c, mod_w[i], mod_b[i])]
        a = modulate(h_lat, norm1_g[i], sh1, sc1)
        if need_ctx_in:
            csh1, csc1, cg1, csh2, csc2, cg2 = adaln(c_ctx, mod_w[i], mod_b[i])
            ac = modulate(h_ctx, norm1_g[i], csh1, csc1)
        if i % 2 == 0:
            y, yc = even_mixer(a, ac, even_w_in[j], q_gain[j], k_gain[j], pool_w[j], pool_scale[j],
                               even_w_out[j], row_ang, col_ang, need_ctx_out)
        else:
            y = odd_mixer(a, odd_w_in[j], sg_gain[j], sg_w[j], sg_b[j], conv_w[j], odd_w_out[j])
            yc = odd_mixer(ac, odd_w_in[j], sg_gain[j], sg_w[j], sg_b[j], conv_w[j], odd_w_out[j]) if need_ctx_out else None
        moe_args = (router_g_w[i], router_g_b[i], router_e_w[i], router_e_b[i], w_gate[i], w_up[i], w_down[i])
        h_lat = h_lat + g1 * y
        h_lat = h_lat + g2 * hier_moe(modulate(h_lat, norm2_g[i], sh2, sc2), *moe_args)
        if need_ctx_out:
            h_ctx = h_ctx + cg1 * yc
            h_ctx = h_ctx + cg2 * hier_moe(modulate(h_ctx, norm2_g[i], csh2, csc2), *moe_args)
    return h_lat
```

```python
import numpy as np
from contextlib import ExitStack
import concourse.bass as bass
import concourse.mybir as mybir
from concourse.bass_utils import run_bass_kernel_spmd

F32 = mybir.dt.float32
BF16 = mybir.dt.bfloat16
AF = mybir.ActivationFunctionType
ALU = mybir.AluOpType
AX = mybir.AxisListType

ENGS = ["pe", "act", "dve", "pool", "sp"]
S = 4096
D = 1024
NCORES = 8
EPS = 1e-6
_CNT = [0]


class Op:
    __slots__ = ("eng", "fn", "reads", "writes", "dma_key", "idx", "waits", "signal", "semval")

    def __init__(self, eng, fn, reads, writes, dma_key):
        self.eng = eng
        self.fn = fn
        self.reads = reads
        self.writes = writes
        self.dma_key = dma_key
        self.waits = []
        self.signal = False
        self.semval = 0


class Prog:
    def __init__(self, nc):
        self.nc = nc
        self.ops = []

    def op(self, eng, fn, reads=(), writes=(), dma_key=None):
        reads = tuple(reads)
        writes = tuple(writes)
        ex = tuple(r for r in reads if r.startswith("ps"))
        o = Op(eng, fn, reads, writes + ex, dma_key)
        o.idx = len(self.ops)
        self.ops.append(o)
        return o

    def pe(self, fn, reads=(), writes=()):
        return self.op("pe", fn, reads, writes)

    def act(self, fn, reads=(), writes=()):
        return self.op("act", fn, reads, writes)

    def dve(self, fn, reads=(), writes=()):
        return self.op("dve", fn, reads, writes)

    def pool(self, fn, reads=(), writes=()):
        return self.op("pool", fn, reads, writes)

    def dma(self, eng, key, fn, reads=(), writes=()):
        return self.op(eng, fn, reads, writes, dma_key=key)

    def finalize(self):
        ops = self.ops

        def tl(o):
            return ("dma", o.dma_key) if o.dma_key is not None else o.eng

        pos = {}
        cnt = {}
        for o in ops:
            t = tl(o)
            cnt[t] = cnt.get(t, 0) + 1
            pos[o.idx] = cnt[t]
        last_writer = {}
        readers = {}
        known = {e: {} for e in ENGS}
        done_clock = {}
        needed = set()
        latest_on_key = {}
        for o in ops:
            deps = set()
            raw = set()
            for r in o.reads:
                w = last_writer.get(r)
                if w is not None:
                    deps.add(w)
                    raw.add(w)
            for r in o.writes:
                w = last_writer.get(r)
                if w is not None:
                    deps.add(w)
                    if r.startswith("ps"):
                        raw.add(w)
                for rd in readers.get(r, ()):
                    deps.add(rd)
            req = {}
            for d in deps:
                if d == o.idx:
                    continue
                po = ops[d]
                t = tl(po)
                if po.dma_key is None and o.dma_key is None and po.eng == o.eng:
                    if o.eng == "pe":
                        continue
                    if d not in raw:
                        continue
                if t not in req or pos[d] > pos[req[t]]:
                    req[t] = d
            kn = known[o.eng]
            for t in list(req.keys()):
                if not isinstance(t, str):
                    req[t] = latest_on_key[t]
            waits = []
            for t, d in req.items():
                if kn.get(t, 0) >= pos[d]:
                    continue
                waits.append(d)
                needed.add(d)
                for t2, p2 in done_clock[d].items():
                    if kn.get(t2, 0) < p2:
                        kn[t2] = p2
            o.waits = waits
            dc = dict(kn)
            dc[tl(o)] = max(dc.get(tl(o), 0), pos[o.idx])
            done_clock[o.idx] = dc
            if o.dma_key is not None:
                latest_on_key[tl(o)] = o.idx
            for r in o.writes:
                last_writer[r] = o.idx
                readers[r] = []
            for r in o.reads:
                if r not in o.writes:
                    readers.setdefault(r, []).append(o.idx)
        semcnt = {}
        for o in ops:
            t = tl(o)
            if o.dma_key is not None:
                semcnt[t] = semcnt.get(t, 0) + 16
                o.signal = True
                o.semval = semcnt[t]
            elif o.idx in needed:
                semcnt[t] = semcnt.get(t, 0) + 1
                o.signal = True
                o.semval = semcnt[t]
        self.timelines = sorted(set(tl(o) for o in ops if o.signal), key=str)
        self._tl = tl
        return self

    def emit(self):
        nc = self.nc
        ops = self.ops
        tl = self._tl
        with ExitStack() as es:
            sems = {}
            for i, t in enumerate(self.timelines):
                _CNT[0] += 1
                sems[t] = es.enter_context(nc.semaphore("sem%d" % _CNT[0]))
            block = es.enter_context(nc.Block())
            by_eng = {e: [o for o in ops if o.eng == e] for e in ENGS}
            final_dma = {}
            for o in ops:
                if o.dma_key is not None:
                    final_dma[tl(o)] = o.semval

            def run(engname, eng):
                for o in by_eng[engname]:
                    ws = list(o.waits)
                    att = None
                    if ws and engname != "pe":
                        att = ws.pop()
                    for d in ws:
                        po = ops[d]
                        eng.wait_ge(sems[tl(po)], po.semval)
                    if att is not None:
                        rec = _Rec(eng)
                        ins = o.fn(rec)
                        po = ops[att]
                        rec.first._wait_ge(sems[tl(po)], po.semval)
                    else:
                        ins = o.fn(eng)
                    if o.signal:
                        ins.then_inc(sems[tl(o)], 16 if o.dma_key is not None else 1)
                if engname == "sp":
                    for t, v in final_dma.items():
                        eng.wait_ge(sems[t], v)

            @block.tensor
            def _(eng):
                run("pe", eng)

            @block.scalar
            def _(eng):
                run("act", eng)

            @block.vector
            def _(eng):
                run("dve", eng)

            @block.gpsimd
            def _(eng):
                run("pool", eng)

            @block.sync
            def _(eng):
                run("sp", eng)


class _Rec:
    def __init__(self, eng):
        self._eng = eng
        self.first = None

    def __getattr__(self, name):
        f = getattr(self._eng, name)

        def g(*a, **k):
            r = f(*a, **k)
            if self.first is None:
                self.first = r
            return r
        return g


class Phase:
    def __init__(self, nc):
        self.nc = nc
        self.es = ExitStack()
        self.p = Prog(nc)
        self._n = 0

    def sb(self, shape, dt):
        self._n += 1
        _CNT[0] += 1
        return self.es.enter_context(self.nc.sbuf_tensor("sb%d" % _CNT[0], list(shape), dt))

    def psum4(self):
        r = []
        for _ in range(4):
            _CNT[0] += 1
            r.append(self.es.enter_context(self.nc.psum_tensor("ps%d" % _CNT[0], [128, 1024], F32)))
        return r

    def done(self):
        self.p.finalize()
        self.p.emit()
        self.es.close()


def build(stage=4, tail_only=False):
    nc = bass.Bass("TRN2", target_bir_lowering=False)

    def din(name, shape, dt=F32):
        return nc.dram_tensor(name, list(shape), dt, kind="ExternalInput").ap()

    def dscr(name, shape, dt):
        return nc.dram_tensor(name, list(shape), dt, kind="Internal").ap()

    x_d = din("x", [S, D])
    ctx_d = din("ctx", [256, D])
    cvec_d = din("cvec", [128, 16])
    ident_d = din("ident", [128, 128])
    modw_d = din("mod_w", [2, D, 6144])
    modb_d = din("mod_bT", [128, 96])
    ng_d = din("norm_g", [128, 32])
    wqkp_d = din("w_qkp", [D, 1792])
    wv_d = din("w_v", [D, 128])
    gains_d = din("gains", [128, 4])
    cos_d = din("cos_t", [128, S])
    sin_d = din("sin_t", [128, S])
    poolw_d = din("pool_wT", [128, 512])
    poolsc_d = din("pool_sc", [128, 4])
    pooledge_d = din("pool_edge", [128, 64])
    wout0_d = din("w_out0", [D, D])
    wout1_d = din("w_out1", [D, D])
    win1_d = din("w_in1", [D, 2560])
    sggain_d = din("sg_gain_b", [128, 512])
    sgw_d = din("sg_wT", [128, 512])
    sgb_d = din("sg_b_b", [128, 512])
    convw_d = din("conv_wT", [128, 12])
    rw_d = din("rw", [2, D, 20])
    rb_d = din("rb_b", [128, 160])
    wg_d = din("w_gate", [2, 16, D, 256])
    wu_d = din("w_up", [2, 16, D, 256])
    wd_d = din("w_down", [2, 16, 256, D])
    sel_d = din("sel", [32, 2048])
    out_d = nc.dram_tensor("out", [S, D], F32, kind="ExternalOutput").ap()

    A2_d = dscr("A2s", [8, 128, S], BF16)
    QT_d = dscr("QTs", [4, 128, S], BF16)
    MIX_d = dscr("MIXs", [8, 128, S], BF16)
    PT_d = dscr("PTs", [4, 128, S], F32)
    BG_d = dscr("BGs", [4, 128, S], F32)
    KT_d = dscr("KTs", [128, 4352], BF16)
    VS_d = dscr("VSs", [128, 34, 193], BF16)

    with ExitStack() as top:
        def psb(shape, dt):
            _CNT[0] += 1
            return top.enter_context(nc.sbuf_tensor("pt%d" % _CNT[0], list(shape), dt))

        HT = psb([128, 8, S], F32)
        ident = psb([128, 128], F32)
        ident_bf = psb([128, 128], BF16)
        ones_bf = psb([128, 128], BF16)
        onesblk_bf = psb([128, 128], BF16)
        ones_f = psb([128, 128], F32)
        MOD = psb([128, 2, 48], F32)
        MODC = psb([128, 16], F32)
        NG = psb([128, 32], F32)
        GS = psb([128, 4, 8], F32)
        GSC = psb([128, 8], F32)
        combT = psb([32, S], BF16)
        EPSB = psb([128, 1], F32)

        ph = Phase(nc)
        p = ph.p
        PS = ph.psum4()
        cvec = ph.sb([128, 16], F32)
        scv = ph.sb([128, 16], F32)
        modb = ph.sb([128, 96], F32)
        mrow = ph.sb([2, 6144], F32)
        mwbuf = [ph.sb([128, 8, 512], F32) for _ in range(2)]
        xin = [ph.sb([128, D], F32) for _ in range(2)]
        p.dma("sp", "c0", lambda e: e.dma_start(out=ident[:], in_=ident_d[:, :]), writes=["ident"])
        p.dma("sp", "c0", lambda e: e.dma_start(out=cvec[:], in_=cvec_d[:, :]), writes=["cvec"])
        p.dma("sp", "c0", lambda e: e.dma_start(out=modb[:], in_=modb_d[:, :]), writes=["modb"])
        p.dma("sp", "c0", lambda e: e.dma_start(out=NG[:], in_=ng_d[:, :]), writes=["NG"])
        p.dve(lambda e: e.tensor_copy(ident_bf[:], ident[:]), reads=["ident"], writes=["ident_bf"])
        p.dve(lambda e: e.memset(ones_bf[:], 1.0 / 1024.0), writes=["ones_bf"])
        p.dve(lambda e: e.memset(ones_f[:], 1.0), writes=["ones_f"])
        p.dve(lambda e: e.memset(EPSB[:], EPS), writes=["epsb"])
        p.dve(lambda e: e.memset(onesblk_bf[:], 0.0), writes=["onesblk0"])
        p.dve(lambda e: e.memset(onesblk_bf[0:64, 0:64], 1.0 / 64.0), reads=["onesblk0"], writes=["onesblk1"])
        p.dve(lambda e: e.memset(onesblk_bf[64:128, 64:128], 1.0 / 64.0), reads=["onesblk1"], writes=["onesblk"])
        p.act(lambda e: e.activation(scv[:], cvec[:], AF.Silu), reads=["cvec"], writes=["scv"])
        for l in range(2):
            for fb in range(12):
                it = l * 12 + fb
                buf = mwbuf[it % 2]
                bk = "mw%d" % (it % 2)
                p.dma("sp", bk, lambda e, buf=buf, l=l, fb=fb: e.dma_start(
                    out=buf[:], in_=modw_d[l, :, fb * 512:(fb + 1) * 512].rearrange("(c p) n -> p c n", p=128)),
                    writes=[bk])
                pb = "psA%d" % (it % 2)
                pst = PS[0][:, (it % 2) * 512:(it % 2) * 512 + 512]

                def mm(e, buf=buf, pst=pst):
                    for c in range(8):
                        ins = e.matmul(pst[0:2, :], scv[:, c * 2:c * 2 + 2], buf[:, c, :], start=(c == 0), stop=(c == 7))
                    return ins
                p.pe(mm, reads=[bk, "scv"], writes=[pb])
                if it % 2 == 0:
                    p.act(lambda e, pst=pst, fb=fb: e.activation(mrow[0:2, fb * 512:(fb + 1) * 512], pst[0:2, :], AF.Identity),
                          reads=[pb], writes=["mrow_%d" % fb])
                else:
                    p.dve(lambda e, pst=pst, fb=fb: e.tensor_copy(mrow[0:2, fb * 512:(fb + 1) * 512], pst[0:2, :]),
                          reads=[pb], writes=["mrow_%d" % fb])

            def tr(e):
                for j in range(48):
                    ins = e.matmul(PS[1][:, j * 2:j * 2 + 2], mrow[0:2, j * 128:(j + 1) * 128], ident[0:2, 0:2], start=True, stop=True)
                return ins
            p.pe(tr, reads=["mrow_%d" % fb for fb in range(12)] + ["ident"], writes=["psB0"])
            p.dve(lambda e, l=l: e.tensor_tensor(MOD[:, l, :], PS[1][:, 0:96].rearrange("p (j t) -> p j t", t=2)[:, :, 0],
                                                modb[:, l * 48:(l + 1) * 48], ALU.add),
                  reads=["psB0", "modb"], writes=["MOD%d" % l])
            if l == 0:
                p.dve(lambda e: e.tensor_tensor(MODC[:], PS[1][:, 0:32].rearrange("p (j t) -> p j t", t=2)[:, :, 1],
                                                modb[:, 0:16], ALU.add),
                      reads=["psB0", "modb"], writes=["MODC"])
        for l in range(2):
            for n in range(2):
                sc = MOD[:, l, (1 + 3 * n) * 8:(2 + 3 * n) * 8]
                p.dve(lambda e, l=l, n=n, sc=sc: e.scalar_tensor_tensor(GS[:, l * 2 + n, :], sc, 1.0, NG[:, n * 16 + l * 8:n * 16 + l * 8 + 8],
                                                                      op0=ALU.add, op1=ALU.mult),
                      reads=["MOD%d" % l, "NG"], writes=["GS%d%d" % (l, n)])
        p.dve(lambda e: e.scalar_tensor_tensor(GSC[:], MODC[:, 8:16], 1.0, NG[:, 0:8], op0=ALU.add, op1=ALU.mult),
              reads=["MODC", "NG"], writes=["GSC"])
        for tt in range(32):
            xb = xin[tt % 2]
            xk = "xin%d" % (tt % 2)
            p.dma("sp", xk, lambda e, xb=xb, tt=tt: e.dma_start(out=xb[:], in_=x_d[tt * 128:(tt + 1) * 128, :]), writes=[xk])
            pst = PS[2 + tt % 2]
            pk = "psX%d" % (tt % 2)

            def trx(e, xb=xb, pst=pst):
                for c in range(8):
                    ins = e.matmul(pst[:, c * 128:(c + 1) * 128], xb[:, c * 128:(c + 1) * 128], ident[:], start=True, stop=True)
                return ins
            p.pe(trx, reads=[xk, "ident"], writes=[pk])
            dst = HT[:, :, tt * 128:(tt + 1) * 128]
            src = pst[:, :].rearrange("p (c t) -> p c t", t=128)
            if tt % 2 == 0:
                p.act(lambda e, dst=dst, src=src: e.activation(dst, src, AF.Identity), reads=[pk], writes=["HT%d" % tt])
            else:
                p.dve(lambda e, dst=dst, src=src: e.tensor_copy(dst, src), reads=[pk], writes=["HT%d" % tt])
        ph.done()

        def norm_block(p, PSst, pskey, t0, T, gs, sh, sqb, rstd, tmpb, dst_fn, tag, src=None, srckey="HT"):
            srcT = HT if src is None else src
            for c in range(8):
                sq = sqb[c % 2]
                p.act(lambda e, sq=sq, c=c: e.activation(sq[:, 0:T], srcT[:, c, t0:t0 + T], AF.Square),
                      reads=[srckey], writes=["sq%s%d" % (tag, c % 2)])
                p.pe(lambda e, sq=sq, c=c: e.matmul(PSst[:, 0:T], ones_bf[:], sq[:, 0:T], start=(c == 0), stop=(c == 7)),
                     reads=["sq%s%d" % (tag, c % 2), "ones_bf"], writes=[pskey])
            p.act(lambda e: e.activation(rstd[:, 0:T], PSst[:, 0:T], AF.Sqrt, bias=EPSB[:, 0:1]), reads=[pskey], writes=["rstd0" + tag])
            p.dve(lambda e: e.reciprocal(rstd[:, 0:T], rstd[:, 0:T]), reads=["rstd0" + tag], writes=["rstd" + tag])
            for c in range(8):
                tb = tmpb[c % 2]
                p.dve(lambda e, tb=tb, c=c: e.scalar_tensor_tensor(tb[:, 0:T], srcT[:, c, t0:t0 + T], gs[:, c:c + 1], rstd[:, 0:T],
                                                                  op0=ALU.mult, op1=ALU.mult),
                      reads=[srckey, "rstd" + tag], writes=["tmp%s%d" % (tag, c % 2)])
                dst, dkey = dst_fn(c)
                p.act(lambda e, tb=tb, c=c, dst=dst: e.activation(dst, tb[:, 0:T], AF.Identity, bias=sh[:, c:c + 1]),
                      reads=["tmp%s%d" % (tag, c % 2)], writes=[dkey])

        def wout_phase(wout_d, l):
            ph = Phase(nc)
            p = ph.p
            PS = ph.psum4()
            w = ph.sb([128, 8, D], BF16)
            mixb = [ph.sb([128, 8, 512], BF16) for _ in range(2)]
            for c in range(8):
                p.dma("pool", "w", lambda e, c=c: e.dma_start(out=w[:, c, :], in_=wout_d[c * 128:(c + 1) * 128, :]), writes=["w"])
            for b in range(8):
                mb = mixb[b % 2]
                mk = "mix%d" % (b % 2)
                p.dma("sp", mk, lambda e, mb=mb, b=b: e.dma_start(out=mb[:], in_=MIX_d[:, :, b * 512:(b + 1) * 512].rearrange("c p t -> p c t")),
                      writes=[mk])
                for f in range(8):
                    pst = PS[f % 4][:, 0:512]
                    pk = "psY%d" % (f % 4)

                    def mm(e, mb=mb, f=f, pst=pst):
                        for k in range(8):
                            ins = e.matmul(pst, w[:, k, f * 128:(f + 1) * 128], mb[:, k, :], start=(k == 0), stop=(k == 7))
                        return ins
                    p.pe(mm, reads=["w", mk], writes=[pk])
                    hsl = HT[:, f, b * 512:(b + 1) * 512]
                    p.dve(lambda e, pst=pst, f=f, hsl=hsl: e.scalar_tensor_tensor(hsl, pst, MOD[:, l, 16 + f:17 + f], hsl, op0=ALU.mult, op1=ALU.add),
                          reads=[pk], writes=["HT"])
            ph.done()

        def router_moe(l):
            ph = Phase(nc)
            p = ph.p
            PS = ph.psum4()
            sqb = [ph.sb([128, 512], BF16) for _ in range(2)]
            rstd = ph.sb([128, 512], F32)
            tmpb = [ph.sb([128, 512], F32) for _ in range(2)]
            a2f = ph.sb([128, 8, 512], F32)
            a2b = [ph.sb([128, 8, 512], BF16) for _ in range(2)]
            rw = ph.sb([128, 8, 20], F32)
            rbb = ph.sb([128, 80], F32)
            lgT = ph.sb([32, 512], F32)
            LG = ph.sb([128, 32, 20], F32)
            p.dma("sp", "rw", lambda e: e.dma_start(out=rw[:], in_=rw_d[l].rearrange("(c p) n -> p c n", p=128)), writes=["rw"])
            p.dma("sp", "rw", lambda e: e.dma_start(out=rbb[:], in_=rb_d[:, l * 80:(l + 1) * 80]), writes=["rbb"])
            gs = GS[:, l * 2 + 1, :]
            sh = MOD[:, l, 24:32]
            for b in range(8):
                af = a2f
                ab = a2b[b % 2]
                abk = "a2b%d" % (b % 2)
                norm_block(p, PS[0], "psS", b * 512, 512, gs, sh, sqb, rstd, tmpb,
                           lambda c, af=af: (af[:, c, :], "a2f_%d" % c), "n")
                akeys = ["a2f_%d" % c for c in range(8)]
                p.pool(lambda e, af=af, ab=ab: e.tensor_copy(ab[:], af[:]), reads=akeys, writes=[abk])
                p.dma("sp", "a2st%d" % (b % 2), lambda e, ab=ab, b=b: e.dma_start(
                    out=A2_d[:, :, b * 512:(b + 1) * 512].rearrange("c p t -> p c t"), in_=ab[:]), reads=[abk], writes=["A2"])

                def rmm(e, af=af):
                    for c in range(8):
                        ins = e.matmul(PS[1][0:20, 0:512], rw[:, c, :], af[:, c, :], start=(c == 0), stop=(c == 7))
                    return ins
                p.pe(rmm, reads=["rw"] + akeys, writes=["psR"])
                p.act(lambda e: e.activation(lgT[0:20, :], PS[1][0:20, 0:512], AF.Identity), reads=["psR"], writes=["lgT"])

                def rtr(e):
                    for tt in range(4):
                        ins = e.matmul(PS[2][:, tt * 32:tt * 32 + 20], lgT[0:20, tt * 128:(tt + 1) * 128], ident[0:20, 0:20], start=True, stop=True)
                    return ins
                p.pe(rtr, reads=["lgT", "ident"], writes=["psT"])
                p.dve(lambda e, b=b: e.tensor_tensor(LG[:, b * 4:(b + 1) * 4, :], PS[2][:, 0:128].rearrange("p (t n) -> p t n", n=32)[:, :, 0:20],
                                                    rbb[:].rearrange("p (t n) -> p t n", n=20), ALU.add),
                      reads=["psT", "rbb"], writes=["LG"])
            NT = 32
            gmax = ph.sb([128, NT], F32)
            ohg = ph.sb([128, NT, 4], F32)
            gd = ph.sb([128, NT, 4], F32)
            gsum = ph.sb([128, NT], F32)
            gp = ph.sb([128, NT], F32)
            t44 = ph.sb([128, NT, 4, 4], F32)
            esel = ph.sb([128, NT, 4], F32)
            e1 = ph.sb([128, NT], F32)
            sel1 = ph.sb([128, NT, 4], F32)
            em = ph.sb([128, NT, 4], F32)
            e2 = ph.sb([128, NT], F32)
            sel2 = ph.sb([128, NT, 4], F32)
            dd = ph.sb([128, NT], F32)
            w1 = ph.sb([128, NT], F32)
            w2 = ph.sb([128, NT], F32)
            ce = ph.sb([128, NT, 4], F32)
            ce2 = ph.sb([128, NT, 4], F32)
            comb = ph.sb([128, NT, 16], F32)
            chl = ph.sb([128, NT, 32], BF16)
            chf = ph.sb([128, NT, 16], F32)
            gl = LG[:, :, 0:4]
            el = LG[:, :, 4:20].rearrange("p n (g i) -> p n g i", i=4)

            def b3(ap2):
                return ap2.unsqueeze(2).to_broadcast([128, NT, 4])
            p.dve(lambda e: e.tensor_reduce(gmax[:], gl, AX.X, ALU.max), reads=["LG"], writes=["gmax"])
            p.dve(lambda e: e.tensor_tensor(ohg[:], gl, b3(gmax[:]), ALU.is_equal), reads=["LG", "gmax"], writes=["ohg"])
            p.dve(lambda e: e.tensor_tensor(gd[:], gl, b3(gmax[:]), ALU.subtract), reads=["LG", "gmax"], writes=["gd"])
            p.act(lambda e: e.activation(gd[:], gd[:], AF.Exp), reads=["gd"], writes=["ge"])
            p.dve(lambda e: e.tensor_reduce(gsum[:], gd[:], AX.X, ALU.add), reads=["ge"], writes=["gsum"])
            p.dve(lambda e: e.reciprocal(gp[:], gsum[:]), reads=["gsum"], writes=["gp"])
            p.dve(lambda e: e.tensor_tensor(t44[:], el, ohg[:].unsqueeze(3).to_broadcast([128, NT, 4, 4]), ALU.mult),
                  reads=["LG", "ohg"], writes=["t44"])
            p.dve(lambda e: e.tensor_reduce(esel[:], t44[:].rearrange("p n g i -> p n i g"), AX.X, ALU.add), reads=["t44"], writes=["esel"])
            p.dve(lambda e: e.tensor_reduce(e1[:], esel[:], AX.X, ALU.max), reads=["esel"], writes=["e1"])
            p.dve(lambda e: e.tensor_tensor(sel1[:], esel[:], b3(e1[:]), ALU.is_equal), reads=["esel", "e1"], writes=["sel1"])
            p.dve(lambda e: e.scalar_tensor_tensor(em[:], sel1[:], -1e30, esel[:], op0=ALU.mult, op1=ALU.add), reads=["sel1", "esel"], writes=["em"])
            p.dve(lambda e: e.tensor_reduce(e2[:], em[:], AX.X, ALU.max), reads=["em"], writes=["e2"])
            p.dve(lambda e: e.tensor_tensor(sel2[:], em[:], b3(e2[:]), ALU.is_equal), reads=["em", "e2"], writes=["sel2"])
            p.dve(lambda e: e.tensor_tensor(dd[:], e2[:], e1[:], ALU.subtract), reads=["e1", "e2"], writes=["dd"])
            p.act(lambda e: e.activation(dd[:], dd[:], AF.Exp), reads=["dd"], writes=["ex"])
            p.dve(lambda e: e.tensor_scalar(w1[:], dd[:], 1.0, None, op0=ALU.add), reads=["ex"], writes=["w1a"])
            p.dve(lambda e: e.reciprocal(w1[:], w1[:]), reads=["w1a"], writes=["w1"])
            p.dve(lambda e: e.tensor_tensor(w2[:], dd[:], w1[:], ALU.mult), reads=["ex", "w1"], writes=["w2a"])
            p.dve(lambda e: e.tensor_tensor(w1[:], w1[:], gp[:], ALU.mult), reads=["w1", "gp", "w2a"], writes=["wt1"])
            p.dve(lambda e: e.tensor_tensor(w2[:], w2[:], gp[:], ALU.mult), reads=["w2a", "gp"], writes=["wt2"])
            p.dve(lambda e: e.tensor_tensor(ce[:], sel1[:], b3(w1[:]), ALU.mult), reads=["sel1", "wt1"], writes=["ce"])
            p.dve(lambda e: e.tensor_tensor(ce2[:], sel2[:], b3(w2[:]), ALU.mult), reads=["sel2", "wt2"], writes=["ce2"])
            p.dve(lambda e: e.tensor_tensor(ce[:], ce[:], ce2[:], ALU.add), reads=["ce", "ce2"], writes=["cef"])
            p.dve(lambda e: e.tensor_tensor(comb[:].rearrange("p n (g i) -> p n g i", i=4),
                                            ohg[:].unsqueeze(3).to_broadcast([128, NT, 4, 4]),
                                            ce[:].unsqueeze(2).to_broadcast([128, NT, 4, 4]), ALU.mult),
                  reads=["ohg", "cef"], writes=["comb"])
            p.dve(lambda e: e.tensor_copy(chl[:, :, 0:16], comb[:]), reads=["comb"], writes=["chi"])
            p.dve(lambda e: e.tensor_copy(chf[:], chl[:, :, 0:16]), reads=["chi"], writes=["chf"])
            p.dve(lambda e: e.tensor_tensor(chf[:], comb[:], chf[:], ALU.subtract), reads=["comb", "chf"], writes=["clo"])
            p.dve(lambda e: e.tensor_copy(chl[:, :, 16:32], chf[:]), reads=["clo"], writes=["chl"])
            for q in range(8):
                pst = PS[3][:, (q % 2) * 512:(q % 2) * 512 + 512]
                pk = "psC%d" % (q % 2)

                def ctr(e, q=q, pst=pst):
                    for tt in range(4):
                        ins = e.matmul(pst[0:32, tt * 128:(tt + 1) * 128], chl[:, q * 4 + tt, :], ident_bf[:], start=True, stop=True)
                    return ins
                p.pe(ctr, reads=["chl", "chi", "ident_bf"], writes=[pk])
                p.act(lambda e, q=q, pst=pst: e.activation(combT[:, q * 512:(q + 1) * 512], pst[0:32, :], AF.Identity), reads=[pk], writes=["combT"])
            ph.done()
            if stage == 10 + l:
                return
            ph = Phase(nc)
            p = ph.p
            PS = ph.psum4()
            T = 512
            NB = S // T
            wslot = [(ph.sb([128, 8, 256], BF16), ph.sb([128, 8, 256], BF16), ph.sb([128, 2, D], BF16)) for _ in range(3)]
            a2 = [ph.sb([128, 8, T], BF16) for _ in range(2)]
            sel = ph.sb([32, 2048], BF16)
            cb = ph.sb([128, T], F32)
            sg = [ph.sb([128, T], F32)] * 2
            tt_ = ph.sb([128, T], F32)
            hb0 = [ph.sb([128, 2, T], BF16) for _ in range(2)]
            hb1 = ph.sb([128, 2, T], BF16)
            p.dma("pool", "sel", lambda e: e.dma_start(out=sel[:, 0:1024], in_=sel_d[:, 0:1024]), writes=["sel"])
            p.dma("pool", "sel", lambda e: e.dma_start(out=sel[:, 1024:2048], in_=sel_d[:, 1024:2048]), writes=["sel"])
            g2 = MOD[:, l, 40:48]
            RB = [PS[0][:, 0:512], PS[0][:, 512:1024], PS[1][:, 0:512]]
            RBK = ["psR0", "psR1", "psR2"]
            CBP = PS[1][:, 512:1024]
            ACC = [PS[2][:, 0:512], PS[2][:, 512:1024], PS[3][:, 0:512], PS[3][:, 512:1024]]

            def load_expert(ex):
                sl = ex % 3
                wg, wu, wd = wslot[sl]
                wk = "w%d" % sl
                p.dma("pool", wk, lambda e: e.dma_start(out=wg[:], in_=wg_d[l, ex].rearrange("(c p) n -> p c n", p=128)), writes=[wk + "g"])
                p.dma("pool", wk, lambda e: e.dma_start(out=wu[:], in_=wu_d[l, ex].rearrange("(c p) n -> p c n", p=128)), writes=[wk + "u"])
                p.dma("pool", wk, lambda e: e.dma_start(out=wd[:], in_=wd_d[l, ex].rearrange("(c p) n -> p c n", p=128)), writes=[wk + "d"])
            st = {"step": 0, "rk": 0, "sgi": 0}

            def hbuf(b, j):
                if j == 0:
                    return hb0[b % 2], "h0_%d" % (b % 2)
                return hb1, "h1"

            def G(pr, b, j):
                ex = pr * 2 + j
                sl = ex % 3
                wg, wu, wd = wslot[sl]
                wk = "w%d" % sl
                if j == 0:
                    ab = a2[st["step"] % 2]
                    ak = "a2_%d" % (st["step"] % 2)
                    st["cur"] = (ab, ak)
                    st["step"] += 1
                    p.dma("sp", ak, lambda e: e.dma_start(out=ab[:], in_=A2_d[:, :, b * T:(b + 1) * T].rearrange("c p t -> p c t")), writes=[ak])
                ab, ak = st["cur"]
                hbb, hk = hbuf(b, j)
                p.pe(lambda e: e.matmul(CBP, sel[:, ex * 128:(ex + 1) * 128], combT[:, b * T:(b + 1) * T], start=True, stop=True),
                     reads=["sel", "combT"], writes=["psCB"])
                p.act(lambda e: e.activation(cb[:], CBP, AF.Identity), reads=["psCB"], writes=["cb"])
                for f2 in range(2):
                    pg = RB[st["rk"] % 3]
                    pgk = RBK[st["rk"] % 3]
                    st["rk"] += 1
                    pu = RB[st["rk"] % 3]
                    puk = RBK[st["rk"] % 3]
                    st["rk"] += 1

                    def gmm(e, pg=pg, f2=f2):
                        for k in range(8):
                            ins = e.matmul(pg, wg[:, k, f2 * 128:(f2 + 1) * 128], ab[:, k, :], start=(k == 0), stop=(k == 7))
                        return ins

                    def umm(e, pu=pu, f2=f2):
                        for k in range(8):
                            ins = e.matmul(pu, wu[:, k, f2 * 128:(f2 + 1) * 128], ab[:, k, :], start=(k == 0), stop=(k == 7))
                        return ins
                    p.pe(gmm, reads=[wk + "g", ak], writes=[pgk])
                    p.pe(umm, reads=[wk + "u", ak], writes=[puk])
                    sgb = sg[st["sgi"] % 2]
                    sgk = "sg0"
                    st["sgi"] += 1
                    p.act(lambda e, sgb=sgb, pg=pg: e.activation(sgb[:], pg, AF.Silu), reads=[pgk], writes=[sgk])
                    p.dve(lambda e, pu=pu: e.tensor_tensor(tt_[:], pu, cb[:], ALU.mult), reads=[puk, "cb"], writes=["tt"])
                    p.pool(lambda e, sgb=sgb, f2=f2: e.tensor_tensor(hbb[:, f2, :], tt_[:], sgb[:], ALU.mult),
                           reads=["tt", sgk], writes=[hk + "_%d" % f2])

            def DOWN(pr, b, half):
                hs = []
                for j in range(2):
                    ex = pr * 2 + j
                    wg, wu, wd = wslot[ex % 3]
                    hbb, hk = hbuf(b, j)
                    hs.append((wd, "w%dd" % (ex % 3), hbb, hk))
                for fi in range(4):
                    f = half * 4 + fi

                    def dmm(e, fi=fi, f=f):
                        for j in range(2):
                            wd, wdk, hbb, hk = hs[j]
                            for k in range(2):
                                ins = e.matmul(ACC[fi], wd[:, k, f * 128:(f + 1) * 128], hbb[:, k, :], start=(j == 0 and k == 0), stop=(j == 1 and k == 1))
                        return ins
                    p.pe(dmm, reads=[hs[0][1], hs[1][1], hs[0][3] + "_0", hs[0][3] + "_1", hs[1][3] + "_0", hs[1][3] + "_1"], writes=["psD%d" % fi])
                for fi in range(4):
                    f = half * 4 + fi
                    hsl = HT[:, f, b * T:(b + 1) * T]
                    p.dve(lambda e, fi=fi, f=f, hsl=hsl: e.scalar_tensor_tensor(hsl, ACC[fi], g2[:, f:f + 1], hsl, op0=ALU.mult, op1=ALU.add),
                          reads=["psD%d" % fi], writes=["HT"])

            load_expert(0)
            load_expert(1)
            for pr in range(8):
                if pr > 0:
                    load_expert(2 * pr + 1)
                G(pr, 0, 0)
                G(pr, 0, 1)
                if pr < 7:
                    load_expert(2 * pr + 2)
                for b in range(NB):
                    DOWN(pr, b, 0)
                    if b + 1 < NB:
                        G(pr, b + 1, 0)
                    DOWN(pr, b, 1)
                    if b + 1 < NB:
                        G(pr, b + 1, 1)
            ph.done()

        def store_phase():
            ph = Phase(nc)
            p = ph.p
            PS = ph.psum4()
            ob = [ph.sb([128, D], F32) for _ in range(2)]
            for tt in range(32):
                pst = PS[tt % 2]
                pk = "psO%d" % (tt % 2)

                def tr(e, tt=tt, pst=pst):
                    for c in range(8):
                        ins = e.matmul(pst[:, c * 128:(c + 1) * 128], HT[:, c, tt * 128:(tt + 1) * 128], ident[:], start=True, stop=True)
                    return ins
                p.pe(tr, reads=["HT", "ident"], writes=[pk])
                o = ob[tt % 2]
                ok = "ob%d" % (tt % 2)
                if tt % 2 == 0:
                    p.act(lambda e, o=o, pst=pst: e.activation(o[:], pst[:, :], AF.Identity), reads=[pk], writes=[ok])
                else:
                    p.dve(lambda e, o=o, pst=pst: e.tensor_copy(o[:], pst[:, :]), reads=[pk], writes=[ok])
                p.dma("sp", "ost%d" % (tt % 2), lambda e, o=o, tt=tt: e.dma_start(out=out_d[tt * 128:(tt + 1) * 128, :], in_=o[:]), reads=[ok], writes=["out"])
            ph.done()

        def qk_chain(p, PS, psA, kA, psB, kB, psC, T, gcol, gpcol, cosb, sinb, tq, dst, dstkey, rope=True):
            sqq, rsq, t1, t2 = tq
            p.act(lambda e: e.activation(sqq[:, 0:T], psA, AF.Square), reads=[kA], writes=["sqq"])
            p.pe(lambda e: e.matmul(psC[:, 0:T], onesblk_bf[:], sqq[:, 0:T], start=True, stop=True), reads=["sqq", "onesblk"], writes=["psC"])
            p.act(lambda e: e.activation(rsq[:, 0:T], psC[:, 0:T], AF.Sqrt, bias=EPSB[:, 0:1]), reads=["psC"], writes=["rsq0"])
            p.dve(lambda e: e.reciprocal(rsq[:, 0:T], rsq[:, 0:T]), reads=["rsq0"], writes=["rsq"])
            if rope:
                p.dve(lambda e: e.scalar_tensor_tensor(t1[:, 0:T], psA, gcol, cosb[:, 0:T], op0=ALU.mult, op1=ALU.mult),
                      reads=[kA, "cosb"], writes=["t1"])
                p.dve(lambda e: e.scalar_tensor_tensor(t2[:, 0:T], psB, gpcol, sinb[:, 0:T], op0=ALU.mult, op1=ALU.mult),
                      reads=[kB, "sinb"], writes=["t2"])
                p.pool(lambda e: e.tensor_tensor(t1[:, 0:T], t1[:, 0:T], t2[:, 0:T], ALU.add), reads=["t1", "t2"], writes=["t3"])
                p.dve(lambda e: e.tensor_tensor(dst, t1[:, 0:T], rsq[:, 0:T], ALU.mult), reads=["t3", "rsq"], writes=[dstkey])
            else:
                p.dve(lambda e: e.scalar_tensor_tensor(dst, psA, gcol, rsq[:, 0:T], op0=ALU.mult, op1=ALU.mult),
                      reads=[kA, "rsq"], writes=[dstkey])

        def layer0_mixer():
            ph = Phase(nc)
            p = ph.p
            PS = ph.psum4()
            T = 256
            wq = ph.sb([128, 8, 1792], BF16)
            wv = ph.sb([128, 8, 128], BF16)
            gains = ph.sb([128, 4], F32)
            sqb = [ph.sb([128, T], BF16) for _ in range(2)]
            rstd = ph.sb([128, T], F32)
            tmpb = [ph.sb([128, T], F32) for _ in range(2)]
            aT = ph.sb([128, 8, T], BF16)
            cosb = ph.sb([128, T], F32)
            sinb = ph.sb([128, T], F32)
            tq = (ph.sb([128, T], BF16), ph.sb([128, T], F32), ph.sb([128, T], F32), ph.sb([128, T], F32))
            qf = [ph.sb([128, T], BF16) for _ in range(2)]
            pst_ = [ph.sb([128, T], F32) for _ in range(2)]
            vst = [ph.sb([128, 2, 193], BF16) for _ in range(2)]
            ctin = ph.sb([128, D], F32)
            CT = ph.sb([128, 8, 256], F32)
            p.dma("sp", "c1", lambda e: e.dma_start(out=gains[:], in_=gains_d[:, :]), writes=["gains"])
            for vi in range(2):
                p.dve(lambda e, vi=vi: e.memset(vst[vi][:], 0.0), writes=["vst%d" % vi])
                p.dve(lambda e, vi=vi: e.memset(vst[vi][:, :, 64:66], 1.0), reads=["vst%d" % vi], writes=["vst%d" % vi])
            for c in range(8):
                p.dma("pool", "wq", lambda e, c=c: e.dma_start(out=wq[:, c, :], in_=wqkp_d[c * 128:(c + 1) * 128, :]), writes=["wq"])
            p.dma("pool", "wq", lambda e: e.dma_start(out=wv[:], in_=wv_d.rearrange("(c p) n -> p c n", p=128)), writes=["wv"])
            B_ST = PS[0][:, 0:512]
            B_A = [PS[0][:, 512:1024], PS[1][:, 0:512]]
            B_B = [PS[1][:, 512:1024], PS[2][:, 0:512]]
            B_C = PS[2][:, 512:1024]
            B_M = [PS[3][:, 0:512], PS[3][:, 512:1024]]
            for tt in range(2):
                p.dma("sp", "ctin", lambda e, tt=tt: e.dma_start(out=ctin[:], in_=ctx_d[tt * 128:(tt + 1) * 128, :]), writes=["ctin"])
                for half in range(2):
                    bm = B_M[half]

                    def trc(e, half=half, bm=bm):
                        for c in range(4):
                            cc = half * 4 + c
                            ins = e.matmul(bm[:, c * 128:(c + 1) * 128], ctin[:, cc * 128:(cc + 1) * 128], ident[:], start=True, stop=True)
                        return ins
                    p.pe(trc, reads=["ctin", "ident"], writes=["psM%d" % half])
                    p.dve(lambda e, half=half, bm=bm, tt=tt: e.tensor_copy(CT[:, half * 4:half * 4 + 4, tt * 128:(tt + 1) * 128],
                                                                          bm.rearrange("p (c t) -> p c t", t=128)),
                          reads=["psM%d" % half], writes=["CT"])
            mcnt = [0]

            def proj(p, col0, bank, bkey, T=T):
                def mm(e):
                    for k in range(8):
                        ins = e.matmul(bank[:, 0:T], wq[:, k, col0:col0 + 128], aT[:, k, :], start=(k == 0), stop=(k == 7))
                    return ins
                p.pe(mm, reads=["wq"] + ["aT_%d" % c for c in range(8)], writes=[bkey])

            def vproj(p, tile0):
                i = mcnt[0] % 2
                mcnt[0] += 1
                bm = B_M[i]
                bk = "psM%d" % i
                vs_ = vst[i]

                def mm(e):
                    for t2 in range(2):
                        for k in range(8):
                            ins = e.matmul(bm[:, t2 * 128:(t2 + 1) * 128], aT[:, k, t2 * 128:(t2 + 1) * 128], wv[:, k, :], start=(k == 0), stop=(k == 7))
                    return ins
                p.pe(mm, reads=["wv"] + ["aT_%d" % c for c in range(8)], writes=[bk])
                p.dve(lambda e: e.tensor_copy(vs_[:, :, 0:64], bm[:, 0:256].rearrange("p (t n) -> p t n", n=128)[:, :, 0:64]),
                      reads=[bk], writes=["vst%da" % i])
                p.dve(lambda e: e.tensor_copy(vs_[:, :, 129:193], bm[:, 0:256].rearrange("p (t n) -> p t n", n=128)[:, :, 64:128]),
                      reads=[bk, "vst%da" % i], writes=["vst%d" % i])
                p.dma("sp", "vsst%d" % i, lambda e: e.dma_start(out=VS_d[:, tile0:tile0 + 2, :], in_=vs_[:]), reads=["vst%d" % i, "vst%da" % i], writes=["VSd"])

            norm_block(p, B_ST, "psST", 0, 256, GSC, MODC, sqb, rstd, tmpb, lambda c: (aT[:, c, :], "aT_%d" % c), "c", src=CT, srckey="CT")
            proj(p, 512, B_A[0], "psA0")
            qk_chain(p, PS, B_A[0][:, 0:T], "psA0", None, None, B_C, T, gains[:, 2:3], None, None, None, tq, qf[0][:, 0:T], "qf0", rope=False)
            p.dma("sp", "qst0", lambda e: e.dma_start(out=KT_d[:, 0:256], in_=qf[0][:, 0:T]), reads=["qf0"], writes=["KTd"])
            vproj(p, 0)
            qi = 1
            for b in range(S // T):
                t0 = b * T
                p.dma("sp", "cs", lambda e, t0=t0: e.dma_start(out=cosb[:], in_=cos_d[:, t0:t0 + T]), writes=["cosb"])
                p.dma("sp", "cs", lambda e, t0=t0: e.dma_start(out=sinb[:], in_=sin_d[:, t0:t0 + T]), writes=["sinb"])
                norm_block(p, B_ST, "psST", t0, T, GS[:, 0, :], MOD[:, 0, 0:8], sqb, rstd, tmpb, lambda c: (aT[:, c, :], "aT_%d" % c), "m")
                for j in range(5):
                    i = qi % 2
                    qi += 1
                    col = j * 128 if j < 4 else 512
                    colp = 1152 + j * 128 if j < 4 else 1664
                    proj(p, col, B_A[i], "psA%d" % i)
                    proj(p, colp, B_B[i], "psB%d" % i)
                    gcol = gains[:, 0:1] if j < 4 else gains[:, 2:3]
                    gpcol = gains[:, 1:2] if j < 4 else gains[:, 3:4]
                    qk_chain(p, PS, B_A[i][:, 0:T], "psA%d" % i, B_B[i][:, 0:T], "psB%d" % i, B_C, T, gcol, gpcol, cosb, sinb, tq,
                             qf[i][:, 0:T], "qf%d" % i)
                    if j < 4:
                        p.dma("sp", "qst%d" % i, lambda e, i=i, j=j, t0=t0: e.dma_start(out=QT_d[j, :, t0:t0 + T], in_=qf[i][:, 0:T]),
                              reads=["qf%d" % i], writes=["QTd"])
                    else:
                        p.dma("sp", "qst%d" % i, lambda e, i=i, t0=t0: e.dma_start(out=KT_d[:, 256 + t0:256 + t0 + T], in_=qf[i][:, 0:T]),
                              reads=["qf%d" % i], writes=["KTd"])
                for g in range(4):
                    i = mcnt[0] % 2
                    mcnt[0] += 1
                    proj(p, 640 + g * 128, B_M[i], "psM%d" % i)
                    p.act(lambda e, i=i: e.activation(pst_[i][:, 0:T], B_M[i][:, 0:T], AF.Identity), reads=["psM%d" % i], writes=["pst%d" % i])
                    p.dma("sp", "pst%d" % i, lambda e, i=i, g=g, t0=t0: e.dma_start(out=PT_d[g, :, t0:t0 + T], in_=pst_[i][:, 0:T]),
                          reads=["pst%d" % i], writes=["PTd"])
                vproj(p, 2 + 2 * b)
            ph.done()
            if stage == 20:
                return
            ph = Phase(nc)
            p = ph.p
            PS = ph.psum4()
            KT = ph.sb([128, 4352], BF16)
            VS = ph.sb([128, 34, 193], BF16)
            Qb = [ph.sb([128, 512], BF16) for _ in range(2)]
            Pb = [ph.sb([128, 1024], BF16) for _ in range(3)]
            rr = ph.sb([128, 512], F32)
            bcs = ph.sb([128, 512], F32)
            mixo = [ph.sb([128, 512], BF16) for _ in range(2)]
            p.dma("sp", "kt", lambda e: e.dma_start(out=KT[:], in_=KT_d[:, :]), writes=["KT"])
            p.dma("sp", "kt", lambda e: e.dma_start(out=VS[:], in_=VS_d[:, :, :]), writes=["VS"])
            SB_ = [PS[0], PS[1]]
            OA = PS[2][:, 0:512]
            OB = PS[2][:, 512:1024]
            BCA = PS[3][:, 0:512]
            BCB = PS[3][:, 512:1024]
            u = 0
            it = 0
            for j in range(4):
                for qb in range(8):
                    qt = Qb[u % 2]
                    qk_ = "Qb%d" % (u % 2)
                    p.dma("sp", qk_, lambda e, qt=qt, j=j, qb=qb: e.dma_start(out=qt[:], in_=QT_d[j, :, qb * 512:(qb + 1) * 512]), writes=[qk_])
                    for kt in range(34):
                        sb_ = SB_[it % 2]
                        sk = "psS%d" % (it % 2)
                        pb = Pb[it % 3]
                        pk = "P%d" % (it % 3)
                        it += 1

                        def smm(e, sb_=sb_, qt=qt, kt=kt):
                            e.matmul(sb_[:, 0:512], KT[0:64, kt * 128:(kt + 1) * 128], qt[0:64, :], start=True, stop=True)
                            return e.matmul(sb_[:, 512:1024], KT[64:128, kt * 128:(kt + 1) * 128], qt[64:128, :], start=True, stop=True)
                        p.pe(smm, reads=["KT", qk_], writes=[sk])
                        p.act(lambda e, pb=pb, sb_=sb_: e.activation(pb[:], sb_[:, :], AF.Exp, scale=0.125), reads=[sk], writes=[pk])

                        def pv(e, pb=pb, kt=kt):
                            e.matmul(OA[0:65, :], VS[:, kt, 0:65], pb[:, 0:512], start=(kt == 0), stop=(kt == 33))
                            return e.matmul(OB[:, :], VS[:, kt, 65:193], pb[:, 512:1024], start=(kt == 0), stop=(kt == 33))
                        p.pe(pv, reads=["VS", pk], writes=["psOA", "psOB"])
                    p.dve(lambda e: e.reciprocal(rr[64:65, :], OA[64:65, :]), reads=["psOA"], writes=["rrA"])
                    p.dve(lambda e: e.reciprocal(rr[0:1, :], OB[0:1, :]), reads=["psOB"], writes=["rrB"])
                    p.pe(lambda e: e.matmul(BCA[0:64, :], ones_f[64:65, 0:64], rr[64:65, :], start=True, stop=True), reads=["rrA", "ones_f"], writes=["psBCA"])
                    p.pe(lambda e: e.matmul(BCB[:, :], ones_f[0:1, :], rr[0:1, :], start=True, stop=True), reads=["rrB", "ones_f"], writes=["psBCB"])
                    p.act(lambda e: e.activation(bcs[0:64, :], BCA[0:64, :], AF.Identity), reads=["psBCA"], writes=["bcsA"])
                    p.act(lambda e: e.activation(bcs[64:128, :], BCB[64:128, :], AF.Identity), reads=["psBCB"], writes=["bcsB"])
                    mo = mixo[u % 2]
                    mk = "mixo%d" % (u % 2)
                    p.dve(lambda e, mo=mo: e.tensor_tensor(mo[0:64, :], OA[0:64, :], bcs[0:64, :], ALU.mult), reads=["psOA", "bcsA"], writes=[mk + "a"])
                    p.dve(lambda e, mo=mo: e.tensor_tensor(mo[64:128, :], OB[64:128, :], bcs[64:128, :], ALU.mult), reads=["psOB", "bcsB"], writes=[mk + "b"])
                    p.dma("sp", "mst%d" % (u % 2), lambda e, mo=mo, j=j, qb=qb: e.dma_start(out=MIX_d[j, :, qb * 512:(qb + 1) * 512], in_=mo[:]),
                          reads=[mk + "a", mk + "b"], writes=["MIXd"])
                    u += 1
            ph.done()
            ph = Phase(nc)
            p = ph.p
            PS = ph.psum4()
            W = S + 16
            Pf = ph.sb([128, W], F32)
            sa = ph.sb([128, W], F32)
            sb2 = ph.sb([128, W], F32)
            dbf = ph.sb([128, S], BF16)
            pw = ph.sb([128, 512], BF16)
            psc = ph.sb([128, 4], F32)
            edg = ph.sb([128, 64], F32)
            et = ph.sb([128, 16], F32)
            po = [ph.sb([128, 512], BF16) for _ in range(2)]
            p.dma("pool", "pw", lambda e: e.dma_start(out=pw[:], in_=poolw_d[:, :]), writes=["pw"])
            p.dma("sp", "pc", lambda e: e.dma_start(out=psc[:], in_=poolsc_d[:, :]), writes=["psc"])
            p.dma("sp", "pc", lambda e: e.dma_start(out=edg[:], in_=pooledge_d[:, :]), writes=["edg"])
            p.dve(lambda e: e.memset(Pf[:, 0:8], 0.0), writes=["PfL"])
            p.dve(lambda e: e.memset(Pf[:, W - 8:W], 0.0), writes=["PfR"])
            oc = 0
            for g in range(4):
                w_ = 2 ** (g + 1)
                p.dma("sp", "pf", lambda e, g=g: e.dma_start(out=Pf[:, 8:8 + S], in_=PT_d[g, :, :]), writes=["Pf"])
                p.dve(lambda e: e.tensor_tensor(sa[:, 1:W], Pf[:, 0:W - 1], Pf[:, 1:W], ALU.add), reads=["Pf", "PfL", "PfR"], writes=["sa"])
                cur, ck = sa, "sa"
                oth, ok_ = sb2, "sb"
                lo, hi, sh_ = 1, W, 1
                for st in range(g):
                    nlo, nhi = lo + sh_, hi - sh_
                    eng = p.pool if st % 2 == 0 else p.dve
                    eng(lambda e, cur=cur, oth=oth, nlo=nlo, nhi=nhi, sh_=sh_: e.tensor_tensor(oth[:, nlo:nhi], cur[:, nlo - sh_:nhi - sh_], cur[:, nlo + sh_:nhi + sh_], ALU.add),
                        reads=[ck], writes=[ok_])
                    cur, ck, oth, ok_ = oth, ok_, cur, ck
                    lo, hi = nlo, nhi
                    sh_ *= 2
                assert lo <= 8 and hi >= 8 + S
                p.dve(lambda e, cur=cur, w_=w_: e.scalar_tensor_tensor(dbf[:, :], cur[:, 8:8 + S], 1.0 / w_, Pf[:, 8:8 + S], op0=ALU.mult, op1=ALU.subtract),
                      reads=[ck, "Pf"], writes=["dbf0"])
                p.dve(lambda e, cur=cur, g=g: e.tensor_tensor(et[:, 0:8], cur[:, 8:16], edg[:, g * 16:g * 16 + 8], ALU.mult), reads=[ck, "edg"], writes=["et0"])
                p.dve(lambda e, cur=cur, g=g: e.tensor_tensor(et[:, 8:16], cur[:, S:8 + S], edg[:, g * 16 + 8:g * 16 + 16], ALU.mult), reads=[ck, "edg", "et0"], writes=["et1"])
                p.dve(lambda e: e.tensor_tensor(dbf[:, 0:8], et[:, 0:8], Pf[:, 8:16], ALU.subtract), reads=["et1", "Pf", "dbf0"], writes=["dbf1"])
                p.dve(lambda e: e.tensor_tensor(dbf[:, S - 8:S], et[:, 8:16], Pf[:, S:8 + S], ALU.subtract), reads=["et1", "Pf", "dbf1"], writes=["dbf"])
                for b in range(8):
                    i = oc % 2
                    oc += 1
                    bank = PS[i][:, 0:512]
                    p.pe(lambda e, bank=bank, g=g, b=b: e.matmul(bank, pw[:, g * 128:(g + 1) * 128], dbf[:, b * 512:(b + 1) * 512], start=True, stop=True),
                         reads=["pw", "dbf"], writes=["psP%d" % i])
                    p.act(lambda e, bank=bank, i=i, g=g: e.activation(po[i][:], bank, AF.Identity, scale=psc[:, g:g + 1]), reads=["psP%d" % i, "psc"], writes=["po%d" % i])
                    p.dma("sp", "post%d" % i, lambda e, i=i, g=g, b=b: e.dma_start(out=MIX_d[4 + g, :, b * 512:(b + 1) * 512], in_=po[i][:]),
                          reads=["po%d" % i], writes=["MIXd"])
            ph.done()
            wout_phase(wout0_d, 0)

        def layer1_mixer():
            ph = Phase(nc)
            p = ph.p
            PS = ph.psum4()
            T = 256
            w1 = ph.sb([128, 8, 2560], BF16)
            sqb = [ph.sb([128, T], BF16) for _ in range(2)]
            rstd = ph.sb([128, T], F32)
            tmpb = [ph.sb([128, T], F32) for _ in range(2)]
            aT = ph.sb([128, 8, T], BF16)
            sggain = ph.sb([128, 512], F32)
            sgwT = ph.sb([128, 512], BF16)
            sgbb = ph.sb([128, 512], F32)
            usb = ph.sb([128, 4, T], F32)
            hxs = [ph.sb([128, T], F32)] * 2
            zs = hxs
            bgs = [ph.sb([128, T], F32)] * 2
            sqv = ph.sb([128, 512], F32)
            ssum = ph.sb([128, 4], F32)
            vt = sqv
            vn = ph.sb([128, 4, 128], BF16)
            st_ = ph.sb([128, 4, 128], F32)
            yc = [ph.sb([128, 4, T], BF16) for _ in range(2)]
            for c in range(8):
                p.dma("pool", "w1", lambda e, c=c: e.dma_start(out=w1[:, c, 0:1280], in_=win1_d[c * 128:(c + 1) * 128, 0:1280]), writes=["w1"])
                p.dma("pool", "w1", lambda e, c=c: e.dma_start(out=w1[:, c, 1280:2560], in_=win1_d[c * 128:(c + 1) * 128, 1280:2560]), writes=["w1"])
            p.dma("pool", "w1", lambda e: e.dma_start(out=sgwT[:], in_=sgw_d[:, :]), writes=["sgwT"])
            p.dma("sp", "c2", lambda e: e.dma_start(out=sggain[:], in_=sggain_d[:, :]), writes=["sggain"])
            p.dma("sp", "c2", lambda e: e.dma_start(out=sgbb[:], in_=sgb_d[:, :]), writes=["sgbb"])
            B_ST = PS[0][:, 0:512]
            B_U = [PS[0][:, 512:1024], PS[1][:, 0:512]]
            B_H = [PS[1][:, 512:1024], PS[2][:, 0:512]]
            B_G = PS[2][:, 512:1024]
            B_V = PS[3][:, 0:512]
            B_S = PS[3][:, 512:1024]
            akeys = ["aT_%d" % c for c in range(8)]

            def proj(col0, tgt, bkey):
                def mm(e):
                    for k in range(8):
                        ins = e.matmul(tgt, w1[:, k, col0:col0 + 128], aT[:, k, :], start=(k == 0), stop=(k == 7))
                    return ins
                p.pe(mm, reads=["w1"] + akeys, writes=[bkey])
            hi_ = 0
            for b in range(S // T):
                t0 = b * T
                norm_block(p, B_ST, "psST", t0, T, GS[:, 2, :], MOD[:, 1, 0:8], sqb, rstd, tmpb, lambda c: (aT[:, c, :], "aT_%d" % c), "m")
                for half in range(2):
                    for q in range(2):
                        g = half * 2 + q
                        proj(g * 128, B_U[half][:, q * T:(q + 1) * T], "psU%d" % half)
                    p.act(lambda e, half=half: e.activation(usb[:, half * 2:half * 2 + 2, :], B_U[half][:, 0:2 * T].rearrange("p (q t) -> p q t", t=T), AF.Identity),
                          reads=["psU%d" % half], writes=["usb%d" % half])
                for c in range(4):
                    i = hi_ % 2
                    hi_ += 1
                    proj(1024 + c * 128, B_H[i][:, 0:T], "psH%d" % i)
                    proj(2048 + c * 128, B_H[i][:, T:2 * T], "psH%d" % i)
                    p.act(lambda e, i=i: e.activation(hxs[i][:], B_H[i][:, 0:T], AF.Identity), reads=["psH%d" % i], writes=["hxs0"])
                    p.dve(lambda e, i=i: e.tensor_tensor(zs[i][:], B_H[i][:, T:2 * T], hxs[i][:], ALU.mult), reads=["psH%d" % i, "hxs0"], writes=["hxs0"])
                    p.dma("sp", "zst%d" % i, lambda e, i=i, c=c, t0=t0: e.dma_start(out=PT_d[c, :, t0:t0 + T], in_=zs[i][:]), reads=["hxs0"], writes=["PTd"])
                    proj(1536 + c * 128, B_G[:, 0:T], "psG")
                    p.act(lambda e, i=i: e.activation(bgs[i][:], B_G[:, 0:T], AF.Identity), reads=["psG"], writes=["bgs0"])
                    p.dma("sp", "bst%d" % i, lambda e, i=i, c=c, t0=t0: e.dma_start(out=BG_d[c, :, t0:t0 + T], in_=bgs[i][:]), reads=["bgs0"], writes=["BGd"])
                yb = yc[b % 2]
                yk = "yc%d" % (b % 2)
                for n in range(2):
                    def vmm(e, n=n):
                        for g in range(4):
                            for k in range(8):
                                ins = e.matmul(B_V[:, g * 128:(g + 1) * 128], aT[:, k, n * 128:(n + 1) * 128], w1[:, k, 512 + g * 128:512 + (g + 1) * 128],
                                               start=(k == 0), stop=(k == 7))
                        return ins
                    p.pe(vmm, reads=["w1"] + akeys, writes=["psV"])
                    p.act(lambda e: e.activation(sqv[:], B_V, AF.Square), reads=["psV"], writes=["sqv"])
                    p.dve(lambda e: e.tensor_reduce(ssum[:], sqv[:].rearrange("p (g c) -> p g c", c=128), AX.X, ALU.add), reads=["sqv"], writes=["ssum0"])
                    p.act(lambda e: e.activation(ssum[:], ssum[:], AF.Sqrt, bias=EPSB[:, 0:1], scale=1.0 / 128.0), reads=["ssum0"], writes=["ssum1"])
                    p.dve(lambda e: e.reciprocal(ssum[:], ssum[:]), reads=["ssum1"], writes=["ssum"])
                    p.dve(lambda e: e.tensor_tensor(vt[:].rearrange("p (g c) -> p g c", c=128), B_V.rearrange("p (g c) -> p g c", c=128), ssum[:].unsqueeze(2).to_broadcast([128, 4, 128]), ALU.mult),
                          reads=["psV", "ssum", "sqv"], writes=["sqv"])
                    p.pool(lambda e: e.tensor_tensor(vn[:].rearrange("p g c -> p (g c)"), vt[:], sggain[:], ALU.mult), reads=["sqv", "sggain"], writes=["vn"])

                    def smm(e):
                        for g in range(4):
                            ins = e.matmul(B_S[:, g * 128:(g + 1) * 128], vn[:, g, :], sgwT[:, g * 128:(g + 1) * 128], start=True, stop=True)
                        return ins
                    p.pe(smm, reads=["vn", "sgwT"], writes=["psS"])
                    p.dve(lambda e: e.tensor_tensor(st_[:], B_S.rearrange("p (g c) -> p g c", c=128), sgbb[:].rearrange("p (g c) -> p g c", c=128), ALU.add),
                          reads=["psS", "sgbb"], writes=["st"])
                    p.pool(lambda e, n=n, yb=yb: e.tensor_tensor(yb[:, :, n * 128:(n + 1) * 128], st_[:], usb[:, :, n * 128:(n + 1) * 128], ALU.mult),
                           reads=["st", "usb0", "usb1"], writes=[yk + "_%d" % n])
                p.dma("sp", "yst%d" % (b % 2), lambda e, yb=yb, t0=t0: e.dma_start(out=MIX_d[0:4, :, t0:t0 + T].rearrange("c p t -> p c t"), in_=yb[:]),
                      reads=[yk + "_0", yk + "_1"], writes=["MIXd"])
            ph.done()
            ph = Phase(nc)
            p = ph.p
            W = S + 2
            Z = ph.sb([128, W], F32)
            BGr = ph.sb([128, S], F32)
            t1 = ph.sb([128, S], F32)
            yo = ph.sb([128, S], BF16)
            cw = ph.sb([128, 12], F32)
            p.dma("sp", "cw", lambda e: e.dma_start(out=cw[:], in_=convw_d[:, :]), writes=["cw"])
            p.dve(lambda e: e.memset(Z[:, 0:1], 0.0), writes=["ZL"])
            p.dve(lambda e: e.memset(Z[:, W - 1:W], 0.0), writes=["ZR"])
            for c in range(4):
                p.dma("sp", "z", lambda e, c=c: e.dma_start(out=Z[:, 1:1 + S], in_=PT_d[c, :, :]), writes=["Z"])
                p.dma("sp", "bg", lambda e, c=c: e.dma_start(out=BGr[:], in_=BG_d[c, :, :]), writes=["BGr"])
                p.dve(lambda e, c=c: e.tensor_scalar(t1[:], Z[:, 1:1 + S], cw[:, c * 3 + 1:c * 3 + 2], None, op0=ALU.mult), reads=["Z", "cw"], writes=["t1a"])
                p.dve(lambda e, c=c: e.scalar_tensor_tensor(t1[:], Z[:, 0:S], cw[:, c * 3:c * 3 + 1], t1[:], op0=ALU.mult, op1=ALU.add),
                      reads=["Z", "ZL", "cw", "t1a"], writes=["t1b"])
                p.dve(lambda e, c=c: e.scalar_tensor_tensor(t1[:], Z[:, 2:2 + S], cw[:, c * 3 + 2:c * 3 + 3], t1[:], op0=ALU.mult, op1=ALU.add),
                      reads=["Z", "ZR", "cw", "t1b"], writes=["t1c"])
                p.pool(lambda e: e.tensor_tensor(yo[:], t1[:], BGr[:], ALU.mult), reads=["t1c", "BGr"], writes=["yo"])
                p.dma("sp", "yo", lambda e, c=c: e.dma_start(out=MIX_d[4 + c, :, :], in_=yo[:]), reads=["yo"], writes=["MIXd"])
            ph.done()
            wout_phase(wout1_d, 1)

        if tail_only:
            router_moe(1)
        else:
            if stage >= 1:
                layer0_mixer()
            if stage >= 2 and stage < 20:
                router_moe(0)
            if stage >= 3 and stage < 10 and stage != 5:
                layer1_mixer()
            if stage >= 4 and stage < 10 and stage != 6:
                router_moe(1)
        if stage == 6:
            for _ in range(3):
                ph = Phase(nc)
                ph.p.dve(lambda e: e.memset(EPSB[:], EPS), writes=["epsb"])
                ph.done()
        store_phase()
    return nc


def _perm64():
    d = np.arange(64)
    return np.where((d % 32) < 16, d + 16, d - 16)


def prep_inputs(inputs):
    f = lambda a: np.ascontiguousarray(np.asarray(a, dtype=np.float32))
    I = {k: np.asarray(v) for k, v in inputs.items()}
    shared = {}
    shared["ident"] = np.eye(128, dtype=np.float32)
    shared["mod_w"] = f(I["mod_w"])
    shared["mod_bT"] = f(I["mod_b"].reshape(2, 48, 128).transpose(2, 0, 1).reshape(128, 96))
    ng = np.stack([I["norm1_g"], I["norm2_g"]], 0)
    shared["norm_g"] = f(ng.reshape(2, 2, 8, 128).transpose(3, 0, 1, 2).reshape(128, 32))
    w_in0 = I["even_w_in"][0]
    pi = _perm64()
    qcols, qpcols = [], []
    for j in range(4):
        for h in (j, j + 4):
            qcols.append(h * 64 + np.arange(64))
            qpcols.append(h * 64 + pi)
    qcols = np.concatenate(qcols)
    qpcols = np.concatenate(qpcols)
    kcols = 512 + np.arange(128)
    kpcols = 512 + np.concatenate([pi, 64 + pi])
    pcols = 768 + np.arange(512)
    allc = np.concatenate([qcols, kcols, pcols, qpcols, kpcols])
    shared["w_qkp"] = f(w_in0[:, allc])
    shared["w_v"] = f(w_in0[:, 640:768])
    qg = I["q_gain"][0]
    kg = I["k_gain"][0]
    d = np.arange(128) % 64
    shared["gains"] = f(np.stack([qg[d], qg[pi[d]], kg[d], kg[pi[d]]], 1))
    t = np.arange(S)
    row = (t // 64).astype(np.float32)
    col = (t % 64).astype(np.float32)
    inv = (10000.0 ** (-np.arange(0, 16, dtype=np.float32) * 2 / 32.0)).astype(np.float32)
    cos_t = np.zeros((128, S), np.float32)
    sin_t = np.zeros((128, S), np.float32)
    for pp in range(128):
        dd = pp % 64
        jj = dd % 16
        pos = row if dd < 32 else col
        ang = (pos * inv[jj]).astype(np.float32)
        sgn = -1.0 if (dd % 32) < 16 else 1.0
        cos_t[pp] = np.cos(ang)
        sin_t[pp] = sgn * np.sin(ang)
    shared["cos_t"] = cos_t
    shared["sin_t"] = sin_t
    shared["pool_wT"] = f(I["pool_w"][0].transpose(1, 0, 2).reshape(128, 512))
    shared["pool_sc"] = f(I["pool_scale"][0].reshape(4, 128).T)
    edge = np.zeros((128, 4, 16), np.float32)
    for g, w in enumerate((2, 4, 8, 16)):
        for jx in range(16):
            tpos = jx if jx < 8 else S - 16 + jx
            lo = max(tpos - w // 2, 0)
            hi = min(tpos + w - w // 2, S)
            edge[:, g, jx] = 1.0 / (hi - lo)
    shared["pool_edge"] = edge.reshape(128, 64)
    w_out0 = I["even_w_out"][0]
    rows = []
    for c in range(4):
        for h in (c, c + 4):
            rows.append(h * 64 + np.arange(64))
    rows.append(512 + np.arange(512))
    shared["w_out0"] = f(w_out0[np.concatenate(rows), :])
    shared["w_out1"] = f(I["odd_w_out"][0])
    shared["w_in1"] = f(I["odd_w_in"][0])
    shared["sg_gain_b"] = f(np.broadcast_to(I["sg_gain"][0].reshape(1, 512), (128, 512)))
    shared["sg_wT"] = f(I["sg_w"][0].transpose(2, 0, 1).reshape(128, 512))
    shared["sg_b_b"] = f(np.broadcast_to(I["sg_b"][0].reshape(1, 512), (128, 512)))
    shared["conv_wT"] = f(I["conv_w"][0][:, 0, :].reshape(3, 4, 128).transpose(2, 1, 0).reshape(128, 12))
    shared["rw"] = f(np.concatenate([I["router_g_w"], I["router_e_w"]], axis=2))
    rb = np.concatenate([I["router_g_b"], I["router_e_b"]], axis=1)
    shared["rb_b"] = f(np.broadcast_to(np.tile(rb[:, None, :], (1, 4, 1)).reshape(1, 160), (128, 160)))
    shared["w_gate"] = f(I["w_gate"])
    shared["w_up"] = f(I["w_up"])
    shared["w_down"] = f(I["w_down"])
    sel = np.zeros((32, 16, 128), np.float32)
    for ex in range(16):
        sel[ex, ex, :] = 1.0
        sel[16 + ex, ex, :] = 1.0
    shared["sel"] = sel.reshape(32, 2048)
    in_maps = []
    for b in range(NCORES):
        m = dict(shared)
        m["x"] = f(I["x"][b])
        m["ctx"] = f(I["ctx"][b])
        cv = np.stack([I["c"][b], I["c_ctx"]], 0)
        m["cvec"] = f(cv.reshape(2, 8, 128).transpose(2, 1, 0).reshape(128, 16))
        in_maps.append(m)
    return in_maps


_NC_CACHE = {}


def kernel(**inputs):
    in_maps = prep_inputs(inputs)
    if "nc" not in _NC_CACHE:
        _NC_CACHE["nc"] = (build(stage=3), build(tail_only=True))
    nc1, nc2 = _NC_CACHE["nc"]
    res = run_bass_kernel_spmd(nc1, in_maps, core_ids=list(range(NCORES)))
    for b in range(NCORES):
        in_maps[b]["x"] = np.ascontiguousarray(np.asarray(res.results[b]["out"], dtype=np.float32))
    res = run_bass_kernel_spmd(nc2, in_maps, core_ids=list(range(NCORES)))
    out = np.stack([np.asarray(r["out"]) for r in res.results], axis=0)
    return out.astype(np.float32)
```

```python
import numpy as np
from contextlib import ExitStack
import concourse.bass as bass
import concourse.mybir as mybir
from concourse.bass_utils import run_bass_kernel_spmd

F32 = mybir.dt.float32
BF16 = mybir.dt.bfloat16
AF = mybir.ActivationFunctionType
ALU = mybir.AluOpType
AX = mybir.AxisListType

ENGS = ["pe", "act", "dve", "pool", "sp"]
S = 4096
D = 1024
NCORES = 8
EPS = 1e-6
_CNT = [0]
_PHASE = [0]
_SEMPOOL = [[], []]
NSEM = 16


class Op:
    __slots__ = ("eng", "fn", "reads", "writes", "dma_key", "idx", "waits", "signal", "semval")

    def __init__(self, eng, fn, reads, writes, dma_key):
        self.eng = eng
        self.fn = fn
        self.reads = reads
        self.writes = writes
        self.dma_key = dma_key
        self.waits = []
        self.signal = False
        self.semval = 0


class Prog:
    def __init__(self, nc):
        self.nc = nc
        self.ops = []

    def op(self, eng, fn, reads=(), writes=(), dma_key=None):
        reads = tuple(reads)
        writes = tuple(writes)
        ex = tuple(r for r in reads if r.startswith("ps"))
        o = Op(eng, fn, reads, writes + ex, dma_key)
        o.idx = len(self.ops)
        self.ops.append(o)
        return o

    def pe(self, fn, reads=(), writes=()):
        return self.op("pe", fn, reads, writes)

    def act(self, fn, reads=(), writes=()):
        return self.op("act", fn, reads, writes)

    def dve(self, fn, reads=(), writes=()):
        return self.op("dve", fn, reads, writes)

    def pool(self, fn, reads=(), writes=()):
        return self.op("pool", fn, reads, writes)

    def dma(self, eng, key, fn, reads=(), writes=()):
        return self.op(eng, fn, reads, writes, dma_key=key)

    def finalize(self):
        ops = self.ops

        def tl(o):
            return ("dma", o.dma_key) if o.dma_key is not None else o.eng

        pos = {}
        cnt = {}
        for o in ops:
            t = tl(o)
            cnt[t] = cnt.get(t, 0) + 1
            pos[o.idx] = cnt[t]
        last_writer = {}
        readers = {}
        known = {e: {} for e in ENGS}
        done_clock = {}
        needed = set()
        latest_on_key = {}
        for o in ops:
            deps = set()
            raw = set()
            for r in o.reads:
                w = last_writer.get(r)
                if w is not None:
                    deps.add(w)
                    raw.add(w)
            for r in o.writes:
                w = last_writer.get(r)
                if w is not None:
                    deps.add(w)
                    if r.startswith("ps"):
                        raw.add(w)
                for rd in readers.get(r, ()):
                    deps.add(rd)
            req = {}
            for d in deps:
                if d == o.idx:
                    continue
                po = ops[d]
                t = tl(po)
                if po.dma_key is None and o.dma_key is None and po.eng == o.eng:
                    if o.eng == "pe":
                        continue
                    if d not in raw:
                        continue
                if t not in req or pos[d] > pos[req[t]]:
                    req[t] = d
            kn = known[o.eng]
            for t in list(req.keys()):
                if not isinstance(t, str):
                    req[t] = latest_on_key[t]
            waits = []
            for t, d in req.items():
                if kn.get(t, 0) >= pos[d]:
                    continue
                waits.append(d)
                needed.add(d)
                for t2, p2 in done_clock[d].items():
                    if kn.get(t2, 0) < p2:
                        kn[t2] = p2
            o.waits = waits
            dc = dict(kn)
            dc[tl(o)] = max(dc.get(tl(o), 0), pos[o.idx])
            done_clock[o.idx] = dc
            if o.dma_key is not None:
                latest_on_key[tl(o)] = o.idx
            for r in o.writes:
                last_writer[r] = o.idx
                readers[r] = []
            for r in o.reads:
                if r not in o.writes:
                    readers.setdefault(r, []).append(o.idx)
        semcnt = {}
        for o in ops:
            t = tl(o)
            if o.dma_key is not None:
                semcnt[t] = semcnt.get(t, 0) + 16
                o.signal = True
                o.semval = semcnt[t]
            elif o.idx in needed:
                semcnt[t] = semcnt.get(t, 0) + 1
                o.signal = True
                o.semval = semcnt[t]
        self.timelines = sorted(set(tl(o) for o in ops if o.signal), key=str)
        self._tl = tl
        return self

    def emit(self):
        nc = self.nc
        ops = self.ops
        tl = self._tl
        with ExitStack() as es:
            pidx = _PHASE[0] % 2
            _PHASE[0] += 1
            mypool = _SEMPOOL[pidx]
            other = _SEMPOOL[1 - pidx]
            assert len(self.timelines) <= len(mypool), len(self.timelines)
            sems = {}
            for i, t in enumerate(self.timelines):
                sems[t] = mypool[i]
            block = es.enter_context(nc.Block())
            by_eng = {e: [o for o in ops if o.eng == e] for e in ENGS}
            final_dma = {}
            for o in ops:
                if o.dma_key is not None:
                    final_dma[tl(o)] = o.semval

            def run(engname, eng):
                for o in by_eng[engname]:
                    ws = list(o.waits)
                    att = None
                    if ws and engname != "pe":
                        att = ws.pop()
                    for d in ws:
                        po = ops[d]
                        eng.wait_ge(sems[tl(po)], po.semval)
                    if att is not None:
                        rec = _Rec(eng)
                        ins = o.fn(rec)
                        po = ops[att]
                        rec.first._wait_ge(sems[tl(po)], po.semval)
                    else:
                        ins = o.fn(eng)
                    if o.signal:
                        ins.then_inc(sems[tl(o)], 16 if o.dma_key is not None else 1)
                if engname == "sp":
                    for t, v in final_dma.items():
                        eng.wait_ge(sems[t], v)
                    for sm in other:
                        eng.sem_clear(sm)

            @block.tensor
            def _(eng):
                run("pe", eng)

            @block.scalar
            def _(eng):
                run("act", eng)

            @block.vector
            def _(eng):
                run("dve", eng)

            @block.gpsimd
            def _(eng):
                run("pool", eng)

            @block.sync
            def _(eng):
                run("sp", eng)


class _Rec:
    def __init__(self, eng):
        self._eng = eng
        self.first = None

    def __getattr__(self, name):
        f = getattr(self._eng, name)

        def g(*a, **k):
            r = f(*a, **k)
            if self.first is None:
                self.first = r
            return r
        return g


class Phase:
    def __init__(self, nc):
        self.nc = nc
        self.es = ExitStack()
        self.p = Prog(nc)
        self._n = 0

    def sb(self, shape, dt):
        self._n += 1
        _CNT[0] += 1
        return self.es.enter_context(self.nc.sbuf_tensor("sb%d" % _CNT[0], list(shape), dt))

    def psum4(self):
        r = []
        for _ in range(4):
            _CNT[0] += 1
            r.append(self.es.enter_context(self.nc.psum_tensor("ps%d" % _CNT[0], [128, 1024], F32)))
        return r

    def done(self):
        self.p.finalize()
        self.p.emit()
        self.es.close()


def build(stage=4, tail_only=False):
    nc = bass.Bass("TRN2", target_bir_lowering=False)

    def din(name, shape, dt=F32):
        return nc.dram_tensor(name, list(shape), dt, kind="ExternalInput").ap()

    def dscr(name, shape, dt):
        return nc.dram_tensor(name, list(shape), dt, kind="Internal").ap()

    x_d = din("x", [S, D])
    ctx_d = din("ctx", [256, D])
    cvec_d = din("cvec", [128, 16])
    ident_d = din("ident", [128, 128])
    modw_d = din("mod_w", [2, D, 6144])
    modb_d = din("mod_bT", [128, 96])
    ng_d = din("norm_g", [128, 32])
    wqkp_d = din("w_qkp", [D, 1792])
    wv_d = din("w_v", [D, 128])
    gains_d = din("gains", [128, 4])
    cos_d = din("cos_t", [128, S])
    sin_d = din("sin_t", [128, S])
    poolw_d = din("pool_wT", [128, 512])
    poolsc_d = din("pool_sc", [128, 4])
    pooledge_d = din("pool_edge", [128, 64])
    wout0_d = din("w_out0", [D, D])
    wout1_d = din("w_out1", [D, D])
    win1_d = din("w_in1", [D, 2560])
    sggain_d = din("sg_gain_b", [128, 512])
    sgw_d = din("sg_wT", [128, 512])
    sgb_d = din("sg_b_b", [128, 512])
    convw_d = din("conv_wT", [128, 12])
    rw_d = din("rw", [2, D, 20])
    rb_d = din("rb_b", [128, 160])
    wg_d = din("w_gate", [2, 16, D, 256])
    wu_d = din("w_up", [2, 16, D, 256])
    wd_d = din("w_down", [2, 16, 256, D])
    sel_d = din("sel", [32, 2048])
    out_d = nc.dram_tensor("out", [S, D], F32, kind="ExternalOutput").ap()

    A2_d = dscr("A2s", [8, 128, S], BF16)
    QT_d = dscr("QTs", [4, 128, S], BF16)
    MIX_d = dscr("MIXs", [8, 128, S], BF16)
    PT_d = dscr("PTs", [4, 128, S], F32)
    BG_d = dscr("BGs", [4, 128, S], F32)
    KT_d = dscr("KTs", [128, 4352], BF16)
    VS_d = dscr("VSs", [128, 34, 193], BF16)

    with ExitStack() as top:
        def psb(shape, dt):
            _CNT[0] += 1
            return top.enter_context(nc.sbuf_tensor("pt%d" % _CNT[0], list(shape), dt))

        _PHASE[0] = 0
        for pi_ in range(2):
            _SEMPOOL[pi_] = []
            for si_ in range(NSEM):
                _SEMPOOL[pi_].append(top.enter_context(nc.semaphore("sp%d_%d" % (pi_, si_))))
        HT = psb([128, 8, S], F32)
        ident = psb([128, 128], F32)
        ident_bf = psb([128, 128], BF16)
        ones_bf = psb([128, 128], BF16)
        onesblk_bf = psb([128, 128], BF16)
        ones_f = psb([128, 128], F32)
        MOD = psb([128, 2, 48], F32)
        MODC = psb([128, 16], F32)
        NG = psb([128, 32], F32)
        GS = psb([128, 4, 8], F32)
        GSC = psb([128, 8], F32)
        combT = psb([32, S], BF16)
        EPSB = psb([128, 1], F32)

        ph = Phase(nc)
        p = ph.p
        PS = ph.psum4()
        cvec = ph.sb([128, 16], F32)
        scv = ph.sb([128, 16], F32)
        modb = ph.sb([128, 96], F32)
        mrow = ph.sb([2, 6144], F32)
        mwbuf = [ph.sb([128, 8, 512], F32) for _ in range(2)]
        xin = [ph.sb([128, D], F32) for _ in range(2)]
        p.dma("sp", "c0", lambda e: e.dma_start(out=ident[:], in_=ident_d[:, :]), writes=["ident"])
        p.dma("sp", "c0", lambda e: e.dma_start(out=cvec[:], in_=cvec_d[:, :]), writes=["cvec"])
        p.dma("sp", "c0", lambda e: e.dma_start(out=modb[:], in_=modb_d[:, :]), writes=["modb"])
        p.dma("sp", "c0", lambda e: e.dma_start(out=NG[:], in_=ng_d[:, :]), writes=["NG"])
        p.dve(lambda e: e.tensor_copy(ident_bf[:], ident[:]), reads=["ident"], writes=["ident_bf"])
        p.dve(lambda e: e.memset(ones_bf[:], 1.0 / 1024.0), writes=["ones_bf"])
        p.dve(lambda e: e.memset(ones_f[:], 1.0), writes=["ones_f"])
        p.dve(lambda e: e.memset(EPSB[:], EPS), writes=["epsb"])
        p.dve(lambda e: e.memset(onesblk_bf[:], 0.0), writes=["onesblk0"])
        p.dve(lambda e: e.memset(onesblk_bf[0:64, 0:64], 1.0 / 64.0), reads=["onesblk0"], writes=["onesblk1"])
        p.dve(lambda e: e.memset(onesblk_bf[64:128, 64:128], 1.0 / 64.0), reads=["onesblk1"], writes=["onesblk"])
        p.act(lambda e: e.activation(scv[:], cvec[:], AF.Silu), reads=["cvec"], writes=["scv"])
        for l in range(2):
            for fb in range(12):
                it = l * 12 + fb
                buf = mwbuf[it % 2]
                bk = "mw%d" % (it % 2)
                p.dma("sp", bk, lambda e, buf=buf, l=l, fb=fb: e.dma_start(
                    out=buf[:], in_=modw_d[l, :, fb * 512:(fb + 1) * 512].rearrange("(c p) n -> p c n", p=128)),
                    writes=[bk])
                pb = "psA%d" % (it % 2)
                pst = PS[0][:, (it % 2) * 512:(it % 2) * 512 + 512]

                def mm(e, buf=buf, pst=pst):
                    for c in range(8):
                        ins = e.matmul(pst[0:2, :], scv[:, c * 2:c * 2 + 2], buf[:, c, :], start=(c == 0), stop=(c == 7))
                    return ins
                p.pe(mm, reads=[bk, "scv"], writes=[pb])
                if it % 2 == 0:
                    p.act(lambda e, pst=pst, fb=fb: e.activation(mrow[0:2, fb * 512:(fb + 1) * 512], pst[0:2, :], AF.Identity),
                          reads=[pb], writes=["mrow_%d" % fb])
                else:
                    p.dve(lambda e, pst=pst, fb=fb: e.tensor_copy(mrow[0:2, fb * 512:(fb + 1) * 512], pst[0:2, :]),
                          reads=[pb], writes=["mrow_%d" % fb])

            def tr(e):
                for j in range(48):
                    ins = e.matmul(PS[1][:, j * 2:j * 2 + 2], mrow[0:2, j * 128:(j + 1) * 128], ident[0:2, 0:2], start=True, stop=True)
                return ins
            p.pe(tr, reads=["mrow_%d" % fb for fb in range(12)] + ["ident"], writes=["psB0"])
            p.dve(lambda e, l=l: e.tensor_tensor(MOD[:, l, :], PS[1][:, 0:96].rearrange("p (j t) -> p j t", t=2)[:, :, 0],
                                                modb[:, l * 48:(l + 1) * 48], ALU.add),
                  reads=["psB0", "modb"], writes=["MOD%d" % l])
            if l == 0:
                p.dve(lambda e: e.tensor_tensor(MODC[:], PS[1][:, 0:32].rearrange("p (j t) -> p j t", t=2)[:, :, 1],
                                                modb[:, 0:16], ALU.add),
                      reads=["psB0", "modb"], writes=["MODC"])
        for l in range(2):
            for n in range(2):
                sc = MOD[:, l, (1 + 3 * n) * 8:(2 + 3 * n) * 8]
                p.dve(lambda e, l=l, n=n, sc=sc: e.scalar_tensor_tensor(GS[:, l * 2 + n, :], sc, 1.0, NG[:, n * 16 + l * 8:n * 16 + l * 8 + 8],
                                                                      op0=ALU.add, op1=ALU.mult),
                      reads=["MOD%d" % l, "NG"], writes=["GS%d%d" % (l, n)])
        p.dve(lambda e: e.scalar_tensor_tensor(GSC[:], MODC[:, 8:16], 1.0, NG[:, 0:8], op0=ALU.add, op1=ALU.mult),
              reads=["MODC", "NG"], writes=["GSC"])
        for tt in range(32):
            xb = xin[tt % 2]
            xk = "xin%d" % (tt % 2)
            p.dma("sp", xk, lambda e, xb=xb, tt=tt: e.dma_start(out=xb[:], in_=x_d[tt * 128:(tt + 1) * 128, :]), writes=[xk])
            pst = PS[2 + tt % 2]
            pk = "psX%d" % (tt % 2)

            def trx(e, xb=xb, pst=pst):
                for c in range(8):
                    ins = e.matmul(pst[:, c * 128:(c + 1) * 128], xb[:, c * 128:(c + 1) * 128], ident[:], start=True, stop=True)
                return ins
            p.pe(trx, reads=[xk, "ident"], writes=[pk])
            dst = HT[:, :, tt * 128:(tt + 1) * 128]
            src = pst[:, :].rearrange("p (c t) -> p c t", t=128)
            if tt % 2 == 0:
                p.act(lambda e, dst=dst, src=src: e.activation(dst, src, AF.Identity), reads=[pk], writes=["HT%d" % tt])
            else:
                p.dve(lambda e, dst=dst, src=src: e.tensor_copy(dst, src), reads=[pk], writes=["HT%d" % tt])
        ph.done()

        def norm_block(p, PSst, pskey, t0, T, gs, sh, sqb, rstd, tmpb, dst_fn, tag, src=None, srckey="HT"):
            srcT = HT if src is None else src
            for c in range(8):
                sq = sqb[c % 2]
                p.act(lambda e, sq=sq, c=c: e.activation(sq[:, 0:T], srcT[:, c, t0:t0 + T], AF.Square),
                      reads=[srckey], writes=["sq%s%d" % (tag, c % 2)])
                p.pe(lambda e, sq=sq, c=c: e.matmul(PSst[:, 0:T], ones_bf[:], sq[:, 0:T], start=(c == 0), stop=(c == 7)),
                     reads=["sq%s%d" % (tag, c % 2), "ones_bf"], writes=[pskey])
            p.act(lambda e: e.activation(rstd[:, 0:T], PSst[:, 0:T], AF.Sqrt, bias=EPSB[:, 0:1]), reads=[pskey], writes=["rstd0" + tag])
            p.dve(lambda e: e.reciprocal(rstd[:, 0:T], rstd[:, 0:T]), reads=["rstd0" + tag], writes=["rstd" + tag])
            for c in range(8):
                tb = tmpb[c % 2]
                p.dve(lambda e, tb=tb, c=c: e.scalar_tensor_tensor(tb[:, 0:T], srcT[:, c, t0:t0 + T], gs[:, c:c + 1], rstd[:, 0:T],
                                                                  op0=ALU.mult, op1=ALU.mult),
                      reads=[srckey, "rstd" + tag], writes=["tmp%s%d" % (tag, c % 2)])
                dst, dkey = dst_fn(c)
                p.act(lambda e, tb=tb, c=c, dst=dst: e.activation(dst, tb[:, 0:T], AF.Identity, bias=sh[:, c:c + 1]),
                      reads=["tmp%s%d" % (tag, c % 2)], writes=[dkey])

        def wout_phase(wout_d, l):
            ph = Phase(nc)
            p = ph.p
            PS = ph.psum4()
            w = ph.sb([128, 8, D], BF16)
            mixb = [ph.sb([128, 8, 512], BF16) for _ in range(2)]
            for c in range(8):
                p.dma("pool", "w", lambda e, c=c: e.dma_start(out=w[:, c, :], in_=wout_d[c * 128:(c + 1) * 128, :]), writes=["w"])
            for b in range(8):
                mb = mixb[b % 2]
                mk = "mix%d" % (b % 2)
                p.dma("sp", mk, lambda e, mb=mb, b=b: e.dma_start(out=mb[:], in_=MIX_d[:, :, b * 512:(b + 1) * 512].rearrange("c p t -> p c t")),
                      writes=[mk])
                for f in range(8):
                    pst = PS[f % 4][:, 0:512]
                    pk = "psY%d" % (f % 4)

                    def mm(e, mb=mb, f=f, pst=pst):
                        for k in range(8):
                            ins = e.matmul(pst, w[:, k, f * 128:(f + 1) * 128], mb[:, k, :], start=(k == 0), stop=(k == 7))
                        return ins
                    p.pe(mm, reads=["w", mk], writes=[pk])
                    hsl = HT[:, f, b * 512:(b + 1) * 512]
                    p.dve(lambda e, pst=pst, f=f, hsl=hsl: e.scalar_tensor_tensor(hsl, pst, MOD[:, l, 16 + f:17 + f], hsl, op0=ALU.mult, op1=ALU.add),
                          reads=[pk], writes=["HT"])
            ph.done()

        def router_moe(l):
            ph = Phase(nc)
            p = ph.p
            PS = ph.psum4()
            sqb = [ph.sb([128, 512], BF16) for _ in range(2)]
            rstd = ph.sb([128, 512], F32)
            tmpb = [ph.sb([128, 512], F32) for _ in range(2)]
            a2f = ph.sb([128, 8, 512], F32)
            a2b = [ph.sb([128, 8, 512], BF16) for _ in range(2)]
            rw = ph.sb([128, 8, 20], F32)
            rbb = ph.sb([128, 80], F32)
            lgT = ph.sb([32, 512], F32)
            LG = ph.sb([128, 32, 20], F32)
            p.dma("sp", "rw", lambda e: e.dma_start(out=rw[:], in_=rw_d[l].rearrange("(c p) n -> p c n", p=128)), writes=["rw"])
            p.dma("sp", "rw", lambda e: e.dma_start(out=rbb[:], in_=rb_d[:, l * 80:(l + 1) * 80]), writes=["rbb"])
            gs = GS[:, l * 2 + 1, :]
            sh = MOD[:, l, 24:32]
            for b in range(8):
                af = a2f
                ab = a2b[b % 2]
                abk = "a2b%d" % (b % 2)
                norm_block(p, PS[0], "psS", b * 512, 512, gs, sh, sqb, rstd, tmpb,
                           lambda c, af=af: (af[:, c, :], "a2f_%d" % c), "n")
                akeys = ["a2f_%d" % c for c in range(8)]
                p.pool(lambda e, af=af, ab=ab: e.tensor_copy(ab[:], af[:]), reads=akeys, writes=[abk])
                p.dma("sp", "a2st%d" % (b % 2), lambda e, ab=ab, b=b: e.dma_start(
                    out=A2_d[:, :, b * 512:(b + 1) * 512].rearrange("c p t -> p c t"), in_=ab[:]), reads=[abk], writes=["A2"])

                def rmm(e, af=af):
                    for c in range(8):
                        ins = e.matmul(PS[1][0:20, 0:512], rw[:, c, :], af[:, c, :], start=(c == 0), stop=(c == 7))
                    return ins
                p.pe(rmm, reads=["rw"] + akeys, writes=["psR"])
                p.act(lambda e: e.activation(lgT[0:20, :], PS[1][0:20, 0:512], AF.Identity), reads=["psR"], writes=["lgT"])

                def rtr(e):
                    for tt in range(4):
                        ins = e.matmul(PS[2][:, tt * 32:tt * 32 + 20], lgT[0:20, tt * 128:(tt + 1) * 128], ident[0:20, 0:20], start=True, stop=True)
                    return ins
                p.pe(rtr, reads=["lgT", "ident"], writes=["psT"])
                p.dve(lambda e, b=b: e.tensor_tensor(LG[:, b * 4:(b + 1) * 4, :], PS[2][:, 0:128].rearrange("p (t n) -> p t n", n=32)[:, :, 0:20],
                                                    rbb[:].rearrange("p (t n) -> p t n", n=20), ALU.add),
                      reads=["psT", "rbb"], writes=["LG"])
            NT = 32
            gmax = ph.sb([128, NT], F32)
            ohg = ph.sb([128, NT, 4], F32)
            gd = ph.sb([128, NT, 4], F32)
            gsum = ph.sb([128, NT], F32)
            gp = ph.sb([128, NT], F32)
            t44 = ph.sb([128, NT, 4, 4], F32)
            esel = ph.sb([128, NT, 4], F32)
            e1 = ph.sb([128, NT], F32)
            sel1 = ph.sb([128, NT, 4], F32)
            em = ph.sb([128, NT, 4], F32)
            e2 = ph.sb([128, NT], F32)
            sel2 = ph.sb([128, NT, 4], F32)
            dd = ph.sb([128, NT], F32)
            w1 = ph.sb([128, NT], F32)
            w2 = ph.sb([128, NT], F32)
            ce = ph.sb([128, NT, 4], F32)
            ce2 = ph.sb([128, NT, 4], F32)
            comb = ph.sb([128, NT, 16], F32)
            chl = ph.sb([128, NT, 32], BF16)
            chf = ph.sb([128, NT, 16], F32)
            gl = LG[:, :, 0:4]
            el = LG[:, :, 4:20].rearrange("p n (g i) -> p n g i", i=4)

            def b3(ap2):
                return ap2.unsqueeze(2).to_broadcast([128, NT, 4])
            p.dve(lambda e: e.tensor_reduce(gmax[:], gl, AX.X, ALU.max), reads=["LG"], writes=["gmax"])
            p.dve(lambda e: e.tensor_tensor(ohg[:], gl, b3(gmax[:]), ALU.is_equal), reads=["LG", "gmax"], writes=["ohg"])
            p.dve(lambda e: e.tensor_tensor(gd[:], gl, b3(gmax[:]), ALU.subtract), reads=["LG", "gmax"], writes=["gd"])
            p.act(lambda e: e.activation(gd[:], gd[:], AF.Exp), reads=["gd"], writes=["ge"])
            p.dve(lambda e: e.tensor_reduce(gsum[:], gd[:], AX.X, ALU.add), reads=["ge"], writes=["gsum"])
            p.dve(lambda e: e.reciprocal(gp[:], gsum[:]), reads=["gsum"], writes=["gp"])
            p.dve(lambda e: e.tensor_tensor(t44[:], el, ohg[:].unsqueeze(3).to_broadcast([128, NT, 4, 4]), ALU.mult),
                  reads=["LG", "ohg"], writes=["t44"])
            p.dve(lambda e: e.tensor_reduce(esel[:], t44[:].rearrange("p n g i -> p n i g"), AX.X, ALU.add), reads=["t44"], writes=["esel"])
            p.dve(lambda e: e.tensor_reduce(e1[:], esel[:], AX.X, ALU.max), reads=["esel"], writes=["e1"])
            p.dve(lambda e: e.tensor_tensor(sel1[:], esel[:], b3(e1[:]), ALU.is_equal), reads=["esel", "e1"], writes=["sel1"])
            p.dve(lambda e: e.scalar_tensor_tensor(em[:], sel1[:], -1e30, esel[:], op0=ALU.mult, op1=ALU.add), reads=["sel1", "esel"], writes=["em"])
            p.dve(lambda e: e.tensor_reduce(e2[:], em[:], AX.X, ALU.max), reads=["em"], writes=["e2"])
            p.dve(lambda e: e.tensor_tensor(sel2[:], em[:], b3(e2[:]), ALU.is_equal), reads=["em", "e2"], writes=["sel2"])
            p.dve(lambda e: e.tensor_tensor(dd[:], e2[:], e1[:], ALU.subtract), reads=["e1", "e2"], writes=["dd"])
            p.act(lambda e: e.activation(dd[:], dd[:], AF.Exp), reads=["dd"], writes=["ex"])
            p.dve(lambda e: e.tensor_scalar(w1[:], dd[:], 1.0, None, op0=ALU.add), reads=["ex"], writes=["w1a"])
            p.dve(lambda e: e.reciprocal(w1[:], w1[:]), reads=["w1a"], writes=["w1"])
            p.dve(lambda e: e.tensor_tensor(w2[:], dd[:], w1[:], ALU.mult), reads=["ex", "w1"], writes=["w2a"])
            p.dve(lambda e: e.tensor_tensor(w1[:], w1[:], gp[:], ALU.mult), reads=["w1", "gp", "w2a"], writes=["wt1"])
            p.dve(lambda e: e.tensor_tensor(w2[:], w2[:], gp[:], ALU.mult), reads=["w2a", "gp"], writes=["wt2"])
            p.dve(lambda e: e.tensor_tensor(ce[:], sel1[:], b3(w1[:]), ALU.mult), reads=["sel1", "wt1"], writes=["ce"])
            p.dve(lambda e: e.tensor_tensor(ce2[:], sel2[:], b3(w2[:]), ALU.mult), reads=["sel2", "wt2"], writes=["ce2"])
            p.dve(lambda e: e.tensor_tensor(ce[:], ce[:], ce2[:], ALU.add), reads=["ce", "ce2"], writes=["cef"])
            p.dve(lambda e: e.tensor_tensor(comb[:].rearrange("p n (g i) -> p n g i", i=4),
                                            ohg[:].unsqueeze(3).to_broadcast([128, NT, 4, 4]),
                                            ce[:].unsqueeze(2).to_broadcast([128, NT, 4, 4]), ALU.mult),
                  reads=["ohg", "cef"], writes=["comb"])
            p.dve(lambda e: e.tensor_copy(chl[:, :, 0:16], comb[:]), reads=["comb"], writes=["chi"])
            p.dve(lambda e: e.tensor_copy(chf[:], chl[:, :, 0:16]), reads=["chi"], writes=["chf"])
            p.dve(lambda e: e.tensor_tensor(chf[:], comb[:], chf[:], ALU.subtract), reads=["comb", "chf"], writes=["clo"])
            p.dve(lambda e: e.tensor_copy(chl[:, :, 16:32], chf[:]), reads=["clo"], writes=["chl"])
            for q in range(8):
                pst = PS[3][:, (q % 2) * 512:(q % 2) * 512 + 512]
                pk = "psC%d" % (q % 2)

                def ctr(e, q=q, pst=pst):
                    for tt in range(4):
                        ins = e.matmul(pst[0:32, tt * 128:(tt + 1) * 128], chl[:, q * 4 + tt, :], ident_bf[:], start=True, stop=True)
                    return ins
                p.pe(ctr, reads=["chl", "chi", "ident_bf"], writes=[pk])
                p.act(lambda e, q=q, pst=pst: e.activation(combT[:, q * 512:(q + 1) * 512], pst[0:32, :], AF.Identity), reads=[pk], writes=["combT"])
            ph.done()
            if stage == 10 + l:
                return
            ph = Phase(nc)
            p = ph.p
            PS = ph.psum4()
            T = 512
            NB = S // T
            wslot = [(ph.sb([128, 8, 256], BF16), ph.sb([128, 8, 256], BF16), ph.sb([128, 2, D], BF16)) for _ in range(3)]
            a2 = [ph.sb([128, 8, T], BF16) for _ in range(2)]
            sel = ph.sb([32, 2048], BF16)
            cb = ph.sb([128, T], F32)
            sg = [ph.sb([128, T], F32)] * 2
            tt_ = ph.sb([128, T], F32)
            hb0 = [ph.sb([128, 2, T], BF16) for _ in range(2)]
            hb1 = ph.sb([128, 2, T], BF16)
            p.dma("pool", "sel", lambda e: e.dma_start(out=sel[:, 0:1024], in_=sel_d[:, 0:1024]), writes=["sel"])
            p.dma("pool", "sel", lambda e: e.dma_start(out=sel[:, 1024:2048], in_=sel_d[:, 1024:2048]), writes=["sel"])
            g2 = MOD[:, l, 40:48]
            RB = [PS[0][:, 0:512], PS[0][:, 512:1024], PS[1][:, 0:512]]
            RBK = ["psR0", "psR1", "psR2"]
            CBP = PS[1][:, 512:1024]
            ACC = [PS[2][:, 0:512], PS[2][:, 512:1024], PS[3][:, 0:512], PS[3][:, 512:1024]]

            def load_expert(ex):
                sl = ex % 3
                wg, wu, wd = wslot[sl]
                wk = "w%d" % sl
                p.dma("pool", wk, lambda e: e.dma_start(out=wg[:], in_=wg_d[l, ex].rearrange("(c p) n -> p c n", p=128)), writes=[wk + "g"])
                p.dma("pool", wk, lambda e: e.dma_start(out=wu[:], in_=wu_d[l, ex].rearrange("(c p) n -> p c n", p=128)), writes=[wk + "u"])
                p.dma("pool", wk, lambda e: e.dma_start(out=wd[:], in_=wd_d[l, ex].rearrange("(c p) n -> p c n", p=128)), writes=[wk + "d"])
            st = {"step": 0, "rk": 0, "sgi": 0}

            def hbuf(b, j):
                if j == 0:
                    return hb0[b % 2], "h0_%d" % (b % 2)
                return hb1, "h1"

            def G(pr, b, j):
                ex = pr * 2 + j
                sl = ex % 3
                wg, wu, wd = wslot[sl]
                wk = "w%d" % sl
                if j == 0:
                    ab = a2[st["step"] % 2]
                    ak = "a2_%d" % (st["step"] % 2)
                    st["cur"] = (ab, ak)
                    st["step"] += 1
                    p.dma("sp", ak, lambda e: e.dma_start(out=ab[:], in_=A2_d[:, :, b * T:(b + 1) * T].rearrange("c p t -> p c t")), writes=[ak])
                ab, ak = st["cur"]
                hbb, hk = hbuf(b, j)
                p.pe(lambda e: e.matmul(CBP, sel[:, ex * 128:(ex + 1) * 128], combT[:, b * T:(b + 1) * T], start=True, stop=True),
                     reads=["sel", "combT"], writes=["psCB"])
                p.act(lambda e: e.activation(cb[:], CBP, AF.Identity), reads=["psCB"], writes=["cb"])
                for f2 in range(2):
                    pg = RB[st["rk"] % 3]
                    pgk = RBK[st["rk"] % 3]
                    st["rk"] += 1
                    pu = RB[st["rk"] % 3]
                    puk = RBK[st["rk"] % 3]
                    st["rk"] += 1

                    def gmm(e, pg=pg, f2=f2):
                        for k in range(8):
                            ins = e.matmul(pg, wg[:, k, f2 * 128:(f2 + 1) * 128], ab[:, k, :], start=(k == 0), stop=(k == 7))
                        return ins

                    def umm(e, pu=pu, f2=f2):
                        for k in range(8):
                            ins = e.matmul(pu, wu[:, k, f2 * 128:(f2 + 1) * 128], ab[:, k, :], start=(k == 0), stop=(k == 7))
                        return ins
                    p.pe(gmm, reads=[wk + "g", ak], writes=[pgk])
                    p.pe(umm, reads=[wk + "u", ak], writes=[puk])
                    sgb = sg[st["sgi"] % 2]
                    sgk = "sg0"
                    st["sgi"] += 1
                    p.act(lambda e, sgb=sgb, pg=pg: e.activation(sgb[:], pg, AF.Silu), reads=[pgk], writes=[sgk])
                    p.dve(lambda e, pu=pu: e.tensor_tensor(tt_[:], pu, cb[:], ALU.mult), reads=[puk, "cb"], writes=["tt"])
                    p.pool(lambda e, sgb=sgb, f2=f2: e.tensor_tensor(hbb[:, f2, :], tt_[:], sgb[:], ALU.mult),
                           reads=["tt", sgk], writes=[hk + "_%d" % f2])

            def DOWN(pr, b, half):
                hs = []
                for j in range(2):
                    ex = pr * 2 + j
                    wg, wu, wd = wslot[ex % 3]
                    hbb, hk = hbuf(b, j)
                    hs.append((wd, "w%dd" % (ex % 3), hbb, hk))
                for fi in range(4):
                    f = half * 4 + fi

                    def dmm(e, fi=fi, f=f):
                        for j in range(2):
                            wd, wdk, hbb, hk = hs[j]
                            for k in range(2):
                                ins = e.matmul(ACC[fi], wd[:, k, f * 128:(f + 1) * 128], hbb[:, k, :], start=(j == 0 and k == 0), stop=(j == 1 and k == 1))
                        return ins
                    p.pe(dmm, reads=[hs[0][1], hs[1][1], hs[0][3] + "_0", hs[0][3] + "_1", hs[1][3] + "_0", hs[1][3] + "_1"], writes=["psD%d" % fi])
                for fi in range(4):
                    f = half * 4 + fi
                    hsl = HT[:, f, b * T:(b + 1) * T]
                    p.dve(lambda e, fi=fi, f=f, hsl=hsl: e.scalar_tensor_tensor(hsl, ACC[fi], g2[:, f:f + 1], hsl, op0=ALU.mult, op1=ALU.add),
                          reads=["psD%d" % fi], writes=["HT"])

            load_expert(0)
            load_expert(1)
            for pr in range(8):
                if pr > 0:
                    load_expert(2 * pr + 1)
                G(pr, 0, 0)
                G(pr, 0, 1)
                if pr < 7:
                    load_expert(2 * pr + 2)
                for b in range(NB):
                    DOWN(pr, b, 0)
                    if b + 1 < NB:
                        G(pr, b + 1, 0)
                    DOWN(pr, b, 1)
                    if b + 1 < NB:
                        G(pr, b + 1, 1)
            ph.done()

        def store_phase():
            ph = Phase(nc)
            p = ph.p
            PS = ph.psum4()
            ob = [ph.sb([128, D], F32) for _ in range(2)]
            for tt in range(32):
                pst = PS[tt % 2]
                pk = "psO%d" % (tt % 2)

                def tr(e, tt=tt, pst=pst):
                    for c in range(8):
                        ins = e.matmul(pst[:, c * 128:(c + 1) * 128], HT[:, c, tt * 128:(tt + 1) * 128], ident[:], start=True, stop=True)
                    return ins
                p.pe(tr, reads=["HT", "ident"], writes=[pk])
                o = ob[tt % 2]
                ok = "ob%d" % (tt % 2)
                if tt % 2 == 0:
                    p.act(lambda e, o=o, pst=pst: e.activation(o[:], pst[:, :], AF.Identity), reads=[pk], writes=[ok])
                else:
                    p.dve(lambda e, o=o, pst=pst: e.tensor_copy(o[:], pst[:, :]), reads=[pk], writes=[ok])
                p.dma("sp", "ost%d" % (tt % 2), lambda e, o=o, tt=tt: e.dma_start(out=out_d[tt * 128:(tt + 1) * 128, :], in_=o[:]), reads=[ok], writes=["out"])
            ph.done()

        def qk_chain(p, PS, psA, kA, psB, kB, psC, T, gcol, gpcol, cosb, sinb, tq, dst, dstkey, rope=True):
            sqq, rsq, t1, t2 = tq
            p.act(lambda e: e.activation(sqq[:, 0:T], psA, AF.Square), reads=[kA], writes=["sqq"])
            p.pe(lambda e: e.matmul(psC[:, 0:T], onesblk_bf[:], sqq[:, 0:T], start=True, stop=True), reads=["sqq", "onesblk"], writes=["psC"])
            p.act(lambda e: e.activation(rsq[:, 0:T], psC[:, 0:T], AF.Sqrt, bias=EPSB[:, 0:1]), reads=["psC"], writes=["rsq0"])
            p.dve(lambda e: e.reciprocal(rsq[:, 0:T], rsq[:, 0:T]), reads=["rsq0"], writes=["rsq"])
            if rope:
                p.dve(lambda e: e.scalar_tensor_tensor(t1[:, 0:T], psA, gcol, cosb[:, 0:T], op0=ALU.mult, op1=ALU.mult),
                      reads=[kA, "cosb"], writes=["t1"])
                p.dve(lambda e: e.scalar_tensor_tensor(t2[:, 0:T], psB, gpcol, sinb[:, 0:T], op0=ALU.mult, op1=ALU.mult),
                      reads=[kB, "sinb"], writes=["t2"])
                p.pool(lambda e: e.tensor_tensor(t1[:, 0:T], t1[:, 0:T], t2[:, 0:T], ALU.add), reads=["t1", "t2"], writes=["t3"])
                p.dve(lambda e: e.tensor_tensor(dst, t1[:, 0:T], rsq[:, 0:T], ALU.mult), reads=["t3", "rsq"], writes=[dstkey])
            else:
                p.dve(lambda e: e.scalar_tensor_tensor(dst, psA, gcol, rsq[:, 0:T], op0=ALU.mult, op1=ALU.mult),
                      reads=[kA, "rsq"], writes=[dstkey])

        def layer0_mixer():
            ph = Phase(nc)
            p = ph.p
            PS = ph.psum4()
            T = 256
            wq = ph.sb([128, 8, 1792], BF16)
            wv = ph.sb([128, 8, 128], BF16)
            gains = ph.sb([128, 4], F32)
            sqb = [ph.sb([128, T], BF16) for _ in range(2)]
            rstd = ph.sb([128, T], F32)
            tmpb = [ph.sb([128, T], F32) for _ in range(2)]
            aT = ph.sb([128, 8, T], BF16)
            cosb = ph.sb([128, T], F32)
            sinb = ph.sb([128, T], F32)
            tq = (ph.sb([128, T], BF16), ph.sb([128, T], F32), ph.sb([128, T], F32), ph.sb([128, T], F32))
            qf = [ph.sb([128, T], BF16) for _ in range(2)]
            pst_ = [ph.sb([128, T], F32) for _ in range(2)]
            vst = [ph.sb([128, 2, 193], BF16) for _ in range(2)]
            ctin = ph.sb([128, D], F32)
            CT = ph.sb([128, 8, 256], F32)
            p.dma("sp", "c1", lambda e: e.dma_start(out=gains[:], in_=gains_d[:, :]), writes=["gains"])
            for vi in range(2):
                p.dve(lambda e, vi=vi: e.memset(vst[vi][:], 0.0), writes=["vst%d" % vi])
                p.dve(lambda e, vi=vi: e.memset(vst[vi][:, :, 64:66], 1.0), reads=["vst%d" % vi], writes=["vst%d" % vi])
            for c in range(8):
                p.dma("pool", "wq", lambda e, c=c: e.dma_start(out=wq[:, c, :], in_=wqkp_d[c * 128:(c + 1) * 128, :]), writes=["wq"])
            p.dma("pool", "wq", lambda e: e.dma_start(out=wv[:], in_=wv_d.rearrange("(c p) n -> p c n", p=128)), writes=["wv"])
            B_ST = PS[0][:, 0:512]
            B_A = [PS[0][:, 512:1024], PS[1][:, 0:512]]
            B_B = [PS[1][:, 512:1024], PS[2][:, 0:512]]
            B_C = PS[2][:, 512:1024]
            B_M = [PS[3][:, 0:512], PS[3][:, 512:1024]]
            for tt in range(2):
                p.dma("sp", "ctin", lambda e, tt=tt: e.dma_start(out=ctin[:], in_=ctx_d[tt * 128:(tt + 1) * 128, :]), writes=["ctin"])
                for half in range(2):
                    bm = B_M[half]

                    def trc(e, half=half, bm=bm):
                        for c in range(4):
                            cc = half * 4 + c
                            ins = e.matmul(bm[:, c * 128:(c + 1) * 128], ctin[:, cc * 128:(cc + 1) * 128], ident[:], start=True, stop=True)
                        return ins
                    p.pe(trc, reads=["ctin", "ident"], writes=["psM%d" % half])
                    p.dve(lambda e, half=half, bm=bm, tt=tt: e.tensor_copy(CT[:, half * 4:half * 4 + 4, tt * 128:(tt + 1) * 128],
                                                                          bm.rearrange("p (c t) -> p c t", t=128)),
                          reads=["psM%d" % half], writes=["CT"])
            mcnt = [0]

            def proj(p, col0, bank, bkey, T=T):
                def mm(e):
                    for k in range(8):
                        ins = e.matmul(bank[:, 0:T], wq[:, k, col0:col0 + 128], aT[:, k, :], start=(k == 0), stop=(k == 7))
                    return ins
                p.pe(mm, reads=["wq"] + ["aT_%d" % c for c in range(8)], writes=[bkey])

            def vproj(p, tile0):
                i = mcnt[0] % 2
                mcnt[0] += 1
                bm = B_M[i]
                bk = "psM%d" % i
                vs_ = vst[i]

                def mm(e):
                    for t2 in range(2):
                        for k in range(8):
                            ins = e.matmul(bm[:, t2 * 128:(t2 + 1) * 128], aT[:, k, t2 * 128:(t2 + 1) * 128], wv[:, k, :], start=(k == 0), stop=(k == 7))
                    return ins
                p.pe(mm, reads=["wv"] + ["aT_%d" % c for c in range(8)], writes=[bk])
                p.dve(lambda e: e.tensor_copy(vs_[:, :, 0:64], bm[:, 0:256].rearrange("p (t n) -> p t n", n=128)[:, :, 0:64]),
                      reads=[bk], writes=["vst%da" % i])
                p.dve(lambda e: e.tensor_copy(vs_[:, :, 129:193], bm[:, 0:256].rearrange("p (t n) -> p t n", n=128)[:, :, 64:128]),
                      reads=[bk, "vst%da" % i], writes=["vst%d" % i])
                p.dma("sp", "vsst%d" % i, lambda e: e.dma_start(out=VS_d[:, tile0:tile0 + 2, :], in_=vs_[:]), reads=["vst%d" % i, "vst%da" % i], writes=["VSd"])

            norm_block(p, B_ST, "psST", 0, 256, GSC, MODC, sqb, rstd, tmpb, lambda c: (aT[:, c, :], "aT_%d" % c), "c", src=CT, srckey="CT")
            proj(p, 512, B_A[0], "psA0")
            qk_chain(p, PS, B_A[0][:, 0:T], "psA0", None, None, B_C, T, gains[:, 2:3], None, None, None, tq, qf[0][:, 0:T], "qf0", rope=False)
            p.dma("sp", "qst0", lambda e: e.dma_start(out=KT_d[:, 0:256], in_=qf[0][:, 0:T]), reads=["qf0"], writes=["KTd"])
            vproj(p, 0)
            qi = 1
            for b in range(S // T):
                t0 = b * T
                p.dma("sp", "cs", lambda e, t0=t0: e.dma_start(out=cosb[:], in_=cos_d[:, t0:t0 + T]), writes=["cosb"])
                p.dma("sp", "cs", lambda e, t0=t0: e.dma_start(out=sinb[:], in_=sin_d[:, t0:t0 + T]), writes=["sinb"])
                norm_block(p, B_ST, "psST", t0, T, GS[:, 0, :], MOD[:, 0, 0:8], sqb, rstd, tmpb, lambda c: (aT[:, c, :], "aT_%d" % c), "m")
                for j in range(5):
                    i = qi % 2
                    qi += 1
                    col = j * 128 if j < 4 else 512
                    colp = 1152 + j * 128 if j < 4 else 1664
                    proj(p, col, B_A[i], "psA%d" % i)
                    proj(p, colp, B_B[i], "psB%d" % i)
                    gcol = gains[:, 0:1] if j < 4 else gains[:, 2:3]
                    gpcol = gains[:, 1:2] if j < 4 else gains[:, 3:4]
                    qk_chain(p, PS, B_A[i][:, 0:T], "psA%d" % i, B_B[i][:, 0:T], "psB%d" % i, B_C, T, gcol, gpcol, cosb, sinb, tq,
                             qf[i][:, 0:T], "qf%d" % i)
                    if j < 4:
                        p.dma("sp", "qst%d" % i, lambda e, i=i, j=j, t0=t0: e.dma_start(out=QT_d[j, :, t0:t0 + T], in_=qf[i][:, 0:T]),
                              reads=["qf%d" % i], writes=["QTd"])
                    else:
                        p.dma("sp", "qst%d" % i, lambda e, i=i, t0=t0: e.dma_start(out=KT_d[:, 256 + t0:256 + t0 + T], in_=qf[i][:, 0:T]),
                              reads=["qf%d" % i], writes=["KTd"])
                for g in range(4):
                    i = mcnt[0] % 2
                    mcnt[0] += 1
                    proj(p, 640 + g * 128, B_M[i], "psM%d" % i)
                    p.act(lambda e, i=i: e.activation(pst_[i][:, 0:T], B_M[i][:, 0:T], AF.Identity), reads=["psM%d" % i], writes=["pst%d" % i])
                    p.dma("sp", "pst%d" % i, lambda e, i=i, g=g, t0=t0: e.dma_start(out=PT_d[g, :, t0:t0 + T], in_=pst_[i][:, 0:T]),
                          reads=["pst%d" % i], writes=["PTd"])
                vproj(p, 2 + 2 * b)
            ph.done()
            if stage == 20:
                return
            ph = Phase(nc)
            p = ph.p
            PS = ph.psum4()
            KT = ph.sb([128, 4352], BF16)
            VS = ph.sb([128, 34, 193], BF16)
            Qb = [ph.sb([128, 512], BF16) for _ in range(2)]
            Pb = [ph.sb([128, 1024], BF16) for _ in range(3)]
            rr = ph.sb([128, 512], F32)
            bcs = ph.sb([128, 512], F32)
            mixo = [ph.sb([128, 512], BF16) for _ in range(2)]
            p.dma("sp", "kt", lambda e: e.dma_start(out=KT[:], in_=KT_d[:, :]), writes=["KT"])
            p.dma("sp", "kt", lambda e: e.dma_start(out=VS[:], in_=VS_d[:, :, :]), writes=["VS"])
            SB_ = [PS[0], PS[1]]
            OA = PS[2][:, 0:512]
            OB = PS[2][:, 512:1024]
            BCA = PS[3][:, 0:512]
            BCB = PS[3][:, 512:1024]
            u = 0
            it = 0
            for j in range(4):
                for qb in range(8):
                    qt = Qb[u % 2]
                    qk_ = "Qb%d" % (u % 2)
                    p.dma("sp", qk_, lambda e, qt=qt, j=j, qb=qb: e.dma_start(out=qt[:], in_=QT_d[j, :, qb * 512:(qb + 1) * 512]), writes=[qk_])
                    for kt in range(24 if stage == 7 else 34):
                        sb_ = SB_[it % 2]
                        sk = "psS%d" % (it % 2)
                        pb = Pb[it % 3]
                        pk = "P%d" % (it % 3)
                        it += 1

                        def smm(e, sb_=sb_, qt=qt, kt=kt):
                            e.matmul(sb_[:, 0:512], KT[0:64, kt * 128:(kt + 1) * 128], qt[0:64, :], start=True, stop=True)
                            return e.matmul(sb_[:, 512:1024], KT[64:128, kt * 128:(kt + 1) * 128], qt[64:128, :], start=True, stop=True)
                        p.pe(smm, reads=["KT", qk_], writes=[sk])
                        p.act(lambda e, pb=pb, sb_=sb_: e.activation(pb[:], sb_[:, :], AF.Exp, scale=0.125), reads=[sk], writes=[pk])

                        def pv(e, pb=pb, kt=kt):
                            e.matmul(OA[0:65, :], VS[:, kt, 0:65], pb[:, 0:512], start=(kt == 0), stop=(kt == (23 if stage == 7 else 33)))
                            return e.matmul(OB[:, :], VS[:, kt, 65:193], pb[:, 512:1024], start=(kt == 0), stop=(kt == (23 if stage == 7 else 33)))
                        p.pe(pv, reads=["VS", pk], writes=["psOA", "psOB"])
                    p.dve(lambda e: e.reciprocal(rr[64:65, :], OA[64:65, :]), reads=["psOA"], writes=["rrA"])
                    p.dve(lambda e: e.reciprocal(rr[0:1, :], OB[0:1, :]), reads=["psOB"], writes=["rrB"])
                    p.pe(lambda e: e.matmul(BCA[0:64, :], ones_f[64:65, 0:64], rr[64:65, :], start=True, stop=True), reads=["rrA", "ones_f"], writes=["psBCA"])
                    p.pe(lambda e: e.matmul(BCB[:, :], ones_f[0:1, :], rr[0:1, :], start=True, stop=True), reads=["rrB", "ones_f"], writes=["psBCB"])
                    p.act(lambda e: e.activation(bcs[0:64, :], BCA[0:64, :], AF.Identity), reads=["psBCA"], writes=["bcsA"])
                    p.act(lambda e: e.activation(bcs[64:128, :], BCB[64:128, :], AF.Identity), reads=["psBCB"], writes=["bcsB"])
                    mo = mixo[u % 2]
                    mk = "mixo%d" % (u % 2)
                    p.dve(lambda e, mo=mo: e.tensor_tensor(mo[0:64, :], OA[0:64, :], bcs[0:64, :], ALU.mult), reads=["psOA", "bcsA"], writes=[mk + "a"])
                    p.dve(lambda e, mo=mo: e.tensor_tensor(mo[64:128, :], OB[64:128, :], bcs[64:128, :], ALU.mult), reads=["psOB", "bcsB"], writes=[mk + "b"])
                    p.dma("sp", "mst%d" % (u % 2), lambda e, mo=mo, j=j, qb=qb: e.dma_start(out=MIX_d[j, :, qb * 512:(qb + 1) * 512], in_=mo[:]),
                          reads=[mk + "a", mk + "b"], writes=["MIXd"])
                    u += 1
            ph.done()
            ph = Phase(nc)
            p = ph.p
            PS = ph.psum4()
            W = S + 16
            Pf = ph.sb([128, W], F32)
            sa = ph.sb([128, W], F32)
            sb2 = ph.sb([128, W], F32)
            dbf = ph.sb([128, S], BF16)
            pw = ph.sb([128, 512], BF16)
            psc = ph.sb([128, 4], F32)
            edg = ph.sb([128, 64], F32)
            et = ph.sb([128, 16], F32)
            po = [ph.sb([128, 512], BF16) for _ in range(2)]
            p.dma("pool", "pw", lambda e: e.dma_start(out=pw[:], in_=poolw_d[:, :]), writes=["pw"])
            p.dma("sp", "pc", lambda e: e.dma_start(out=psc[:], in_=poolsc_d[:, :]), writes=["psc"])
            p.dma("sp", "pc", lambda e: e.dma_start(out=edg[:], in_=pooledge_d[:, :]), writes=["edg"])
            p.dve(lambda e: e.memset(Pf[:, 0:8], 0.0), writes=["PfL"])
            p.dve(lambda e: e.memset(Pf[:, W - 8:W], 0.0), writes=["PfR"])
            oc = 0
            for g in range(4):
                w_ = 2 ** (g + 1)
                p.dma("sp", "pf", lambda e, g=g: e.dma_start(out=Pf[:, 8:8 + S], in_=PT_d[g, :, :]), writes=["Pf"])
                p.dve(lambda e: e.tensor_tensor(sa[:, 1:W], Pf[:, 0:W - 1], Pf[:, 1:W], ALU.add), reads=["Pf", "PfL", "PfR"], writes=["sa"])
                cur, ck = sa, "sa"
                oth, ok_ = sb2, "sb"
                lo, hi, sh_ = 1, W, 1
                for st in range(g):
                    nlo, nhi = lo + sh_, hi - sh_
                    eng = p.pool if st % 2 == 0 else p.dve
                    eng(lambda e, cur=cur, oth=oth, nlo=nlo, nhi=nhi, sh_=sh_: e.tensor_tensor(oth[:, nlo:nhi], cur[:, nlo - sh_:nhi - sh_], cur[:, nlo + sh_:nhi + sh_], ALU.add),
                        reads=[ck], writes=[ok_])
                    cur, ck, oth, ok_ = oth, ok_, cur, ck
                    lo, hi = nlo, nhi
                    sh_ *= 2
                assert lo <= 8 and hi >= 8 + S
                p.dve(lambda e, cur=cur, w_=w_: e.scalar_tensor_tensor(dbf[:, :], cur[:, 8:8 + S], 1.0 / w_, Pf[:, 8:8 + S], op0=ALU.mult, op1=ALU.subtract),
                      reads=[ck, "Pf"], writes=["dbf0"])
                p.dve(lambda e, cur=cur, g=g: e.tensor_tensor(et[:, 0:8], cur[:, 8:16], edg[:, g * 16:g * 16 + 8], ALU.mult), reads=[ck, "edg"], writes=["et0"])
                p.dve(lambda e, cur=cur, g=g: e.tensor_tensor(et[:, 8:16], cur[:, S:8 + S], edg[:, g * 16 + 8:g * 16 + 16], ALU.mult), reads=[ck, "edg", "et0"], writes=["et1"])
                p.dve(lambda e: e.tensor_tensor(dbf[:, 0:8], et[:, 0:8], Pf[:, 8:16], ALU.subtract), reads=["et1", "Pf", "dbf0"], writes=["dbf1"])
                p.dve(lambda e: e.tensor_tensor(dbf[:, S - 8:S], et[:, 8:16], Pf[:, S:8 + S], ALU.subtract), reads=["et1", "Pf", "dbf1"], writes=["dbf"])
                for b in range(8):
                    i = oc % 2
                    oc += 1
                    bank = PS[i][:, 0:512]
                    p.pe(lambda e, bank=bank, g=g, b=b: e.matmul(bank, pw[:, g * 128:(g + 1) * 128], dbf[:, b * 512:(b + 1) * 512], start=True, stop=True),
                         reads=["pw", "dbf"], writes=["psP%d" % i])
                    p.act(lambda e, bank=bank, i=i, g=g: e.activation(po[i][:], bank, AF.Identity, scale=psc[:, g:g + 1]), reads=["psP%d" % i, "psc"], writes=["po%d" % i])
                    p.dma("sp", "post%d" % i, lambda e, i=i, g=g, b=b: e.dma_start(out=MIX_d[4 + g, :, b * 512:(b + 1) * 512], in_=po[i][:]),
                          reads=["po%d" % i], writes=["MIXd"])
            ph.done()
            wout_phase(wout0_d, 0)

        def layer1_mixer():
            ph = Phase(nc)
            p = ph.p
            PS = ph.psum4()
            T = 256
            w1 = ph.sb([128, 8, 2560], BF16)
            sqb = [ph.sb([128, T], BF16) for _ in range(2)]
            rstd = ph.sb([128, T], F32)
            tmpb = [ph.sb([128, T], F32) for _ in range(2)]
            aT = ph.sb([128, 8, T], BF16)
            sggain = ph.sb([128, 512], F32)
            sgwT = ph.sb([128, 512], BF16)
            sgbb = ph.sb([128, 512], F32)
            usb = ph.sb([128, 4, T], F32)
            hxs = [ph.sb([128, T], F32)] * 2
            zs = hxs
            bgs = [ph.sb([128, T], F32)] * 2
            sqv = ph.sb([128, 512], F32)
            ssum = ph.sb([128, 4], F32)
            vt = sqv
            vn = ph.sb([128, 4, 128], BF16)
            st_ = ph.sb([128, 4, 128], F32)
            yc = [ph.sb([128, 4, T], BF16) for _ in range(2)]
            for c in range(8):
                p.dma("pool", "w1", lambda e, c=c: e.dma_start(out=w1[:, c, 0:1280], in_=win1_d[c * 128:(c + 1) * 128, 0:1280]), writes=["w1"])
                p.dma("pool", "w1", lambda e, c=c: e.dma_start(out=w1[:, c, 1280:2560], in_=win1_d[c * 128:(c + 1) * 128, 1280:2560]), writes=["w1"])
            p.dma("pool", "w1", lambda e: e.dma_start(out=sgwT[:], in_=sgw_d[:, :]), writes=["sgwT"])
            p.dma("sp", "c2", lambda e: e.dma_start(out=sggain[:], in_=sggain_d[:, :]), writes=["sggain"])
            p.dma("sp", "c2", lambda e: e.dma_start(out=sgbb[:], in_=sgb_d[:, :]), writes=["sgbb"])
            B_ST = PS[0][:, 0:512]
            B_U = [PS[0][:, 512:1024], PS[1][:, 0:512]]
            B_H = [PS[1][:, 512:1024], PS[2][:, 0:512]]
            B_G = PS[2][:, 512:1024]
            B_V = PS[3][:, 0:512]
            B_S = PS[3][:, 512:1024]
            akeys = ["aT_%d" % c for c in range(8)]

            def proj(col0, tgt, bkey):
                def mm(e):
                    for k in range(8):
                        ins = e.matmul(tgt, w1[:, k, col0:col0 + 128], aT[:, k, :], start=(k == 0), stop=(k == 7))
                    return ins
                p.pe(mm, reads=["w1"] + akeys, writes=[bkey])
            hi_ = 0
            for b in range(S // T):
                t0 = b * T
                norm_block(p, B_ST, "psST", t0, T, GS[:, 2, :], MOD[:, 1, 0:8], sqb, rstd, tmpb, lambda c: (aT[:, c, :], "aT_%d" % c), "m")
                for half in range(2):
                    for q in range(2):
                        g = half * 2 + q
                        proj(g * 128, B_U[half][:, q * T:(q + 1) * T], "psU%d" % half)
                    p.act(lambda e, half=half: e.activation(usb[:, half * 2:half * 2 + 2, :], B_U[half][:, 0:2 * T].rearrange("p (q t) -> p q t", t=T), AF.Identity),
                          reads=["psU%d" % half], writes=["usb%d" % half])
                for c in range(4):
                    i = hi_ % 2
                    hi_ += 1
                    proj(1024 + c * 128, B_H[i][:, 0:T], "psH%d" % i)
                    proj(2048 + c * 128, B_H[i][:, T:2 * T], "psH%d" % i)
                    p.act(lambda e, i=i: e.activation(hxs[i][:], B_H[i][:, 0:T], AF.Identity), reads=["psH%d" % i], writes=["hxs0"])
                    p.dve(lambda e, i=i: e.tensor_tensor(zs[i][:], B_H[i][:, T:2 * T], hxs[i][:], ALU.mult), reads=["psH%d" % i, "hxs0"], writes=["hxs0"])
                    p.dma("sp", "zst%d" % i, lambda e, i=i, c=c, t0=t0: e.dma_start(out=PT_d[c, :, t0:t0 + T], in_=zs[i][:]), reads=["hxs0"], writes=["PTd"])
                    proj(1536 + c * 128, B_G[:, 0:T], "psG")
                    p.act(lambda e, i=i: e.activation(bgs[i][:], B_G[:, 0:T], AF.Identity), reads=["psG"], writes=["bgs0"])
                    p.dma("sp", "bst%d" % i, lambda e, i=i, c=c, t0=t0: e.dma_start(out=BG_d[c, :, t0:t0 + T], in_=bgs[i][:]), reads=["bgs0"], writes=["BGd"])
                yb = yc[b % 2]
                yk = "yc%d" % (b % 2)
                for n in range(2):
                    def vmm(e, n=n):
                        for g in range(4):
                            for k in range(8):
                                ins = e.matmul(B_V[:, g * 128:(g + 1) * 128], aT[:, k, n * 128:(n + 1) * 128], w1[:, k, 512 + g * 128:512 + (g + 1) * 128],
                                               start=(k == 0), stop=(k == 7))
                        return ins
                    p.pe(vmm, reads=["w1"] + akeys, writes=["psV"])
                    p.act(lambda e: e.activation(sqv[:], B_V, AF.Square), reads=["psV"], writes=["sqv"])
                    p.dve(lambda e: e.tensor_reduce(ssum[:], sqv[:].rearrange("p (g c) -> p g c", c=128), AX.X, ALU.add), reads=["sqv"], writes=["ssum0"])
                    p.act(lambda e: e.activation(ssum[:], ssum[:], AF.Sqrt, bias=EPSB[:, 0:1], scale=1.0 / 128.0), reads=["ssum0"], writes=["ssum1"])
                    p.dve(lambda e: e.reciprocal(ssum[:], ssum[:]), reads=["ssum1"], writes=["ssum"])
                    p.dve(lambda e: e.tensor_tensor(vt[:].rearrange("p (g c) -> p g c", c=128), B_V.rearrange("p (g c) -> p g c", c=128), ssum[:].unsqueeze(2).to_broadcast([128, 4, 128]), ALU.mult),
                          reads=["psV", "ssum", "sqv"], writes=["sqv"])
                    p.pool(lambda e: e.tensor_tensor(vn[:].rearrange("p g c -> p (g c)"), vt[:], sggain[:], ALU.mult), reads=["sqv", "sggain"], writes=["vn"])

                    def smm(e):
                        for g in range(4):
                            ins = e.matmul(B_S[:, g * 128:(g + 1) * 128], vn[:, g, :], sgwT[:, g * 128:(g + 1) * 128], start=True, stop=True)
                        return ins
                    p.pe(smm, reads=["vn", "sgwT"], writes=["psS"])
                    p.dve(lambda e: e.tensor_tensor(st_[:], B_S.rearrange("p (g c) -> p g c", c=128), sgbb[:].rearrange("p (g c) -> p g c", c=128), ALU.add),
                          reads=["psS", "sgbb"], writes=["st"])
                    p.pool(lambda e, n=n, yb=yb: e.tensor_tensor(yb[:, :, n * 128:(n + 1) * 128], st_[:], usb[:, :, n * 128:(n + 1) * 128], ALU.mult),
                           reads=["st", "usb0", "usb1"], writes=[yk + "_%d" % n])
                p.dma("sp", "yst%d" % (b % 2), lambda e, yb=yb, t0=t0: e.dma_start(out=MIX_d[0:4, :, t0:t0 + T].rearrange("c p t -> p c t"), in_=yb[:]),
                      reads=[yk + "_0", yk + "_1"], writes=["MIXd"])
            ph.done()
            ph = Phase(nc)
            p = ph.p
            W = S + 2
            Z = ph.sb([128, W], F32)
            BGr = ph.sb([128, S], F32)
            t1 = ph.sb([128, S], F32)
            yo = ph.sb([128, S], BF16)
            cw = ph.sb([128, 12], F32)
            p.dma("sp", "cw", lambda e: e.dma_start(out=cw[:], in_=convw_d[:, :]), writes=["cw"])
            p.dve(lambda e: e.memset(Z[:, 0:1], 0.0), writes=["ZL"])
            p.dve(lambda e: e.memset(Z[:, W - 1:W], 0.0), writes=["ZR"])
            for c in range(4):
                p.dma("sp", "z", lambda e, c=c: e.dma_start(out=Z[:, 1:1 + S], in_=PT_d[c, :, :]), writes=["Z"])
                p.dma("sp", "bg", lambda e, c=c: e.dma_start(out=BGr[:], in_=BG_d[c, :, :]), writes=["BGr"])
                p.dve(lambda e, c=c: e.tensor_scalar(t1[:], Z[:, 1:1 + S], cw[:, c * 3 + 1:c * 3 + 2], None, op0=ALU.mult), reads=["Z", "cw"], writes=["t1a"])
                p.dve(lambda e, c=c: e.scalar_tensor_tensor(t1[:], Z[:, 0:S], cw[:, c * 3:c * 3 + 1], t1[:], op0=ALU.mult, op1=ALU.add),
                      reads=["Z", "ZL", "cw", "t1a"], writes=["t1b"])
                p.dve(lambda e, c=c: e.scalar_tensor_tensor(t1[:], Z[:, 2:2 + S], cw[:, c * 3 + 2:c * 3 + 3], t1[:], op0=ALU.mult, op1=ALU.add),
                      reads=["Z", "ZR", "cw", "t1b"], writes=["t1c"])
                p.pool(lambda e: e.tensor_tensor(yo[:], t1[:], BGr[:], ALU.mult), reads=["t1c", "BGr"], writes=["yo"])
                p.dma("sp", "yo", lambda e, c=c: e.dma_start(out=MIX_d[4 + c, :, :], in_=yo[:]), reads=["yo"], writes=["MIXd"])
            ph.done()
            wout_phase(wout1_d, 1)

        if tail_only:
            router_moe(1)
        else:
            if stage >= 1:
                layer0_mixer()
            if stage >= 2 and stage < 20:
                router_moe(0)
            if stage >= 3 and stage < 10 and stage != 5:
                layer1_mixer()
            if stage >= 4 and stage < 10 and stage != 6:
                router_moe(1)
        if stage == 6:
            for _ in range(3):
                ph = Phase(nc)
                ph.p.dve(lambda e: e.memset(EPSB[:], EPS), writes=["epsb"])
                ph.done()
        store_phase()
    return nc


def _perm64():
    d = np.arange(64)
    return np.where((d % 32) < 16, d + 16, d - 16)


def prep_inputs(inputs):
    f = lambda a: np.ascontiguousarray(np.asarray(a, dtype=np.float32))
    I = {k: np.asarray(v) for k, v in inputs.items()}
    shared = {}
    shared["ident"] = np.eye(128, dtype=np.float32)
    shared["mod_w"] = f(I["mod_w"])
    shared["mod_bT"] = f(I["mod_b"].reshape(2, 48, 128).transpose(2, 0, 1).reshape(128, 96))
    ng = np.stack([I["norm1_g"], I["norm2_g"]], 0)
    shared["norm_g"] = f(ng.reshape(2, 2, 8, 128).transpose(3, 0, 1, 2).reshape(128, 32))
    w_in0 = I["even_w_in"][0]
    pi = _perm64()
    qcols, qpcols = [], []
    for j in range(4):
        for h in (j, j + 4):
            qcols.append(h * 64 + np.arange(64))
            qpcols.append(h * 64 + pi)
    qcols = np.concatenate(qcols)
    qpcols = np.concatenate(qpcols)
    kcols = 512 + np.arange(128)
    kpcols = 512 + np.concatenate([pi, 64 + pi])
    pcols = 768 + np.arange(512)
    allc = np.concatenate([qcols, kcols, pcols, qpcols, kpcols])
    shared["w_qkp"] = f(w_in0[:, allc])
    shared["w_v"] = f(w_in0[:, 640:768])
    qg = I["q_gain"][0]
    kg = I["k_gain"][0]
    d = np.arange(128) % 64
    shared["gains"] = f(np.stack([qg[d], qg[pi[d]], kg[d], kg[pi[d]]], 1))
    t = np.arange(S)
    row = (t // 64).astype(np.float32)
    col = (t % 64).astype(np.float32)
    inv = (10000.0 ** (-np.arange(0, 16, dtype=np.float32) * 2 / 32.0)).astype(np.float32)
    cos_t = np.zeros((128, S), np.float32)
    sin_t = np.zeros((128, S), np.float32)
    for pp in range(128):
        dd = pp % 64
        jj = dd % 16
        pos = row if dd < 32 else col
        ang = (pos * inv[jj]).astype(np.float32)
        sgn = -1.0 if (dd % 32) < 16 else 1.0
        cos_t[pp] = np.cos(ang)
        sin_t[pp] = sgn * np.sin(ang)
    shared["cos_t"] = cos_t
    shared["sin_t"] = sin_t
    shared["pool_wT"] = f(I["pool_w"][0].transpose(1, 0, 2).reshape(128, 512))
    shared["pool_sc"] = f(I["pool_scale"][0].reshape(4, 128).T)
    edge = np.zeros((128, 4, 16), np.float32)
    for g, w in enumerate((2, 4, 8, 16)):
        for jx in range(16):
            tpos = jx if jx < 8 else S - 16 + jx
            lo = max(tpos - w // 2, 0)
            hi = min(tpos + w - w // 2, S)
            edge[:, g, jx] = 1.0 / (hi - lo)
    shared["pool_edge"] = edge.reshape(128, 64)
    w_out0 = I["even_w_out"][0]
    rows = []
    for c in range(4):
        for h in (c, c + 4):
            rows.append(h * 64 + np.arange(64))
    rows.append(512 + np.arange(512))
    shared["w_out0"] = f(w_out0[np.concatenate(rows), :])
    shared["w_out1"] = f(I["odd_w_out"][0])
    shared["w_in1"] = f(I["odd_w_in"][0])
    shared["sg_gain_b"] = f(np.broadcast_to(I["sg_gain"][0].reshape(1, 512), (128, 512)))
    shared["sg_wT"] = f(I["sg_w"][0].transpose(2, 0, 1).reshape(128, 512))
    shared["sg_b_b"] = f(np.broadcast_to(I["sg_b"][0].reshape(1, 512), (128, 512)))
    shared["conv_wT"] = f(I["conv_w"][0][:, 0, :].reshape(3, 4, 128).transpose(2, 1, 0).reshape(128, 12))
    shared["rw"] = f(np.concatenate([I["router_g_w"], I["router_e_w"]], axis=2))
    rb = np.concatenate([I["router_g_b"], I["router_e_b"]], axis=1)
    shared["rb_b"] = f(np.broadcast_to(np.tile(rb[:, None, :], (1, 4, 1)).reshape(1, 160), (128, 160)))
    shared["w_gate"] = f(I["w_gate"])
    shared["w_up"] = f(I["w_up"])
    shared["w_down"] = f(I["w_down"])
    sel = np.zeros((32, 16, 128), np.float32)
    for ex in range(16):
        sel[ex, ex, :] = 1.0
        sel[16 + ex, ex, :] = 1.0
    shared["sel"] = sel.reshape(32, 2048)
    in_maps = []
    for b in range(NCORES):
        m = dict(shared)
        m["x"] = f(I["x"][b])
        m["ctx"] = f(I["ctx"][b])
        cv = np.stack([I["c"][b], I["c_ctx"]], 0)
        m["cvec"] = f(cv.reshape(2, 8, 128).transpose(2, 1, 0).reshape(128, 16))
        in_maps.append(m)
    return in_maps


_NC_CACHE = {}


def kernel(**inputs):
    in_maps = prep_inputs(inputs)
    if "nc" not in _NC_CACHE:
        _NC_CACHE["nc"] = (build(stage=3), build(tail_only=True))
    nc1, nc2 = _NC_CACHE["nc"]
    res = run_bass_kernel_spmd(nc1, in_maps, core_ids=list(range(NCORES)))
    for b in range(NCORES):
        in_maps[b]["x"] = np.ascontiguousarray(np.asarray(res.results[b]["out"], dtype=np.float32))
    res = run_bass_kernel_spmd(nc2, in_maps, core_ids=list(range(NCORES)))
    out = np.stack([np.asarray(r["out"]) for r in res.results], axis=0)
    return out.astype(np.float32)
```

```python
import numpy as np
from contextlib import ExitStack
import concourse.bass as bass
import concourse.mybir as mybir
from concourse.bass_utils import run_bass_kernel_spmd

F32 = mybir.dt.float32
BF16 = mybir.dt.bfloat16
AF = mybir.ActivationFunctionType
ALU = mybir.AluOpType
AX = mybir.AxisListType

ENGS = ["pe", "act", "dve", "pool", "sp"]
S = 4096
D = 1024
NCORES = 8
EPS = 1e-6
_CNT = [0]
_PHASE = [0]
_SEMPOOL = [[], []]
NSEM = 16


class Op:
    __slots__ = ("eng", "fn", "reads", "writes", "dma_key", "idx", "waits", "signal", "semval")

    def __init__(self, eng, fn, reads, writes, dma_key):
        self.eng = eng
        self.fn = fn
        self.reads = reads
        self.writes = writes
        self.dma_key = dma_key
        self.waits = []
        self.signal = False
        self.semval = 0


class Prog:
    def __init__(self, nc):
        self.nc = nc
        self.ops = []

    def op(self, eng, fn, reads=(), writes=(), dma_key=None):
        reads = tuple(reads)
        writes = tuple(writes)
        ex = tuple(r for r in reads if r.startswith("ps"))
        o = Op(eng, fn, reads, writes + ex, dma_key)
        o.idx = len(self.ops)
        self.ops.append(o)
        return o

    def pe(self, fn, reads=(), writes=()):
        return self.op("pe", fn, reads, writes)

    def act(self, fn, reads=(), writes=()):
        return self.op("act", fn, reads, writes)

    def dve(self, fn, reads=(), writes=()):
        return self.op("dve", fn, reads, writes)

    def pool(self, fn, reads=(), writes=()):
        return self.op("pool", fn, reads, writes)

    def dma(self, eng, key, fn, reads=(), writes=()):
        return self.op(eng, fn, reads, writes, dma_key=key)

    def finalize(self):
        ops = self.ops

        def tl(o):
            return ("dma", o.dma_key) if o.dma_key is not None else o.eng

        pos = {}
        cnt = {}
        for o in ops:
            t = tl(o)
            cnt[t] = cnt.get(t, 0) + 1
            pos[o.idx] = cnt[t]
        last_writer = {}
        readers = {}
        known = {e: {} for e in ENGS}
        done_clock = {}
        needed = set()
        latest_on_key = {}
        for o in ops:
            deps = set()
            raw = set()
            for r in o.reads:
                w = last_writer.get(r)
                if w is not None:
                    deps.add(w)
                    raw.add(w)
            for r in o.writes:
                w = last_writer.get(r)
                if w is not None:
                    deps.add(w)
                    if r.startswith("ps"):
                        raw.add(w)
                for rd in readers.get(r, ()):
                    deps.add(rd)
            req = {}
            for d in deps:
                if d == o.idx:
                    continue
                po = ops[d]
                t = tl(po)
                if po.dma_key is None and o.dma_key is None and po.eng == o.eng:
                    if o.eng == "pe":
                        continue
                    if d not in raw:
                        continue
                if t not in req or pos[d] > pos[req[t]]:
                    req[t] = d
            kn = known[o.eng]
            for t in list(req.keys()):
                if not isinstance(t, str):
                    req[t] = latest_on_key[t]
            waits = []
            for t, d in req.items():
                if kn.get(t, 0) >= pos[d]:
                    continue
                waits.append(d)
                needed.add(d)
                for t2, p2 in done_clock[d].items():
                    if kn.get(t2, 0) < p2:
                        kn[t2] = p2
            o.waits = waits
            dc = dict(kn)
            dc[tl(o)] = max(dc.get(tl(o), 0), pos[o.idx])
            done_clock[o.idx] = dc
            if o.dma_key is not None:
                latest_on_key[tl(o)] = o.idx
            for r in o.writes:
                last_writer[r] = o.idx
                readers[r] = []
            for r in o.reads:
                if r not in o.writes:
                    readers.setdefault(r, []).append(o.idx)
        semcnt = {}
        for o in ops:
            t = tl(o)
            if o.dma_key is not None:
                semcnt[t] = semcnt.get(t, 0) + 16
                o.signal = True
                o.semval = semcnt[t]
            elif o.idx in needed:
                semcnt[t] = semcnt.get(t, 0) + 1
                o.signal = True
                o.semval = semcnt[t]
        self.timelines = sorted(set(tl(o) for o in ops if o.signal), key=str)
        self._tl = tl
        return self

    def emit(self):
        nc = self.nc
        ops = self.ops
        tl = self._tl
        with ExitStack() as es:
            pidx = _PHASE[0] % 2
            _PHASE[0] += 1
            mypool = _SEMPOOL[pidx]
            other = _SEMPOOL[1 - pidx]
            assert len(self.timelines) <= len(mypool), len(self.timelines)
            sems = {}
            for i, t in enumerate(self.timelines):
                sems[t] = mypool[i]
            block = es.enter_context(nc.Block())
            by_eng = {e: [o for o in ops if o.eng == e] for e in ENGS}
            final_dma = {}
            for o in ops:
                if o.dma_key is not None:
                    final_dma[tl(o)] = o.semval

            def run(engname, eng):
                for o in by_eng[engname]:
                    ws = list(o.waits)
                    att = None
                    if ws and engname != "pe":
                        att = ws.pop()
                    for d in ws:
                        po = ops[d]
                        eng.wait_ge(sems[tl(po)], po.semval)
                    if att is not None:
                        rec = _Rec(eng)
                        ins = o.fn(rec)
                        po = ops[att]
                        rec.first._wait_ge(sems[tl(po)], po.semval)
                    else:
                        ins = o.fn(eng)
                    if o.signal:
                        ins.then_inc(sems[tl(o)], 16 if o.dma_key is not None else 1)
                if engname == "sp":
                    for t, v in final_dma.items():
                        eng.wait_ge(sems[t], v)
                    for sm in other:
                        eng.sem_clear(sm)

            @block.tensor
            def _(eng):
                run("pe", eng)

            @block.scalar
            def _(eng):
                run("act", eng)

            @block.vector
            def _(eng):
                run("dve", eng)

            @block.gpsimd
            def _(eng):
                run("pool", eng)

            @block.sync
            def _(eng):
                run("sp", eng)


class _Rec:
    def __init__(self, eng):
        self._eng = eng
        self.first = None

    def __getattr__(self, name):
        f = getattr(self._eng, name)

        def g(*a, **k):
            r = f(*a, **k)
            if self.first is None:
                self.first = r
            return r
        return g


class Phase:
    def __init__(self, nc):
        self.nc = nc
        self.es = ExitStack()
        self.p = Prog(nc)
        self._n = 0

    def sb(self, shape, dt):
        self._n += 1
        _CNT[0] += 1
        return self.es.enter_context(self.nc.sbuf_tensor("sb%d" % _CNT[0], list(shape), dt))

    def psum4(self):
        r = []
        for _ in range(4):
            _CNT[0] += 1
            r.append(self.es.enter_context(self.nc.psum_tensor("ps%d" % _CNT[0], [128, 1024], F32)))
        return r

    def done(self):
        self.p.finalize()
        self.p.emit()
        self.es.close()


def build(stage=4, tail_only=False):
    nc = bass.Bass("TRN2", target_bir_lowering=False)

    def din(name, shape, dt=F32):
        return nc.dram_tensor(name, list(shape), dt, kind="ExternalInput").ap()

    def dscr(name, shape, dt):
        return nc.dram_tensor(name, list(shape), dt, kind="Internal").ap()

    x_d = din("x", [S, D])
    ctx_d = din("ctx", [256, D])
    cvec_d = din("cvec", [128, 16])
    ident_d = din("ident", [128, 128])
    modw_d = din("mod_w", [2, D, 6144])
    modb_d = din("mod_bT", [128, 96])
    ng_d = din("norm_g", [128, 32])
    wqkp_d = din("w_qkp", [D, 1792])
    wv_d = din("w_v", [D, 128])
    gains_d = din("gains", [128, 4])
    cos_d = din("cos_t", [128, S])
    sin_d = din("sin_t", [128, S])
    poolw_d = din("pool_wT", [128, 512])
    poolsc_d = din("pool_sc", [128, 4])
    pooledge_d = din("pool_edge", [128, 64])
    wout0_d = din("w_out0", [D, D])
    wout1_d = din("w_out1", [D, D])
    win1_d = din("w_in1", [D, 2560])
    sggain_d = din("sg_gain_b", [128, 512])
    sgw_d = din("sg_wT", [128, 512])
    sgb_d = din("sg_b_b", [128, 512])
    convw_d = din("conv_wT", [128, 12])
    rw_d = din("rw", [2, D, 20])
    rb_d = din("rb_b", [128, 160])
    wg_d = din("w_gate", [2, 16, D, 256])
    wu_d = din("w_up", [2, 16, D, 256])
    wd_d = din("w_down", [2, 16, 256, D])
    sel_d = din("sel", [32, 2048])
    out_d = nc.dram_tensor("out", [S, D], F32, kind="ExternalOutput").ap()

    A2_d = dscr("A2s", [8, 128, S], BF16)
    QT_d = dscr("QTs", [4, 128, S], BF16)
    MIX_d = dscr("MIXs", [8, 128, S], BF16)
    PT_d = dscr("PTs", [4, 128, S], F32)
    BG_d = dscr("BGs", [4, 128, S], F32)
    KT_d = dscr("KTs", [128, 4352], BF16)
    VS_d = dscr("VSs", [128, 34, 193], BF16)

    with ExitStack() as top:
        def psb(shape, dt):
            _CNT[0] += 1
            return top.enter_context(nc.sbuf_tensor("pt%d" % _CNT[0], list(shape), dt))

        _PHASE[0] = 0
        for pi_ in range(2):
            _SEMPOOL[pi_] = []
            for si_ in range(NSEM):
                _SEMPOOL[pi_].append(top.enter_context(nc.semaphore("sp%d_%d" % (pi_, si_))))
        HT = psb([128, 8, S], F32)
        ident = psb([128, 128], F32)
        ident_bf = psb([128, 128], BF16)
        ones_bf = psb([128, 128], BF16)
        onesblk_bf = psb([128, 128], BF16)
        ones_f = psb([128, 128], F32)
        MOD = psb([128, 2, 48], F32)
        MODC = psb([128, 16], F32)
        NG = psb([128, 32], F32)
        GS = psb([128, 4, 8], F32)
        GSC = psb([128, 8], F32)
        combT = psb([32, S], BF16)
        EPSB = psb([128, 1], F32)

        ph = Phase(nc)
        p = ph.p
        PS = ph.psum4()
        cvec = ph.sb([128, 16], F32)
        scv = ph.sb([128, 16], F32)
        modb = ph.sb([128, 96], F32)
        mrow = ph.sb([2, 6144], F32)
        mwbuf = [ph.sb([128, 8, 512], F32) for _ in range(2)]
        xin = [ph.sb([128, D], F32) for _ in range(2)]
        p.dma("sp", "c0", lambda e: e.dma_start(out=ident[:], in_=ident_d[:, :]), writes=["ident"])
        p.dma("sp", "c0", lambda e: e.dma_start(out=cvec[:], in_=cvec_d[:, :]), writes=["cvec"])
        p.dma("sp", "c0", lambda e: e.dma_start(out=modb[:], in_=modb_d[:, :]), writes=["modb"])
        p.dma("sp", "c0", lambda e: e.dma_start(out=NG[:], in_=ng_d[:, :]), writes=["NG"])
        p.dve(lambda e: e.tensor_copy(ident_bf[:], ident[:]), reads=["ident"], writes=["ident_bf"])
        p.dve(lambda e: e.memset(ones_bf[:], 1.0 / 1024.0), writes=["ones_bf"])
        p.dve(lambda e: e.memset(ones_f[:], 1.0), writes=["ones_f"])
        p.dve(lambda e: e.memset(EPSB[:], EPS), writes=["epsb"])
        p.dve(lambda e: e.memset(onesblk_bf[:], 0.0), writes=["onesblk0"])
        p.dve(lambda e: e.memset(onesblk_bf[0:64, 0:64], 1.0 / 64.0), reads=["onesblk0"], writes=["onesblk1"])
        p.dve(lambda e: e.memset(onesblk_bf[64:128, 64:128], 1.0 / 64.0), reads=["onesblk1"], writes=["onesblk"])
        p.act(lambda e: e.activation(scv[:], cvec[:], AF.Silu), reads=["cvec"], writes=["scv"])
        for l in range(2):
            for fb in range(12):
                it = l * 12 + fb
                buf = mwbuf[it % 2]
                bk = "mw%d" % (it % 2)
                p.dma("sp", bk, lambda e, buf=buf, l=l, fb=fb: e.dma_start(
                    out=buf[:], in_=modw_d[l, :, fb * 512:(fb + 1) * 512].rearrange("(c p) n -> p c n", p=128)),
                    writes=[bk])
                pb = "psA%d" % (it % 2)
                pst = PS[0][:, (it % 2) * 512:(it % 2) * 512 + 512]

                def mm(e, buf=buf, pst=pst):
                    for c in range(8):
                        ins = e.matmul(pst[0:2, :], scv[:, c * 2:c * 2 + 2], buf[:, c, :], start=(c == 0), stop=(c == 7))
                    return ins
                p.pe(mm, reads=[bk, "scv"], writes=[pb])
                if it % 2 == 0:
                    p.act(lambda e, pst=pst, fb=fb: e.activation(mrow[0:2, fb * 512:(fb + 1) * 512], pst[0:2, :], AF.Identity),
                          reads=[pb], writes=["mrow_%d" % fb])
                else:
                    p.dve(lambda e, pst=pst, fb=fb: e.tensor_copy(mrow[0:2, fb * 512:(fb + 1) * 512], pst[0:2, :]),
                          reads=[pb], writes=["mrow_%d" % fb])

            def tr(e):
                for j in range(48):
                    ins = e.matmul(PS[1][:, j * 2:j * 2 + 2], mrow[0:2, j * 128:(j + 1) * 128], ident[0:2, 0:2], start=True, stop=True)
                return ins
            p.pe(tr, reads=["mrow_%d" % fb for fb in range(12)] + ["ident"], writes=["psB0"])
            p.dve(lambda e, l=l: e.tensor_tensor(MOD[:, l, :], PS[1][:, 0:96].rearrange("p (j t) -> p j t", t=2)[:, :, 0],
                                                modb[:, l * 48:(l + 1) * 48], ALU.add),
                  reads=["psB0", "modb"], writes=["MOD%d" % l])
            if l == 0:
                p.dve(lambda e: e.tensor_tensor(MODC[:], PS[1][:, 0:32].rearrange("p (j t) -> p j t", t=2)[:, :, 1],
                                                modb[:, 0:16], ALU.add),
                      reads=["psB0", "modb"], writes=["MODC"])
        for l in range(2):
            for n in range(2):
                sc = MOD[:, l, (1 + 3 * n) * 8:(2 + 3 * n) * 8]
                p.dve(lambda e, l=l, n=n, sc=sc: e.scalar_tensor_tensor(GS[:, l * 2 + n, :], sc, 1.0, NG[:, n * 16 + l * 8:n * 16 + l * 8 + 8],
                                                                      op0=ALU.add, op1=ALU.mult),
                      reads=["MOD%d" % l, "NG"], writes=["GS%d%d" % (l, n)])
        p.dve(lambda e: e.scalar_tensor_tensor(GSC[:], MODC[:, 8:16], 1.0, NG[:, 0:8], op0=ALU.add, op1=ALU.mult),
              reads=["MODC", "NG"], writes=["GSC"])
        for tt in range(32):
            xb = xin[tt % 2]
            xk = "xin%d" % (tt % 2)
            p.dma("sp", xk, lambda e, xb=xb, tt=tt: e.dma_start(out=xb[:], in_=x_d[tt * 128:(tt + 1) * 128, :]), writes=[xk])
            pst = PS[2 + tt % 2]
            pk = "psX%d" % (tt % 2)

            def trx(e, xb=xb, pst=pst):
                for c in range(8):
                    ins = e.matmul(pst[:, c * 128:(c + 1) * 128], xb[:, c * 128:(c + 1) * 128], ident[:], start=True, stop=True)
                return ins
            p.pe(trx, reads=[xk, "ident"], writes=[pk])
            dst = HT[:, :, tt * 128:(tt + 1) * 128]
            src = pst[:, :].rearrange("p (c t) -> p c t", t=128)
            if tt % 2 == 0:
                p.act(lambda e, dst=dst, src=src: e.activation(dst, src, AF.Identity), reads=[pk], writes=["HT%d" % tt])
            else:
                p.dve(lambda e, dst=dst, src=src: e.tensor_copy(dst, src), reads=[pk], writes=["HT%d" % tt])
        ph.done()

        def norm_block(p, PSst, pskey, t0, T, gs, sh, sqb, rstd, tmpb, dst_fn, tag, src=None, srckey="HT"):
            srcT = HT if src is None else src
            for c in range(8):
                sq = sqb[c % 2]
                p.act(lambda e, sq=sq, c=c: e.activation(sq[:, 0:T], srcT[:, c, t0:t0 + T], AF.Square),
                      reads=[srckey], writes=["sq%s%d" % (tag, c % 2)])
                p.pe(lambda e, sq=sq, c=c: e.matmul(PSst[:, 0:T], ones_bf[:], sq[:, 0:T], start=(c == 0), stop=(c == 7)),
                     reads=["sq%s%d" % (tag, c % 2), "ones_bf"], writes=[pskey])
            p.act(lambda e: e.activation(rstd[:, 0:T], PSst[:, 0:T], AF.Sqrt, bias=EPSB[:, 0:1]), reads=[pskey], writes=["rstd0" + tag])
            p.dve(lambda e: e.reciprocal(rstd[:, 0:T], rstd[:, 0:T]), reads=["rstd0" + tag], writes=["rstd" + tag])
            for c in range(8):
                tb = tmpb[c % 2]
                p.dve(lambda e, tb=tb, c=c: e.scalar_tensor_tensor(tb[:, 0:T], srcT[:, c, t0:t0 + T], gs[:, c:c + 1], rstd[:, 0:T],
                                                                  op0=ALU.mult, op1=ALU.mult),
                      reads=[srckey, "rstd" + tag], writes=["tmp%s%d" % (tag, c % 2)])
                dst, dkey = dst_fn(c)
                p.act(lambda e, tb=tb, c=c, dst=dst: e.activation(dst, tb[:, 0:T], AF.Identity, bias=sh[:, c:c + 1]),
                      reads=["tmp%s%d" % (tag, c % 2)], writes=[dkey])

        def wout_phase(wout_d, l):
            ph = Phase(nc)
            p = ph.p
            PS = ph.psum4()
            w = ph.sb([128, 8, D], BF16)
            mixb = [ph.sb([128, 8, 512], BF16) for _ in range(2)]
            p.dma("pool", "w", lambda e: e.dma_start(out=w[:], in_=wout_d.rearrange("(c p) n -> p c n", p=128)), writes=["w"])
            for b in range(8):
                mb = mixb[b % 2]
                mk = "mix%d" % (b % 2)
                p.dma("sp", mk, lambda e, mb=mb, b=b: e.dma_start(out=mb[:], in_=MIX_d[:, :, b * 512:(b + 1) * 512].rearrange("c p t -> p c t")),
                      writes=[mk])
                for f in range(8):
                    pst = PS[f % 4][:, 0:512]
                    pk = "psY%d" % (f % 4)

                    def mm(e, mb=mb, f=f, pst=pst):
                        for k in range(8):
                            ins = e.matmul(pst, w[:, k, f * 128:(f + 1) * 128], mb[:, k, :], start=(k == 0), stop=(k == 7))
                        return ins
                    p.pe(mm, reads=["w", mk], writes=[pk])
                    hsl = HT[:, f, b * 512:(b + 1) * 512]
                    p.dve(lambda e, pst=pst, f=f, hsl=hsl: e.scalar_tensor_tensor(hsl, pst, MOD[:, l, 16 + f:17 + f], hsl, op0=ALU.mult, op1=ALU.add),
                          reads=[pk], writes=["HT"])
            ph.done()

        def router_moe(l):
            ph = Phase(nc)
            p = ph.p
            PS = ph.psum4()
            sqb = [ph.sb([128, 512], BF16) for _ in range(2)]
            rstd = ph.sb([128, 512], F32)
            tmpb = [ph.sb([128, 512], F32) for _ in range(2)]
            a2f = ph.sb([128, 8, 512], F32)
            a2b = [ph.sb([128, 8, 512], BF16) for _ in range(2)]
            rw = ph.sb([128, 8, 20], F32)
            rbb = ph.sb([128, 80], F32)
            lgT = ph.sb([32, 512], F32)
            LG = ph.sb([128, 32, 20], F32)
            p.dma("sp", "rw", lambda e: e.dma_start(out=rw[:], in_=rw_d[l].rearrange("(c p) n -> p c n", p=128)), writes=["rw"])
            p.dma("sp", "rw", lambda e: e.dma_start(out=rbb[:], in_=rb_d[:, l * 80:(l + 1) * 80]), writes=["rbb"])
            gs = GS[:, l * 2 + 1, :]
            sh = MOD[:, l, 24:32]
            for b in range(8):
                af = a2f
                ab = a2b[b % 2]
                abk = "a2b%d" % (b % 2)
                norm_block(p, PS[0], "psS", b * 512, 512, gs, sh, sqb, rstd, tmpb,
                           lambda c, af=af: (af[:, c, :], "a2f_%d" % c), "n")
                akeys = ["a2f_%d" % c for c in range(8)]
                p.pool(lambda e, af=af, ab=ab: e.tensor_copy(ab[:], af[:]), reads=akeys, writes=[abk])
                p.dma("sp", "a2st%d" % (b % 2), lambda e, ab=ab, b=b: e.dma_start(
                    out=A2_d[:, :, b * 512:(b + 1) * 512].rearrange("c p t -> p c t"), in_=ab[:]), reads=[abk], writes=["A2"])

                def rmm(e, af=af):
                    for c in range(8):
                        ins = e.matmul(PS[1][0:20, 0:512], rw[:, c, :], af[:, c, :], start=(c == 0), stop=(c == 7))
                    return ins
                p.pe(rmm, reads=["rw"] + akeys, writes=["psR"])
                p.act(lambda e: e.activation(lgT[0:20, :], PS[1][0:20, 0:512], AF.Identity), reads=["psR"], writes=["lgT"])

                def rtr(e):
                    for tt in range(4):
                        ins = e.matmul(PS[2][:, tt * 32:tt * 32 + 20], lgT[0:20, tt * 128:(tt + 1) * 128], ident[0:20, 0:20], start=True, stop=True)
                    return ins
                p.pe(rtr, reads=["lgT", "ident"], writes=["psT"])
                p.dve(lambda e, b=b: e.tensor_tensor(LG[:, b * 4:(b + 1) * 4, :], PS[2][:, 0:128].rearrange("p (t n) -> p t n", n=32)[:, :, 0:20],
                                                    rbb[:].rearrange("p (t n) -> p t n", n=20), ALU.add),
                      reads=["psT", "rbb"], writes=["LG"])
            NT = 32
            gmax = ph.sb([128, NT], F32)
            ohg = ph.sb([128, NT, 4], F32)
            gd = ph.sb([128, NT, 4], F32)
            gsum = ph.sb([128, NT], F32)
            gp = ph.sb([128, NT], F32)
            t44 = ph.sb([128, NT, 4, 4], F32)
            esel = ph.sb([128, NT, 4], F32)
            e1 = ph.sb([128, NT], F32)
            sel1 = ph.sb([128, NT, 4], F32)
            em = ph.sb([128, NT, 4], F32)
            e2 = ph.sb([128, NT], F32)
            sel2 = ph.sb([128, NT, 4], F32)
            dd = ph.sb([128, NT], F32)
            w1 = ph.sb([128, NT], F32)
            w2 = ph.sb([128, NT], F32)
            ce = ph.sb([128, NT, 4], F32)
            ce2 = ph.sb([128, NT, 4], F32)
            comb = ph.sb([128, NT, 16], F32)
            chl = ph.sb([128, NT, 32], BF16)
            chf = ph.sb([128, NT, 16], F32)
            gl = LG[:, :, 0:4]
            el = LG[:, :, 4:20].rearrange("p n (g i) -> p n g i", i=4)

            def b3(ap2):
                return ap2.unsqueeze(2).to_broadcast([128, NT, 4])
            p.dve(lambda e: e.tensor_reduce(gmax[:], gl, AX.X, ALU.max), reads=["LG"], writes=["gmax"])
            p.dve(lambda e: e.tensor_tensor(ohg[:], gl, b3(gmax[:]), ALU.is_equal), reads=["LG", "gmax"], writes=["ohg"])
            p.dve(lambda e: e.tensor_tensor(gd[:], gl, b3(gmax[:]), ALU.subtract), reads=["LG", "gmax"], writes=["gd"])
            p.act(lambda e: e.activation(gd[:], gd[:], AF.Exp), reads=["gd"], writes=["ge"])
            p.dve(lambda e: e.tensor_reduce(gsum[:], gd[:], AX.X, ALU.add), reads=["ge"], writes=["gsum"])
            p.dve(lambda e: e.reciprocal(gp[:], gsum[:]), reads=["gsum"], writes=["gp"])
            p.dve(lambda e: e.tensor_tensor(t44[:], el, ohg[:].unsqueeze(3).to_broadcast([128, NT, 4, 4]), ALU.mult),
                  reads=["LG", "ohg"], writes=["t44"])
            p.dve(lambda e: e.tensor_reduce(esel[:], t44[:].rearrange("p n g i -> p n i g"), AX.X, ALU.add), reads=["t44"], writes=["esel"])
            p.dve(lambda e: e.tensor_reduce(e1[:], esel[:], AX.X, ALU.max), reads=["esel"], writes=["e1"])
            p.dve(lambda e: e.tensor_tensor(sel1[:], esel[:], b3(e1[:]), ALU.is_equal), reads=["esel", "e1"], writes=["sel1"])
            p.dve(lambda e: e.scalar_tensor_tensor(em[:], sel1[:], -1e30, esel[:], op0=ALU.mult, op1=ALU.add), reads=["sel1", "esel"], writes=["em"])
            p.dve(lambda e: e.tensor_reduce(e2[:], em[:], AX.X, ALU.max), reads=["em"], writes=["e2"])
            p.dve(lambda e: e.tensor_tensor(sel2[:], em[:], b3(e2[:]), ALU.is_equal), reads=["em", "e2"], writes=["sel2"])
            p.dve(lambda e: e.tensor_tensor(dd[:], e2[:], e1[:], ALU.subtract), reads=["e1", "e2"], writes=["dd"])
            p.act(lambda e: e.activation(dd[:], dd[:], AF.Exp), reads=["dd"], writes=["ex"])
            p.dve(lambda e: e.tensor_scalar(w1[:], dd[:], 1.0, None, op0=ALU.add), reads=["ex"], writes=["w1a"])
            p.dve(lambda e: e.reciprocal(w1[:], w1[:]), reads=["w1a"], writes=["w1"])
            p.dve(lambda e: e.tensor_tensor(w2[:], dd[:], w1[:], ALU.mult), reads=["ex", "w1"], writes=["w2a"])
            p.dve(lambda e: e.tensor_tensor(w1[:], w1[:], gp[:], ALU.mult), reads=["w1", "gp", "w2a"], writes=["wt1"])
            p.dve(lambda e: e.tensor_tensor(w2[:], w2[:], gp[:], ALU.mult), reads=["w2a", "gp"], writes=["wt2"])
            p.dve(lambda e: e.tensor_tensor(ce[:], sel1[:], b3(w1[:]), ALU.mult), reads=["sel1", "wt1"], writes=["ce"])
            p.dve(lambda e: e.tensor_tensor(ce2[:], sel2[:], b3(w2[:]), ALU.mult), reads=["sel2", "wt2"], writes=["ce2"])
            p.dve(lambda e: e.tensor_tensor(ce[:], ce[:], ce2[:], ALU.add), reads=["ce", "ce2"], writes=["cef"])
            p.dve(lambda e: e.tensor_tensor(comb[:].rearrange("p n (g i) -> p n g i", i=4),
                                            ohg[:].unsqueeze(3).to_broadcast([128, NT, 4, 4]),
                                            ce[:].unsqueeze(2).to_broadcast([128, NT, 4, 4]), ALU.mult),
                  reads=["ohg", "cef"], writes=["comb"])
            p.dve(lambda e: e.tensor_copy(chl[:, :, 0:16], comb[:]), reads=["comb"], writes=["chi"])
            p.dve(lambda e: e.tensor_copy(chf[:], chl[:, :, 0:16]), reads=["chi"], writes=["chf"])
            p.dve(lambda e: e.tensor_tensor(chf[:], comb[:], chf[:], ALU.subtract), reads=["comb", "chf"], writes=["clo"])
            p.dve(lambda e: e.tensor_copy(chl[:, :, 16:32], chf[:]), reads=["clo"], writes=["chl"])
            for q in range(8):
                pst = PS[3][:, (q % 2) * 512:(q % 2) * 512 + 512]
                pk = "psC%d" % (q % 2)

                def ctr(e, q=q, pst=pst):
                    for tt in range(4):
                        ins = e.matmul(pst[0:32, tt * 128:(tt + 1) * 128], chl[:, q * 4 + tt, :], ident_bf[:], start=True, stop=True)
                    return ins
                p.pe(ctr, reads=["chl", "chi", "ident_bf"], writes=[pk])
                p.act(lambda e, q=q, pst=pst: e.activation(combT[:, q * 512:(q + 1) * 512], pst[0:32, :], AF.Identity), reads=[pk], writes=["combT"])
            ph.done()
            if stage == 10 + l or (stage == 8 and l == 1):
                return
            ph = Phase(nc)
            p = ph.p
            PS = ph.psum4()
            T = 512
            NB = S // T
            wslot = [(ph.sb([128, 8, 256], BF16), ph.sb([128, 8, 256], BF16), ph.sb([128, 2, D], BF16)) for _ in range(3)]
            a2 = [ph.sb([128, 8, T], BF16) for _ in range(2)]
            sel = ph.sb([32, 2048], BF16)
            cb = ph.sb([128, T], F32)
            sg = [ph.sb([128, T], F32)] * 2
            tt_ = ph.sb([128, T], F32)
            hb0 = [ph.sb([128, 2, T], BF16) for _ in range(2)]
            hb1 = ph.sb([128, 2, T], BF16)
            p.dma("pool", "sel", lambda e: e.dma_start(out=sel[:, 0:1024], in_=sel_d[:, 0:1024]), writes=["sel"])
            p.dma("pool", "sel", lambda e: e.dma_start(out=sel[:, 1024:2048], in_=sel_d[:, 1024:2048]), writes=["sel"])
            g2 = MOD[:, l, 40:48]
            RB = [PS[0][:, 0:512], PS[0][:, 512:1024], PS[1][:, 0:512]]
            RBK = ["psR0", "psR1", "psR2"]
            CBP = PS[1][:, 512:1024]
            ACC = [PS[2][:, 0:512], PS[2][:, 512:1024], PS[3][:, 0:512], PS[3][:, 512:1024]]

            def load_expert(ex):
                sl = ex % 3
                wg, wu, wd = wslot[sl]
                wk = "w%d" % sl
                p.dma("pool", wk, lambda e: e.dma_start(out=wg[:], in_=wg_d[l, ex].rearrange("(c p) n -> p c n", p=128)), writes=[wk + "g"])
                p.dma("pool", wk, lambda e: e.dma_start(out=wu[:], in_=wu_d[l, ex].rearrange("(c p) n -> p c n", p=128)), writes=[wk + "u"])
                p.dma("pool", wk, lambda e: e.dma_start(out=wd[:], in_=wd_d[l, ex].rearrange("(c p) n -> p c n", p=128)), writes=[wk + "d"])
            st = {"step": 0, "rk": 0, "sgi": 0}

            def hbuf(b, j):
                if j == 0:
                    return hb0[b % 2], "h0_%d" % (b % 2)
                return hb1, "h1"

            def G(pr, b, j):
                ex = pr * 2 + j
                sl = ex % 3
                wg, wu, wd = wslot[sl]
                wk = "w%d" % sl
                if j == 0:
                    ab = a2[st["step"] % 2]
                    ak = "a2_%d" % (st["step"] % 2)
                    st["cur"] = (ab, ak)
                    st["step"] += 1
                    p.dma("sp", ak, lambda e: e.dma_start(out=ab[:], in_=A2_d[:, :, b * T:(b + 1) * T].rearrange("c p t -> p c t")), writes=[ak])
                ab, ak = st["cur"]
                hbb, hk = hbuf(b, j)
                p.pe(lambda e: e.matmul(CBP, sel[:, ex * 128:(ex + 1) * 128], combT[:, b * T:(b + 1) * T], start=True, stop=True),
                     reads=["sel", "combT"], writes=["psCB"])
                p.act(lambda e: e.activation(cb[:], CBP, AF.Identity), reads=["psCB"], writes=["cb"])
                for f2 in range(2):
                    pg = RB[st["rk"] % 3]
                    pgk = RBK[st["rk"] % 3]
                    st["rk"] += 1
                    pu = RB[st["rk"] % 3]
                    puk = RBK[st["rk"] % 3]
                    st["rk"] += 1

                    def gmm(e, pg=pg, f2=f2):
                        for k in range(8):
                            ins = e.matmul(pg, wg[:, k, f2 * 128:(f2 + 1) * 128], ab[:, k, :], start=(k == 0), stop=(k == 7))
                        return ins

                    def umm(e, pu=pu, f2=f2):
                        for k in range(8):
                            ins = e.matmul(pu, wu[:, k, f2 * 128:(f2 + 1) * 128], ab[:, k, :], start=(k == 0), stop=(k == 7))
                        return ins
                    p.pe(gmm, reads=[wk + "g", ak], writes=[pgk])
                    p.pe(umm, reads=[wk + "u", ak], writes=[puk])
                    sgb = sg[st["sgi"] % 2]
                    sgk = "sg0"
                    st["sgi"] += 1
                    p.act(lambda e, sgb=sgb, pg=pg: e.activation(sgb[:], pg, AF.Silu), reads=[pgk], writes=[sgk])
                    p.dve(lambda e, pu=pu: e.tensor_tensor(tt_[:], pu, cb[:], ALU.mult), reads=[puk, "cb"], writes=["tt"])
                    p.pool(lambda e, sgb=sgb, f2=f2: e.tensor_tensor(hbb[:, f2, :], tt_[:], sgb[:], ALU.mult),
                           reads=["tt", sgk], writes=[hk + "_%d" % f2])

            def DOWN(pr, b, half):
                hs = []
                for j in range(2):
                    ex = pr * 2 + j
                    wg, wu, wd = wslot[ex % 3]
                    hbb, hk = hbuf(b, j)
                    hs.append((wd, "w%dd" % (ex % 3), hbb, hk))
                for fi in range(4):
                    f = half * 4 + fi

                    def dmm(e, fi=fi, f=f):
                        for j in range(2):
                            wd, wdk, hbb, hk = hs[j]
                            for k in range(2):
                                ins = e.matmul(ACC[fi], wd[:, k, f * 128:(f + 1) * 128], hbb[:, k, :], start=(j == 0 and k == 0), stop=(j == 1 and k == 1))
                        return ins
                    p.pe(dmm, reads=[hs[0][1], hs[1][1], hs[0][3] + "_0", hs[0][3] + "_1", hs[1][3] + "_0", hs[1][3] + "_1"], writes=["psD%d" % fi])
                for fi in range(4):
                    f = half * 4 + fi
                    hsl = HT[:, f, b * T:(b + 1) * T]
                    p.dve(lambda e, fi=fi, f=f, hsl=hsl: e.scalar_tensor_tensor(hsl, ACC[fi], g2[:, f:f + 1], hsl, op0=ALU.mult, op1=ALU.add),
                          reads=["psD%d" % fi], writes=["HT"])

            load_expert(0)
            load_expert(1)
            for pr in range(8):
                if pr > 0:
                    load_expert(2 * pr + 1)
                G(pr, 0, 0)
                G(pr, 0, 1)
                if pr < 7:
                    load_expert(2 * pr + 2)
                for b in range(NB):
                    DOWN(pr, b, 0)
                    if b + 1 < NB:
                        G(pr, b + 1, 0)
                    DOWN(pr, b, 1)
                    if b + 1 < NB:
                        G(pr, b + 1, 1)
            ph.done()

        def store_phase():
            ph = Phase(nc)
            p = ph.p
            PS = ph.psum4()
            ob = [ph.sb([128, 4, D], F32) for _ in range(2)]
            for tt in range(32):
                pst = PS[tt % 2]
                pk = "psO%d" % (tt % 2)

                def tr(e, tt=tt, pst=pst):
                    for c in range(8):
                        ins = e.matmul(pst[:, c * 128:(c + 1) * 128], HT[:, c, tt * 128:(tt + 1) * 128], ident[:], start=True, stop=True)
                    return ins
                p.pe(tr, reads=["HT", "ident"], writes=[pk])
                g4 = tt // 4
                o = ob[g4 % 2]
                ok = "ob%d_%d" % (g4 % 2, tt % 4)
                if tt % 2 == 0:
                    p.act(lambda e, o=o, pst=pst, tt=tt: e.activation(o[:, tt % 4, :], pst[:, :], AF.Identity), reads=[pk], writes=[ok])
                else:
                    p.dve(lambda e, o=o, pst=pst, tt=tt: e.tensor_copy(o[:, tt % 4, :], pst[:, :]), reads=[pk], writes=[ok])
                if tt % 4 == 3:
                    p.dma("sp", "ost%d" % (g4 % 2), lambda e, o=o, g4=g4: e.dma_start(
                        out=out_d[g4 * 512:(g4 + 1) * 512, :].rearrange("(t p) n -> p t n", p=128), in_=o[:]),
                        reads=["ob%d_%d" % (g4 % 2, q) for q in range(4)], writes=["out"])
            ph.done()

        def qk_chain(p, PS, psA, kA, psB, kB, psC, T, gcol, gpcol, cosb, sinb, tq, dst, dstkey, rope=True):
            sqq, rsq, t1, t2 = tq
            p.act(lambda e: e.activation(sqq[:, 0:T], psA, AF.Square), reads=[kA], writes=["sqq"])
            p.pe(lambda e: e.matmul(psC[:, 0:T], onesblk_bf[:], sqq[:, 0:T], start=True, stop=True), reads=["sqq", "onesblk"], writes=["psC"])
            p.act(lambda e: e.activation(rsq[:, 0:T], psC[:, 0:T], AF.Sqrt, bias=EPSB[:, 0:1]), reads=["psC"], writes=["rsq0"])
            p.dve(lambda e: e.reciprocal(rsq[:, 0:T], rsq[:, 0:T]), reads=["rsq0"], writes=["rsq"])
            if rope:
                p.dve(lambda e: e.scalar_tensor_tensor(t1[:, 0:T], psA, gcol, cosb[:, 0:T], op0=ALU.mult, op1=ALU.mult),
                      reads=[kA, "cosb"], writes=["t1"])
                p.dve(lambda e: e.scalar_tensor_tensor(t2[:, 0:T], psB, gpcol, sinb[:, 0:T], op0=ALU.mult, op1=ALU.mult),
                      reads=[kB, "sinb"], writes=["t2"])
                p.pool(lambda e: e.tensor_tensor(t1[:, 0:T], t1[:, 0:T], t2[:, 0:T], ALU.add), reads=["t1", "t2"], writes=["t3"])
                p.dve(lambda e: e.tensor_tensor(dst, t1[:, 0:T], rsq[:, 0:T], ALU.mult), reads=["t3", "rsq"], writes=[dstkey])
            else:
                p.dve(lambda e: e.scalar_tensor_tensor(dst, psA, gcol, rsq[:, 0:T], op0=ALU.mult, op1=ALU.mult),
                      reads=[kA, "rsq"], writes=[dstkey])

        def layer0_mixer():
            ph = Phase(nc)
            p = ph.p
            PS = ph.psum4()
            T = 256
            wq = ph.sb([128, 8, 1792], BF16)
            wv = ph.sb([128, 8, 128], BF16)
            gains = ph.sb([128, 4], F32)
            sqb = [ph.sb([128, T], BF16) for _ in range(2)]
            rstd = ph.sb([128, T], F32)
            tmpb = [ph.sb([128, T], F32) for _ in range(2)]
            aT = ph.sb([128, 8, T], BF16)
            cosb = ph.sb([128, T], F32)
            sinb = ph.sb([128, T], F32)
            tq = (ph.sb([128, T], BF16), ph.sb([128, T], F32), ph.sb([128, T], F32), ph.sb([128, T], F32))
            qf = [ph.sb([128, T], BF16) for _ in range(2)]
            qf4 = ph.sb([128, 4, T], BF16)
            pst4 = ph.sb([128, 4, T], F32)
            vst = [ph.sb([128, 2, 193], BF16) for _ in range(2)]
            ctin = ph.sb([128, D], F32)
            CT = ph.sb([128, 8, 256], F32)
            p.dma("sp", "c1", lambda e: e.dma_start(out=gains[:], in_=gains_d[:, :]), writes=["gains"])
            for vi in range(2):
                p.dve(lambda e, vi=vi: e.memset(vst[vi][:], 0.0), writes=["vst%d" % vi])
                p.dve(lambda e, vi=vi: e.memset(vst[vi][:, :, 64:66], 1.0), reads=["vst%d" % vi], writes=["vst%d" % vi])
            p.dma("pool", "wq", lambda e: e.dma_start(out=wq[:], in_=wqkp_d.rearrange("(c p) n -> p c n", p=128)), writes=["wq"])
            p.dma("pool", "wq", lambda e: e.dma_start(out=wv[:], in_=wv_d.rearrange("(c p) n -> p c n", p=128)), writes=["wv"])
            B_ST = PS[0][:, 0:512]
            B_A = [PS[0][:, 512:1024], PS[1][:, 0:512]]
            B_B = [PS[1][:, 512:1024], PS[2][:, 0:512]]
            B_C = PS[2][:, 512:1024]
            B_M = [PS[3][:, 0:512], PS[3][:, 512:1024]]
            for tt in range(2):
                p.dma("sp", "ctin", lambda e, tt=tt: e.dma_start(out=ctin[:], in_=ctx_d[tt * 128:(tt + 1) * 128, :]), writes=["ctin"])
                for half in range(2):
                    bm = B_M[half]

                    def trc(e, half=half, bm=bm):
                        for c in range(4):
                            cc = half * 4 + c
                            ins = e.matmul(bm[:, c * 128:(c + 1) * 128], ctin[:, cc * 128:(cc + 1) * 128], ident[:], start=True, stop=True)
                        return ins
                    p.pe(trc, reads=["ctin", "ident"], writes=["psM%d" % half])
                    p.dve(lambda e, half=half, bm=bm, tt=tt: e.tensor_copy(CT[:, half * 4:half * 4 + 4, tt * 128:(tt + 1) * 128],
                                                                          bm.rearrange("p (c t) -> p c t", t=128)),
                          reads=["psM%d" % half], writes=["CT"])
            mcnt = [0]

            def proj(p, col0, bank, bkey, T=T):
                def mm(e):
                    for k in range(8):
                        ins = e.matmul(bank[:, 0:T], wq[:, k, col0:col0 + 128], aT[:, k, :], start=(k == 0), stop=(k == 7))
                    return ins
                p.pe(mm, reads=["wq"] + ["aT_%d" % c for c in range(8)], writes=[bkey])

            def vproj(p, tile0):
                i = mcnt[0] % 2
                mcnt[0] += 1
                bm = B_M[i]
                bk = "psM%d" % i
                vs_ = vst[i]

                def mm(e):
                    for t2 in range(2):
                        for k in range(8):
                            ins = e.matmul(bm[:, t2 * 128:(t2 + 1) * 128], aT[:, k, t2 * 128:(t2 + 1) * 128], wv[:, k, :], start=(k == 0), stop=(k == 7))
                    return ins
                p.pe(mm, reads=["wv"] + ["aT_%d" % c for c in range(8)], writes=[bk])
                p.dve(lambda e: e.tensor_copy(vs_[:, :, 0:64], bm[:, 0:256].rearrange("p (t n) -> p t n", n=128)[:, :, 0:64]),
                      reads=[bk], writes=["vst%da" % i])
                p.dve(lambda e: e.tensor_copy(vs_[:, :, 129:193], bm[:, 0:256].rearrange("p (t n) -> p t n", n=128)[:, :, 64:128]),
                      reads=[bk, "vst%da" % i], writes=["vst%d" % i])
                p.dma("sp", "vsst%d" % i, lambda e: e.dma_start(out=VS_d[:, tile0:tile0 + 2, :], in_=vs_[:]), reads=["vst%d" % i, "vst%da" % i], writes=["VSd"])

            norm_block(p, B_ST, "psST", 0, 256, GSC, MODC, sqb, rstd, tmpb, lambda c: (aT[:, c, :], "aT_%d" % c), "c", src=CT, srckey="CT")
            proj(p, 512, B_A[0], "psA0")
            qk_chain(p, PS, B_A[0][:, 0:T], "psA0", None, None, B_C, T, gains[:, 2:3], None, None, None, tq, qf[0][:, 0:T], "qf0", rope=False)
            p.dma("sp", "qst0", lambda e: e.dma_start(out=KT_d[:, 0:256], in_=qf[0][:, 0:T]), reads=["qf0"], writes=["KTd"])
            vproj(p, 0)
            qi = 1
            for b in range(S // T):
                t0 = b * T
                p.dma("sp", "cs", lambda e, t0=t0: e.dma_start(out=cosb[:], in_=cos_d[:, t0:t0 + T]), writes=["cosb"])
                p.dma("sp", "cs", lambda e, t0=t0: e.dma_start(out=sinb[:], in_=sin_d[:, t0:t0 + T]), writes=["sinb"])
                norm_block(p, B_ST, "psST", t0, T, GS[:, 0, :], MOD[:, 0, 0:8], sqb, rstd, tmpb, lambda c: (aT[:, c, :], "aT_%d" % c), "m")
                for j in range(5):
                    i = qi % 2
                    qi += 1
                    col = j * 128 if j < 4 else 512
                    colp = 1152 + j * 128 if j < 4 else 1664
                    proj(p, col, B_A[i], "psA%d" % i)
                    proj(p, colp, B_B[i], "psB%d" % i)
                    gcol = gains[:, 0:1] if j < 4 else gains[:, 2:3]
                    gpcol = gains[:, 1:2] if j < 4 else gains[:, 3:4]
                    if j < 4:
                        qk_chain(p, PS, B_A[i][:, 0:T], "psA%d" % i, B_B[i][:, 0:T], "psB%d" % i, B_C, T, gcol, gpcol, cosb, sinb, tq,
                                 qf4[:, j, :], "qf4_%d" % j)
                        if j == 3:
                            p.dma("sp", "qst4", lambda e, t0=t0: e.dma_start(out=QT_d[:, :, t0:t0 + T].rearrange("c p t -> p c t"), in_=qf4[:]),
                                  reads=["qf4_%d" % q for q in range(4)], writes=["QTd"])
                    else:
                        qk_chain(p, PS, B_A[i][:, 0:T], "psA%d" % i, B_B[i][:, 0:T], "psB%d" % i, B_C, T, gcol, gpcol, cosb, sinb, tq,
                                 qf[i][:, 0:T], "qf%d" % i)
                        p.dma("sp", "qst%d" % i, lambda e, i=i, t0=t0: e.dma_start(out=KT_d[:, 256 + t0:256 + t0 + T], in_=qf[i][:, 0:T]),
                              reads=["qf%d" % i], writes=["KTd"])
                for g in range(4):
                    i = mcnt[0] % 2
                    mcnt[0] += 1
                    proj(p, 640 + g * 128, B_M[i], "psM%d" % i)
                    p.act(lambda e, i=i, g=g: e.activation(pst4[:, g, :], B_M[i][:, 0:T], AF.Identity), reads=["psM%d" % i], writes=["qpst4_%d" % g])
                    if g == 3:
                        p.dma("sp", "pst4", lambda e, t0=t0: e.dma_start(out=PT_d[:, :, t0:t0 + T].rearrange("c p t -> p c t"), in_=pst4[:]),
                              reads=["qpst4_%d" % q for q in range(4)], writes=["PTd"])
                vproj(p, 2 + 2 * b)
            ph.done()
            if stage == 20:
                return
            ph = Phase(nc)
            p = ph.p
            PS = ph.psum4()
            KT = ph.sb([128, 4352], BF16)
            VS = ph.sb([128, 34, 193], BF16)
            Qb = [ph.sb([128, 512], BF16) for _ in range(2)]
            Pb = [ph.sb([128, 1024], BF16) for _ in range(3)]
            rr = ph.sb([128, 512], F32)
            bcs = ph.sb([128, 512], F32)
            mixo = [ph.sb([128, 512], BF16) for _ in range(2)]
            p.dma("sp", "kt", lambda e: e.dma_start(out=KT[:], in_=KT_d[:, :]), writes=["KT"])
            p.dma("sp", "kt", lambda e: e.dma_start(out=VS[:], in_=VS_d[:, :, :]), writes=["VS"])
            SB_ = [PS[0], PS[1]]
            OA = PS[2][:, 0:512]
            OB = PS[2][:, 512:1024]
            BCA = PS[3][:, 0:512]
            BCB = PS[3][:, 512:1024]
            u = 0
            it = 0
            for j in range(4):
                for qb in range(8):
                    qt = Qb[u % 2]
                    qk_ = "Qb%d" % (u % 2)
                    p.dma("sp", qk_, lambda e, qt=qt, j=j, qb=qb: e.dma_start(out=qt[:], in_=QT_d[j, :, qb * 512:(qb + 1) * 512]), writes=[qk_])
                    for kt in range(24 if stage == 7 else 34):
                        sb_ = SB_[it % 2]
                        sk = "psS%d" % (it % 2)
                        pb = Pb[it % 3]
                        pk = "P%d" % (it % 3)
                        it += 1

                        def smm(e, sb_=sb_, qt=qt, kt=kt):
                            e.matmul(sb_[:, 0:512], KT[0:64, kt * 128:(kt + 1) * 128], qt[0:64, :], start=True, stop=True)
                            return e.matmul(sb_[:, 512:1024], KT[64:128, kt * 128:(kt + 1) * 128], qt[64:128, :], start=True, stop=True)
                        p.pe(smm, reads=["KT", qk_], writes=[sk])
                        p.act(lambda e, pb=pb, sb_=sb_: e.activation(pb[:], sb_[:, :], AF.Exp, scale=0.125), reads=[sk], writes=[pk])

                        def pv(e, pb=pb, kt=kt):
                            e.matmul(OA[0:65, :], VS[:, kt, 0:65], pb[:, 0:512], start=(kt == 0), stop=(kt == (23 if stage == 7 else 33)))
                            return e.matmul(OB[:, :], VS[:, kt, 65:193], pb[:, 512:1024], start=(kt == 0), stop=(kt == (23 if stage == 7 else 33)))
                        p.pe(pv, reads=["VS", pk], writes=["psOA", "psOB"])
                    p.dve(lambda e: e.reciprocal(rr[64:65, :], OA[64:65, :]), reads=["psOA"], writes=["rrA"])
                    p.dve(lambda e: e.reciprocal(rr[0:1, :], OB[0:1, :]), reads=["psOB"], writes=["rrB"])
                    p.pe(lambda e: e.matmul(BCA[0:64, :], ones_f[64:65, 0:64], rr[64:65, :], start=True, stop=True), reads=["rrA", "ones_f"], writes=["psBCA"])
                    p.pe(lambda e: e.matmul(BCB[:, :], ones_f[0:1, :], rr[0:1, :], start=True, stop=True), reads=["rrB", "ones_f"], writes=["psBCB"])
                    p.act(lambda e: e.activation(bcs[0:64, :], BCA[0:64, :], AF.Identity), reads=["psBCA"], writes=["bcsA"])
                    p.act(lambda e: e.activation(bcs[64:128, :], BCB[64:128, :], AF.Identity), reads=["psBCB"], writes=["bcsB"])
                    mo = mixo[u % 2]
                    mk = "mixo%d" % (u % 2)
                    p.dve(lambda e, mo=mo: e.tensor_tensor(mo[0:64, :], OA[0:64, :], bcs[0:64, :], ALU.mult), reads=["psOA", "bcsA"], writes=[mk + "a"])
                    p.dve(lambda e, mo=mo: e.tensor_tensor(mo[64:128, :], OB[64:128, :], bcs[64:128, :], ALU.mult), reads=["psOB", "bcsB"], writes=[mk + "b"])
                    p.dma("sp", "mst%d" % (u % 2), lambda e, mo=mo, j=j, qb=qb: e.dma_start(out=MIX_d[j, :, qb * 512:(qb + 1) * 512], in_=mo[:]),
                          reads=[mk + "a", mk + "b"], writes=["MIXd"])
                    u += 1
            ph.done()
            ph = Phase(nc)
            p = ph.p
            PS = ph.psum4()
            W = S + 16
            Pf = ph.sb([128, W], F32)
            sa = ph.sb([128, W], F32)
            sb2 = ph.sb([128, W], F32)
            dbf = ph.sb([128, S], BF16)
            pw = ph.sb([128, 512], BF16)
            psc = ph.sb([128, 4], F32)
            edg = ph.sb([128, 64], F32)
            et = ph.sb([128, 16], F32)
            po = [ph.sb([128, 512], BF16) for _ in range(2)]
            p.dma("pool", "pw", lambda e: e.dma_start(out=pw[:], in_=poolw_d[:, :]), writes=["pw"])
            p.dma("sp", "pc", lambda e: e.dma_start(out=psc[:], in_=poolsc_d[:, :]), writes=["psc"])
            p.dma("sp", "pc", lambda e: e.dma_start(out=edg[:], in_=pooledge_d[:, :]), writes=["edg"])
            p.dve(lambda e: e.memset(Pf[:, 0:8], 0.0), writes=["PfL"])
            p.dve(lambda e: e.memset(Pf[:, W - 8:W], 0.0), writes=["PfR"])
            oc = 0
            for g in range(4):
                w_ = 2 ** (g + 1)
                p.dma("sp", "pf", lambda e, g=g: e.dma_start(out=Pf[:, 8:8 + S], in_=PT_d[g, :, :]), writes=["Pf"])
                p.dve(lambda e: e.tensor_tensor(sa[:, 1:W], Pf[:, 0:W - 1], Pf[:, 1:W], ALU.add), reads=["Pf", "PfL", "PfR"], writes=["sa"])
                cur, ck = sa, "sa"
                oth, ok_ = sb2, "sb"
                lo, hi, sh_ = 1, W, 1
                for st in range(g):
                    nlo, nhi = lo + sh_, hi - sh_
                    eng = p.pool if st % 2 == 0 else p.dve
                    eng(lambda e, cur=cur, oth=oth, nlo=nlo, nhi=nhi, sh_=sh_: e.tensor_tensor(oth[:, nlo:nhi], cur[:, nlo - sh_:nhi - sh_], cur[:, nlo + sh_:nhi + sh_], ALU.add),
                        reads=[ck], writes=[ok_])
                    cur, ck, oth, ok_ = oth, ok_, cur, ck
                    lo, hi = nlo, nhi
                    sh_ *= 2
                assert lo <= 8 and hi >= 8 + S
                p.dve(lambda e, cur=cur, w_=w_: e.scalar_tensor_tensor(dbf[:, :], cur[:, 8:8 + S], 1.0 / w_, Pf[:, 8:8 + S], op0=ALU.mult, op1=ALU.subtract),
                      reads=[ck, "Pf"], writes=["dbf0"])
                p.dve(lambda e, cur=cur, g=g: e.tensor_tensor(et[:, 0:8], cur[:, 8:16], edg[:, g * 16:g * 16 + 8], ALU.mult), reads=[ck, "edg"], writes=["et0"])
                p.dve(lambda e, cur=cur, g=g: e.tensor_tensor(et[:, 8:16], cur[:, S:8 + S], edg[:, g * 16 + 8:g * 16 + 16], ALU.mult), reads=[ck, "edg", "et0"], writes=["et1"])
                p.dve(lambda e: e.tensor_tensor(dbf[:, 0:8], et[:, 0:8], Pf[:, 8:16], ALU.subtract), reads=["et1", "Pf", "dbf0"], writes=["dbf1"])
                p.dve(lambda e: e.tensor_tensor(dbf[:, S - 8:S], et[:, 8:16], Pf[:, S:8 + S], ALU.subtract), reads=["et1", "Pf", "dbf1"], writes=["dbf"])
                for b in range(8):
                    i = oc % 2
                    oc += 1
                    bank = PS[i][:, 0:512]
                    p.pe(lambda e, bank=bank, g=g, b=b: e.matmul(bank, pw[:, g * 128:(g + 1) * 128], dbf[:, b * 512:(b + 1) * 512], start=True, stop=True),
                         reads=["pw", "dbf"], writes=["psP%d" % i])
                    p.act(lambda e, bank=bank, i=i, g=g: e.activation(po[i][:], bank, AF.Identity, scale=psc[:, g:g + 1]), reads=["psP%d" % i, "psc"], writes=["po%d" % i])
                    p.dma("sp", "post%d" % i, lambda e, i=i, g=g, b=b: e.dma_start(out=MIX_d[4 + g, :, b * 512:(b + 1) * 512], in_=po[i][:]),
                          reads=["po%d" % i], writes=["MIXd"])
            ph.done()
            wout_phase(wout0_d, 0)

        def layer1_mixer():
            ph = Phase(nc)
            p = ph.p
            PS = ph.psum4()
            T = 256
            w1 = ph.sb([128, 8, 2560], BF16)
            sqb = [ph.sb([128, T], BF16) for _ in range(2)]
            rstd = ph.sb([128, T], F32)
            tmpb = [ph.sb([128, T], F32) for _ in range(2)]
            aT = ph.sb([128, 8, T], BF16)
            sggain = ph.sb([128, 512], F32)
            sgwT = ph.sb([128, 512], BF16)
            sgbb = ph.sb([128, 512], F32)
            usb = ph.sb([128, 4, T], F32)
            hxs = [ph.sb([128, T], F32)] * 2
            zs = hxs
            bgs = [ph.sb([128, T], F32)] * 2
            sqv = ph.sb([128, 512], F32)
            ssum = ph.sb([128, 4], F32)
            vt = sqv
            vn = ph.sb([128, 4, 128], BF16)
            st_ = ph.sb([128, 4, 128], F32)
            yc = [ph.sb([128, 4, T], BF16) for _ in range(2)]
            p.dma("pool", "w1", lambda e: e.dma_start(out=w1[:, :, 0:1280], in_=win1_d[:, 0:1280].rearrange("(c p) n -> p c n", p=128)), writes=["w1"])
            p.dma("pool", "w1", lambda e: e.dma_start(out=w1[:, :, 1280:2560], in_=win1_d[:, 1280:2560].rearrange("(c p) n -> p c n", p=128)), writes=["w1"])
            p.dma("pool", "w1", lambda e: e.dma_start(out=sgwT[:], in_=sgw_d[:, :]), writes=["sgwT"])
            p.dma("sp", "c2", lambda e: e.dma_start(out=sggain[:], in_=sggain_d[:, :]), writes=["sggain"])
            p.dma("sp", "c2", lambda e: e.dma_start(out=sgbb[:], in_=sgb_d[:, :]), writes=["sgbb"])
            B_ST = PS[0][:, 0:512]
            B_U = [PS[0][:, 512:1024], PS[1][:, 0:512]]
            B_H = [PS[1][:, 512:1024], PS[2][:, 0:512]]
            B_G = PS[2][:, 512:1024]
            B_V = PS[3][:, 0:512]
            B_S = PS[3][:, 512:1024]
            akeys = ["aT_%d" % c for c in range(8)]

            def proj(col0, tgt, bkey):
                def mm(e):
                    for k in range(8):
                        ins = e.matmul(tgt, w1[:, k, col0:col0 + 128], aT[:, k, :], start=(k == 0), stop=(k == 7))
                    return ins
                p.pe(mm, reads=["w1"] + akeys, writes=[bkey])
            hi_ = 0
            for b in range(S // T):
                t0 = b * T
                norm_block(p, B_ST, "psST", t0, T, GS[:, 2, :], MOD[:, 1, 0:8], sqb, rstd, tmpb, lambda c: (aT[:, c, :], "aT_%d" % c), "m")
                for half in range(2):
                    for q in range(2):
                        g = half * 2 + q
                        proj(g * 128, B_U[half][:, q * T:(q + 1) * T], "psU%d" % half)
                    p.act(lambda e, half=half: e.activation(usb[:, half * 2:half * 2 + 2, :], B_U[half][:, 0:2 * T].rearrange("p (q t) -> p q t", t=T), AF.Identity),
                          reads=["psU%d" % half], writes=["usb%d" % half])
                for c in range(4):
                    i = hi_ % 2
                    hi_ += 1
                    proj(1024 + c * 128, B_H[i][:, 0:T], "psH%d" % i)
                    proj(2048 + c * 128, B_H[i][:, T:2 * T], "psH%d" % i)
                    p.act(lambda e, i=i: e.activation(hxs[i][:], B_H[i][:, 0:T], AF.Identity), reads=["psH%d" % i], writes=["hxs0"])
                    p.dve(lambda e, i=i: e.tensor_tensor(zs[i][:], B_H[i][:, T:2 * T], hxs[i][:], ALU.mult), reads=["psH%d" % i, "hxs0"], writes=["hxs0"])
                    p.dma("sp", "zst%d" % i, lambda e, i=i, c=c, t0=t0: e.dma_start(out=PT_d[c, :, t0:t0 + T], in_=zs[i][:]), reads=["hxs0"], writes=["PTd"])
                    proj(1536 + c * 128, B_G[:, 0:T], "psG")
                    p.act(lambda e, i=i: e.activation(bgs[i][:], B_G[:, 0:T], AF.Identity), reads=["psG"], writes=["bgs0"])
                    p.dma("sp", "bst%d" % i, lambda e, i=i, c=c, t0=t0: e.dma_start(out=BG_d[c, :, t0:t0 + T], in_=bgs[i][:]), reads=["bgs0"], writes=["BGd"])
                yb = yc[b % 2]
                yk = "yc%d" % (b % 2)
                for n in range(2):
                    def vmm(e, n=n):
                        for g in range(4):
                            for k in range(8):
                                ins = e.matmul(B_V[:, g * 128:(g + 1) * 128], aT[:, k, n * 128:(n + 1) * 128], w1[:, k, 512 + g * 128:512 + (g + 1) * 128],
                                               start=(k == 0), stop=(k == 7))
                        return ins
                    p.pe(vmm, reads=["w1"] + akeys, writes=["psV"])
                    p.act(lambda e: e.activation(sqv[:], B_V, AF.Square), reads=["psV"], writes=["sqv"])
                    p.dve(lambda e: e.tensor_reduce(ssum[:], sqv[:].rearrange("p (g c) -> p g c", c=128), AX.X, ALU.add), reads=["sqv"], writes=["ssum0"])
                    p.act(lambda e: e.activation(ssum[:], ssum[:], AF.Sqrt, bias=EPSB[:, 0:1], scale=1.0 / 128.0), reads=["ssum0"], writes=["ssum1"])
                    p.dve(lambda e: e.reciprocal(ssum[:], ssum[:]), reads=["ssum1"], writes=["ssum"])
                    p.dve(lambda e: e.tensor_tensor(vt[:].rearrange("p (g c) -> p g c", c=128), B_V.rearrange("p (g c) -> p g c", c=128), ssum[:].unsqueeze(2).to_broadcast([128, 4, 128]), ALU.mult),
                          reads=["psV", "ssum", "sqv"], writes=["sqv"])
                    p.pool(lambda e: e.tensor_tensor(vn[:].rearrange("p g c -> p (g c)"), vt[:], sggain[:], ALU.mult), reads=["sqv", "sggain"], writes=["vn"])

                    def smm(e):
                        for g in range(4):
                            ins = e.matmul(B_S[:, g * 128:(g + 1) * 128], vn[:, g, :], sgwT[:, g * 128:(g + 1) * 128], start=True, stop=True)
                        return ins
                    p.pe(smm, reads=["vn", "sgwT"], writes=["psS"])
                    p.dve(lambda e: e.tensor_tensor(st_[:], B_S.rearrange("p (g c) -> p g c", c=128), sgbb[:].rearrange("p (g c) -> p g c", c=128), ALU.add),
                          reads=["psS", "sgbb"], writes=["st"])
                    p.pool(lambda e, n=n, yb=yb: e.tensor_tensor(yb[:, :, n * 128:(n + 1) * 128], st_[:], usb[:, :, n * 128:(n + 1) * 128], ALU.mult),
                           reads=["st", "usb0", "usb1"], writes=[yk + "_%d" % n])
                p.dma("sp", "yst%d" % (b % 2), lambda e, yb=yb, t0=t0: e.dma_start(out=MIX_d[0:4, :, t0:t0 + T].rearrange("c p t -> p c t"), in_=yb[:]),
                      reads=[yk + "_0", yk + "_1"], writes=["MIXd"])
            ph.done()
            ph = Phase(nc)
            p = ph.p
            W = S + 2
            Z = ph.sb([128, W], F32)
            BGr = ph.sb([128, S], F32)
            t1 = ph.sb([128, S], F32)
            yo = ph.sb([128, S], BF16)
            cw = ph.sb([128, 12], F32)
            p.dma("sp", "cw", lambda e: e.dma_start(out=cw[:], in_=convw_d[:, :]), writes=["cw"])
            p.dve(lambda e: e.memset(Z[:, 0:1], 0.0), writes=["ZL"])
            p.dve(lambda e: e.memset(Z[:, W - 1:W], 0.0), writes=["ZR"])
            for c in range(4):
                p.dma("sp", "z", lambda e, c=c: e.dma_start(out=Z[:, 1:1 + S], in_=PT_d[c, :, :]), writes=["Z"])
                p.dma("sp", "bg", lambda e, c=c: e.dma_start(out=BGr[:], in_=BG_d[c, :, :]), writes=["BGr"])
                p.dve(lambda e, c=c: e.tensor_scalar(t1[:], Z[:, 1:1 + S], cw[:, c * 3 + 1:c * 3 + 2], None, op0=ALU.mult), reads=["Z", "cw"], writes=["t1a"])
                p.dve(lambda e, c=c: e.scalar_tensor_tensor(t1[:], Z[:, 0:S], cw[:, c * 3:c * 3 + 1], t1[:], op0=ALU.mult, op1=ALU.add),
                      reads=["Z", "ZL", "cw", "t1a"], writes=["t1b"])
                p.dve(lambda e, c=c: e.scalar_tensor_tensor(t1[:], Z[:, 2:2 + S], cw[:, c * 3 + 2:c * 3 + 3], t1[:], op0=ALU.mult, op1=ALU.add),
                      reads=["Z", "ZR", "cw", "t1b"], writes=["t1c"])
                p.pool(lambda e: e.tensor_tensor(yo[:], t1[:], BGr[:], ALU.mult), reads=["t1c", "BGr"], writes=["yo"])
                p.dma("sp", "yo", lambda e, c=c: e.dma_start(out=MIX_d[4 + c, :, :], in_=yo[:]), reads=["yo"], writes=["MIXd"])
            ph.done()
            wout_phase(wout1_d, 1)

        if tail_only:
            router_moe(1)
        else:
            if stage >= 1:
                layer0_mixer()
            if stage >= 2 and stage < 20:
                router_moe(0)
            if stage == 9:
                router_moe(1)
                layer1_mixer()
            if stage >= 3 and stage < 9 and stage != 5:
                layer1_mixer()
            if stage >= 4 and stage < 9 and stage != 6:
                router_moe(1)
        if stage == 12:
            ph = Phase(nc)
            big = ph.sb([128, 4096], F32)
            for i_ in range(4000):
                ph.p.dve(lambda e: e.memset(big[:], 1.0), writes=["big"])
            ph.done()
        if stage == 6:
            for _ in range(3):
                ph = Phase(nc)
                ph.p.dve(lambda e: e.memset(EPSB[:], EPS), writes=["epsb"])
                ph.done()
        store_phase()
    return nc


def _perm64():
    d = np.arange(64)
    return np.where((d % 32) < 16, d + 16, d - 16)


def prep_inputs(inputs):
    f = lambda a: np.ascontiguousarray(np.asarray(a, dtype=np.float32))
    I = {k: np.asarray(v) for k, v in inputs.items()}
    shared = {}
    shared["ident"] = np.eye(128, dtype=np.float32)
    shared["mod_w"] = f(I["mod_w"])
    shared["mod_bT"] = f(I["mod_b"].reshape(2, 48, 128).transpose(2, 0, 1).reshape(128, 96))
    ng = np.stack([I["norm1_g"], I["norm2_g"]], 0)
    shared["norm_g"] = f(ng.reshape(2, 2, 8, 128).transpose(3, 0, 1, 2).reshape(128, 32))
    w_in0 = I["even_w_in"][0]
    pi = _perm64()
    qcols, qpcols = [], []
    for j in range(4):
        for h in (j, j + 4):
            qcols.append(h * 64 + np.arange(64))
            qpcols.append(h * 64 + pi)
    qcols = np.concatenate(qcols)
    qpcols = np.concatenate(qpcols)
    kcols = 512 + np.arange(128)
    kpcols = 512 + np.concatenate([pi, 64 + pi])
    pcols = 768 + np.arange(512)
    allc = np.concatenate([qcols, kcols, pcols, qpcols, kpcols])
    shared["w_qkp"] = f(w_in0[:, allc])
    shared["w_v"] = f(w_in0[:, 640:768])
    qg = I["q_gain"][0]
    kg = I["k_gain"][0]
    d = np.arange(128) % 64
    shared["gains"] = f(np.stack([qg[d], qg[pi[d]], kg[d], kg[pi[d]]], 1))
    t = np.arange(S)
    row = (t // 64).astype(np.float32)
    col = (t % 64).astype(np.float32)
    inv = (10000.0 ** (-np.arange(0, 16, dtype=np.float32) * 2 / 32.0)).astype(np.float32)
    cos_t = np.zeros((128, S), np.float32)
    sin_t = np.zeros((128, S), np.float32)
    for pp in range(128):
        dd = pp % 64
        jj = dd % 16
        pos = row if dd < 32 else col
        ang = (pos * inv[jj]).astype(np.float32)
        sgn = -1.0 if (dd % 32) < 16 else 1.0
        cos_t[pp] = np.cos(ang)
        sin_t[pp] = sgn * np.sin(ang)
    shared["cos_t"] = cos_t
    shared["sin_t"] = sin_t
    shared["pool_wT"] = f(I["pool_w"][0].transpose(1, 0, 2).reshape(128, 512))
    shared["pool_sc"] = f(I["pool_scale"][0].reshape(4, 128).T)
    edge = np.zeros((128, 4, 16), np.float32)
    for g, w in enumerate((2, 4, 8, 16)):
        for jx in range(16):
            tpos = jx if jx < 8 else S - 16 + jx
            lo = max(tpos - w // 2, 0)
            hi = min(tpos + w - w // 2, S)
            edge[:, g, jx] = 1.0 / (hi - lo)
    shared["pool_edge"] = edge.reshape(128, 64)
    w_out0 = I["even_w_out"][0]
    rows = []
    for c in range(4):
        for h in (c, c + 4):
            rows.append(h * 64 + np.arange(64))
    rows.append(512 + np.arange(512))
    shared["w_out0"] = f(w_out0[np.concatenate(rows), :])
    shared["w_out1"] = f(I["odd_w_out"][0])
    shared["w_in1"] = f(I["odd_w_in"][0])
    shared["sg_gain_b"] = f(np.broadcast_to(I["sg_gain"][0].reshape(1, 512), (128, 512)))
    shared["sg_wT"] = f(I["sg_w"][0].transpose(2, 0, 1).reshape(128, 512))
    shared["sg_b_b"] = f(np.broadcast_to(I["sg_b"][0].reshape(1, 512), (128, 512)))
    shared["conv_wT"] = f(I["conv_w"][0][:, 0, :].reshape(3, 4, 128).transpose(2, 1, 0).reshape(128, 12))
    shared["rw"] = f(np.concatenate([I["router_g_w"], I["router_e_w"]], axis=2))
    rb = np.concatenate([I["router_g_b"], I["router_e_b"]], axis=1)
    shared["rb_b"] = f(np.broadcast_to(np.tile(rb[:, None, :], (1, 4, 1)).reshape(1, 160), (128, 160)))
    shared["w_gate"] = f(I["w_gate"])
    shared["w_up"] = f(I["w_up"])
    shared["w_down"] = f(I["w_down"])
    sel = np.zeros((32, 16, 128), np.float32)
    for ex in range(16):
        sel[ex, ex, :] = 1.0
        sel[16 + ex, ex, :] = 1.0
    shared["sel"] = sel.reshape(32, 2048)
    in_maps = []
    for b in range(NCORES):
        m = dict(shared)
        m["x"] = f(I["x"][b])
        m["ctx"] = f(I["ctx"][b])
        cv = np.stack([I["c"][b], I["c_ctx"]], 0)
        m["cvec"] = f(cv.reshape(2, 8, 128).transpose(2, 1, 0).reshape(128, 16))
        in_maps.append(m)
    return in_maps


_NC_CACHE = {}


def kernel(**inputs):
    in_maps = prep_inputs(inputs)
    if "nc" not in _NC_CACHE:
        _NC_CACHE["nc"] = (build(stage=3), build(tail_only=True))
    nc1, nc2 = _NC_CACHE["nc"]
    res = run_bass_kernel_spmd(nc1, in_maps, core_ids=list(range(NCORES)))
    for b in range(NCORES):
        in_maps[b]["x"] = np.ascontiguousarray(np.asarray(res.results[b]["out"], dtype=np.float32))
    res = run_bass_kernel_spmd(nc2, in_maps, core_ids=list(range(NCORES)))
    out = np.stack([np.asarray(r["out"]) for r in res.results], axis=0)
    return out.astype(np.float32)
```

```python
import numpy as np
from contextlib import ExitStack
import concourse.bass as bass
import concourse.mybir as mybir
from concourse.bass_utils import run_bass_kernel_spmd

F32 = mybir.dt.float32
BF16 = mybir.dt.bfloat16
AF = mybir.ActivationFunctionType
ALU = mybir.AluOpType
AX = mybir.AxisListType

ENGS = ["pe", "act", "dve", "pool", "sp"]
S = 4096
D = 1024
NCORES = 8
EPS = 1e-6
_CNT = [0]
_PHASE = [0]
_SEMPOOL = [[], []]
NSEM = 16


class Op:
    __slots__ = ("eng", "fn", "reads", "writes", "dma_key", "idx", "waits", "signal", "semval")

    def __init__(self, eng, fn, reads, writes, dma_key):
        self.eng = eng
        self.fn = fn
        self.reads = reads
        self.writes = writes
        self.dma_key = dma_key
        self.waits = []
        self.signal = False
        self.semval = 0


class Prog:
    def __init__(self, nc):
        self.nc = nc
        self.ops = []

    def op(self, eng, fn, reads=(), writes=(), dma_key=None):
        reads = tuple(reads)
        writes = tuple(writes)
        ex = tuple(r for r in reads if r.startswith("ps"))
        o = Op(eng, fn, reads, writes + ex, dma_key)
        o.idx = len(self.ops)
        self.ops.append(o)
        return o

    def pe(self, fn, reads=(), writes=()):
        return self.op("pe", fn, reads, writes)

    def act(self, fn, reads=(), writes=()):
        return self.op("act", fn, reads, writes)

    def dve(self, fn, reads=(), writes=()):
        return self.op("dve", fn, reads, writes)

    def pool(self, fn, reads=(), writes=()):
        return self.op("pool", fn, reads, writes)

    def dma(self, eng, key, fn, reads=(), writes=()):
        return self.op(eng, fn, reads, writes, dma_key=key)

    def finalize(self):
        ops = self.ops

        def tl(o):
            return ("dma", o.dma_key) if o.dma_key is not None else o.eng

        pos = {}
        cnt = {}
        for o in ops:
            t = tl(o)
            cnt[t] = cnt.get(t, 0) + 1
            pos[o.idx] = cnt[t]
        last_writer = {}
        readers = {}
        known = {e: {} for e in ENGS}
        done_clock = {}
        needed = set()
        latest_on_key = {}
        for o in ops:
            deps = set()
            raw = set()
            for r in o.reads:
                w = last_writer.get(r)
                if w is not None:
                    deps.add(w)
                    raw.add(w)
            for r in o.writes:
                w = last_writer.get(r)
                if w is not None:
                    deps.add(w)
                    if r.startswith("ps"):
                        raw.add(w)
                for rd in readers.get(r, ()):
                    deps.add(rd)
            req = {}
            for d in deps:
                if d == o.idx:
                    continue
                po = ops[d]
                t = tl(po)
                if po.dma_key is None and o.dma_key is None and po.eng == o.eng:
                    if o.eng == "pe":
                        continue
                    if d not in raw:
                        continue
                if t not in req or pos[d] > pos[req[t]]:
                    req[t] = d
            kn = known[o.eng]
            for t in list(req.keys()):
                if not isinstance(t, str):
                    req[t] = latest_on_key[t]
            waits = []
            for t, d in req.items():
                if kn.get(t, 0) >= pos[d]:
                    continue
                waits.append(d)
                needed.add(d)
                for t2, p2 in done_clock[d].items():
                    if kn.get(t2, 0) < p2:
                        kn[t2] = p2
            o.waits = waits
            dc = dict(kn)
            dc[tl(o)] = max(dc.get(tl(o), 0), pos[o.idx])
            done_clock[o.idx] = dc
            if o.dma_key is not None:
                latest_on_key[tl(o)] = o.idx
            for r in o.writes:
                last_writer[r] = o.idx
                readers[r] = []
            for r in o.reads:
                if r not in o.writes:
                    readers.setdefault(r, []).append(o.idx)
        semcnt = {}
        for o in ops:
            t = tl(o)
            if o.dma_key is not None:
                semcnt[t] = semcnt.get(t, 0) + 16
                o.signal = True
                o.semval = semcnt[t]
            elif o.idx in needed:
                semcnt[t] = semcnt.get(t, 0) + 1
                o.signal = True
                o.semval = semcnt[t]
        self.timelines = sorted(set(tl(o) for o in ops if o.signal), key=str)
        self._tl = tl
        return self

    def emit(self):
        nc = self.nc
        ops = self.ops
        tl = self._tl
        with ExitStack() as es:
            pidx = _PHASE[0] % 2
            _PHASE[0] += 1
            mypool = _SEMPOOL[pidx]
            other = _SEMPOOL[1 - pidx]
            assert len(self.timelines) <= len(mypool), len(self.timelines)
            sems = {}
            for i, t in enumerate(self.timelines):
                sems[t] = mypool[i]
            block = es.enter_context(nc.Block())
            by_eng = {e: [o for o in ops if o.eng == e] for e in ENGS}
            final_dma = {}
            for o in ops:
                if o.dma_key is not None:
                    final_dma[tl(o)] = o.semval

            def run(engname, eng):
                for o in by_eng[engname]:
                    ws = list(o.waits)
                    att = None
                    if ws and engname != "pe":
                        att = ws.pop()
                    for d in ws:
                        po = ops[d]
                        eng.wait_ge(sems[tl(po)], po.semval)
                    if att is not None:
                        rec = _Rec(eng)
                        ins = o.fn(rec)
                        po = ops[att]
                        rec.first._wait_ge(sems[tl(po)], po.semval)
                    else:
                        ins = o.fn(eng)
                    if o.signal:
                        ins.then_inc(sems[tl(o)], 16 if o.dma_key is not None else 1)
                if engname == "sp":
                    for t, v in final_dma.items():
                        eng.wait_ge(sems[t], v)
                    for sm in other:
                        eng.sem_clear(sm)

            @block.tensor
            def _(eng):
                run("pe", eng)

            @block.scalar
            def _(eng):
                run("act", eng)

            @block.vector
            def _(eng):
                run("dve", eng)

            @block.gpsimd
            def _(eng):
                run("pool", eng)

            @block.sync
            def _(eng):
                run("sp", eng)


class _Rec:
    def __init__(self, eng):
        self._eng = eng
        self.first = None

    def __getattr__(self, name):
        f = getattr(self._eng, name)

        def g(*a, **k):
            r = f(*a, **k)
            if self.first is None:
                self.first = r
            return r
        return g


class Phase:
    def __init__(self, nc):
        self.nc = nc
        self.es = ExitStack()
        self.p = Prog(nc)
        self._n = 0

    def sb(self, shape, dt):
        self._n += 1
        _CNT[0] += 1
        return self.es.enter_context(self.nc.sbuf_tensor("sb%d" % _CNT[0], list(shape), dt))

    def psum4(self):
        r = []
        for _ in range(4):
            _CNT[0] += 1
            r.append(self.es.enter_context(self.nc.psum_tensor("ps%d" % _CNT[0], [128, 1024], F32)))
        return r

    def done(self):
        self.p.finalize()
        self.p.emit()
        self.es.close()


def build(stage=4, tail_only=False):
    nc = bass.Bass("TRN2", target_bir_lowering=False)

    def din(name, shape, dt=F32):
        return nc.dram_tensor(name, list(shape), dt, kind="ExternalInput").ap()

    def dscr(name, shape, dt):
        return nc.dram_tensor(name, list(shape), dt, kind="Internal").ap()

    x_d = din("x", [S, D])
    ctx_d = din("ctx", [256, D])
    cvec_d = din("cvec", [128, 16])
    ident_d = din("ident", [128, 128])
    modw_d = din("mod_w", [2, D, 6144])
    modb_d = din("mod_bT", [128, 96])
    ng_d = din("norm_g", [128, 32])
    wqkp_d = din("w_qkp", [D, 1792])
    wv_d = din("w_v", [D, 128])
    gains_d = din("gains", [128, 4])
    cos_d = din("cos_t", [128, S])
    sin_d = din("sin_t", [128, S])
    poolw_d = din("pool_wT", [128, 512])
    poolsc_d = din("pool_sc", [128, 4])
    pooledge_d = din("pool_edge", [128, 64])
    wout0_d = din("w_out0", [D, D])
    wout1_d = din("w_out1", [D, D])
    win1_d = din("w_in1", [D, 2560])
    sggain_d = din("sg_gain_b", [128, 512])
    sgw_d = din("sg_wT", [128, 512])
    sgb_d = din("sg_b_b", [128, 512])
    convw_d = din("conv_wT", [128, 12])
    rw_d = din("rw", [2, D, 20])
    rb_d = din("rb_b", [128, 160])
    wg_d = din("w_gate", [2, 16, D, 256])
    wu_d = din("w_up", [2, 16, D, 256])
    wd_d = din("w_down", [2, 16, 256, D])
    sel_d = din("sel", [32, 2048])
    out_d = nc.dram_tensor("out", [S, D], F32, kind="ExternalOutput").ap()

    A2_d = dscr("A2s", [8, 128, S], BF16)
    QT_d = dscr("QTs", [4, 128, S], BF16)
    MIX_d = dscr("MIXs", [8, 128, S], BF16)
    PT_d = dscr("PTs", [4, 128, S], F32)
    BG_d = dscr("BGs", [4, 128, S], F32)
    KT_d = dscr("KTs", [128, 4352], BF16)
    VS_d = dscr("VSs", [128, 34, 193], BF16)

    with ExitStack() as top:
        def psb(shape, dt):
            _CNT[0] += 1
            return top.enter_context(nc.sbuf_tensor("pt%d" % _CNT[0], list(shape), dt))

        _PHASE[0] = 0
        for pi_ in range(2):
            _SEMPOOL[pi_] = []
            for si_ in range(NSEM):
                _SEMPOOL[pi_].append(top.enter_context(nc.semaphore("sp%d_%d" % (pi_, si_))))
        HT = psb([128, 8, S], F32)
        ident = psb([128, 128], F32)
        ident_bf = psb([128, 128], BF16)
        ones_bf = psb([128, 128], BF16)
        onesblk_bf = psb([128, 128], BF16)
        ones_f = psb([128, 128], F32)
        MOD = psb([128, 2, 48], F32)
        MODC = psb([128, 16], F32)
        NG = psb([128, 32], F32)
        GS = psb([128, 4, 8], F32)
        GSC = psb([128, 8], F32)
        combT = psb([32, S], BF16)
        EPSB = psb([128, 1], F32)

        ph = Phase(nc)
        p = ph.p
        PS = ph.psum4()
        cvec = ph.sb([128, 16], F32)
        scv = ph.sb([128, 16], F32)
        modb = ph.sb([128, 96], F32)
        mrow = ph.sb([2, 6144], F32)
        mwbuf = [ph.sb([128, 8, 512], F32) for _ in range(2)]
        xin = [ph.sb([128, D], F32) for _ in range(2)]
        p.dma("sp", "c0", lambda e: e.dma_start(out=ident[:], in_=ident_d[:, :]), writes=["ident"])
        p.dma("sp", "c0", lambda e: e.dma_start(out=cvec[:], in_=cvec_d[:, :]), writes=["cvec"])
        p.dma("sp", "c0", lambda e: e.dma_start(out=modb[:], in_=modb_d[:, :]), writes=["modb"])
        p.dma("sp", "c0", lambda e: e.dma_start(out=NG[:], in_=ng_d[:, :]), writes=["NG"])
        p.dve(lambda e: e.tensor_copy(ident_bf[:], ident[:]), reads=["ident"], writes=["ident_bf"])
        p.dve(lambda e: e.memset(ones_bf[:], 1.0 / 1024.0), writes=["ones_bf"])
        p.dve(lambda e: e.memset(ones_f[:], 1.0), writes=["ones_f"])
        p.dve(lambda e: e.memset(EPSB[:], EPS), writes=["epsb"])
        p.dve(lambda e: e.memset(onesblk_bf[:], 0.0), writes=["onesblk0"])
        p.dve(lambda e: e.memset(onesblk_bf[0:64, 0:64], 1.0 / 64.0), reads=["onesblk0"], writes=["onesblk1"])
        p.dve(lambda e: e.memset(onesblk_bf[64:128, 64:128], 1.0 / 64.0), reads=["onesblk1"], writes=["onesblk"])
        p.act(lambda e: e.activation(scv[:], cvec[:], AF.Silu), reads=["cvec"], writes=["scv"])
        for l in range(2):
            for fb in range(12):
                it = l * 12 + fb
                buf = mwbuf[it % 2]
                bk = "mw%d" % (it % 2)
                p.dma("sp", bk, lambda e, buf=buf, l=l, fb=fb: e.dma_start(
                    out=buf[:], in_=modw_d[l, :, fb * 512:(fb + 1) * 512].rearrange("(c p) n -> p c n", p=128)),
                    writes=[bk])
                pb = "psA%d" % (it % 2)
                pst = PS[0][:, (it % 2) * 512:(it % 2) * 512 + 512]

                def mm(e, buf=buf, pst=pst):
                    for c in range(8):
                        ins = e.matmul(pst[0:2, :], scv[:, c * 2:c * 2 + 2], buf[:, c, :], start=(c == 0), stop=(c == 7))
                    return ins
                p.pe(mm, reads=[bk, "scv"], writes=[pb])
                if it % 2 == 0:
                    p.act(lambda e, pst=pst, fb=fb: e.activation(mrow[0:2, fb * 512:(fb + 1) * 512], pst[0:2, :], AF.Identity),
                          reads=[pb], writes=["mrow_%d" % fb])
                else:
                    p.dve(lambda e, pst=pst, fb=fb: e.tensor_copy(mrow[0:2, fb * 512:(fb + 1) * 512], pst[0:2, :]),
                          reads=[pb], writes=["mrow_%d" % fb])

            def tr(e):
                for j in range(48):
                    ins = e.matmul(PS[1][:, j * 2:j * 2 + 2], mrow[0:2, j * 128:(j + 1) * 128], ident[0:2, 0:2], start=True, stop=True)
                return ins
            p.pe(tr, reads=["mrow_%d" % fb for fb in range(12)] + ["ident"], writes=["psB0"])
            p.dve(lambda e, l=l: e.tensor_tensor(MOD[:, l, :], PS[1][:, 0:96].rearrange("p (j t) -> p j t", t=2)[:, :, 0],
                                                modb[:, l * 48:(l + 1) * 48], ALU.add),
                  reads=["psB0", "modb"], writes=["MOD%d" % l])
            if l == 0:
                p.dve(lambda e: e.tensor_tensor(MODC[:], PS[1][:, 0:32].rearrange("p (j t) -> p j t", t=2)[:, :, 1],
                                                modb[:, 0:16], ALU.add),
                      reads=["psB0", "modb"], writes=["MODC"])
        for l in range(2):
            for n in range(2):
                sc = MOD[:, l, (1 + 3 * n) * 8:(2 + 3 * n) * 8]
                p.dve(lambda e, l=l, n=n, sc=sc: e.scalar_tensor_tensor(GS[:, l * 2 + n, :], sc, 1.0, NG[:, n * 16 + l * 8:n * 16 + l * 8 + 8],
                                                                      op0=ALU.add, op1=ALU.mult),
                      reads=["MOD%d" % l, "NG"], writes=["GS%d%d" % (l, n)])
        p.dve(lambda e: e.scalar_tensor_tensor(GSC[:], MODC[:, 8:16], 1.0, NG[:, 0:8], op0=ALU.add, op1=ALU.mult),
              reads=["MODC", "NG"], writes=["GSC"])
        for tt in range(32):
            xb = xin[tt % 2]
            xk = "xin%d" % (tt % 2)
            p.dma("sp", xk, lambda e, xb=xb, tt=tt: e.dma_start(out=xb[:], in_=x_d[tt * 128:(tt + 1) * 128, :]), writes=[xk])
            pst = PS[2 + tt % 2]
            pk = "psX%d" % (tt % 2)

            def trx(e, xb=xb, pst=pst):
                for c in range(8):
                    ins = e.matmul(pst[:, c * 128:(c + 1) * 128], xb[:, c * 128:(c + 1) * 128], ident[:], start=True, stop=True)
                return ins
            p.pe(trx, reads=[xk, "ident"], writes=[pk])
            dst = HT[:, :, tt * 128:(tt + 1) * 128]
            src = pst[:, :].rearrange("p (c t) -> p c t", t=128)
            if tt % 2 == 0:
                p.act(lambda e, dst=dst, src=src: e.activation(dst, src, AF.Identity), reads=[pk], writes=["HT%d" % tt])
            else:
                p.dve(lambda e, dst=dst, src=src: e.tensor_copy(dst, src), reads=[pk], writes=["HT%d" % tt])
        ph.done()

        def norm_block(p, PSst, pskey, t0, T, gs, sh, sqb, rstd, tmpb, dst_fn, tag, src=None, srckey="HT"):
            srcT = HT if src is None else src
            for c in range(8):
                sq = sqb[c % 2]
                p.act(lambda e, sq=sq, c=c: e.activation(sq[:, 0:T], srcT[:, c, t0:t0 + T], AF.Square),
                      reads=[srckey], writes=["sq%s%d" % (tag, c % 2)])
                p.pe(lambda e, sq=sq, c=c: e.matmul(PSst[:, 0:T], ones_bf[:], sq[:, 0:T], start=(c == 0), stop=(c == 7)),
                     reads=["sq%s%d" % (tag, c % 2), "ones_bf"], writes=[pskey])
            p.act(lambda e: e.activation(rstd[:, 0:T], PSst[:, 0:T], AF.Sqrt, bias=EPSB[:, 0:1]), reads=[pskey], writes=["rstd0" + tag])
            p.dve(lambda e: e.reciprocal(rstd[:, 0:T], rstd[:, 0:T]), reads=["rstd0" + tag], writes=["rstd" + tag])
            for c in range(8):
                tb = tmpb[c % 2]
                p.dve(lambda e, tb=tb, c=c: e.scalar_tensor_tensor(tb[:, 0:T], srcT[:, c, t0:t0 + T], gs[:, c:c + 1], rstd[:, 0:T],
                                                                  op0=ALU.mult, op1=ALU.mult),
                      reads=[srckey, "rstd" + tag], writes=["tmp%s%d" % (tag, c % 2)])
                dst, dkey = dst_fn(c)
                p.act(lambda e, tb=tb, c=c, dst=dst: e.activation(dst, tb[:, 0:T], AF.Identity, bias=sh[:, c:c + 1]),
                      reads=["tmp%s%d" % (tag, c % 2)], writes=[dkey])

        def wout_phase(wout_d, l):
            ph = Phase(nc)
            p = ph.p
            PS = ph.psum4()
            w = ph.sb([128, 8, D], BF16)
            mixb = [ph.sb([128, 8, 512], BF16) for _ in range(2)]
            p.dma("pool", "w", lambda e: e.dma_start(out=w[:], in_=wout_d.rearrange("(c p) n -> p c n", p=128)), writes=["w"])
            for b in range(8):
                mb = mixb[b % 2]
                mk = "mix%d" % (b % 2)
                p.dma("sp", mk, lambda e, mb=mb, b=b: e.dma_start(out=mb[:], in_=MIX_d[:, :, b * 512:(b + 1) * 512].rearrange("c p t -> p c t")),
                      writes=[mk])
                for f in range(8):
                    pst = PS[f % 4][:, 0:512]
                    pk = "psY%d" % (f % 4)

                    def mm(e, mb=mb, f=f, pst=pst):
                        for k in range(8):
                            ins = e.matmul(pst, w[:, k, f * 128:(f + 1) * 128], mb[:, k, :], start=(k == 0), stop=(k == 7))
                        return ins
                    p.pe(mm, reads=["w", mk], writes=[pk])
                    hsl = HT[:, f, b * 512:(b + 1) * 512]
                    p.dve(lambda e, pst=pst, f=f, hsl=hsl: e.scalar_tensor_tensor(hsl, pst, MOD[:, l, 16 + f:17 + f], hsl, op0=ALU.mult, op1=ALU.add),
                          reads=[pk], writes=["HT"])
            ph.done()

        def router_moe(l):
            ph = Phase(nc)
            p = ph.p
            PS = ph.psum4()
            sqb = [ph.sb([128, 512], BF16) for _ in range(2)]
            rstd = ph.sb([128, 512], F32)
            tmpb = [ph.sb([128, 512], F32) for _ in range(2)]
            a2f = ph.sb([128, 8, 512], F32)
            a2b = [ph.sb([128, 8, 512], BF16) for _ in range(2)]
            rw = ph.sb([128, 8, 20], F32)
            rbb = ph.sb([128, 80], F32)
            lgT = ph.sb([32, 512], F32)
            LG = ph.sb([128, 32, 20], F32)
            p.dma("sp", "rw", lambda e: e.dma_start(out=rw[:], in_=rw_d[l].rearrange("(c p) n -> p c n", p=128)), writes=["rw"])
            p.dma("sp", "rw", lambda e: e.dma_start(out=rbb[:], in_=rb_d[:, l * 80:(l + 1) * 80]), writes=["rbb"])
            gs = GS[:, l * 2 + 1, :]
            sh = MOD[:, l, 24:32]
            for b in range(8):
                af = a2f
                ab = a2b[b % 2]
                abk = "a2b%d" % (b % 2)
                norm_block(p, PS[0], "psS", b * 512, 512, gs, sh, sqb, rstd, tmpb,
                           lambda c, af=af: (af[:, c, :], "a2f_%d" % c), "n")
                akeys = ["a2f_%d" % c for c in range(8)]
                p.pool(lambda e, af=af, ab=ab: e.tensor_copy(ab[:], af[:]), reads=akeys, writes=[abk])
                p.dma("sp", "a2st%d" % (b % 2), lambda e, ab=ab, b=b: e.dma_start(
                    out=A2_d[:, :, b * 512:(b + 1) * 512].rearrange("c p t -> p c t"), in_=ab[:]), reads=[abk], writes=["A2"])

                def rmm(e, af=af):
                    for c in range(8):
                        ins = e.matmul(PS[1][0:20, 0:512], rw[:, c, :], af[:, c, :], start=(c == 0), stop=(c == 7))
                    return ins
                p.pe(rmm, reads=["rw"] + akeys, writes=["psR"])
                p.act(lambda e: e.activation(lgT[0:20, :], PS[1][0:20, 0:512], AF.Identity), reads=["psR"], writes=["lgT"])

                def rtr(e):
                    for tt in range(4):
                        ins = e.matmul(PS[2][:, tt * 32:tt * 32 + 20], lgT[0:20, tt * 128:(tt + 1) * 128], ident[0:20, 0:20], start=True, stop=True)
                    return ins
                p.pe(rtr, reads=["lgT", "ident"], writes=["psT"])
                p.dve(lambda e, b=b: e.tensor_tensor(LG[:, b * 4:(b + 1) * 4, :], PS[2][:, 0:128].rearrange("p (t n) -> p t n", n=32)[:, :, 0:20],
                                                    rbb[:].rearrange("p (t n) -> p t n", n=20), ALU.add),
                      reads=["psT", "rbb"], writes=["LG"])
            NT = 32
            gmax = ph.sb([128, NT], F32)
            ohg = ph.sb([128, NT, 4], F32)
            gd = ph.sb([128, NT, 4], F32)
            gsum = ph.sb([128, NT], F32)
            gp = ph.sb([128, NT], F32)
            t44 = ph.sb([128, NT, 4, 4], F32)
            esel = ph.sb([128, NT, 4], F32)
            e1 = ph.sb([128, NT], F32)
            sel1 = ph.sb([128, NT, 4], F32)
            em = ph.sb([128, NT, 4], F32)
            e2 = ph.sb([128, NT], F32)
            sel2 = ph.sb([128, NT, 4], F32)
            dd = ph.sb([128, NT], F32)
            w1 = ph.sb([128, NT], F32)
            w2 = ph.sb([128, NT], F32)
            ce = ph.sb([128, NT, 4], F32)
            ce2 = ph.sb([128, NT, 4], F32)
            comb = ph.sb([128, NT, 16], F32)
            chl = ph.sb([128, NT, 32], BF16)
            chf = ph.sb([128, NT, 16], F32)
            gl = LG[:, :, 0:4]
            el = LG[:, :, 4:20].rearrange("p n (g i) -> p n g i", i=4)

            def b3(ap2):
                return ap2.unsqueeze(2).to_broadcast([128, NT, 4])
            p.dve(lambda e: e.tensor_reduce(gmax[:], gl, AX.X, ALU.max), reads=["LG"], writes=["gmax"])
            p.dve(lambda e: e.tensor_tensor(ohg[:], gl, b3(gmax[:]), ALU.is_equal), reads=["LG", "gmax"], writes=["ohg"])
            p.dve(lambda e: e.tensor_tensor(gd[:], gl, b3(gmax[:]), ALU.subtract), reads=["LG", "gmax"], writes=["gd"])
            p.act(lambda e: e.activation(gd[:], gd[:], AF.Exp), reads=["gd"], writes=["ge"])
            p.dve(lambda e: e.tensor_reduce(gsum[:], gd[:], AX.X, ALU.add), reads=["ge"], writes=["gsum"])
            p.dve(lambda e: e.reciprocal(gp[:], gsum[:]), reads=["gsum"], writes=["gp"])
            p.dve(lambda e: e.tensor_tensor(t44[:], el, ohg[:].unsqueeze(3).to_broadcast([128, NT, 4, 4]), ALU.mult),
                  reads=["LG", "ohg"], writes=["t44"])
            p.dve(lambda e: e.tensor_reduce(esel[:], t44[:].rearrange("p n g i -> p n i g"), AX.X, ALU.add), reads=["t44"], writes=["esel"])
            p.dve(lambda e: e.tensor_reduce(e1[:], esel[:], AX.X, ALU.max), reads=["esel"], writes=["e1"])
            p.dve(lambda e: e.tensor_tensor(sel1[:], esel[:], b3(e1[:]), ALU.is_equal), reads=["esel", "e1"], writes=["sel1"])
            p.dve(lambda e: e.scalar_tensor_tensor(em[:], sel1[:], -1e30, esel[:], op0=ALU.mult, op1=ALU.add), reads=["sel1", "esel"], writes=["em"])
            p.dve(lambda e: e.tensor_reduce(e2[:], em[:], AX.X, ALU.max), reads=["em"], writes=["e2"])
            p.dve(lambda e: e.tensor_tensor(sel2[:], em[:], b3(e2[:]), ALU.is_equal), reads=["em", "e2"], writes=["sel2"])
            p.dve(lambda e: e.tensor_tensor(dd[:], e2[:], e1[:], ALU.subtract), reads=["e1", "e2"], writes=["dd"])
            p.act(lambda e: e.activation(dd[:], dd[:], AF.Exp), reads=["dd"], writes=["ex"])
            p.dve(lambda e: e.tensor_scalar(w1[:], dd[:], 1.0, None, op0=ALU.add), reads=["ex"], writes=["w1a"])
            p.dve(lambda e: e.reciprocal(w1[:], w1[:]), reads=["w1a"], writes=["w1"])
            p.dve(lambda e: e.tensor_tensor(w2[:], dd[:], w1[:], ALU.mult), reads=["ex", "w1"], writes=["w2a"])
            p.dve(lambda e: e.tensor_tensor(w1[:], w1[:], gp[:], ALU.mult), reads=["w1", "gp", "w2a"], writes=["wt1"])
            p.dve(lambda e: e.tensor_tensor(w2[:], w2[:], gp[:], ALU.mult), reads=["w2a", "gp"], writes=["wt2"])
            p.dve(lambda e: e.tensor_tensor(ce[:], sel1[:], b3(w1[:]), ALU.mult), reads=["sel1", "wt1"], writes=["ce"])
            p.dve(lambda e: e.tensor_tensor(ce2[:], sel2[:], b3(w2[:]), ALU.mult), reads=["sel2", "wt2"], writes=["ce2"])
            p.dve(lambda e: e.tensor_tensor(ce[:], ce[:], ce2[:], ALU.add), reads=["ce", "ce2"], writes=["cef"])
            p.dve(lambda e: e.tensor_tensor(comb[:].rearrange("p n (g i) -> p n g i", i=4),
                                            ohg[:].unsqueeze(3).to_broadcast([128, NT, 4, 4]),
                                            ce[:].unsqueeze(2).to_broadcast([128, NT, 4, 4]), ALU.mult),
                  reads=["ohg", "cef"], writes=["comb"])
            p.dve(lambda e: e.tensor_copy(chl[:, :, 0:16], comb[:]), reads=["comb"], writes=["chi"])
            p.dve(lambda e: e.tensor_copy(chf[:], chl[:, :, 0:16]), reads=["chi"], writes=["chf"])
            p.dve(lambda e: e.tensor_tensor(chf[:], comb[:], chf[:], ALU.subtract), reads=["comb", "chf"], writes=["clo"])
            p.dve(lambda e: e.tensor_copy(chl[:, :, 16:32], chf[:]), reads=["clo"], writes=["chl"])
            for q in range(8):
                pst = PS[3][:, (q % 2) * 512:(q % 2) * 512 + 512]
                pk = "psC%d" % (q % 2)

                def ctr(e, q=q, pst=pst):
                    for tt in range(4):
                        ins = e.matmul(pst[0:32, tt * 128:(tt + 1) * 128], chl[:, q * 4 + tt, :], ident_bf[:], start=True, stop=True)
                    return ins
                p.pe(ctr, reads=["chl", "chi", "ident_bf"], writes=[pk])
                p.act(lambda e, q=q, pst=pst: e.activation(combT[:, q * 512:(q + 1) * 512], pst[0:32, :], AF.Identity), reads=[pk], writes=["combT"])
            ph.done()
            if stage == 10 + l or (stage == 8 and l == 1):
                return
            ph = Phase(nc)
            p = ph.p
            PS = ph.psum4()
            T = 512
            NB = S // T
            wslot = [(ph.sb([128, 8, 256], BF16), ph.sb([128, 8, 256], BF16), ph.sb([128, 2, D], BF16)) for _ in range(3)]
            a2 = [ph.sb([128, 8, T], BF16) for _ in range(2)]
            sel = ph.sb([32, 2048], BF16)
            cb = ph.sb([128, T], F32)
            sg = [ph.sb([128, T], F32)] * 2
            tt_ = ph.sb([128, T], F32)
            hb0 = [ph.sb([128, 2, T], BF16) for _ in range(2)]
            hb1 = ph.sb([128, 2, T], BF16)
            p.dma("pool", "sel", lambda e: e.dma_start(out=sel[:, 0:1024], in_=sel_d[:, 0:1024]), writes=["sel"])
            p.dma("pool", "sel", lambda e: e.dma_start(out=sel[:, 1024:2048], in_=sel_d[:, 1024:2048]), writes=["sel"])
            g2 = MOD[:, l, 40:48]
            RB = [PS[0][:, 0:512], PS[0][:, 512:1024], PS[1][:, 0:512]]
            RBK = ["psR0", "psR1", "psR2"]
            CBP = PS[1][:, 512:1024]
            ACC = [PS[2][:, 0:512], PS[2][:, 512:1024], PS[3][:, 0:512], PS[3][:, 512:1024]]

            def load_expert(ex):
                sl = ex % 3
                wg, wu, wd = wslot[sl]
                wk = "w%d" % sl
                p.dma("pool", wk, lambda e: e.dma_start(out=wg[:], in_=wg_d[l, ex].rearrange("(c p) n -> p c n", p=128)), writes=[wk + "g"])
                p.dma("pool", wk, lambda e: e.dma_start(out=wu[:], in_=wu_d[l, ex].rearrange("(c p) n -> p c n", p=128)), writes=[wk + "u"])
                p.dma("pool", wk, lambda e: e.dma_start(out=wd[:], in_=wd_d[l, ex].rearrange("(c p) n -> p c n", p=128)), writes=[wk + "d"])
            st = {"step": 0, "rk": 0, "sgi": 0}

            def hbuf(b, j):
                if j == 0:
                    return hb0[b % 2], "h0_%d" % (b % 2)
                return hb1, "h1"

            def G(pr, b, j):
                ex = pr * 2 + j
                sl = ex % 3
                wg, wu, wd = wslot[sl]
                wk = "w%d" % sl
                if j == 0:
                    ab = a2[st["step"] % 2]
                    ak = "a2_%d" % (st["step"] % 2)
                    st["cur"] = (ab, ak)
                    st["step"] += 1
                    p.dma("sp", ak, lambda e: e.dma_start(out=ab[:], in_=A2_d[:, :, b * T:(b + 1) * T].rearrange("c p t -> p c t")), writes=[ak])
                ab, ak = st["cur"]
                hbb, hk = hbuf(b, j)
                p.pe(lambda e: e.matmul(CBP, sel[:, ex * 128:(ex + 1) * 128], combT[:, b * T:(b + 1) * T], start=True, stop=True),
                     reads=["sel", "combT"], writes=["psCB"])
                p.act(lambda e: e.activation(cb[:], CBP, AF.Identity), reads=["psCB"], writes=["cb"])
                for f2 in range(2):
                    pg = RB[st["rk"] % 3]
                    pgk = RBK[st["rk"] % 3]
                    st["rk"] += 1
                    pu = RB[st["rk"] % 3]
                    puk = RBK[st["rk"] % 3]
                    st["rk"] += 1

                    def gmm(e, pg=pg, f2=f2):
                        for k in range(8):
                            ins = e.matmul(pg, wg[:, k, f2 * 128:(f2 + 1) * 128], ab[:, k, :], start=(k == 0), stop=(k == 7))
                        return ins

                    def umm(e, pu=pu, f2=f2):
                        for k in range(8):
                            ins = e.matmul(pu, wu[:, k, f2 * 128:(f2 + 1) * 128], ab[:, k, :], start=(k == 0), stop=(k == 7))
                        return ins
                    p.pe(gmm, reads=[wk + "g", ak], writes=[pgk])
                    p.pe(umm, reads=[wk + "u", ak], writes=[puk])
                    sgb = sg[st["sgi"] % 2]
                    sgk = "sg0"
                    st["sgi"] += 1
                    p.act(lambda e, sgb=sgb, pg=pg: e.activation(sgb[:], pg, AF.Silu), reads=[pgk], writes=[sgk])
                    p.dve(lambda e, pu=pu: e.tensor_tensor(tt_[:], pu, cb[:], ALU.mult), reads=[puk, "cb"], writes=["tt"])
                    p.pool(lambda e, sgb=sgb, f2=f2: e.tensor_tensor(hbb[:, f2, :], tt_[:], sgb[:], ALU.mult),
                           reads=["tt", sgk], writes=[hk + "_%d" % f2])

            def DOWN(pr, b, half):
                hs = []
                for j in range(2):
                    ex = pr * 2 + j
                    wg, wu, wd = wslot[ex % 3]
                    hbb, hk = hbuf(b, j)
                    hs.append((wd, "w%dd" % (ex % 3), hbb, hk))
                for fi in range(4):
                    f = half * 4 + fi

                    def dmm(e, fi=fi, f=f):
                        for j in range(2):
                            wd, wdk, hbb, hk = hs[j]
                            for k in range(2):
                                ins = e.matmul(ACC[fi], wd[:, k, f * 128:(f + 1) * 128], hbb[:, k, :], start=(j == 0 and k == 0), stop=(j == 1 and k == 1))
                        return ins
                    p.pe(dmm, reads=[hs[0][1], hs[1][1], hs[0][3] + "_0", hs[0][3] + "_1", hs[1][3] + "_0", hs[1][3] + "_1"], writes=["psD%d" % fi])
                for fi in range(4):
                    f = half * 4 + fi
                    hsl = HT[:, f, b * T:(b + 1) * T]
                    p.dve(lambda e, fi=fi, f=f, hsl=hsl: e.scalar_tensor_tensor(hsl, ACC[fi], g2[:, f:f + 1], hsl, op0=ALU.mult, op1=ALU.add),
                          reads=["psD%d" % fi], writes=["HT"])

            load_expert(0)
            load_expert(1)
            for pr in range(8):
                if pr > 0:
                    load_expert(2 * pr + 1)
                G(pr, 0, 0)
                G(pr, 0, 1)
                if pr < 7:
                    load_expert(2 * pr + 2)
                for b in range(NB):
                    DOWN(pr, b, 0)
                    if b + 1 < NB:
                        G(pr, b + 1, 0)
                    DOWN(pr, b, 1)
                    if b + 1 < NB:
                        G(pr, b + 1, 1)
            ph.done()

        def store_phase():
            ph = Phase(nc)
            p = ph.p
            PS = ph.psum4()
            ob = [ph.sb([128, 4, D], F32) for _ in range(2)]
            for tt in range(32):
                pst = PS[tt % 2]
                pk = "psO%d" % (tt % 2)

                def tr(e, tt=tt, pst=pst):
                    for c in range(8):
                        ins = e.matmul(pst[:, c * 128:(c + 1) * 128], HT[:, c, tt * 128:(tt + 1) * 128], ident[:], start=True, stop=True)
                    return ins
                p.pe(tr, reads=["HT", "ident"], writes=[pk])
                g4 = tt // 4
                o = ob[g4 % 2]
                ok = "ob%d_%d" % (g4 % 2, tt % 4)
                if tt % 2 == 0:
                    p.act(lambda e, o=o, pst=pst, tt=tt: e.activation(o[:, tt % 4, :], pst[:, :], AF.Identity), reads=[pk], writes=[ok])
                else:
                    p.dve(lambda e, o=o, pst=pst, tt=tt: e.tensor_copy(o[:, tt % 4, :], pst[:, :]), reads=[pk], writes=[ok])
                if tt % 4 == 3:
                    p.dma("sp", "ost%d" % (g4 % 2), lambda e, o=o, g4=g4: e.dma_start(
                        out=out_d[g4 * 512:(g4 + 1) * 512, :].rearrange("(t p) n -> p t n", p=128), in_=o[:]),
                        reads=["ob%d_%d" % (g4 % 2, q) for q in range(4)], writes=["out"])
            ph.done()

        def qk_chain(p, PS, psA, kA, psB, kB, psC, T, gcol, gpcol, cosb, sinb, tq, dst, dstkey, rope=True):
            sqq, rsq, t1, t2 = tq
            p.act(lambda e: e.activation(sqq[:, 0:T], psA, AF.Square), reads=[kA], writes=["sqq"])
            p.pe(lambda e: e.matmul(psC[:, 0:T], onesblk_bf[:], sqq[:, 0:T], start=True, stop=True), reads=["sqq", "onesblk"], writes=["psC"])
            p.act(lambda e: e.activation(rsq[:, 0:T], psC[:, 0:T], AF.Sqrt, bias=EPSB[:, 0:1]), reads=["psC"], writes=["rsq0"])
            p.dve(lambda e: e.reciprocal(rsq[:, 0:T], rsq[:, 0:T]), reads=["rsq0"], writes=["rsq"])
            if rope:
                p.dve(lambda e: e.scalar_tensor_tensor(t1[:, 0:T], psA, gcol, cosb[:, 0:T], op0=ALU.mult, op1=ALU.mult),
                      reads=[kA, "cosb"], writes=["t1"])
                p.dve(lambda e: e.scalar_tensor_tensor(t2[:, 0:T], psB, gpcol, sinb[:, 0:T], op0=ALU.mult, op1=ALU.mult),
                      reads=[kB, "sinb"], writes=["t2"])
                p.pool(lambda e: e.tensor_tensor(t1[:, 0:T], t1[:, 0:T], t2[:, 0:T], ALU.add), reads=["t1", "t2"], writes=["t3"])
                p.dve(lambda e: e.tensor_tensor(dst, t1[:, 0:T], rsq[:, 0:T], ALU.mult), reads=["t3", "rsq"], writes=[dstkey])
            else:
                p.dve(lambda e: e.scalar_tensor_tensor(dst, psA, gcol, rsq[:, 0:T], op0=ALU.mult, op1=ALU.mult),
                      reads=[kA, "rsq"], writes=[dstkey])

        def layer0_mixer():
            ph = Phase(nc)
            p = ph.p
            PS = ph.psum4()
            T = 256
            wq = ph.sb([128, 8, 1792], BF16)
            wv = ph.sb([128, 8, 128], BF16)
            gains = ph.sb([128, 4], F32)
            sqb = [ph.sb([128, T], BF16) for _ in range(2)]
            rstd = ph.sb([128, T], F32)
            tmpb = [ph.sb([128, T], F32) for _ in range(2)]
            aT = ph.sb([128, 8, T], BF16)
            cosb = ph.sb([128, T], F32)
            sinb = ph.sb([128, T], F32)
            tq = (ph.sb([128, T], BF16), ph.sb([128, T], F32), ph.sb([128, T], F32), ph.sb([128, T], F32))
            qf = [ph.sb([128, T], BF16) for _ in range(2)]
            qf4 = ph.sb([128, 4, T], BF16)
            pst4 = ph.sb([128, 4, T], F32)
            vst = [ph.sb([128, 2, 193], BF16) for _ in range(2)]
            ctin = ph.sb([128, D], F32)
            CT = ph.sb([128, 8, 256], F32)
            p.dma("sp", "c1", lambda e: e.dma_start(out=gains[:], in_=gains_d[:, :]), writes=["gains"])
            for vi in range(2):
                p.dve(lambda e, vi=vi: e.memset(vst[vi][:], 0.0), writes=["vst%d" % vi])
                p.dve(lambda e, vi=vi: e.memset(vst[vi][:, :, 64:66], 1.0), reads=["vst%d" % vi], writes=["vst%d" % vi])
            p.dma("pool", "wq", lambda e: e.dma_start(out=wq[:], in_=wqkp_d.rearrange("(c p) n -> p c n", p=128)), writes=["wq"])
            p.dma("pool", "wq", lambda e: e.dma_start(out=wv[:], in_=wv_d.rearrange("(c p) n -> p c n", p=128)), writes=["wv"])
            B_ST = PS[0][:, 0:512]
            B_A = [PS[0][:, 512:1024], PS[1][:, 0:512]]
            B_B = [PS[1][:, 512:1024], PS[2][:, 0:512]]
            B_C = PS[2][:, 512:1024]
            B_M = [PS[3][:, 0:512], PS[3][:, 512:1024]]
            for tt in range(2):
                p.dma("sp", "ctin", lambda e, tt=tt: e.dma_start(out=ctin[:], in_=ctx_d[tt * 128:(tt + 1) * 128, :]), writes=["ctin"])
                for half in range(2):
                    bm = B_M[half]

                    def trc(e, half=half, bm=bm):
                        for c in range(4):
                            cc = half * 4 + c
                            ins = e.matmul(bm[:, c * 128:(c + 1) * 128], ctin[:, cc * 128:(cc + 1) * 128], ident[:], start=True, stop=True)
                        return ins
                    p.pe(trc, reads=["ctin", "ident"], writes=["psM%d" % half])
                    p.dve(lambda e, half=half, bm=bm, tt=tt: e.tensor_copy(CT[:, half * 4:half * 4 + 4, tt * 128:(tt + 1) * 128],
                                                                          bm.rearrange("p (c t) -> p c t", t=128)),
                          reads=["psM%d" % half], writes=["CT"])
            mcnt = [0]

            def proj(p, col0, bank, bkey, T=T):
                def mm(e):
                    for k in range(8):
                        ins = e.matmul(bank[:, 0:T], wq[:, k, col0:col0 + 128], aT[:, k, :], start=(k == 0), stop=(k == 7))
                    return ins
                p.pe(mm, reads=["wq"] + ["aT_%d" % c for c in range(8)], writes=[bkey])

            def vproj(p, tile0):
                i = mcnt[0] % 2
                mcnt[0] += 1
                bm = B_M[i]
                bk = "psM%d" % i
                vs_ = vst[i]

                def mm(e):
                    for t2 in range(2):
                        for k in range(8):
                            ins = e.matmul(bm[:, t2 * 128:(t2 + 1) * 128], aT[:, k, t2 * 128:(t2 + 1) * 128], wv[:, k, :], start=(k == 0), stop=(k == 7))
                    return ins
                p.pe(mm, reads=["wv"] + ["aT_%d" % c for c in range(8)], writes=[bk])
                p.dve(lambda e: e.tensor_copy(vs_[:, :, 0:64], bm[:, 0:256].rearrange("p (t n) -> p t n", n=128)[:, :, 0:64]),
                      reads=[bk], writes=["vst%da" % i])
                p.dve(lambda e: e.tensor_copy(vs_[:, :, 129:193], bm[:, 0:256].rearrange("p (t n) -> p t n", n=128)[:, :, 64:128]),
                      reads=[bk, "vst%da" % i], writes=["vst%d" % i])
                p.dma("sp", "vsst%d" % i, lambda e: e.dma_start(out=VS_d[:, tile0:tile0 + 2, :], in_=vs_[:]), reads=["vst%d" % i, "vst%da" % i], writes=["VSd"])

            norm_block(p, B_ST, "psST", 0, 256, GSC, MODC, sqb, rstd, tmpb, lambda c: (aT[:, c, :], "aT_%d" % c), "c", src=CT, srckey="CT")
            proj(p, 512, B_A[0], "psA0")
            qk_chain(p, PS, B_A[0][:, 0:T], "psA0", None, None, B_C, T, gains[:, 2:3], None, None, None, tq, qf[0][:, 0:T], "qf0", rope=False)
            p.dma("sp", "qst0", lambda e: e.dma_start(out=KT_d[:, 0:256], in_=qf[0][:, 0:T]), reads=["qf0"], writes=["KTd"])
            vproj(p, 0)
            qi = 1
            for b in range(S // T):
                t0 = b * T
                p.dma("sp", "cs", lambda e, t0=t0: e.dma_start(out=cosb[:], in_=cos_d[:, t0:t0 + T]), writes=["cosb"])
                p.dma("sp", "cs", lambda e, t0=t0: e.dma_start(out=sinb[:], in_=sin_d[:, t0:t0 + T]), writes=["sinb"])
                norm_block(p, B_ST, "psST", t0, T, GS[:, 0, :], MOD[:, 0, 0:8], sqb, rstd, tmpb, lambda c: (aT[:, c, :], "aT_%d" % c), "m")
                for j in range(5):
                    i = qi % 2
                    qi += 1
                    col = j * 128 if j < 4 else 512
                    colp = 1152 + j * 128 if j < 4 else 1664
                    proj(p, col, B_A[i], "psA%d" % i)
                    proj(p, colp, B_B[i], "psB%d" % i)
                    gcol = gains[:, 0:1] if j < 4 else gains[:, 2:3]
                    gpcol = gains[:, 1:2] if j < 4 else gains[:, 3:4]
                    if j < 4:
                        qk_chain(p, PS, B_A[i][:, 0:T], "psA%d" % i, B_B[i][:, 0:T], "psB%d" % i, B_C, T, gcol, gpcol, cosb, sinb, tq,
                                 qf4[:, j, :], "qf4_%d" % j)
                        if j == 3:
                            p.dma("sp", "qst4", lambda e, t0=t0: e.dma_start(out=QT_d[:, :, t0:t0 + T].rearrange("c p t -> p c t"), in_=qf4[:]),
                                  reads=["qf4_%d" % q for q in range(4)], writes=["QTd"])
                    else:
                        qk_chain(p, PS, B_A[i][:, 0:T], "psA%d" % i, B_B[i][:, 0:T], "psB%d" % i, B_C, T, gcol, gpcol, cosb, sinb, tq,
                                 qf[i][:, 0:T], "qf%d" % i)
                        p.dma("sp", "qst%d" % i, lambda e, i=i, t0=t0: e.dma_start(out=KT_d[:, 256 + t0:256 + t0 + T], in_=qf[i][:, 0:T]),
                              reads=["qf%d" % i], writes=["KTd"])
                for g in range(4):
                    i = mcnt[0] % 2
                    mcnt[0] += 1
                    proj(p, 640 + g * 128, B_M[i], "psM%d" % i)
                    p.act(lambda e, i=i, g=g: e.activation(pst4[:, g, :], B_M[i][:, 0:T], AF.Identity), reads=["psM%d" % i], writes=["qpst4_%d" % g])
                    if g == 3:
                        p.dma("sp", "pst4", lambda e, t0=t0: e.dma_start(out=PT_d[:, :, t0:t0 + T].rearrange("c p t -> p c t"), in_=pst4[:]),
                              reads=["qpst4_%d" % q for q in range(4)], writes=["PTd"])
                vproj(p, 2 + 2 * b)
            ph.done()
            if stage == 20:
                return
            ph = Phase(nc)
            p = ph.p
            PS = ph.psum4()
            KT = ph.sb([128, 4352], BF16)
            VS = ph.sb([128, 34, 193], BF16)
            Qb = [ph.sb([128, 512], BF16) for _ in range(2)]
            Pb = [ph.sb([128, 1024], BF16) for _ in range(3)]
            rr = ph.sb([128, 512], F32)
            bcs = ph.sb([128, 512], F32)
            mixo = [ph.sb([128, 512], BF16) for _ in range(2)]
            p.dma("sp", "kt", lambda e: e.dma_start(out=KT[:], in_=KT_d[:, :]), writes=["KT"])
            p.dma("sp", "kt", lambda e: e.dma_start(out=VS[:], in_=VS_d[:, :, :]), writes=["VS"])
            SB_ = [PS[0], PS[1]]
            OA = PS[2][:, 0:512]
            OB = PS[2][:, 512:1024]
            BCA = PS[3][:, 0:512]
            BCB = PS[3][:, 512:1024]
            NKT = 24 if stage == 7 else 34
            units = [(j, qb) for j in range(4) for qb in range(8)]
            steps = [(u, kt) for u in range(len(units)) for kt in range(NKT)]
            qbuf = {}

            def load_q(u):
                j, qb = units[u]
                qt = Qb[u % 2]
                qk_ = "Qb%d" % (u % 2)
                p.dma("sp", qk_, lambda e: e.dma_start(out=qt[:], in_=QT_d[j, :, qb * 512:(qb + 1) * 512]), writes=[qk_])
                qbuf[u] = (qt, qk_)

            def S_(i):
                u, kt = steps[i]
                if kt == 0:
                    load_q(u)
                qt, qk_ = qbuf[u]
                sb_ = SB_[i % 2]
                sk = "psS%d" % (i % 2)

                def smm(e):
                    e.matmul(sb_[:, 0:512], KT[0:64, kt * 128:(kt + 1) * 128], qt[0:64, :], start=True, stop=True)
                    return e.matmul(sb_[:, 512:1024], KT[64:128, kt * 128:(kt + 1) * 128], qt[64:128, :], start=True, stop=True)
                p.pe(smm, reads=["KT", qk_], writes=[sk])

            def finalize(u):
                j, qb = units[u]
                p.dve(lambda e: e.reciprocal(rr[64:65, :], OA[64:65, :]), reads=["psOA"], writes=["rrA"])
                p.dve(lambda e: e.reciprocal(rr[0:1, :], OB[0:1, :]), reads=["psOB"], writes=["rrB"])
                p.pe(lambda e: e.matmul(BCA[0:64, :], ones_f[64:65, 0:64], rr[64:65, :], start=True, stop=True), reads=["rrA", "ones_f"], writes=["psBCA"])
                p.pe(lambda e: e.matmul(BCB[:, :], ones_f[0:1, :], rr[0:1, :], start=True, stop=True), reads=["rrB", "ones_f"], writes=["psBCB"])
                p.act(lambda e: e.activation(bcs[0:64, :], BCA[0:64, :], AF.Identity), reads=["psBCA"], writes=["bcsA"])
                p.act(lambda e: e.activation(bcs[64:128, :], BCB[64:128, :], AF.Identity), reads=["psBCB"], writes=["bcsB"])
                mo = mixo[u % 2]
                mk = "mixo%d" % (u % 2)
                p.dve(lambda e: e.tensor_tensor(mo[0:64, :], OA[0:64, :], bcs[0:64, :], ALU.mult), reads=["psOA", "bcsA"], writes=[mk + "a"])
                p.dve(lambda e: e.tensor_tensor(mo[64:128, :], OB[64:128, :], bcs[64:128, :], ALU.mult), reads=["psOB", "bcsB"], writes=[mk + "b"])
                p.dma("sp", "mst%d" % (u % 2), lambda e: e.dma_start(out=MIX_d[j, :, qb * 512:(qb + 1) * 512], in_=mo[:]),
                      reads=[mk + "a", mk + "b"], writes=["MIXd"])

            S_(0)
            for i in range(len(steps)):
                u, kt = steps[i]
                if i + 1 < len(steps):
                    S_(i + 1)
                sb_ = SB_[i % 2]
                sk = "psS%d" % (i % 2)
                pb = Pb[i % 3]
                pk = "P%d" % (i % 3)
                p.act(lambda e, pb=pb, sb_=sb_: e.activation(pb[:], sb_[:, :], AF.Exp, scale=0.125), reads=[sk], writes=[pk])

                def pv(e, pb=pb, kt=kt):
                    e.matmul(OA[0:65, :], VS[:, kt, 0:65], pb[:, 0:512], start=(kt == 0), stop=(kt == NKT - 1))
                    return e.matmul(OB[:, :], VS[:, kt, 65:193], pb[:, 512:1024], start=(kt == 0), stop=(kt == NKT - 1))
                p.pe(pv, reads=["VS", pk], writes=["psOA", "psOB"])
                if kt == NKT - 1:
                    finalize(u)
            ph.done()
            ph = Phase(nc)
            p = ph.p
            PS = ph.psum4()
            W = S + 16
            Pf = ph.sb([128, W], F32)
            sa = ph.sb([128, W], F32)
            sb2 = ph.sb([128, W], F32)
            dbf = ph.sb([128, S], BF16)
            pw = ph.sb([128, 512], BF16)
            psc = ph.sb([128, 4], F32)
            edg = ph.sb([128, 64], F32)
            et = ph.sb([128, 16], F32)
            po = [ph.sb([128, 512], BF16) for _ in range(2)]
            p.dma("pool", "pw", lambda e: e.dma_start(out=pw[:], in_=poolw_d[:, :]), writes=["pw"])
            p.dma("sp", "pc", lambda e: e.dma_start(out=psc[:], in_=poolsc_d[:, :]), writes=["psc"])
            p.dma("sp", "pc", lambda e: e.dma_start(out=edg[:], in_=pooledge_d[:, :]), writes=["edg"])
            p.dve(lambda e: e.memset(Pf[:, 0:8], 0.0), writes=["PfL"])
            p.dve(lambda e: e.memset(Pf[:, W - 8:W], 0.0), writes=["PfR"])
            oc = 0
            for g in range(4):
                w_ = 2 ** (g + 1)
                p.dma("sp", "pf", lambda e, g=g: e.dma_start(out=Pf[:, 8:8 + S], in_=PT_d[g, :, :]), writes=["Pf"])
                p.dve(lambda e: e.tensor_tensor(sa[:, 1:W], Pf[:, 0:W - 1], Pf[:, 1:W], ALU.add), reads=["Pf", "PfL", "PfR"], writes=["sa"])
                cur, ck = sa, "sa"
                oth, ok_ = sb2, "sb"
                lo, hi, sh_ = 1, W, 1
                for st in range(g):
                    nlo, nhi = lo + sh_, hi - sh_
                    eng = p.pool if st % 2 == 0 else p.dve
                    eng(lambda e, cur=cur, oth=oth, nlo=nlo, nhi=nhi, sh_=sh_: e.tensor_tensor(oth[:, nlo:nhi], cur[:, nlo - sh_:nhi - sh_], cur[:, nlo + sh_:nhi + sh_], ALU.add),
                        reads=[ck], writes=[ok_])
                    cur, ck, oth, ok_ = oth, ok_, cur, ck
                    lo, hi = nlo, nhi
                    sh_ *= 2
                assert lo <= 8 and hi >= 8 + S
                p.dve(lambda e, cur=cur, w_=w_: e.scalar_tensor_tensor(dbf[:, :], cur[:, 8:8 + S], 1.0 / w_, Pf[:, 8:8 + S], op0=ALU.mult, op1=ALU.subtract),
                      reads=[ck, "Pf"], writes=["dbf0"])
                p.dve(lambda e, cur=cur, g=g: e.tensor_tensor(et[:, 0:8], cur[:, 8:16], edg[:, g * 16:g * 16 + 8], ALU.mult), reads=[ck, "edg"], writes=["et0"])
                p.dve(lambda e, cur=cur, g=g: e.tensor_tensor(et[:, 8:16], cur[:, S:8 + S], edg[:, g * 16 + 8:g * 16 + 16], ALU.mult), reads=[ck, "edg", "et0"], writes=["et1"])
                p.dve(lambda e: e.tensor_tensor(dbf[:, 0:8], et[:, 0:8], Pf[:, 8:16], ALU.subtract), reads=["et1", "Pf", "dbf0"], writes=["dbf1"])
                p.dve(lambda e: e.tensor_tensor(dbf[:, S - 8:S], et[:, 8:16], Pf[:, S:8 + S], ALU.subtract), reads=["et1", "Pf", "dbf1"], writes=["dbf"])
                for b in range(8):
                    i = oc % 2
                    oc += 1
                    bank = PS[i][:, 0:512]
                    p.pe(lambda e, bank=bank, g=g, b=b: e.matmul(bank, pw[:, g * 128:(g + 1) * 128], dbf[:, b * 512:(b + 1) * 512], start=True, stop=True),
                         reads=["pw", "dbf"], writes=["psP%d" % i])
                    p.act(lambda e, bank=bank, i=i, g=g: e.activation(po[i][:], bank, AF.Identity, scale=psc[:, g:g + 1]), reads=["psP%d" % i, "psc"], writes=["po%d" % i])
                    p.dma("sp", "post%d" % i, lambda e, i=i, g=g, b=b: e.dma_start(out=MIX_d[4 + g, :, b * 512:(b + 1) * 512], in_=po[i][:]),
                          reads=["po%d" % i], writes=["MIXd"])
            ph.done()
            wout_phase(wout0_d, 0)

        def layer1_mixer():
            ph = Phase(nc)
            p = ph.p
            PS = ph.psum4()
            T = 256
            w1 = ph.sb([128, 8, 2560], BF16)
            sqb = [ph.sb([128, T], BF16) for _ in range(2)]
            rstd = ph.sb([128, T], F32)
            tmpb = [ph.sb([128, T], F32) for _ in range(2)]
            aT = ph.sb([128, 8, T], BF16)
            sggain = ph.sb([128, 512], F32)
            sgwT = ph.sb([128, 512], BF16)
            sgbb = ph.sb([128, 512], F32)
            usb = ph.sb([128, 4, T], F32)
            hxs = [ph.sb([128, T], F32)] * 2
            zs = hxs
            bgs = [ph.sb([128, T], F32)] * 2
            sqv = ph.sb([128, 512], F32)
            ssum = ph.sb([128, 4], F32)
            vt = sqv
            vn = ph.sb([128, 4, 128], BF16)
            st_ = ph.sb([128, 4, 128], F32)
            yc = [ph.sb([128, 4, T], BF16) for _ in range(2)]
            p.dma("pool", "w1", lambda e: e.dma_start(out=w1[:, :, 0:1280], in_=win1_d[:, 0:1280].rearrange("(c p) n -> p c n", p=128)), writes=["w1"])
            p.dma("pool", "w1", lambda e: e.dma_start(out=w1[:, :, 1280:2560], in_=win1_d[:, 1280:2560].rearrange("(c p) n -> p c n", p=128)), writes=["w1"])
            p.dma("pool", "w1", lambda e: e.dma_start(out=sgwT[:], in_=sgw_d[:, :]), writes=["sgwT"])
            p.dma("sp", "c2", lambda e: e.dma_start(out=sggain[:], in_=sggain_d[:, :]), writes=["sggain"])
            p.dma("sp", "c2", lambda e: e.dma_start(out=sgbb[:], in_=sgb_d[:, :]), writes=["sgbb"])
            B_ST = PS[0][:, 0:512]
            B_U = [PS[0][:, 512:1024], PS[1][:, 0:512]]
            B_H = [PS[1][:, 512:1024], PS[2][:, 0:512]]
            B_G = PS[2][:, 512:1024]
            B_V = PS[3][:, 0:512]
            B_S = PS[3][:, 512:1024]
            akeys = ["aT_%d" % c for c in range(8)]

            def proj(col0, tgt, bkey):
                def mm(e):
                    for k in range(8):
                        ins = e.matmul(tgt, w1[:, k, col0:col0 + 128], aT[:, k, :], start=(k == 0), stop=(k == 7))
                    return ins
                p.pe(mm, reads=["w1"] + akeys, writes=[bkey])
            hi_ = 0
            for b in range(S // T):
                t0 = b * T
                norm_block(p, B_ST, "psST", t0, T, GS[:, 2, :], MOD[:, 1, 0:8], sqb, rstd, tmpb, lambda c: (aT[:, c, :], "aT_%d" % c), "m")
                for half in range(2):
                    for q in range(2):
                        g = half * 2 + q
                        proj(g * 128, B_U[half][:, q * T:(q + 1) * T], "psU%d" % half)
                    p.act(lambda e, half=half: e.activation(usb[:, half * 2:half * 2 + 2, :], B_U[half][:, 0:2 * T].rearrange("p (q t) -> p q t", t=T), AF.Identity),
                          reads=["psU%d" % half], writes=["usb%d" % half])
                for c in range(4):
                    i = hi_ % 2
                    hi_ += 1
                    proj(1024 + c * 128, B_H[i][:, 0:T], "psH%d" % i)
                    proj(2048 + c * 128, B_H[i][:, T:2 * T], "psH%d" % i)
                    p.act(lambda e, i=i: e.activation(hxs[i][:], B_H[i][:, 0:T], AF.Identity), reads=["psH%d" % i], writes=["hxs0"])
                    p.dve(lambda e, i=i: e.tensor_tensor(zs[i][:], B_H[i][:, T:2 * T], hxs[i][:], ALU.mult), reads=["psH%d" % i, "hxs0"], writes=["hxs0"])
                    p.dma("sp", "zst%d" % i, lambda e, i=i, c=c, t0=t0: e.dma_start(out=PT_d[c, :, t0:t0 + T], in_=zs[i][:]), reads=["hxs0"], writes=["PTd"])
                    proj(1536 + c * 128, B_G[:, 0:T], "psG")
                    p.act(lambda e, i=i: e.activation(bgs[i][:], B_G[:, 0:T], AF.Identity), reads=["psG"], writes=["bgs0"])
                    p.dma("sp", "bst%d" % i, lambda e, i=i, c=c, t0=t0: e.dma_start(out=BG_d[c, :, t0:t0 + T], in_=bgs[i][:]), reads=["bgs0"], writes=["BGd"])
                yb = yc[b % 2]
                yk = "yc%d" % (b % 2)
                for n in range(2):
                    def vmm(e, n=n):
                        for g in range(4):
                            for k in range(8):
                                ins = e.matmul(B_V[:, g * 128:(g + 1) * 128], aT[:, k, n * 128:(n + 1) * 128], w1[:, k, 512 + g * 128:512 + (g + 1) * 128],
                                               start=(k == 0), stop=(k == 7))
                        return ins
                    p.pe(vmm, reads=["w1"] + akeys, writes=["psV"])
                    p.act(lambda e: e.activation(sqv[:], B_V, AF.Square), reads=["psV"], writes=["sqv"])
                    p.dve(lambda e: e.tensor_reduce(ssum[:], sqv[:].rearrange("p (g c) -> p g c", c=128), AX.X, ALU.add), reads=["sqv"], writes=["ssum0"])
                    p.act(lambda e: e.activation(ssum[:], ssum[:], AF.Sqrt, bias=EPSB[:, 0:1], scale=1.0 / 128.0), reads=["ssum0"], writes=["ssum1"])
                    p.dve(lambda e: e.reciprocal(ssum[:], ssum[:]), reads=["ssum1"], writes=["ssum"])
                    p.dve(lambda e: e.tensor_tensor(vt[:].rearrange("p (g c) -> p g c", c=128), B_V.rearrange("p (g c) -> p g c", c=128), ssum[:].unsqueeze(2).to_broadcast([128, 4, 128]), ALU.mult),
                          reads=["psV", "ssum", "sqv"], writes=["sqv"])
                    p.pool(lambda e: e.tensor_tensor(vn[:].rearrange("p g c -> p (g c)"), vt[:], sggain[:], ALU.mult), reads=["sqv", "sggain"], writes=["vn"])

                    def smm(e):
                        for g in range(4):
                            ins = e.matmul(B_S[:, g * 128:(g + 1) * 128], vn[:, g, :], sgwT[:, g * 128:(g + 1) * 128], start=True, stop=True)
                        return ins
                    p.pe(smm, reads=["vn", "sgwT"], writes=["psS"])
                    p.dve(lambda e: e.tensor_tensor(st_[:], B_S.rearrange("p (g c) -> p g c", c=128), sgbb[:].rearrange("p (g c) -> p g c", c=128), ALU.add),
                          reads=["psS", "sgbb"], writes=["st"])
                    p.pool(lambda e, n=n, yb=yb: e.tensor_tensor(yb[:, :, n * 128:(n + 1) * 128], st_[:], usb[:, :, n * 128:(n + 1) * 128], ALU.mult),
                           reads=["st", "usb0", "usb1"], writes=[yk + "_%d" % n])
                p.dma("sp", "yst%d" % (b % 2), lambda e, yb=yb, t0=t0: e.dma_start(out=MIX_d[0:4, :, t0:t0 + T].rearrange("c p t -> p c t"), in_=yb[:]),
                      reads=[yk + "_0", yk + "_1"], writes=["MIXd"])
            ph.done()
            ph = Phase(nc)
            p = ph.p
            W = S + 2
            Z = ph.sb([128, W], F32)
            BGr = ph.sb([128, S], F32)
            t1 = ph.sb([128, S], F32)
            yo = ph.sb([128, S], BF16)
            cw = ph.sb([128, 12], F32)
            p.dma("sp", "cw", lambda e: e.dma_start(out=cw[:], in_=convw_d[:, :]), writes=["cw"])
            p.dve(lambda e: e.memset(Z[:, 0:1], 0.0), writes=["ZL"])
            p.dve(lambda e: e.memset(Z[:, W - 1:W], 0.0), writes=["ZR"])
            for c in range(4):
                p.dma("sp", "z", lambda e, c=c: e.dma_start(out=Z[:, 1:1 + S], in_=PT_d[c, :, :]), writes=["Z"])
                p.dma("sp", "bg", lambda e, c=c: e.dma_start(out=BGr[:], in_=BG_d[c, :, :]), writes=["BGr"])
                p.dve(lambda e, c=c: e.tensor_scalar(t1[:], Z[:, 1:1 + S], cw[:, c * 3 + 1:c * 3 + 2], None, op0=ALU.mult), reads=["Z", "cw"], writes=["t1a"])
                p.dve(lambda e, c=c: e.scalar_tensor_tensor(t1[:], Z[:, 0:S], cw[:, c * 3:c * 3 + 1], t1[:], op0=ALU.mult, op1=ALU.add),
                      reads=["Z", "ZL", "cw", "t1a"], writes=["t1b"])
                p.dve(lambda e, c=c: e.scalar_tensor_tensor(t1[:], Z[:, 2:2 + S], cw[:, c * 3 + 2:c * 3 + 3], t1[:], op0=ALU.mult, op1=ALU.add),
                      reads=["Z", "ZR", "cw", "t1b"], writes=["t1c"])
                p.pool(lambda e: e.tensor_tensor(yo[:], t1[:], BGr[:], ALU.mult), reads=["t1c", "BGr"], writes=["yo"])
                p.dma("sp", "yo", lambda e, c=c: e.dma_start(out=MIX_d[4 + c, :, :], in_=yo[:]), reads=["yo"], writes=["MIXd"])
            ph.done()
            wout_phase(wout1_d, 1)

        if tail_only:
            router_moe(1)
        else:
            if stage >= 1:
                layer0_mixer()
            if stage >= 2 and stage < 20:
                router_moe(0)
            if stage == 9:
                router_moe(1)
                layer1_mixer()
            if stage >= 3 and stage < 9 and stage != 5:
                layer1_mixer()
            if stage >= 4 and stage < 9 and stage != 6:
                router_moe(1)
        if stage == 12:
            ph = Phase(nc)
            big = ph.sb([128, 4096], F32)
            for i_ in range(4000):
                ph.p.dve(lambda e: e.memset(big[:], 1.0), writes=["big"])
            ph.done()
        if stage == 6:
            for _ in range(3):
                ph = Phase(nc)
                ph.p.dve(lambda e: e.memset(EPSB[:], EPS), writes=["epsb"])
                ph.done()
        store_phase()
    return nc


def _perm64():
    d = np.arange(64)
    return np.where((d % 32) < 16, d + 16, d - 16)


def prep_inputs(inputs):
    f = lambda a: np.ascontiguousarray(np.asarray(a, dtype=np.float32))
    I = {k: np.asarray(v) for k, v in inputs.items()}
    shared = {}
    shared["ident"] = np.eye(128, dtype=np.float32)
    shared["mod_w"] = f(I["mod_w"])
    shared["mod_bT"] = f(I["mod_b"].reshape(2, 48, 128).transpose(2, 0, 1).reshape(128, 96))
    ng = np.stack([I["norm1_g"], I["norm2_g"]], 0)
    shared["norm_g"] = f(ng.reshape(2, 2, 8, 128).transpose(3, 0, 1, 2).reshape(128, 32))
    w_in0 = I["even_w_in"][0]
    pi = _perm64()
    qcols, qpcols = [], []
    for j in range(4):
        for h in (j, j + 4):
            qcols.append(h * 64 + np.arange(64))
            qpcols.append(h * 64 + pi)
    qcols = np.concatenate(qcols)
    qpcols = np.concatenate(qpcols)
    kcols = 512 + np.arange(128)
    kpcols = 512 + np.concatenate([pi, 64 + pi])
    pcols = 768 + np.arange(512)
    allc = np.concatenate([qcols, kcols, pcols, qpcols, kpcols])
    shared["w_qkp"] = f(w_in0[:, allc])
    shared["w_v"] = f(w_in0[:, 640:768])
    qg = I["q_gain"][0]
    kg = I["k_gain"][0]
    d = np.arange(128) % 64
    shared["gains"] = f(np.stack([qg[d], qg[pi[d]], kg[d], kg[pi[d]]], 1))
    t = np.arange(S)
    row = (t // 64).astype(np.float32)
    col = (t % 64).astype(np.float32)
    inv = (10000.0 ** (-np.arange(0, 16, dtype=np.float32) * 2 / 32.0)).astype(np.float32)
    cos_t = np.zeros((128, S), np.float32)
    sin_t = np.zeros((128, S), np.float32)
    for pp in range(128):
        dd = pp % 64
        jj = dd % 16
        pos = row if dd < 32 else col
        ang = (pos * inv[jj]).astype(np.float32)
        sgn = -1.0 if (dd % 32) < 16 else 1.0
        cos_t[pp] = np.cos(ang)
        sin_t[pp] = sgn * np.sin(ang)
    shared["cos_t"] = cos_t
    shared["sin_t"] = sin_t
    shared["pool_wT"] = f(I["pool_w"][0].transpose(1, 0, 2).reshape(128, 512))
    shared["pool_sc"] = f(I["pool_scale"][0].reshape(4, 128).T)
    edge = np.zeros((128, 4, 16), np.float32)
    for g, w in enumerate((2, 4, 8, 16)):
        for jx in range(16):
            tpos = jx if jx < 8 else S - 16 + jx
            lo = max(tpos - w // 2, 0)
            hi = min(tpos + w - w // 2, S)
            edge[:, g, jx] = 1.0 / (hi - lo)
    shared["pool_edge"] = edge.reshape(128, 64)
    w_out0 = I["even_w_out"][0]
    rows = []
    for c in range(4):
        for h in (c, c + 4):
            rows.append(h * 64 + np.arange(64))
    rows.append(512 + np.arange(512))
    shared["w_out0"] = f(w_out0[np.concatenate(rows), :])
    shared["w_out1"] = f(I["odd_w_out"][0])
    shared["w_in1"] = f(I["odd_w_in"][0])
    shared["sg_gain_b"] = f(np.broadcast_to(I["sg_gain"][0].reshape(1, 512), (128, 512)))
    shared["sg_wT"] = f(I["sg_w"][0].transpose(2, 0, 1).reshape(128, 512))
    shared["sg_b_b"] = f(np.broadcast_to(I["sg_b"][0].reshape(1, 512), (128, 512)))
    shared["conv_wT"] = f(I["conv_w"][0][:, 0, :].reshape(3, 4, 128).transpose(2, 1, 0).reshape(128, 12))
    shared["rw"] = f(np.concatenate([I["router_g_w"], I["router_e_w"]], axis=2))
    rb = np.concatenate([I["router_g_b"], I["router_e_b"]], axis=1)
    shared["rb_b"] = f(np.broadcast_to(np.tile(rb[:, None, :], (1, 4, 1)).reshape(1, 160), (128, 160)))
    shared["w_gate"] = f(I["w_gate"])
    shared["w_up"] = f(I["w_up"])
    shared["w_down"] = f(I["w_down"])
    sel = np.zeros((32, 16, 128), np.float32)
    for ex in range(16):
        sel[ex, ex, :] = 1.0
        sel[16 + ex, ex, :] = 1.0
    shared["sel"] = sel.reshape(32, 2048)
    in_maps = []
    for b in range(NCORES):
        m = dict(shared)
        m["x"] = f(I["x"][b])
        m["ctx"] = f(I["ctx"][b])
        cv = np.stack([I["c"][b], I["c_ctx"]], 0)
        m["cvec"] = f(cv.reshape(2, 8, 128).transpose(2, 1, 0).reshape(128, 16))
        in_maps.append(m)
    return in_maps


_NC_CACHE = {}


def kernel(**inputs):
    in_maps = prep_inputs(inputs)
    if "nc" not in _NC_CACHE:
        _NC_CACHE["nc"] = (build(stage=3), build(tail_only=True))
    nc1, nc2 = _NC_CACHE["nc"]
    res = run_bass_kernel_spmd(nc1, in_maps, core_ids=list(range(NCORES)))
    for b in range(NCORES):
        in_maps[b]["x"] = np.ascontiguousarray(np.asarray(res.results[b]["out"], dtype=np.float32))
    res = run_bass_kernel_spmd(nc2, in_maps, core_ids=list(range(NCORES)))
    out = np.stack([np.asarray(r["out"]) for r in res.results], axis=0)
    return out.astype(np.float32)
```

```python
import numpy as np
from contextlib import ExitStack
import concourse.bass as bass
import concourse.mybir as mybir
from concourse.bass_utils import run_bass_kernel_spmd

F32 = mybir.dt.float32
BF16 = mybir.dt.bfloat16
AF = mybir.ActivationFunctionType
ALU = mybir.AluOpType
AX = mybir.AxisListType

ENGS = ["pe", "act", "dve", "pool", "sp"]
S = 4096
D = 1024
NCORES = 8
EPS = 1e-6
_CNT = [0]
_PHASE = [0]
_SEMPOOL = [[], []]
NSEM = 16


class Op:
    __slots__ = ("eng", "fn", "reads", "writes", "dma_key", "idx", "waits", "signal", "semval")

    def __init__(self, eng, fn, reads, writes, dma_key):
        self.eng = eng
        self.fn = fn
        self.reads = reads
        self.writes = writes
        self.dma_key = dma_key
        self.waits = []
        self.signal = False
        self.semval = 0


class Prog:
    def __init__(self, nc):
        self.nc = nc
        self.ops = []

    def op(self, eng, fn, reads=(), writes=(), dma_key=None):
        reads = tuple(reads)
        writes = tuple(writes)
        ex = tuple(r for r in reads if r.startswith("ps"))
        o = Op(eng, fn, reads, writes + ex, dma_key)
        o.idx = len(self.ops)
        self.ops.append(o)
        return o

    def pe(self, fn, reads=(), writes=()):
        return self.op("pe", fn, reads, writes)

    def act(self, fn, reads=(), writes=()):
        return self.op("act", fn, reads, writes)

    def dve(self, fn, reads=(), writes=()):
        return self.op("dve", fn, reads, writes)

    def pool(self, fn, reads=(), writes=()):
        return self.op("pool", fn, reads, writes)

    def dma(self, eng, key, fn, reads=(), writes=()):
        return self.op(eng, fn, reads, writes, dma_key=key)

    def finalize(self):
        ops = self.ops

        def tl(o):
            return ("dma", o.dma_key) if o.dma_key is not None else o.eng

        pos = {}
        cnt = {}
        for o in ops:
            t = tl(o)
            cnt[t] = cnt.get(t, 0) + 1
            pos[o.idx] = cnt[t]
        last_writer = {}
        readers = {}
        known = {e: {} for e in ENGS}
        done_clock = {}
        needed = set()
        latest_on_key = {}
        for o in ops:
            deps = set()
            raw = set()
            for r in o.reads:
                w = last_writer.get(r)
                if w is not None:
                    deps.add(w)
                    raw.add(w)
            for r in o.writes:
                w = last_writer.get(r)
                if w is not None:
                    deps.add(w)
                    if r.startswith("ps"):
                        raw.add(w)
                for rd in readers.get(r, ()):
                    deps.add(rd)
            req = {}
            for d in deps:
                if d == o.idx:
                    continue
                po = ops[d]
                t = tl(po)
                if po.dma_key is None and o.dma_key is None and po.eng == o.eng:
                    if o.eng == "pe":
                        continue
                    if d not in raw:
                        continue
                if t not in req or pos[d] > pos[req[t]]:
                    req[t] = d
            kn = known[o.eng]
            for t in list(req.keys()):
                if not isinstance(t, str):
                    req[t] = latest_on_key[t]
            waits = []
            for t, d in req.items():
                if kn.get(t, 0) >= pos[d]:
                    continue
                waits.append(d)
                needed.add(d)
                for t2, p2 in done_clock[d].items():
                    if kn.get(t2, 0) < p2:
                        kn[t2] = p2
            o.waits = waits
            dc = dict(kn)
            dc[tl(o)] = max(dc.get(tl(o), 0), pos[o.idx])
            done_clock[o.idx] = dc
            if o.dma_key is not None:
                latest_on_key[tl(o)] = o.idx
            for r in o.writes:
                last_writer[r] = o.idx
                readers[r] = []
            for r in o.reads:
                if r not in o.writes:
                    readers.setdefault(r, []).append(o.idx)
        semcnt = {}
        for o in ops:
            t = tl(o)
            if o.dma_key is not None:
                semcnt[t] = semcnt.get(t, 0) + 16
                o.signal = True
                o.semval = semcnt[t]
            elif o.idx in needed:
                semcnt[t] = semcnt.get(t, 0) + 1
                o.signal = True
                o.semval = semcnt[t]
        self.timelines = sorted(set(tl(o) for o in ops if o.signal), key=str)
        self._tl = tl
        return self

    def emit(self):
        nc = self.nc
        ops = self.ops
        tl = self._tl
        with ExitStack() as es:
            pidx = _PHASE[0] % 2
            _PHASE[0] += 1
            mypool = _SEMPOOL[pidx]
            other = _SEMPOOL[1 - pidx]
            assert len(self.timelines) <= len(mypool), len(self.timelines)
            sems = {}
            for i, t in enumerate(self.timelines):
                sems[t] = mypool[i]
            block = es.enter_context(nc.Block())
            by_eng = {e: [o for o in ops if o.eng == e] for e in ENGS}
            final_dma = {}
            for o in ops:
                if o.dma_key is not None:
                    final_dma[tl(o)] = o.semval

            def run(engname, eng):
                for o in by_eng[engname]:
                    ws = list(o.waits)
                    att = None
                    if ws and engname != "pe":
                        att = ws.pop()
                    for d in ws:
                        po = ops[d]
                        eng.wait_ge(sems[tl(po)], po.semval)
                    if att is not None:
                        rec = _Rec(eng)
                        ins = o.fn(rec)
                        po = ops[att]
                        rec.first._wait_ge(sems[tl(po)], po.semval)
                    else:
                        ins = o.fn(eng)
                    if o.signal:
                        ins.then_inc(sems[tl(o)], 16 if o.dma_key is not None else 1)
                if engname == "sp":
                    for t, v in final_dma.items():
                        eng.wait_ge(sems[t], v)
                    for sm in other:
                        eng.sem_clear(sm)

            @block.tensor
            def _(eng):
                run("pe", eng)

            @block.scalar
            def _(eng):
                run("act", eng)

            @block.vector
            def _(eng):
                run("dve", eng)

            @block.gpsimd
            def _(eng):
                run("pool", eng)

            @block.sync
            def _(eng):
                run("sp", eng)


class _Rec:
    def __init__(self, eng):
        self._eng = eng
        self.first = None

    def __getattr__(self, name):
        f = getattr(self._eng, name)

        def g(*a, **k):
            r = f(*a, **k)
            if self.first is None:
                self.first = r
            return r
        return g


class Phase:
    def __init__(self, nc):
        self.nc = nc
        self.es = ExitStack()
        self.p = Prog(nc)
        self._n = 0

    def sb(self, shape, dt):
        self._n += 1
        _CNT[0] += 1
        return self.es.enter_context(self.nc.sbuf_tensor("sb%d" % _CNT[0], list(shape), dt))

    def psum4(self):
        r = []
        for _ in range(4):
            _CNT[0] += 1
            r.append(self.es.enter_context(self.nc.psum_tensor("ps%d" % _CNT[0], [128, 1024], F32)))
        return r

    def done(self):
        self.p.finalize()
        self.p.emit()
        self.es.close()


def build(stage=4, tail_only=False):
    nc = bass.Bass("TRN2", target_bir_lowering=False)

    def din(name, shape, dt=F32):
        return nc.dram_tensor(name, list(shape), dt, kind="ExternalInput").ap()

    def dscr(name, shape, dt):
        return nc.dram_tensor(name, list(shape), dt, kind="Internal").ap()

    x_d = din("x", [S, D])
    ctx_d = din("ctx", [256, D])
    cvec_d = din("cvec", [128, 16])
    ident_d = din("ident", [128, 128])
    modw_d = din("mod_w", [2, D, 6144])
    modb_d = din("mod_bT", [128, 96])
    ng_d = din("norm_g", [128, 32])
    wqkp_d = din("w_qkp", [D, 1792])
    wv_d = din("w_v", [D, 128])
    gains_d = din("gains", [128, 4])
    cos_d = din("cos_t", [128, S])
    sin_d = din("sin_t", [128, S])
    poolw_d = din("pool_wT", [128, 512])
    poolsc_d = din("pool_sc", [128, 4])
    pooledge_d = din("pool_edge", [128, 64])
    wout0_d = din("w_out0", [D, D])
    wout1_d = din("w_out1", [D, D])
    win1_d = din("w_in1", [D, 2560])
    sggain_d = din("sg_gain_b", [128, 512])
    sgw_d = din("sg_wT", [128, 512])
    sgb_d = din("sg_b_b", [128, 512])
    convw_d = din("conv_wT", [128, 12])
    rw_d = din("rw", [2, D, 20])
    rb_d = din("rb_b", [128, 160])
    wg_d = din("w_gate", [2, 16, D, 256])
    wu_d = din("w_up", [2, 16, D, 256])
    wd_d = din("w_down", [2, 16, 256, D])
    sel_d = din("sel", [32, 2048])
    out_d = nc.dram_tensor("out", [S, D], F32, kind="ExternalOutput").ap()

    A2_d = dscr("A2s", [8, 128, S], BF16)
    QT_d = dscr("QTs", [4, 128, S], BF16)
    MIX_d = dscr("MIXs", [8, 128, S], BF16)
    PT_d = dscr("PTs", [4, 128, S], F32)
    BG_d = dscr("BGs", [4, 128, S], F32)
    KT_d = dscr("KTs", [128, 4352], BF16)
    VS_d = dscr("VSs", [128, 34, 193], BF16)

    with ExitStack() as top:
        def psb(shape, dt):
            _CNT[0] += 1
            return top.enter_context(nc.sbuf_tensor("pt%d" % _CNT[0], list(shape), dt))

        _PHASE[0] = 0
        for pi_ in range(2):
            _SEMPOOL[pi_] = []
            for si_ in range(NSEM):
                _SEMPOOL[pi_].append(top.enter_context(nc.semaphore("sp%d_%d" % (pi_, si_))))
        HT = psb([128, 8, S], F32)
        ident = psb([128, 128], F32)
        ident_bf = psb([128, 128], BF16)
        ones_bf = psb([128, 128], BF16)
        onesblk_bf = psb([128, 128], BF16)
        ones_f = psb([128, 128], F32)
        MOD = psb([128, 2, 48], F32)
        MODC = psb([128, 16], F32)
        NG = psb([128, 32], F32)
        GS = psb([128, 4, 8], F32)
        GSC = psb([128, 8], F32)
        EPSB = psb([128, 1], F32)

        ph = Phase(nc)
        p = ph.p
        PS = ph.psum4()
        cvec = ph.sb([128, 16], F32)
        scv = ph.sb([128, 16], F32)
        modb = ph.sb([128, 96], F32)
        mrow = ph.sb([2, 6144], F32)
        mwbuf = [ph.sb([128, 8, 512], F32) for _ in range(2)]
        xin = [ph.sb([128, D], F32) for _ in range(2)]
        p.dma("sp", "c0", lambda e: e.dma_start(out=ident[:], in_=ident_d[:, :]), writes=["ident"])
        p.dma("sp", "c0", lambda e: e.dma_start(out=cvec[:], in_=cvec_d[:, :]), writes=["cvec"])
        p.dma("sp", "c0", lambda e: e.dma_start(out=modb[:], in_=modb_d[:, :]), writes=["modb"])
        p.dma("sp", "c0", lambda e: e.dma_start(out=NG[:], in_=ng_d[:, :]), writes=["NG"])
        p.dve(lambda e: e.tensor_copy(ident_bf[:], ident[:]), reads=["ident"], writes=["ident_bf"])
        p.dve(lambda e: e.memset(ones_bf[:], 1.0 / 1024.0), writes=["ones_bf"])
        p.dve(lambda e: e.memset(ones_f[:], 1.0), writes=["ones_f"])
        p.dve(lambda e: e.memset(EPSB[:], EPS), writes=["epsb"])
        p.dve(lambda e: e.memset(onesblk_bf[:], 0.0), writes=["onesblk0"])
        p.dve(lambda e: e.memset(onesblk_bf[0:64, 0:64], 1.0 / 64.0), reads=["onesblk0"], writes=["onesblk1"])
        p.dve(lambda e: e.memset(onesblk_bf[64:128, 64:128], 1.0 / 64.0), reads=["onesblk1"], writes=["onesblk"])
        p.act(lambda e: e.activation(scv[:], cvec[:], AF.Silu), reads=["cvec"], writes=["scv"])
        for l in range(2):
            for fb in range(12):
                it = l * 12 + fb
                buf = mwbuf[it % 2]
                bk = "mw%d" % (it % 2)
                p.dma("sp", bk, lambda e, buf=buf, l=l, fb=fb: e.dma_start(
                    out=buf[:], in_=modw_d[l, :, fb * 512:(fb + 1) * 512].rearrange("(c p) n -> p c n", p=128)),
                    writes=[bk])
                pb = "psA%d" % (it % 2)
                pst = PS[0][:, (it % 2) * 512:(it % 2) * 512 + 512]

                def mm(e, buf=buf, pst=pst):
                    for c in range(8):
                        ins = e.matmul(pst[0:2, :], scv[:, c * 2:c * 2 + 2], buf[:, c, :], start=(c == 0), stop=(c == 7))
                    return ins
                p.pe(mm, reads=[bk, "scv"], writes=[pb])
                if it % 2 == 0:
                    p.act(lambda e, pst=pst, fb=fb: e.activation(mrow[0:2, fb * 512:(fb + 1) * 512], pst[0:2, :], AF.Identity),
                          reads=[pb], writes=["mrow_%d" % fb])
                else:
                    p.dve(lambda e, pst=pst, fb=fb: e.tensor_copy(mrow[0:2, fb * 512:(fb + 1) * 512], pst[0:2, :]),
                          reads=[pb], writes=["mrow_%d" % fb])

            def tr(e):
                for j in range(48):
                    ins = e.matmul(PS[1][:, j * 2:j * 2 + 2], mrow[0:2, j * 128:(j + 1) * 128], ident[0:2, 0:2], start=True, stop=True)
                return ins
            p.pe(tr, reads=["mrow_%d" % fb for fb in range(12)] + ["ident"], writes=["psB0"])
            p.dve(lambda e, l=l: e.tensor_tensor(MOD[:, l, :], PS[1][:, 0:96].rearrange("p (j t) -> p j t", t=2)[:, :, 0],
                                                modb[:, l * 48:(l + 1) * 48], ALU.add),
                  reads=["psB0", "modb"], writes=["MOD%d" % l])
            if l == 0:
                p.dve(lambda e: e.tensor_tensor(MODC[:], PS[1][:, 0:32].rearrange("p (j t) -> p j t", t=2)[:, :, 1],
                                                modb[:, 0:16], ALU.add),
                      reads=["psB0", "modb"], writes=["MODC"])
        for l in range(2):
            for n in range(2):
                sc = MOD[:, l, (1 + 3 * n) * 8:(2 + 3 * n) * 8]
                p.dve(lambda e, l=l, n=n, sc=sc: e.scalar_tensor_tensor(GS[:, l * 2 + n, :], sc, 1.0, NG[:, n * 16 + l * 8:n * 16 + l * 8 + 8],
                                                                      op0=ALU.add, op1=ALU.mult),
                      reads=["MOD%d" % l, "NG"], writes=["GS%d%d" % (l, n)])
        p.dve(lambda e: e.scalar_tensor_tensor(GSC[:], MODC[:, 8:16], 1.0, NG[:, 0:8], op0=ALU.add, op1=ALU.mult),
              reads=["MODC", "NG"], writes=["GSC"])
        for tt in range(32):
            xb = xin[tt % 2]
            xk = "xin%d" % (tt % 2)
            p.dma("sp", xk, lambda e, xb=xb, tt=tt: e.dma_start(out=xb[:], in_=x_d[tt * 128:(tt + 1) * 128, :]), writes=[xk])
            pst = PS[2 + tt % 2]
            pk = "psX%d" % (tt % 2)

            def trx(e, xb=xb, pst=pst):
                for c in range(8):
                    ins = e.matmul(pst[:, c * 128:(c + 1) * 128], xb[:, c * 128:(c + 1) * 128], ident[:], start=True, stop=True)
                return ins
            p.pe(trx, reads=[xk, "ident"], writes=[pk])
            dst = HT[:, :, tt * 128:(tt + 1) * 128]
            src = pst[:, :].rearrange("p (c t) -> p c t", t=128)
            if tt % 2 == 0:
                p.act(lambda e, dst=dst, src=src: e.activation(dst, src, AF.Identity), reads=[pk], writes=["HT%d" % tt])
            else:
                p.dve(lambda e, dst=dst, src=src: e.tensor_copy(dst, src), reads=[pk], writes=["HT%d" % tt])
        ph.done()

        def norm_block(p, PSst, pskey, t0, T, gs, sh, sqb, rstd, tmpb, dst_fn, tag, src=None, srckey="HT"):
            srcT = HT if src is None else src
            for c in range(8):
                sq = sqb[c % 2]
                p.act(lambda e, sq=sq, c=c: e.activation(sq[:, 0:T], srcT[:, c, t0:t0 + T], AF.Square),
                      reads=[srckey], writes=["sq%s%d" % (tag, c % 2)])
                p.pe(lambda e, sq=sq, c=c: e.matmul(PSst[:, 0:T], ones_bf[:], sq[:, 0:T], start=(c == 0), stop=(c == 7)),
                     reads=["sq%s%d" % (tag, c % 2), "ones_bf"], writes=[pskey])
            p.act(lambda e: e.activation(rstd[:, 0:T], PSst[:, 0:T], AF.Sqrt, bias=EPSB[:, 0:1]), reads=[pskey], writes=["rstd0" + tag])
            p.dve(lambda e: e.reciprocal(rstd[:, 0:T], rstd[:, 0:T]), reads=["rstd0" + tag], writes=["rstd" + tag])
            for c in range(8):
                tb = tmpb[c % 2]
                p.dve(lambda e, tb=tb, c=c: e.scalar_tensor_tensor(tb[:, 0:T], srcT[:, c, t0:t0 + T], gs[:, c:c + 1], rstd[:, 0:T],
                                                                  op0=ALU.mult, op1=ALU.mult),
                      reads=[srckey, "rstd" + tag], writes=["tmp%s%d" % (tag, c % 2)])
                dst, dkey = dst_fn(c)
                p.act(lambda e, tb=tb, c=c, dst=dst: e.activation(dst, tb[:, 0:T], AF.Identity, bias=sh[:, c:c + 1]),
                      reads=["tmp%s%d" % (tag, c % 2)], writes=[dkey])

        def wout_phase(wout_d, l):
            ph = Phase(nc)
            p = ph.p
            PS = ph.psum4()
            w = ph.sb([128, 8, D], BF16)
            mixb = [ph.sb([128, 8, 512], BF16) for _ in range(2)]
            p.dma("pool", "w", lambda e: e.dma_start(out=w[:], in_=wout_d.rearrange("(c p) n -> p c n", p=128)), writes=["w"])
            for b in range(8):
                mb = mixb[b % 2]
                mk = "mix%d" % (b % 2)
                p.dma("sp", mk, lambda e, mb=mb, b=b: e.dma_start(out=mb[:], in_=MIX_d[:, :, b * 512:(b + 1) * 512].rearrange("c p t -> p c t")),
                      writes=[mk])
                for f in range(8):
                    pst = PS[f % 4][:, 0:512]
                    pk = "psY%d" % (f % 4)

                    def mm(e, mb=mb, f=f, pst=pst):
                        for k in range(8):
                            ins = e.matmul(pst, w[:, k, f * 128:(f + 1) * 128], mb[:, k, :], start=(k == 0), stop=(k == 7))
                        return ins
                    p.pe(mm, reads=["w", mk], writes=[pk])
                    hsl = HT[:, f, b * 512:(b + 1) * 512]
                    p.dve(lambda e, pst=pst, f=f, hsl=hsl: e.scalar_tensor_tensor(hsl, pst, MOD[:, l, 16 + f:17 + f], hsl, op0=ALU.mult, op1=ALU.add),
                          reads=[pk], writes=["HT"])
            ph.done()

        def router_moe(l):
            with ExitStack() as rstack:
                _CNT[0] += 1
                combT = rstack.enter_context(nc.sbuf_tensor("combT%d" % _CNT[0], [32, S], BF16))
                router_moe_inner(l, combT)

        def router_moe_inner(l, combT):
            ph = Phase(nc)
            p = ph.p
            PS = ph.psum4()
            sqb = [ph.sb([128, 512], BF16) for _ in range(2)]
            rstd = ph.sb([128, 512], F32)
            tmpb = [ph.sb([128, 512], F32) for _ in range(2)]
            a2f = ph.sb([128, 8, 512], F32)
            a2b = [ph.sb([128, 8, 512], BF16) for _ in range(2)]
            rw = ph.sb([128, 8, 20], F32)
            rbb = ph.sb([128, 80], F32)
            lgT = ph.sb([32, 512], F32)
            LG = ph.sb([128, 32, 20], F32)
            p.dma("sp", "rw", lambda e: e.dma_start(out=rw[:], in_=rw_d[l].rearrange("(c p) n -> p c n", p=128)), writes=["rw"])
            p.dma("sp", "rw", lambda e: e.dma_start(out=rbb[:], in_=rb_d[:, l * 80:(l + 1) * 80]), writes=["rbb"])
            gs = GS[:, l * 2 + 1, :]
            sh = MOD[:, l, 24:32]
            for b in range(8):
                af = a2f
                ab = a2b[b % 2]
                abk = "a2b%d" % (b % 2)
                norm_block(p, PS[0], "psS", b * 512, 512, gs, sh, sqb, rstd, tmpb,
                           lambda c, af=af: (af[:, c, :], "a2f_%d" % c), "n")
                akeys = ["a2f_%d" % c for c in range(8)]
                p.pool(lambda e, af=af, ab=ab: e.tensor_copy(ab[:], af[:]), reads=akeys, writes=[abk])
                p.dma("sp", "a2st%d" % (b % 2), lambda e, ab=ab, b=b: e.dma_start(
                    out=A2_d[:, :, b * 512:(b + 1) * 512].rearrange("c p t -> p c t"), in_=ab[:]), reads=[abk], writes=["A2"])

                def rmm(e, af=af):
                    for c in range(8):
                        ins = e.matmul(PS[1][0:20, 0:512], rw[:, c, :], af[:, c, :], start=(c == 0), stop=(c == 7))
                    return ins
                p.pe(rmm, reads=["rw"] + akeys, writes=["psR"])
                p.act(lambda e: e.activation(lgT[0:20, :], PS[1][0:20, 0:512], AF.Identity), reads=["psR"], writes=["lgT"])

                def rtr(e):
                    for tt in range(4):
                        ins = e.matmul(PS[2][:, tt * 32:tt * 32 + 20], lgT[0:20, tt * 128:(tt + 1) * 128], ident[0:20, 0:20], start=True, stop=True)
                    return ins
                p.pe(rtr, reads=["lgT", "ident"], writes=["psT"])
                p.dve(lambda e, b=b: e.tensor_tensor(LG[:, b * 4:(b + 1) * 4, :], PS[2][:, 0:128].rearrange("p (t n) -> p t n", n=32)[:, :, 0:20],
                                                    rbb[:].rearrange("p (t n) -> p t n", n=20), ALU.add),
                      reads=["psT", "rbb"], writes=["LG"])
            NT = 32
            RT = ph.sb([128, 2880], F32)
            _o = [0]

            def rt2():
                a = _o[0]
                _o[0] += NT
                return RT[:, a:a + NT]

            def rt3():
                a = _o[0]
                _o[0] += NT * 4
                return RT[:, a:a + NT * 4].rearrange("p (n g) -> p n g", g=4)

            def rt4():
                a = _o[0]
                _o[0] += NT * 16
                return RT[:, a:a + NT * 16].rearrange("p (n g i) -> p n g i", g=4, i=4)

            def rt16():
                a = _o[0]
                _o[0] += NT * 16
                return RT[:, a:a + NT * 16].rearrange("p (n k) -> p n k", k=16)
            gmax = rt2()
            ohg = rt3()
            gd = rt3()
            gsum = rt2()
            gp = rt2()
            t44 = rt4()
            esel = rt3()
            e1 = rt2()
            sel1 = rt3()
            em = rt3()
            e2 = rt2()
            sel2 = rt3()
            dd = rt2()
            w1 = rt2()
            w2 = rt2()
            ce = rt3()
            ce2 = rt3()
            comb = rt16()
            chf = rt16()
            assert _o[0] <= 2880
            chl = ph.sb([128, NT, 32], BF16)
            gl = LG[:, :, 0:4]
            el = LG[:, :, 4:20].rearrange("p n (g i) -> p n g i", i=4)

            def b3(ap2):
                return ap2.unsqueeze(2).to_broadcast([128, NT, 4])
            p.dve(lambda e: e.tensor_reduce(gmax[:], gl, AX.X, ALU.max), reads=["LG"], writes=["gmax"])
            p.dve(lambda e: e.tensor_tensor(ohg[:], gl, b3(gmax[:]), ALU.is_equal), reads=["LG", "gmax"], writes=["ohg"])
            p.dve(lambda e: e.tensor_tensor(gd[:], gl, b3(gmax[:]), ALU.subtract), reads=["LG", "gmax"], writes=["gd"])
            p.act(lambda e: e.activation(gd[:], gd[:], AF.Exp), reads=["gd"], writes=["ge"])
            p.dve(lambda e: e.tensor_reduce(gsum[:], gd[:], AX.X, ALU.add), reads=["ge"], writes=["gsum"])
            p.dve(lambda e: e.reciprocal(gp[:], gsum[:]), reads=["gsum"], writes=["gp"])
            p.dve(lambda e: e.tensor_tensor(t44[:], el, ohg[:].unsqueeze(3).to_broadcast([128, NT, 4, 4]), ALU.mult),
                  reads=["LG", "ohg"], writes=["t44"])
            p.dve(lambda e: e.tensor_reduce(esel[:], t44[:].rearrange("p n g i -> p n i g"), AX.X, ALU.add), reads=["t44"], writes=["esel"])
            p.dve(lambda e: e.tensor_reduce(e1[:], esel[:], AX.X, ALU.max), reads=["esel"], writes=["e1"])
            p.dve(lambda e: e.tensor_tensor(sel1[:], esel[:], b3(e1[:]), ALU.is_equal), reads=["esel", "e1"], writes=["sel1"])
            p.dve(lambda e: e.scalar_tensor_tensor(em[:], sel1[:], -1e30, esel[:], op0=ALU.mult, op1=ALU.add), reads=["sel1", "esel"], writes=["em"])
            p.dve(lambda e: e.tensor_reduce(e2[:], em[:], AX.X, ALU.max), reads=["em"], writes=["e2"])
            p.dve(lambda e: e.tensor_tensor(sel2[:], em[:], b3(e2[:]), ALU.is_equal), reads=["em", "e2"], writes=["sel2"])
            p.dve(lambda e: e.tensor_tensor(dd[:], e2[:], e1[:], ALU.subtract), reads=["e1", "e2"], writes=["dd"])
            p.act(lambda e: e.activation(dd[:], dd[:], AF.Exp), reads=["dd"], writes=["ex"])
            p.dve(lambda e: e.tensor_scalar(w1[:], dd[:], 1.0, None, op0=ALU.add), reads=["ex"], writes=["w1a"])
            p.dve(lambda e: e.reciprocal(w1[:], w1[:]), reads=["w1a"], writes=["w1"])
            p.dve(lambda e: e.tensor_tensor(w2[:], dd[:], w1[:], ALU.mult), reads=["ex", "w1"], writes=["w2a"])
            p.dve(lambda e: e.tensor_tensor(w1[:], w1[:], gp[:], ALU.mult), reads=["w1", "gp", "w2a"], writes=["wt1"])
            p.dve(lambda e: e.tensor_tensor(w2[:], w2[:], gp[:], ALU.mult), reads=["w2a", "gp"], writes=["wt2"])
            p.dve(lambda e: e.tensor_tensor(ce[:], sel1[:], b3(w1[:]), ALU.mult), reads=["sel1", "wt1"], writes=["ce"])
            p.dve(lambda e: e.tensor_tensor(ce2[:], sel2[:], b3(w2[:]), ALU.mult), reads=["sel2", "wt2"], writes=["ce2"])
            p.dve(lambda e: e.tensor_tensor(ce[:], ce[:], ce2[:], ALU.add), reads=["ce", "ce2"], writes=["cef"])
            p.dve(lambda e: e.tensor_tensor(comb[:].rearrange("p n (g i) -> p n g i", i=4),
                                            ohg[:].unsqueeze(3).to_broadcast([128, NT, 4, 4]),
                                            ce[:].unsqueeze(2).to_broadcast([128, NT, 4, 4]), ALU.mult),
                  reads=["ohg", "cef"], writes=["comb"])
            p.dve(lambda e: e.tensor_copy(chl[:, :, 0:16], comb[:]), reads=["comb"], writes=["chi"])
            p.dve(lambda e: e.tensor_copy(chf[:], chl[:, :, 0:16]), reads=["chi"], writes=["chf"])
            p.dve(lambda e: e.tensor_tensor(chf[:], comb[:], chf[:], ALU.subtract), reads=["comb", "chf"], writes=["clo"])
            p.dve(lambda e: e.tensor_copy(chl[:, :, 16:32], chf[:]), reads=["clo"], writes=["chl"])
            for q in range(8):
                pst = PS[3][:, (q % 2) * 512:(q % 2) * 512 + 512]
                pk = "psC%d" % (q % 2)

                def ctr(e, q=q, pst=pst):
                    for tt in range(4):
                        ins = e.matmul(pst[0:32, tt * 128:(tt + 1) * 128], chl[:, q * 4 + tt, :], ident_bf[:], start=True, stop=True)
                    return ins
                p.pe(ctr, reads=["chl", "chi", "ident_bf"], writes=[pk])
                p.act(lambda e, q=q, pst=pst: e.activation(combT[:, q * 512:(q + 1) * 512], pst[0:32, :], AF.Identity), reads=[pk], writes=["combT"])
            ph.done()
            if stage == 10 + l or (stage == 8 and l == 1):
                return
            ph = Phase(nc)
            p = ph.p
            PS = ph.psum4()
            T = 512
            NB = S // T
            wslot = [(ph.sb([128, 8, 256], BF16), ph.sb([128, 8, 256], BF16), ph.sb([128, 2, D], BF16)) for _ in range(3)]
            a2 = [ph.sb([128, 8, T], BF16) for _ in range(2)]
            sel = ph.sb([32, 2048], BF16)
            cb = ph.sb([128, T], F32)
            sg = [ph.sb([128, T], F32)] * 2
            tt_ = ph.sb([128, T], F32)
            hb0 = [ph.sb([128, 2, T], BF16) for _ in range(2)]
            hb1 = ph.sb([128, 2, T], BF16)
            p.dma("pool", "sel", lambda e: e.dma_start(out=sel[:, 0:1024], in_=sel_d[:, 0:1024]), writes=["sel"])
            p.dma("pool", "sel", lambda e: e.dma_start(out=sel[:, 1024:2048], in_=sel_d[:, 1024:2048]), writes=["sel"])
            g2 = MOD[:, l, 40:48]
            RB = [PS[0][:, 0:512], PS[0][:, 512:1024], PS[1][:, 0:512]]
            RBK = ["psR0", "psR1", "psR2"]
            CBP = PS[1][:, 512:1024]
            ACC = [PS[2][:, 0:512], PS[2][:, 512:1024], PS[3][:, 0:512], PS[3][:, 512:1024]]

            def load_expert(ex):
                sl = ex % 3
                wg, wu, wd = wslot[sl]
                wk = "w%d" % sl
                p.dma("pool", wk, lambda e: e.dma_start(out=wg[:], in_=wg_d[l, ex].rearrange("(c p) n -> p c n", p=128)), writes=[wk + "g"])
                p.dma("pool", wk, lambda e: e.dma_start(out=wu[:], in_=wu_d[l, ex].rearrange("(c p) n -> p c n", p=128)), writes=[wk + "u"])
                p.dma("pool", wk, lambda e: e.dma_start(out=wd[:], in_=wd_d[l, ex].rearrange("(c p) n -> p c n", p=128)), writes=[wk + "d"])
            st = {"step": 0, "rk": 0, "sgi": 0}

            def hbuf(b, j):
                if j == 0:
                    return hb0[b % 2], "h0_%d" % (b % 2)
                return hb1, "h1"

            def G(pr, b, j):
                ex = pr * 2 + j
                sl = ex % 3
                wg, wu, wd = wslot[sl]
                wk = "w%d" % sl
                if j == 0:
                    ab = a2[st["step"] % 2]
                    ak = "a2_%d" % (st["step"] % 2)
                    st["cur"] = (ab, ak)
                    st["step"] += 1
                    p.dma("sp", ak, lambda e: e.dma_start(out=ab[:], in_=A2_d[:, :, b * T:(b + 1) * T].rearrange("c p t -> p c t")), writes=[ak])
                ab, ak = st["cur"]
                hbb, hk = hbuf(b, j)
                p.pe(lambda e: e.matmul(CBP, sel[:, ex * 128:(ex + 1) * 128], combT[:, b * T:(b + 1) * T], start=True, stop=True),
                     reads=["sel", "combT"], writes=["psCB"])
                p.act(lambda e: e.activation(cb[:], CBP, AF.Identity), reads=["psCB"], writes=["cb"])
                for f2 in range(2):
                    pg = RB[st["rk"] % 3]
                    pgk = RBK[st["rk"] % 3]
                    st["rk"] += 1
                    pu = RB[st["rk"] % 3]
                    puk = RBK[st["rk"] % 3]
                    st["rk"] += 1

                    def gmm(e, pg=pg, f2=f2):
                        for k in range(8):
                            ins = e.matmul(pg, wg[:, k, f2 * 128:(f2 + 1) * 128], ab[:, k, :], start=(k == 0), stop=(k == 7))
                        return ins

                    def umm(e, pu=pu, f2=f2):
                        for k in range(8):
                            ins = e.matmul(pu, wu[:, k, f2 * 128:(f2 + 1) * 128], ab[:, k, :], start=(k == 0), stop=(k == 7))
                        return ins
                    p.pe(gmm, reads=[wk + "g", ak], writes=[pgk])
                    p.pe(umm, reads=[wk + "u", ak], writes=[puk])
                    sgb = sg[st["sgi"] % 2]
                    sgk = "sg0"
                    st["sgi"] += 1
                    p.act(lambda e, sgb=sgb, pg=pg: e.activation(sgb[:], pg, AF.Silu), reads=[pgk], writes=[sgk])
                    p.dve(lambda e, pu=pu: e.tensor_tensor(tt_[:], pu, cb[:], ALU.mult), reads=[puk, "cb"], writes=["tt"])
                    p.pool(lambda e, sgb=sgb, f2=f2: e.tensor_tensor(hbb[:, f2, :], tt_[:], sgb[:], ALU.mult),
                           reads=["tt", sgk], writes=[hk + "_%d" % f2])

            def DOWN(pr, b, half):
                hs = []
                for j in range(2):
                    ex = pr * 2 + j
                    wg, wu, wd = wslot[ex % 3]
                    hbb, hk = hbuf(b, j)
                    hs.append((wd, "w%dd" % (ex % 3), hbb, hk))
                for fi in range(4):
                    f = half * 4 + fi

                    def dmm(e, fi=fi, f=f):
                        for j in range(2):
                            wd, wdk, hbb, hk = hs[j]
                            for k in range(2):
                                ins = e.matmul(ACC[fi], wd[:, k, f * 128:(f + 1) * 128], hbb[:, k, :], start=(j == 0 and k == 0), stop=(j == 1 and k == 1))
                        return ins
                    p.pe(dmm, reads=[hs[0][1], hs[1][1], hs[0][3] + "_0", hs[0][3] + "_1", hs[1][3] + "_0", hs[1][3] + "_1"], writes=["psD%d" % fi])
                for fi in range(4):
                    f = half * 4 + fi
                    hsl = HT[:, f, b * T:(b + 1) * T]
                    p.dve(lambda e, fi=fi, f=f, hsl=hsl: e.scalar_tensor_tensor(hsl, ACC[fi], g2[:, f:f + 1], hsl, op0=ALU.mult, op1=ALU.add),
                          reads=["psD%d" % fi], writes=["HT"])

            load_expert(0)
            load_expert(1)
            for pr in range(8):
                if pr > 0:
                    load_expert(2 * pr + 1)
                G(pr, 0, 0)
                G(pr, 0, 1)
                if pr < 7:
                    load_expert(2 * pr + 2)
                for b in range(NB):
                    DOWN(pr, b, 0)
                    if b + 1 < NB:
                        G(pr, b + 1, 0)
                    DOWN(pr, b, 1)
                    if b + 1 < NB:
                        G(pr, b + 1, 1)
            ph.done()

        def store_phase():
            ph = Phase(nc)
            p = ph.p
            PS = ph.psum4()
            ob = [ph.sb([128, 4, D], F32) for _ in range(2)]
            for tt in range(32):
                pst = PS[tt % 2]
                pk = "psO%d" % (tt % 2)

                def tr(e, tt=tt, pst=pst):
                    for c in range(8):
                        ins = e.matmul(pst[:, c * 128:(c + 1) * 128], HT[:, c, tt * 128:(tt + 1) * 128], ident[:], start=True, stop=True)
                    return ins
                p.pe(tr, reads=["HT", "ident"], writes=[pk])
                g4 = tt // 4
                o = ob[g4 % 2]
                ok = "ob%d_%d" % (g4 % 2, tt % 4)
                if tt % 2 == 0:
                    p.act(lambda e, o=o, pst=pst, tt=tt: e.activation(o[:, tt % 4, :], pst[:, :], AF.Identity), reads=[pk], writes=[ok])
                else:
                    p.dve(lambda e, o=o, pst=pst, tt=tt: e.tensor_copy(o[:, tt % 4, :], pst[:, :]), reads=[pk], writes=[ok])
                if tt % 4 == 3:
                    p.dma("sp", "ost%d" % (g4 % 2), lambda e, o=o, g4=g4: e.dma_start(
                        out=out_d[g4 * 512:(g4 + 1) * 512, :].rearrange("(t p) n -> p t n", p=128), in_=o[:]),
                        reads=["ob%d_%d" % (g4 % 2, q) for q in range(4)], writes=["out"])
            ph.done()

        def qk_chain(p, PS, psA, kA, psB, kB, psC, T, gcol, gpcol, cosb, sinb, tq, dst, dstkey, rope=True):
            sqq, rsq, t1, t2 = tq
            p.act(lambda e: e.activation(sqq[:, 0:T], psA, AF.Square), reads=[kA], writes=["sqq"])
            p.pe(lambda e: e.matmul(psC[:, 0:T], onesblk_bf[:], sqq[:, 0:T], start=True, stop=True), reads=["sqq", "onesblk"], writes=["psC"])
            p.act(lambda e: e.activation(rsq[:, 0:T], psC[:, 0:T], AF.Sqrt, bias=EPSB[:, 0:1]), reads=["psC"], writes=["rsq0"])
            p.dve(lambda e: e.reciprocal(rsq[:, 0:T], rsq[:, 0:T]), reads=["rsq0"], writes=["rsq"])
            if rope:
                p.dve(lambda e: e.scalar_tensor_tensor(t1[:, 0:T], psA, gcol, cosb[:, 0:T], op0=ALU.mult, op1=ALU.mult),
                      reads=[kA, "cosb"], writes=["t1"])
                p.dve(lambda e: e.scalar_tensor_tensor(t2[:, 0:T], psB, gpcol, sinb[:, 0:T], op0=ALU.mult, op1=ALU.mult),
                      reads=[kB, "sinb"], writes=["t2"])
                p.pool(lambda e: e.tensor_tensor(t1[:, 0:T], t1[:, 0:T], t2[:, 0:T], ALU.add), reads=["t1", "t2"], writes=["t3"])
                p.dve(lambda e: e.tensor_tensor(dst, t1[:, 0:T], rsq[:, 0:T], ALU.mult), reads=["t3", "rsq"], writes=[dstkey])
            else:
                p.dve(lambda e: e.scalar_tensor_tensor(dst, psA, gcol, rsq[:, 0:T], op0=ALU.mult, op1=ALU.mult),
                      reads=[kA, "rsq"], writes=[dstkey])

        def layer0_mixer():
            ph = Phase(nc)
            p = ph.p
            PS = ph.psum4()
            T = 256
            wq = ph.sb([128, 8, 1792], BF16)
            wv = ph.sb([128, 8, 128], BF16)
            gains = ph.sb([128, 4], F32)
            sqb = [ph.sb([128, T], BF16) for _ in range(2)]
            rstd = ph.sb([128, T], F32)
            tmpb = [ph.sb([128, T], F32) for _ in range(2)]
            aTs = [ph.sb([128, 8, T], BF16) for _ in range(2)]
            cur = {"aT": aTs[1], "k": ["aT1_%d" % c for c in range(8)]}
            cosb = ph.sb([128, T], F32)
            sinb = ph.sb([128, T], F32)
            tq = (ph.sb([128, T], BF16), ph.sb([128, T], F32), ph.sb([128, T], F32), ph.sb([128, T], F32))
            qf = [ph.sb([128, T], BF16) for _ in range(2)]
            qf4 = ph.sb([128, 4, T], BF16)
            pst4 = ph.sb([128, 4, T], F32)
            vst = [ph.sb([128, 2, 193], BF16) for _ in range(2)]
            ctin = ph.sb([128, D], F32)
            CT = ph.sb([128, 8, 256], F32)
            p.dma("sp", "c1", lambda e: e.dma_start(out=gains[:], in_=gains_d[:, :]), writes=["gains"])
            for vi in range(2):
                p.dve(lambda e, vi=vi: e.memset(vst[vi][:], 0.0), writes=["vst%d" % vi])
                p.dve(lambda e, vi=vi: e.memset(vst[vi][:, :, 64:66], 1.0), reads=["vst%d" % vi], writes=["vst%d" % vi])
            p.dma("pool", "wq", lambda e: e.dma_start(out=wq[:], in_=wqkp_d.rearrange("(c p) n -> p c n", p=128)), writes=["wq"])
            p.dma("pool", "wq", lambda e: e.dma_start(out=wv[:], in_=wv_d.rearrange("(c p) n -> p c n", p=128)), writes=["wv"])
            B_ST = PS[0][:, 0:512]
            B_A = [PS[0][:, 512:1024], PS[1][:, 0:512]]
            B_B = [PS[1][:, 512:1024], PS[2][:, 0:512]]
            B_C = PS[2][:, 512:1024]
            B_M = [PS[3][:, 0:512], PS[3][:, 512:1024]]
            for tt in range(2):
                p.dma("sp", "ctin", lambda e, tt=tt: e.dma_start(out=ctin[:], in_=ctx_d[tt * 128:(tt + 1) * 128, :]), writes=["ctin"])
                for half in range(2):
                    bm = B_M[half]

                    def trc(e, half=half, bm=bm):
                        for c in range(4):
                            cc = half * 4 + c
                            ins = e.matmul(bm[:, c * 128:(c + 1) * 128], ctin[:, cc * 128:(cc + 1) * 128], ident[:], start=True, stop=True)
                        return ins
                    p.pe(trc, reads=["ctin", "ident"], writes=["psM%d" % half])
                    p.dve(lambda e, half=half, bm=bm, tt=tt: e.tensor_copy(CT[:, half * 4:half * 4 + 4, tt * 128:(tt + 1) * 128],
                                                                          bm.rearrange("p (c t) -> p c t", t=128)),
                          reads=["psM%d" % half], writes=["CT"])
            mcnt = [0]

            def proj(p, col0, bank, bkey, T=T):
                aT = cur["aT"]

                def mm(e):
                    for k in range(8):
                        ins = e.matmul(bank[:, 0:T], wq[:, k, col0:col0 + 128], aT[:, k, :], start=(k == 0), stop=(k == 7))
                    return ins
                p.pe(mm, reads=["wq"] + cur["k"], writes=[bkey])

            def vproj(p, tile0):
                i = mcnt[0] % 2
                mcnt[0] += 1
                bm = B_M[i]
                bk = "psM%d" % i
                vs_ = vst[i]
                aT = cur["aT"]

                def mm(e):
                    for t2 in range(2):
                        for k in range(8):
                            ins = e.matmul(bm[:, t2 * 128:(t2 + 1) * 128], aT[:, k, t2 * 128:(t2 + 1) * 128], wv[:, k, :], start=(k == 0), stop=(k == 7))
                    return ins
                p.pe(mm, reads=["wv"] + cur["k"], writes=[bk])
                p.dve(lambda e: e.tensor_copy(vs_[:, :, 0:64], bm[:, 0:256].rearrange("p (t n) -> p t n", n=128)[:, :, 0:64]),
                      reads=[bk], writes=["vst%da" % i])
                p.dve(lambda e: e.tensor_copy(vs_[:, :, 129:193], bm[:, 0:256].rearrange("p (t n) -> p t n", n=128)[:, :, 64:128]),
                      reads=[bk, "vst%da" % i], writes=["vst%d" % i])
                p.dma("sp", "vsst%d" % i, lambda e: e.dma_start(out=VS_d[:, tile0:tile0 + 2, :], in_=vs_[:]), reads=["vst%d" % i, "vst%da" % i], writes=["VSd"])

            def nrm(b):
                bi = b % 2
                norm_block(p, B_ST, "psST", b * T, T, GS[:, 0, :], MOD[:, 0, 0:8], sqb, rstd, tmpb,
                           lambda c: (aTs[bi][:, c, :], "aT%d_%d" % (bi, c)), "m")
            norm_block(p, B_ST, "psST", 0, 256, GSC, MODC, sqb, rstd, tmpb, lambda c: (aTs[1][:, c, :], "aT1_%d" % c), "m", src=CT, srckey="CT")
            nrm(0)
            proj(p, 512, B_A[0], "psA0")
            qk_chain(p, PS, B_A[0][:, 0:T], "psA0", None, None, B_C, T, gains[:, 2:3], None, None, None, tq, qf[0][:, 0:T], "qf0", rope=False)
            p.dma("sp", "qst0", lambda e: e.dma_start(out=KT_d[:, 0:256], in_=qf[0][:, 0:T]), reads=["qf0"], writes=["KTd"])
            vproj(p, 0)
            qi = 1
            for b in range(S // T):
                t0 = b * T
                p.dma("sp", "cs", lambda e, t0=t0: e.dma_start(out=cosb[:], in_=cos_d[:, t0:t0 + T]), writes=["cosb"])
                p.dma("sp", "cs", lambda e, t0=t0: e.dma_start(out=sinb[:], in_=sin_d[:, t0:t0 + T]), writes=["sinb"])
                cur["aT"] = aTs[b % 2]
                cur["k"] = ["aT%d_%d" % (b % 2, c) for c in range(8)]
                if b + 1 < S // T:
                    nrm(b + 1)
                for j in range(5):
                    i = qi % 2
                    qi += 1
                    col = j * 128 if j < 4 else 512
                    colp = 1152 + j * 128 if j < 4 else 1664
                    proj(p, col, B_A[i], "psA%d" % i)
                    proj(p, colp, B_B[i], "psB%d" % i)
                    gcol = gains[:, 0:1] if j < 4 else gains[:, 2:3]
                    gpcol = gains[:, 1:2] if j < 4 else gains[:, 3:4]
                    if j < 4:
                        qk_chain(p, PS, B_A[i][:, 0:T], "psA%d" % i, B_B[i][:, 0:T], "psB%d" % i, B_C, T, gcol, gpcol, cosb, sinb, tq,
                                 qf4[:, j, :], "qf4_%d" % j)
                        if j == 3:
                            p.dma("sp", "qst4", lambda e, t0=t0: e.dma_start(out=QT_d[:, :, t0:t0 + T].rearrange("c p t -> p c t"), in_=qf4[:]),
                                  reads=["qf4_%d" % q for q in range(4)], writes=["QTd"])
                    else:
                        qk_chain(p, PS, B_A[i][:, 0:T], "psA%d" % i, B_B[i][:, 0:T], "psB%d" % i, B_C, T, gcol, gpcol, cosb, sinb, tq,
                                 qf[i][:, 0:T], "qf%d" % i)
                        p.dma("sp", "qst%d" % i, lambda e, i=i, t0=t0: e.dma_start(out=KT_d[:, 256 + t0:256 + t0 + T], in_=qf[i][:, 0:T]),
                              reads=["qf%d" % i], writes=["KTd"])
                for g in range(4):
                    i = mcnt[0] % 2
                    mcnt[0] += 1
                    proj(p, 640 + g * 128, B_M[i], "psM%d" % i)
                    p.act(lambda e, i=i, g=g: e.activation(pst4[:, g, :], B_M[i][:, 0:T], AF.Identity), reads=["psM%d" % i], writes=["qpst4_%d" % g])
                    if g == 3:
                        p.dma("sp", "pst4", lambda e, t0=t0: e.dma_start(out=PT_d[:, :, t0:t0 + T].rearrange("c p t -> p c t"), in_=pst4[:]),
                              reads=["qpst4_%d" % q for q in range(4)], writes=["PTd"])
                vproj(p, 2 + 2 * b)
            ph.done()
            if stage == 20:
                return
            ph = Phase(nc)
            p = ph.p
            PS = ph.psum4()
            KT = ph.sb([128, 4352], BF16)
            VS = ph.sb([128, 34, 193], BF16)
            Qb = [ph.sb([128, 512], BF16) for _ in range(2)]
            Pb = [ph.sb([128, 1024], BF16) for _ in range(3)]
            rr = ph.sb([128, 512], F32)
            bcs = ph.sb([128, 512], F32)
            mixo = [ph.sb([128, 512], BF16) for _ in range(2)]
            p.dma("sp", "kt", lambda e: e.dma_start(out=KT[:], in_=KT_d[:, :]), writes=["KT"])
            p.dma("sp", "kt", lambda e: e.dma_start(out=VS[:], in_=VS_d[:, :, :]), writes=["VS"])
            SB_ = [PS[0], PS[1]]
            OA = PS[2][:, 0:512]
            OB = PS[2][:, 512:1024]
            BCA = PS[3][:, 0:512]
            BCB = PS[3][:, 512:1024]
            NKT = 24 if stage == 7 else 34
            units = [(j, qb) for j in range(4) for qb in range(8)]
            steps = [(u, kt) for u in range(len(units)) for kt in range(NKT)]
            qbuf = {}

            def load_q(u):
                j, qb = units[u]
                qt = Qb[u % 2]
                qk_ = "Qb%d" % (u % 2)
                p.dma("sp", qk_, lambda e: e.dma_start(out=qt[:], in_=QT_d[j, :, qb * 512:(qb + 1) * 512]), writes=[qk_])
                qbuf[u] = (qt, qk_)

            def S_(i):
                u, kt = steps[i]
                if kt == 0:
                    load_q(u)
                qt, qk_ = qbuf[u]
                sb_ = SB_[i % 2]
                sk = "psS%d" % (i % 2)

                def smm(e):
                    e.matmul(sb_[:, 0:512], KT[0:64, kt * 128:(kt + 1) * 128], qt[0:64, :], start=True, stop=True)
                    return e.matmul(sb_[:, 512:1024], KT[64:128, kt * 128:(kt + 1) * 128], qt[64:128, :], start=True, stop=True)
                p.pe(smm, reads=["KT", qk_], writes=[sk])

            def finalize(u):
                j, qb = units[u]
                p.dve(lambda e: e.reciprocal(rr[64:65, :], OA[64:65, :]), reads=["psOA"], writes=["rrA"])
                p.dve(lambda e: e.reciprocal(rr[0:1, :], OB[0:1, :]), reads=["psOB"], writes=["rrB"])
                p.pe(lambda e: e.matmul(BCA[0:64, :], ones_f[64:65, 0:64], rr[64:65, :], start=True, stop=True), reads=["rrA", "ones_f"], writes=["psBCA"])
                p.pe(lambda e: e.matmul(BCB[:, :], ones_f[0:1, :], rr[0:1, :], start=True, stop=True), reads=["rrB", "ones_f"], writes=["psBCB"])
                p.act(lambda e: e.activation(bcs[0:64, :], BCA[0:64, :], AF.Identity), reads=["psBCA"], writes=["bcsA"])
                p.act(lambda e: e.activation(bcs[64:128, :], BCB[64:128, :], AF.Identity), reads=["psBCB"], writes=["bcsB"])
                mo = mixo[u % 2]
                mk = "mixo%d" % (u % 2)
                p.dve(lambda e: e.tensor_tensor(mo[0:64, :], OA[0:64, :], bcs[0:64, :], ALU.mult), reads=["psOA", "bcsA"], writes=[mk + "a"])
                p.dve(lambda e: e.tensor_tensor(mo[64:128, :], OB[64:128, :], bcs[64:128, :], ALU.mult), reads=["psOB", "bcsB"], writes=[mk + "b"])
                p.dma("sp", "mst%d" % (u % 2), lambda e: e.dma_start(out=MIX_d[j, :, qb * 512:(qb + 1) * 512], in_=mo[:]),
                      reads=[mk + "a", mk + "b"], writes=["MIXd"])

            S_(0)
            for i in range(len(steps)):
                u, kt = steps[i]
                if i + 1 < len(steps):
                    S_(i + 1)
                sb_ = SB_[i % 2]
                sk = "psS%d" % (i % 2)
                pb = Pb[i % 3]
                pk = "P%d" % (i % 3)
                p.act(lambda e, pb=pb, sb_=sb_: e.activation(pb[:], sb_[:, :], AF.Exp, scale=0.125), reads=[sk], writes=[pk])

                def pv(e, pb=pb, kt=kt):
                    e.matmul(OA[0:65, :], VS[:, kt, 0:65], pb[:, 0:512], start=(kt == 0), stop=(kt == NKT - 1))
                    return e.matmul(OB[:, :], VS[:, kt, 65:193], pb[:, 512:1024], start=(kt == 0), stop=(kt == NKT - 1))
                p.pe(pv, reads=["VS", pk], writes=["psOA", "psOB"])
                if kt == NKT - 1:
                    finalize(u)
            ph.done()
            ph = Phase(nc)
            p = ph.p
            PS = ph.psum4()
            W = S + 16
            Pf = ph.sb([128, W], F32)
            sa = ph.sb([128, W], F32)
            sb2 = ph.sb([128, W], F32)
            dbf = ph.sb([128, S], BF16)
            pw = ph.sb([128, 512], BF16)
            psc = ph.sb([128, 4], F32)
            edg = ph.sb([128, 64], F32)
            et = ph.sb([128, 16], F32)
            po = [ph.sb([128, 512], BF16) for _ in range(2)]
            p.dma("pool", "pw", lambda e: e.dma_start(out=pw[:], in_=poolw_d[:, :]), writes=["pw"])
            p.dma("sp", "pc", lambda e: e.dma_start(out=psc[:], in_=poolsc_d[:, :]), writes=["psc"])
            p.dma("sp", "pc", lambda e: e.dma_start(out=edg[:], in_=pooledge_d[:, :]), writes=["edg"])
            p.dve(lambda e: e.memset(Pf[:, 0:8], 0.0), writes=["PfL"])
            p.dve(lambda e: e.memset(Pf[:, W - 8:W], 0.0), writes=["PfR"])
            oc = 0
            for g in range(4):
                w_ = 2 ** (g + 1)
                p.dma("sp", "pf", lambda e, g=g: e.dma_start(out=Pf[:, 8:8 + S], in_=PT_d[g, :, :]), writes=["Pf"])
                p.dve(lambda e: e.tensor_tensor(sa[:, 1:W], Pf[:, 0:W - 1], Pf[:, 1:W], ALU.add), reads=["Pf", "PfL", "PfR"], writes=["sa"])
                cur, ck = sa, "sa"
                oth, ok_ = sb2, "sb"
                lo, hi, sh_ = 1, W, 1
                for st in range(g):
                    nlo, nhi = lo + sh_, hi - sh_
                    eng = p.pool if st % 2 == 0 else p.dve
                    eng(lambda e, cur=cur, oth=oth, nlo=nlo, nhi=nhi, sh_=sh_: e.tensor_tensor(oth[:, nlo:nhi], cur[:, nlo - sh_:nhi - sh_], cur[:, nlo + sh_:nhi + sh_], ALU.add),
                        reads=[ck], writes=[ok_])
                    cur, ck, oth, ok_ = oth, ok_, cur, ck
                    lo, hi = nlo, nhi
                    sh_ *= 2
                assert lo <= 8 and hi >= 8 + S
                p.dve(lambda e, cur=cur, w_=w_: e.scalar_tensor_tensor(dbf[:, :], cur[:, 8:8 + S], 1.0 / w_, Pf[:, 8:8 + S], op0=ALU.mult, op1=ALU.subtract),
                      reads=[ck, "Pf"], writes=["dbf0"])
                p.dve(lambda e, cur=cur, g=g: e.tensor_tensor(et[:, 0:8], cur[:, 8:16], edg[:, g * 16:g * 16 + 8], ALU.mult), reads=[ck, "edg"], writes=["et0"])
                p.dve(lambda e, cur=cur, g=g: e.tensor_tensor(et[:, 8:16], cur[:, S:8 + S], edg[:, g * 16 + 8:g * 16 + 16], ALU.mult), reads=[ck, "edg", "et0"], writes=["et1"])
                p.dve(lambda e: e.tensor_tensor(dbf[:, 0:8], et[:, 0:8], Pf[:, 8:16], ALU.subtract), reads=["et1", "Pf", "dbf0"], writes=["dbf1"])
                p.dve(lambda e: e.tensor_tensor(dbf[:, S - 8:S], et[:, 8:16], Pf[:, S:8 + S], ALU.subtract), reads=["et1", "Pf", "dbf1"], writes=["dbf"])
                for b in range(8):
                    i = oc % 2
                    oc += 1
                    bank = PS[i][:, 0:512]
                    p.pe(lambda e, bank=bank, g=g, b=b: e.matmul(bank, pw[:, g * 128:(g + 1) * 128], dbf[:, b * 512:(b + 1) * 512], start=True, stop=True),
                         reads=["pw", "dbf"], writes=["psP%d" % i])
                    p.act(lambda e, bank=bank, i=i, g=g: e.activation(po[i][:], bank, AF.Identity, scale=psc[:, g:g + 1]), reads=["psP%d" % i, "psc"], writes=["po%d" % i])
                    p.dma("sp", "post%d" % i, lambda e, i=i, g=g, b=b: e.dma_start(out=MIX_d[4 + g, :, b * 512:(b + 1) * 512], in_=po[i][:]),
                          reads=["po%d" % i], writes=["MIXd"])
            ph.done()
            wout_phase(wout0_d, 0)

        def layer1_mixer():
            ph = Phase(nc)
            p = ph.p
            PS = ph.psum4()
            T = 256
            w1 = ph.sb([128, 8, 2560], BF16)
            sqb = [ph.sb([128, T], BF16) for _ in range(2)]
            rstd = ph.sb([128, T], F32)
            tmpb = [ph.sb([128, T], F32) for _ in range(2)]
            aTs = [ph.sb([128, 8, T], BF16) for _ in range(2)]
            cur = {"aT": aTs[0], "k": ["aT0_%d" % c for c in range(8)]}
            sggain = ph.sb([128, 512], F32)
            sgwT = ph.sb([128, 512], BF16)
            sgbb = ph.sb([128, 512], F32)
            usb = ph.sb([128, 4, T], F32)
            hxs = [ph.sb([128, T], F32)] * 2
            zs = hxs
            bgs = [ph.sb([128, T], F32)] * 2
            sqv = ph.sb([128, 512], F32)
            ssum = ph.sb([128, 4], F32)
            vt = sqv
            vn = ph.sb([128, 4, 128], BF16)
            st_ = ph.sb([128, 4, 128], F32)
            yc = [ph.sb([128, 4, T], BF16) for _ in range(2)]
            p.dma("pool", "w1", lambda e: e.dma_start(out=w1[:, :, 0:1280], in_=win1_d[:, 0:1280].rearrange("(c p) n -> p c n", p=128)), writes=["w1"])
            p.dma("pool", "w1", lambda e: e.dma_start(out=w1[:, :, 1280:2560], in_=win1_d[:, 1280:2560].rearrange("(c p) n -> p c n", p=128)), writes=["w1"])
            p.dma("pool", "w1", lambda e: e.dma_start(out=sgwT[:], in_=sgw_d[:, :]), writes=["sgwT"])
            p.dma("sp", "c2", lambda e: e.dma_start(out=sggain[:], in_=sggain_d[:, :]), writes=["sggain"])
            p.dma("sp", "c2", lambda e: e.dma_start(out=sgbb[:], in_=sgb_d[:, :]), writes=["sgbb"])
            B_ST = PS[0][:, 0:512]
            B_U = [PS[0][:, 512:1024], PS[1][:, 0:512]]
            B_H = [PS[1][:, 512:1024], PS[2][:, 0:512]]
            B_G = PS[2][:, 512:1024]
            B_V = PS[3][:, 0:512]
            B_S = PS[3][:, 512:1024]
            def proj(col0, tgt, bkey):
                aT = cur["aT"]

                def mm(e):
                    for k in range(8):
                        ins = e.matmul(tgt, w1[:, k, col0:col0 + 128], aT[:, k, :], start=(k == 0), stop=(k == 7))
                    return ins
                p.pe(mm, reads=["w1"] + cur["k"], writes=[bkey])

            def nrm(b):
                bi = b % 2
                norm_block(p, B_ST, "psST", b * T, T, GS[:, 2, :], MOD[:, 1, 0:8], sqb, rstd, tmpb,
                           lambda c: (aTs[bi][:, c, :], "aT%d_%d" % (bi, c)), "m")
            nrm(0)
            hi_ = 0
            for b in range(S // T):
                t0 = b * T
                cur["aT"] = aTs[b % 2]
                cur["k"] = ["aT%d_%d" % (b % 2, c) for c in range(8)]
                akeys = cur["k"]
                aT = cur["aT"]
                if b + 1 < S // T:
                    nrm(b + 1)
                for half in range(2):
                    for q in range(2):
                        g = half * 2 + q
                        proj(g * 128, B_U[half][:, q * T:(q + 1) * T], "psU%d" % half)
                    p.act(lambda e, half=half: e.activation(usb[:, half * 2:half * 2 + 2, :], B_U[half][:, 0:2 * T].rearrange("p (q t) -> p q t", t=T), AF.Identity),
                          reads=["psU%d" % half], writes=["usb%d" % half])
                for c in range(4):
                    i = hi_ % 2
                    hi_ += 1
                    proj(1024 + c * 128, B_H[i][:, 0:T], "psH%d" % i)
                    proj(2048 + c * 128, B_H[i][:, T:2 * T], "psH%d" % i)
                    p.act(lambda e, i=i: e.activation(hxs[i][:], B_H[i][:, 0:T], AF.Identity), reads=["psH%d" % i], writes=["hxs0"])
                    p.dve(lambda e, i=i: e.tensor_tensor(zs[i][:], B_H[i][:, T:2 * T], hxs[i][:], ALU.mult), reads=["psH%d" % i, "hxs0"], writes=["hxs0"])
                    p.dma("sp", "zst%d" % i, lambda e, i=i, c=c, t0=t0: e.dma_start(out=PT_d[c, :, t0:t0 + T], in_=zs[i][:]), reads=["hxs0"], writes=["PTd"])
                    proj(1536 + c * 128, B_G[:, 0:T], "psG")
                    p.act(lambda e, i=i: e.activation(bgs[i][:], B_G[:, 0:T], AF.Identity), reads=["psG"], writes=["bgs0"])
                    p.dma("sp", "bst%d" % i, lambda e, i=i, c=c, t0=t0: e.dma_start(out=BG_d[c, :, t0:t0 + T], in_=bgs[i][:]), reads=["bgs0"], writes=["BGd"])
                yb = yc[b % 2]
                yk = "yc%d" % (b % 2)
                for n in range(2):
                    def vmm(e, n=n, aT=aT):
                        for g in range(4):
                            for k in range(8):
                                ins = e.matmul(B_V[:, g * 128:(g + 1) * 128], aT[:, k, n * 128:(n + 1) * 128], w1[:, k, 512 + g * 128:512 + (g + 1) * 128],
                                               start=(k == 0), stop=(k == 7))
                        return ins
                    p.pe(vmm, reads=["w1"] + akeys, writes=["psV"])
                    p.act(lambda e: e.activation(sqv[:], B_V, AF.Square), reads=["psV"], writes=["sqv"])
                    p.dve(lambda e: e.tensor_reduce(ssum[:], sqv[:].rearrange("p (g c) -> p g c", c=128), AX.X, ALU.add), reads=["sqv"], writes=["ssum0"])
                    p.act(lambda e: e.activation(ssum[:], ssum[:], AF.Sqrt, bias=EPSB[:, 0:1], scale=1.0 / 128.0), reads=["ssum0"], writes=["ssum1"])
                    p.dve(lambda e: e.reciprocal(ssum[:], ssum[:]), reads=["ssum1"], writes=["ssum"])
                    p.dve(lambda e: e.tensor_tensor(vt[:].rearrange("p (g c) -> p g c", c=128), B_V.rearrange("p (g c) -> p g c", c=128), ssum[:].unsqueeze(2).to_broadcast([128, 4, 128]), ALU.mult),
                          reads=["psV", "ssum", "sqv"], writes=["sqv"])
                    p.pool(lambda e: e.tensor_tensor(vn[:].rearrange("p g c -> p (g c)"), vt[:], sggain[:], ALU.mult), reads=["sqv", "sggain"], writes=["vn"])

                    def smm(e):
                        for g in range(4):
                            ins = e.matmul(B_S[:, g * 128:(g + 1) * 128], vn[:, g, :], sgwT[:, g * 128:(g + 1) * 128], start=True, stop=True)
                        return ins
                    p.pe(smm, reads=["vn", "sgwT"], writes=["psS"])
                    p.dve(lambda e: e.tensor_tensor(st_[:], B_S.rearrange("p (g c) -> p g c", c=128), sgbb[:].rearrange("p (g c) -> p g c", c=128), ALU.add),
                          reads=["psS", "sgbb"], writes=["st"])
                    p.pool(lambda e, n=n, yb=yb: e.tensor_tensor(yb[:, :, n * 128:(n + 1) * 128], st_[:], usb[:, :, n * 128:(n + 1) * 128], ALU.mult),
                           reads=["st", "usb0", "usb1"], writes=[yk + "_%d" % n])
                p.dma("sp", "yst%d" % (b % 2), lambda e, yb=yb, t0=t0: e.dma_start(out=MIX_d[0:4, :, t0:t0 + T].rearrange("c p t -> p c t"), in_=yb[:]),
                      reads=[yk + "_0", yk + "_1"], writes=["MIXd"])
            ph.done()
            ph = Phase(nc)
            p = ph.p
            W = S + 2
            Z = ph.sb([128, W], F32)
            BGr = ph.sb([128, S], F32)
            t1 = ph.sb([128, S], F32)
            yo = ph.sb([128, S], BF16)
            cw = ph.sb([128, 12], F32)
            p.dma("sp", "cw", lambda e: e.dma_start(out=cw[:], in_=convw_d[:, :]), writes=["cw"])
            p.dve(lambda e: e.memset(Z[:, 0:1], 0.0), writes=["ZL"])
            p.dve(lambda e: e.memset(Z[:, W - 1:W], 0.0), writes=["ZR"])
            for c in range(4):
                p.dma("sp", "z", lambda e, c=c: e.dma_start(out=Z[:, 1:1 + S], in_=PT_d[c, :, :]), writes=["Z"])
                p.dma("sp", "bg", lambda e, c=c: e.dma_start(out=BGr[:], in_=BG_d[c, :, :]), writes=["BGr"])
                p.dve(lambda e, c=c: e.tensor_scalar(t1[:], Z[:, 1:1 + S], cw[:, c * 3 + 1:c * 3 + 2], None, op0=ALU.mult), reads=["Z", "cw"], writes=["t1a"])
                p.dve(lambda e, c=c: e.scalar_tensor_tensor(t1[:], Z[:, 0:S], cw[:, c * 3:c * 3 + 1], t1[:], op0=ALU.mult, op1=ALU.add),
                      reads=["Z", "ZL", "cw", "t1a"], writes=["t1b"])
                p.dve(lambda e, c=c: e.scalar_tensor_tensor(t1[:], Z[:, 2:2 + S], cw[:, c * 3 + 2:c * 3 + 3], t1[:], op0=ALU.mult, op1=ALU.add),
                      reads=["Z", "ZR", "cw", "t1b"], writes=["t1c"])
                p.pool(lambda e: e.tensor_tensor(yo[:], t1[:], BGr[:], ALU.mult), reads=["t1c", "BGr"], writes=["yo"])
                p.dma("sp", "yo", lambda e, c=c: e.dma_start(out=MIX_d[4 + c, :, :], in_=yo[:]), reads=["yo"], writes=["MIXd"])
            ph.done()
            wout_phase(wout1_d, 1)

        if tail_only:
            router_moe(1)
        else:
            if stage >= 1:
                layer0_mixer()
            if stage >= 2 and stage < 20:
                router_moe(0)
            if stage == 9:
                router_moe(1)
                layer1_mixer()
            if stage >= 3 and stage < 9 and stage != 5:
                layer1_mixer()
            if stage >= 4 and stage < 9 and stage != 6:
                router_moe(1)
        if stage == 12:
            ph = Phase(nc)
            big = ph.sb([128, 4096], F32)
            for i_ in range(4000):
                ph.p.dve(lambda e: e.memset(big[:], 1.0), writes=["big"])
            ph.done()
        if stage == 6:
            for _ in range(3):
                ph = Phase(nc)
                ph.p.dve(lambda e: e.memset(EPSB[:], EPS), writes=["epsb"])
                ph.done()
        store_phase()
    return nc


def _perm64():
    d = np.arange(64)
    return np.where((d % 32) < 16, d + 16, d - 16)


def prep_inputs(inputs):
    f = lambda a: np.ascontiguousarray(np.asarray(a, dtype=np.float32))
    I = {k: np.asarray(v) for k, v in inputs.items()}
    shared = {}
    shared["ident"] = np.eye(128, dtype=np.float32)
    shared["mod_w"] = f(I["mod_w"])
    shared["mod_bT"] = f(I["mod_b"].reshape(2, 48, 128).transpose(2, 0, 1).reshape(128, 96))
    ng = np.stack([I["norm1_g"], I["norm2_g"]], 0)
    shared["norm_g"] = f(ng.reshape(2, 2, 8, 128).transpose(3, 0, 1, 2).reshape(128, 32))
    w_in0 = I["even_w_in"][0]
    pi = _perm64()
    qcols, qpcols = [], []
    for j in range(4):
        for h in (j, j + 4):
            qcols.append(h * 64 + np.arange(64))
            qpcols.append(h * 64 + pi)
    qcols = np.concatenate(qcols)
    qpcols = np.concatenate(qpcols)
    kcols = 512 + np.arange(128)
    kpcols = 512 + np.concatenate([pi, 64 + pi])
    pcols = 768 + np.arange(512)
    allc = np.concatenate([qcols, kcols, pcols, qpcols, kpcols])
    shared["w_qkp"] = f(w_in0[:, allc])
    shared["w_v"] = f(w_in0[:, 640:768])
    qg = I["q_gain"][0]
    kg = I["k_gain"][0]
    d = np.arange(128) % 64
    shared["gains"] = f(np.stack([qg[d], qg[pi[d]], kg[d], kg[pi[d]]], 1))
    t = np.arange(S)
    row = (t // 64).astype(np.float32)
    col = (t % 64).astype(np.float32)
    inv = (10000.0 ** (-np.arange(0, 16, dtype=np.float32) * 2 / 32.0)).astype(np.float32)
    cos_t = np.zeros((128, S), np.float32)
    sin_t = np.zeros((128, S), np.float32)
    for pp in range(128):
        dd = pp % 64
        jj = dd % 16
        pos = row if dd < 32 else col
        ang = (pos * inv[jj]).astype(np.float32)
        sgn = -1.0 if (dd % 32) < 16 else 1.0
        cos_t[pp] = np.cos(ang)
        sin_t[pp] = sgn * np.sin(ang)
    shared["cos_t"] = cos_t
    shared["sin_t"] = sin_t
    shared["pool_wT"] = f(I["pool_w"][0].transpose(1, 0, 2).reshape(128, 512))
    shared["pool_sc"] = f(I["pool_scale"][0].reshape(4, 128).T)
    edge = np.zeros((128, 4, 16), np.float32)
    for g, w in enumerate((2, 4, 8, 16)):
        for jx in range(16):
            tpos = jx if jx < 8 else S - 16 + jx
            lo = max(tpos - w // 2, 0)
            hi = min(tpos + w - w // 2, S)
            edge[:, g, jx] = 1.0 / (hi - lo)
    shared["pool_edge"] = edge.reshape(128, 64)
    w_out0 = I["even_w_out"][0]
    rows = []
    for c in range(4):
        for h in (c, c + 4):
            rows.append(h * 64 + np.arange(64))
    rows.append(512 + np.arange(512))
    shared["w_out0"] = f(w_out0[np.concatenate(rows), :])
    shared["w_out1"] = f(I["odd_w_out"][0])
    shared["w_in1"] = f(I["odd_w_in"][0])
    shared["sg_gain_b"] = f(np.broadcast_to(I["sg_gain"][0].reshape(1, 512), (128, 512)))
    shared["sg_wT"] = f(I["sg_w"][0].transpose(2, 0, 1).reshape(128, 512))
    shared["sg_b_b"] = f(np.broadcast_to(I["sg_b"][0].reshape(1, 512), (128, 512)))
    shared["conv_wT"] = f(I["conv_w"][0][:, 0, :].reshape(3, 4, 128).transpose(2, 1, 0).reshape(128, 12))
    shared["rw"] = f(np.concatenate([I["router_g_w"], I["router_e_w"]], axis=2))
    rb = np.concatenate([I["router_g_b"], I["router_e_b"]], axis=1)
    shared["rb_b"] = f(np.broadcast_to(np.tile(rb[:, None, :], (1, 4, 1)).reshape(1, 160), (128, 160)))
    shared["w_gate"] = f(I["w_gate"])
    shared["w_up"] = f(I["w_up"])
    shared["w_down"] = f(I["w_down"])
    sel = np.zeros((32, 16, 128), np.float32)
    for ex in range(16):
        sel[ex, ex, :] = 1.0
        sel[16 + ex, ex, :] = 1.0
    shared["sel"] = sel.reshape(32, 2048)
    in_maps = []
    for b in range(NCORES):
        m = dict(shared)
        m["x"] = f(I["x"][b])
        m["ctx"] = f(I["ctx"][b])
        cv = np.stack([I["c"][b], I["c_ctx"]], 0)
        m["cvec"] = f(cv.reshape(2, 8, 128).transpose(2, 1, 0).reshape(128, 16))
        in_maps.append(m)
    return in_maps


_NC_CACHE = {}


def kernel(**inputs):
    in_maps = prep_inputs(inputs)
    if "nc" not in _NC_CACHE:
        _NC_CACHE["nc"] = (build(stage=3), build(tail_only=True))
    nc1, nc2 = _NC_CACHE["nc"]
    res = run_bass_kernel_spmd(nc1, in_maps, core_ids=list(range(NCORES)))
    for b in range(NCORES):
        in_maps[b]["x"] = np.ascontiguousarray(np.asarray(res.results[b]["out"], dtype=np.float32))
    res = run_bass_kernel_spmd(nc2, in_maps, core_ids=list(range(NCORES)))
    out = np.stack([np.asarray(r["out"]) for r in res.results], axis=0)
    return out.astype(np.float32)
```

```python
import numpy as np
from contextlib import ExitStack
import concourse.bass as bass
import concourse.mybir as mybir
from concourse.bass_utils import run_bass_kernel_spmd

F32 = mybir.dt.float32
BF16 = mybir.dt.bfloat16
AF = mybir.ActivationFunctionType
ALU = mybir.AluOpType
AX = mybir.AxisListType

ENGS = ["pe", "act", "dve", "pool", "sp"]
S = 4096
D = 1024
NCORES = 8
EPS = 1e-6
_CNT = [0]
_PHASE = [0]
_SEMPOOL = [[], []]
NSEM = 16


class Op:
    __slots__ = ("eng", "fn", "reads", "writes", "dma_key", "idx", "waits", "signal", "semval")

    def __init__(self, eng, fn, reads, writes, dma_key):
        self.eng = eng
        self.fn = fn
        self.reads = reads
        self.writes = writes
        self.dma_key = dma_key
        self.waits = []
        self.signal = False
        self.semval = 0


class Prog:
    def __init__(self, nc):
        self.nc = nc
        self.ops = []

    def op(self, eng, fn, reads=(), writes=(), dma_key=None):
        reads = tuple(reads)
        writes = tuple(writes)
        ex = tuple(r for r in reads if r.startswith("ps"))
        o = Op(eng, fn, reads, writes + ex, dma_key)
        o.idx = len(self.ops)
        self.ops.append(o)
        return o

    def pe(self, fn, reads=(), writes=()):
        return self.op("pe", fn, reads, writes)

    def act(self, fn, reads=(), writes=()):
        return self.op("act", fn, reads, writes)

    def dve(self, fn, reads=(), writes=()):
        return self.op("dve", fn, reads, writes)

    def pool(self, fn, reads=(), writes=()):
        return self.op("pool", fn, reads, writes)

    def dma(self, eng, key, fn, reads=(), writes=()):
        return self.op(eng, fn, reads, writes, dma_key=key)

    def finalize(self):
        ops = self.ops

        def tl(o):
            return ("dma", o.dma_key) if o.dma_key is not None else o.eng

        pos = {}
        cnt = {}
        for o in ops:
            t = tl(o)
            cnt[t] = cnt.get(t, 0) + 1
            pos[o.idx] = cnt[t]
        last_writer = {}
        readers = {}
        known = {e: {} for e in ENGS}
        done_clock = {}
        needed = set()
        latest_on_key = {}
        for o in ops:
            deps = set()
            raw = set()
            for r in o.reads:
                w = last_writer.get(r)
                if w is not None:
                    deps.add(w)
                    raw.add(w)
            for r in o.writes:
                w = last_writer.get(r)
                if w is not None:
                    deps.add(w)
                    if r.startswith("ps"):
                        raw.add(w)
                for rd in readers.get(r, ()):
                    deps.add(rd)
            req = {}
            for d in deps:
                if d == o.idx:
                    continue
                po = ops[d]
                t = tl(po)
                if po.dma_key is None and o.dma_key is None and po.eng == o.eng:
                    if o.eng == "pe":
                        continue
                    if d not in raw:
                        continue
                if t not in req or pos[d] > pos[req[t]]:
                    req[t] = d
            kn = known[o.eng]
            for t in list(req.keys()):
                if not isinstance(t, str):
                    req[t] = latest_on_key[t]
            waits = []
            for t, d in req.items():
                if kn.get(t, 0) >= pos[d]:
                    continue
                waits.append(d)
                needed.add(d)
                for t2, p2 in done_clock[d].items():
                    if kn.get(t2, 0) < p2:
                        kn[t2] = p2
            o.waits = waits
            dc = dict(kn)
            dc[tl(o)] = max(dc.get(tl(o), 0), pos[o.idx])
            done_clock[o.idx] = dc
            if o.dma_key is not None:
                latest_on_key[tl(o)] = o.idx
            for r in o.writes:
                last_writer[r] = o.idx
                readers[r] = []
            for r in o.reads:
                if r not in o.writes:
                    readers.setdefault(r, []).append(o.idx)
        semcnt = {}
        for o in ops:
            t = tl(o)
            if o.dma_key is not None:
                semcnt[t] = semcnt.get(t, 0) + 16
                o.signal = True
                o.semval = semcnt[t]
            elif o.idx in needed:
                semcnt[t] = semcnt.get(t, 0) + 1
                o.signal = True
                o.semval = semcnt[t]
        self.timelines = sorted(set(tl(o) for o in ops if o.signal), key=str)
        self._tl = tl
        return self

    def emit(self):
        nc = self.nc
        ops = self.ops
        tl = self._tl
        with ExitStack() as es:
            pidx = _PHASE[0] % 2
            _PHASE[0] += 1
            mypool = _SEMPOOL[pidx]
            other = _SEMPOOL[1 - pidx]
            assert len(self.timelines) <= len(mypool), len(self.timelines)
            sems = {}
            for i, t in enumerate(self.timelines):
                sems[t] = mypool[i]
            block = es.enter_context(nc.Block())
            by_eng = {e: [o for o in ops if o.eng == e] for e in ENGS}
            final_dma = {}
            for o in ops:
                if o.dma_key is not None:
                    final_dma[tl(o)] = o.semval

            def run(engname, eng):
                for o in by_eng[engname]:
                    ws = list(o.waits)
                    att = None
                    if ws and engname != "pe":
                        att = ws.pop()
                    for d in ws:
                        po = ops[d]
                        eng.wait_ge(sems[tl(po)], po.semval)
                    if att is not None:
                        rec = _Rec(eng)
                        ins = o.fn(rec)
                        po = ops[att]
                        rec.first._wait_ge(sems[tl(po)], po.semval)
                    else:
                        ins = o.fn(eng)
                    if o.signal:
                        ins.then_inc(sems[tl(o)], 16 if o.dma_key is not None else 1)
                if engname == "sp":
                    for t, v in final_dma.items():
                        eng.wait_ge(sems[t], v)
                    for sm in other:
                        eng.sem_clear(sm)

            @block.tensor
            def _(eng):
                run("pe", eng)

            @block.scalar
            def _(eng):
                run("act", eng)

            @block.vector
            def _(eng):
                run("dve", eng)

            @block.gpsimd
            def _(eng):
                run("pool", eng)

            @block.sync
            def _(eng):
                run("sp", eng)


class _Rec:
    def __init__(self, eng):
        self._eng = eng
        self.first = None

    def __getattr__(self, name):
        f = getattr(self._eng, name)

        def g(*a, **k):
            r = f(*a, **k)
            if self.first is None:
                self.first = r
            return r
        return g


class Phase:
    def __init__(self, nc):
        self.nc = nc
        self.es = ExitStack()
        self.p = Prog(nc)
        self._n = 0

    def sb(self, shape, dt):
        self._n += 1
        _CNT[0] += 1
        return self.es.enter_context(self.nc.sbuf_tensor("sb%d" % _CNT[0], list(shape), dt))

    def psum4(self):
        r = []
        for _ in range(4):
            _CNT[0] += 1
            r.append(self.es.enter_context(self.nc.psum_tensor("ps%d" % _CNT[0], [128, 1024], F32)))
        return r

    def done(self):
        self.p.finalize()
        self.p.emit()
        self.es.close()


def build(stage=4, tail_only=False):
    nc = bass.Bass("TRN2", target_bir_lowering=False)

    def din(name, shape, dt=F32):
        return nc.dram_tensor(name, list(shape), dt, kind="ExternalInput").ap()

    def dscr(name, shape, dt):
        return nc.dram_tensor(name, list(shape), dt, kind="Internal").ap()

    x_d = din("x", [S, D])
    ctx_d = din("ctx", [256, D])
    cvec_d = din("cvec", [128, 16])
    ident_d = din("ident", [128, 128])
    modw_d = din("mod_w", [2, D, 6144])
    modb_d = din("mod_bT", [128, 96])
    ng_d = din("norm_g", [128, 32])
    wqkp_d = din("w_qkp", [D, 1792])
    wv_d = din("w_v", [D, 128])
    gains_d = din("gains", [128, 4])
    cos_d = din("cos_t", [128, S])
    sin_d = din("sin_t", [128, S])
    poolw_d = din("pool_wT", [128, 512])
    poolsc_d = din("pool_sc", [128, 4])
    pooledge_d = din("pool_edge", [128, 64])
    wout0_d = din("w_out0", [D, D])
    wout1_d = din("w_out1", [D, D])
    win1_d = din("w_in1", [D, 2560])
    sggain_d = din("sg_gain_b", [128, 512])
    sgw_d = din("sg_wT", [128, 512])
    sgb_d = din("sg_b_b", [128, 512])
    convw_d = din("conv_wT", [128, 12])
    rw_d = din("rw", [2, D, 20])
    rb_d = din("rb_b", [128, 160])
    wg_d = din("w_gate", [2, 16, D, 256])
    wu_d = din("w_up", [2, 16, D, 256])
    wd_d = din("w_down", [2, 16, 256, D])
    sel_d = din("sel", [32, 2048])
    out_d = nc.dram_tensor("out", [S, D], F32, kind="ExternalOutput").ap()

    A2_d = dscr("A2s", [8, 128, 8 * 512], BF16)
    QT_d = dscr("QTs", [4, 128, S], BF16)
    MIX_d = dscr("MIXs", [8, 128, S], BF16)
    PT_d = dscr("PTs", [4, 128, S], F32)
    BG_d = dscr("BGs", [4, 128, S], F32)
    KT_d = dscr("KTs", [128, 4352], BF16)
    VS_d = dscr("VSs", [128, 34, 193], BF16)

    with ExitStack() as top:
        def psb(shape, dt):
            _CNT[0] += 1
            return top.enter_context(nc.sbuf_tensor("pt%d" % _CNT[0], list(shape), dt))

        _PHASE[0] = 0
        for pi_ in range(2):
            _SEMPOOL[pi_] = []
            for si_ in range(NSEM):
                _SEMPOOL[pi_].append(top.enter_context(nc.semaphore("sp%d_%d" % (pi_, si_))))
        HT = psb([128, 8, S], F32)
        ident = psb([128, 128], F32)
        ident_bf = psb([128, 128], BF16)
        ones_bf = psb([128, 128], BF16)
        onesblk_bf = psb([128, 128], BF16)
        ones_f = psb([128, 128], F32)
        MOD = psb([128, 2, 48], F32)
        MODC = psb([128, 16], F32)
        NG = psb([128, 32], F32)
        GS = psb([128, 4, 8], F32)
        GSC = psb([128, 8], F32)
        EPSB = psb([128, 1], F32)

        ph = Phase(nc)
        p = ph.p
        PS = ph.psum4()
        cvec = ph.sb([128, 16], F32)
        scv = ph.sb([128, 16], F32)
        modb = ph.sb([128, 96], F32)
        mrow = ph.sb([2, 6144], F32)
        mwbuf = [ph.sb([128, 8, 512], F32) for _ in range(2)]
        xin = [ph.sb([128, D], F32) for _ in range(2)]
        p.dma("sp", "c0", lambda e: e.dma_start(out=ident[:], in_=ident_d[:, :]), writes=["ident"])
        p.dma("sp", "c0", lambda e: e.dma_start(out=cvec[:], in_=cvec_d[:, :]), writes=["cvec"])
        p.dma("sp", "c0", lambda e: e.dma_start(out=modb[:], in_=modb_d[:, :]), writes=["modb"])
        p.dma("sp", "c0", lambda e: e.dma_start(out=NG[:], in_=ng_d[:, :]), writes=["NG"])
        p.dve(lambda e: e.tensor_copy(ident_bf[:], ident[:]), reads=["ident"], writes=["ident_bf"])
        p.dve(lambda e: e.memset(ones_bf[:], 1.0 / 1024.0), writes=["ones_bf"])
        p.dve(lambda e: e.memset(ones_f[:], 1.0), writes=["ones_f"])
        p.dve(lambda e: e.memset(EPSB[:], EPS), writes=["epsb"])
        p.dve(lambda e: e.memset(onesblk_bf[:], 0.0), writes=["onesblk0"])
        p.dve(lambda e: e.memset(onesblk_bf[0:64, 0:64], 1.0 / 64.0), reads=["onesblk0"], writes=["onesblk1"])
        p.dve(lambda e: e.memset(onesblk_bf[64:128, 64:128], 1.0 / 64.0), reads=["onesblk1"], writes=["onesblk"])
        p.act(lambda e: e.activation(scv[:], cvec[:], AF.Silu), reads=["cvec"], writes=["scv"])
        for l in range(2):
            for fb in range(12):
                it = l * 12 + fb
                buf = mwbuf[it % 2]
                bk = "mw%d" % (it % 2)
                p.dma("sp", bk, lambda e, buf=buf, l=l, fb=fb: e.dma_start(
                    out=buf[:], in_=modw_d[l, :, fb * 512:(fb + 1) * 512].rearrange("(c p) n -> p c n", p=128)),
                    writes=[bk])
                pb = "psA%d" % (it % 2)
                pst = PS[0][:, (it % 2) * 512:(it % 2) * 512 + 512]

                def mm(e, buf=buf, pst=pst):
                    for c in range(8):
                        ins = e.matmul(pst[0:2, :], scv[:, c * 2:c * 2 + 2], buf[:, c, :], start=(c == 0), stop=(c == 7))
                    return ins
                p.pe(mm, reads=[bk, "scv"], writes=[pb])
                if it % 2 == 0:
                    p.act(lambda e, pst=pst, fb=fb: e.activation(mrow[0:2, fb * 512:(fb + 1) * 512], pst[0:2, :], AF.Identity),
                          reads=[pb], writes=["mrow_%d" % fb])
                else:
                    p.dve(lambda e, pst=pst, fb=fb: e.tensor_copy(mrow[0:2, fb * 512:(fb + 1) * 512], pst[0:2, :]),
                          reads=[pb], writes=["mrow_%d" % fb])

            def tr(e):
                for j in range(48):
                    ins = e.matmul(PS[1][:, j * 2:j * 2 + 2], mrow[0:2, j * 128:(j + 1) * 128], ident[0:2, 0:2], start=True, stop=True)
                return ins
            p.pe(tr, reads=["mrow_%d" % fb for fb in range(12)] + ["ident"], writes=["psB0"])
            p.dve(lambda e, l=l: e.tensor_tensor(MOD[:, l, :], PS[1][:, 0:96].rearrange("p (j t) -> p j t", t=2)[:, :, 0],
                                                modb[:, l * 48:(l + 1) * 48], ALU.add),
                  reads=["psB0", "modb"], writes=["MOD%d" % l])
            if l == 0:
                p.dve(lambda e: e.tensor_tensor(MODC[:], PS[1][:, 0:32].rearrange("p (j t) -> p j t", t=2)[:, :, 1],
                                                modb[:, 0:16], ALU.add),
                      reads=["psB0", "modb"], writes=["MODC"])
        for l in range(2):
            for n in range(2):
                sc = MOD[:, l, (1 + 3 * n) * 8:(2 + 3 * n) * 8]
                p.dve(lambda e, l=l, n=n, sc=sc: e.scalar_tensor_tensor(GS[:, l * 2 + n, :], sc, 1.0, NG[:, n * 16 + l * 8:n * 16 + l * 8 + 8],
                                                                      op0=ALU.add, op1=ALU.mult),
                      reads=["MOD%d" % l, "NG"], writes=["GS%d%d" % (l, n)])
        p.dve(lambda e: e.scalar_tensor_tensor(GSC[:], MODC[:, 8:16], 1.0, NG[:, 0:8], op0=ALU.add, op1=ALU.mult),
              reads=["MODC", "NG"], writes=["GSC"])
        for tt in range(32):
            xb = xin[tt % 2]
            xk = "xin%d" % (tt % 2)
            p.dma("sp", xk, lambda e, xb=xb, tt=tt: e.dma_start(out=xb[:], in_=x_d[tt * 128:(tt + 1) * 128, :]), writes=[xk])
            pst = PS[2 + tt % 2]
            pk = "psX%d" % (tt % 2)

            def trx(e, xb=xb, pst=pst):
                for c in range(8):
                    ins = e.matmul(pst[:, c * 128:(c + 1) * 128], xb[:, c * 128:(c + 1) * 128], ident[:], start=True, stop=True)
                return ins
            p.pe(trx, reads=[xk, "ident"], writes=[pk])
            dst = HT[:, :, tt * 128:(tt + 1) * 128]
            src = pst[:, :].rearrange("p (c t) -> p c t", t=128)
            if tt % 2 == 0:
                p.act(lambda e, dst=dst, src=src: e.activation(dst, src, AF.Identity), reads=[pk], writes=["HT%d" % tt])
            else:
                p.dve(lambda e, dst=dst, src=src: e.tensor_copy(dst, src), reads=[pk], writes=["HT%d" % tt])
        ph.done()

        def norm_block(p, PSst, pskey, t0, T, gs, sh, sqb, rstd, tmpb, dst_fn, tag, src=None, srckey="HT"):
            srcT = HT if src is None else src
            for c in range(8):
                sq = sqb[c % 2]
                p.act(lambda e, sq=sq, c=c: e.activation(sq[:, 0:T], srcT[:, c, t0:t0 + T], AF.Square),
                      reads=[srckey], writes=["sq%s%d" % (tag, c % 2)])
                p.pe(lambda e, sq=sq, c=c: e.matmul(PSst[:, 0:T], ones_bf[:], sq[:, 0:T], start=(c == 0), stop=(c == 7)),
                     reads=["sq%s%d" % (tag, c % 2), "ones_bf"], writes=[pskey])
            p.act(lambda e: e.activation(rstd[:, 0:T], PSst[:, 0:T], AF.Sqrt, bias=EPSB[:, 0:1]), reads=[pskey], writes=["rstd0" + tag])
            p.dve(lambda e: e.reciprocal(rstd[:, 0:T], rstd[:, 0:T]), reads=["rstd0" + tag], writes=["rstd" + tag])
            for c in range(8):
                tb = tmpb[c % 2]
                p.dve(lambda e, tb=tb, c=c: e.scalar_tensor_tensor(tb[:, 0:T], srcT[:, c, t0:t0 + T], gs[:, c:c + 1], rstd[:, 0:T],
                                                                  op0=ALU.mult, op1=ALU.mult),
                      reads=[srckey, "rstd" + tag], writes=["tmp%s%d" % (tag, c % 2)])
                dst, dkey = dst_fn(c)
                p.act(lambda e, tb=tb, c=c, dst=dst: e.activation(dst, tb[:, 0:T], AF.Identity, bias=sh[:, c:c + 1]),
                      reads=["tmp%s%d" % (tag, c % 2)], writes=[dkey])

        def wout_phase(wout_d, l):
            ph = Phase(nc)
            p = ph.p
            PS = ph.psum4()
            w = ph.sb([128, 8, D], BF16)
            mixb = [ph.sb([128, 8, 512], BF16) for _ in range(2)]
            p.dma("pool", "w", lambda e: e.dma_start(out=w[:], in_=wout_d.rearrange("(c p) n -> p c n", p=128)), writes=["w"])
            for b in range(8):
                mb = mixb[b % 2]
                mk = "mix%d" % (b % 2)
                p.dma("sp", mk, lambda e, mb=mb, b=b: e.dma_start(out=mb[:], in_=MIX_d[:, :, b * 512:(b + 1) * 512].rearrange("c p t -> p c t")),
                      writes=[mk])
                for f in range(8):
                    pst = PS[f % 4][:, 0:512]
                    pk = "psY%d" % (f % 4)

                    def mm(e, mb=mb, f=f, pst=pst):
                        for k in range(8):
                            ins = e.matmul(pst, w[:, k, f * 128:(f + 1) * 128], mb[:, k, :], start=(k == 0), stop=(k == 7))
                        return ins
                    p.pe(mm, reads=["w", mk], writes=[pk])
                    hsl = HT[:, f, b * 512:(b + 1) * 512]
                    p.dve(lambda e, pst=pst, f=f, hsl=hsl: e.scalar_tensor_tensor(hsl, pst, MOD[:, l, 16 + f:17 + f], hsl, op0=ALU.mult, op1=ALU.add),
                          reads=[pk], writes=["HT"])
            ph.done()

        def router_moe(l):
            with ExitStack() as rstack:
                _CNT[0] += 1
                combT = rstack.enter_context(nc.sbuf_tensor("combT%d" % _CNT[0], [32, S], BF16))
                router_moe_inner(l, combT)

        def router_moe_inner(l, combT):
            ph = Phase(nc)
            p = ph.p
            PS = ph.psum4()
            sqb = [ph.sb([128, 512], BF16) for _ in range(2)]
            rstd = ph.sb([128, 512], F32)
            tmpb = [ph.sb([128, 512], F32) for _ in range(2)]
            a2f = ph.sb([128, 8, 512], F32)
            a2b = [ph.sb([128, 8, 512], BF16) for _ in range(2)]
            rw = ph.sb([128, 8, 20], F32)
            rbb = ph.sb([128, 80], F32)
            lgT = ph.sb([32, 512], F32)
            LG = ph.sb([128, 32, 20], F32)
            p.dma("sp", "rw", lambda e: e.dma_start(out=rw[:], in_=rw_d[l].rearrange("(c p) n -> p c n", p=128)), writes=["rw"])
            p.dma("sp", "rw", lambda e: e.dma_start(out=rbb[:], in_=rb_d[:, l * 80:(l + 1) * 80]), writes=["rbb"])
            gs = GS[:, l * 2 + 1, :]
            sh = MOD[:, l, 24:32]
            for b in range(8):
                af = a2f
                ab = a2b[b % 2]
                abk = "a2b%d" % (b % 2)
                norm_block(p, PS[0], "psS", b * 512, 512, gs, sh, sqb, rstd, tmpb,
                           lambda c, af=af: (af[:, c, :], "a2f_%d" % c), "n")
                akeys = ["a2f_%d" % c for c in range(8)]
                p.pool(lambda e, af=af, ab=ab: e.tensor_copy(ab[:], af[:]), reads=akeys, writes=[abk])
                p.dma("sp", "a2st%d" % (b % 2), lambda e, ab=ab, b=b: e.dma_start(
                    out=A2_d[b, :, :], in_=ab[:].rearrange("p c t -> p (c t)")), reads=[abk], writes=["A2"])

                def rmm(e, af=af):
                    for c in range(8):
                        ins = e.matmul(PS[1][0:20, 0:512], rw[:, c, :], af[:, c, :], start=(c == 0), stop=(c == 7))
                    return ins
                p.pe(rmm, reads=["rw"] + akeys, writes=["psR"])
                p.act(lambda e: e.activation(lgT[0:20, :], PS[1][0:20, 0:512], AF.Identity), reads=["psR"], writes=["lgT"])

                def rtr(e):
                    for tt in range(4):
                        ins = e.matmul(PS[2][:, tt * 32:tt * 32 + 20], lgT[0:20, tt * 128:(tt + 1) * 128], ident[0:20, 0:20], start=True, stop=True)
                    return ins
                p.pe(rtr, reads=["lgT", "ident"], writes=["psT"])
                p.dve(lambda e, b=b: e.tensor_tensor(LG[:, b * 4:(b + 1) * 4, :], PS[2][:, 0:128].rearrange("p (t n) -> p t n", n=32)[:, :, 0:20],
                                                    rbb[:].rearrange("p (t n) -> p t n", n=20), ALU.add),
                      reads=["psT", "rbb"], writes=["LG"])
            NT = 32
            RT = ph.sb([128, 2880], F32)
            _o = [0]

            def rt2():
                a = _o[0]
                _o[0] += NT
                return RT[:, a:a + NT]

            def rt3():
                a = _o[0]
                _o[0] += NT * 4
                return RT[:, a:a + NT * 4].rearrange("p (n g) -> p n g", g=4)

            def rt4():
                a = _o[0]
                _o[0] += NT * 16
                return RT[:, a:a + NT * 16].rearrange("p (n g i) -> p n g i", g=4, i=4)

            def rt16():
                a = _o[0]
                _o[0] += NT * 16
                return RT[:, a:a + NT * 16].rearrange("p (n k) -> p n k", k=16)
            gmax = rt2()
            ohg = rt3()
            gd = rt3()
            gsum = rt2()
            gp = rt2()
            t44 = rt4()
            esel = rt3()
            e1 = rt2()
            sel1 = rt3()
            em = rt3()
            e2 = rt2()
            sel2 = rt3()
            dd = rt2()
            w1 = rt2()
            w2 = rt2()
            ce = rt3()
            ce2 = rt3()
            comb = rt16()
            chf = rt16()
            assert _o[0] <= 2880
            chl = ph.sb([128, NT, 32], BF16)
            gl = LG[:, :, 0:4]
            el = LG[:, :, 4:20].rearrange("p n (g i) -> p n g i", i=4)

            def b3(ap2):
                return ap2.unsqueeze(2).to_broadcast([128, NT, 4])
            p.dve(lambda e: e.tensor_reduce(gmax[:], gl, AX.X, ALU.max), reads=["LG"], writes=["gmax"])
            p.dve(lambda e: e.tensor_tensor(ohg[:], gl, b3(gmax[:]), ALU.is_equal), reads=["LG", "gmax"], writes=["ohg"])
            p.dve(lambda e: e.tensor_tensor(gd[:], gl, b3(gmax[:]), ALU.subtract), reads=["LG", "gmax"], writes=["gd"])
            p.act(lambda e: e.activation(gd[:], gd[:], AF.Exp), reads=["gd"], writes=["ge"])
            p.dve(lambda e: e.tensor_reduce(gsum[:], gd[:], AX.X, ALU.add), reads=["ge"], writes=["gsum"])
            p.dve(lambda e: e.reciprocal(gp[:], gsum[:]), reads=["gsum"], writes=["gp"])
            p.dve(lambda e: e.tensor_tensor(t44[:], el, ohg[:].unsqueeze(3).to_broadcast([128, NT, 4, 4]), ALU.mult),
                  reads=["LG", "ohg"], writes=["t44"])
            p.dve(lambda e: e.tensor_reduce(esel[:], t44[:].rearrange("p n g i -> p n i g"), AX.X, ALU.add), reads=["t44"], writes=["esel"])
            p.dve(lambda e: e.tensor_reduce(e1[:], esel[:], AX.X, ALU.max), reads=["esel"], writes=["e1"])
            p.dve(lambda e: e.tensor_tensor(sel1[:], esel[:], b3(e1[:]), ALU.is_equal), reads=["esel", "e1"], writes=["sel1"])
            p.dve(lambda e: e.scalar_tensor_tensor(em[:], sel1[:], -1e30, esel[:], op0=ALU.mult, op1=ALU.add), reads=["sel1", "esel"], writes=["em"])
            p.dve(lambda e: e.tensor_reduce(e2[:], em[:], AX.X, ALU.max), reads=["em"], writes=["e2"])
            p.dve(lambda e: e.tensor_tensor(sel2[:], em[:], b3(e2[:]), ALU.is_equal), reads=["em", "e2"], writes=["sel2"])
            p.dve(lambda e: e.tensor_tensor(dd[:], e2[:], e1[:], ALU.subtract), reads=["e1", "e2"], writes=["dd"])
            p.act(lambda e: e.activation(dd[:], dd[:], AF.Exp), reads=["dd"], writes=["ex"])
            p.dve(lambda e: e.tensor_scalar(w1[:], dd[:], 1.0, None, op0=ALU.add), reads=["ex"], writes=["w1a"])
            p.dve(lambda e: e.reciprocal(w1[:], w1[:]), reads=["w1a"], writes=["w1"])
            p.dve(lambda e: e.tensor_tensor(w2[:], dd[:], w1[:], ALU.mult), reads=["ex", "w1"], writes=["w2a"])
            p.dve(lambda e: e.tensor_tensor(w1[:], w1[:], gp[:], ALU.mult), reads=["w1", "gp", "w2a"], writes=["wt1"])
            p.dve(lambda e: e.tensor_tensor(w2[:], w2[:], gp[:], ALU.mult), reads=["w2a", "gp"], writes=["wt2"])
            p.dve(lambda e: e.tensor_tensor(ce[:], sel1[:], b3(w1[:]), ALU.mult), reads=["sel1", "wt1"], writes=["ce"])
            p.dve(lambda e: e.tensor_tensor(ce2[:], sel2[:], b3(w2[:]), ALU.mult), reads=["sel2", "wt2"], writes=["ce2"])
            p.dve(lambda e: e.tensor_tensor(ce[:], ce[:], ce2[:], ALU.add), reads=["ce", "ce2"], writes=["cef"])
            p.dve(lambda e: e.tensor_tensor(comb[:].rearrange("p n (g i) -> p n g i", i=4),
                                            ohg[:].unsqueeze(3).to_broadcast([128, NT, 4, 4]),
                                            ce[:].unsqueeze(2).to_broadcast([128, NT, 4, 4]), ALU.mult),
                  reads=["ohg", "cef"], writes=["comb"])
            p.dve(lambda e: e.tensor_copy(chl[:, :, 0:16], comb[:]), reads=["comb"], writes=["chi"])
            p.dve(lambda e: e.tensor_copy(chf[:], chl[:, :, 0:16]), reads=["chi"], writes=["chf"])
            p.dve(lambda e: e.tensor_tensor(chf[:], comb[:], chf[:], ALU.subtract), reads=["comb", "chf"], writes=["clo"])
            p.dve(lambda e: e.tensor_copy(chl[:, :, 16:32], chf[:]), reads=["clo"], writes=["chl"])
            for q in range(8):
                pst = PS[3][:, (q % 2) * 512:(q % 2) * 512 + 512]
                pk = "psC%d" % (q % 2)

                def ctr(e, q=q, pst=pst):
                    for tt in range(4):
                        ins = e.matmul(pst[0:32, tt * 128:(tt + 1) * 128], chl[:, q * 4 + tt, :], ident_bf[:], start=True, stop=True)
                    return ins
                p.pe(ctr, reads=["chl", "chi", "ident_bf"], writes=[pk])
                p.act(lambda e, q=q, pst=pst: e.activation(combT[:, q * 512:(q + 1) * 512], pst[0:32, :], AF.Identity), reads=[pk], writes=["combT"])
            ph.done()
            if stage == 10 + l or (stage == 8 and l == 1):
                return
            ph = Phase(nc)
            p = ph.p
            PS = ph.psum4()
            T = 512
            NB = S // T
            wslot = [(ph.sb([128, 8, 256], BF16), ph.sb([128, 8, 256], BF16), ph.sb([128, 2, D], BF16)) for _ in range(3)]
            a2 = [ph.sb([128, 8, T], BF16) for _ in range(2)]
            sel = ph.sb([32, 2048], BF16)
            cb = ph.sb([128, T], F32)
            sg = [ph.sb([128, T], F32)] * 2
            tt_ = ph.sb([128, T], F32)
            hb0 = [ph.sb([128, 2, T], BF16) for _ in range(2)]
            hb1 = ph.sb([128, 2, T], BF16)
            p.dma("pool", "sel", lambda e: e.dma_start(out=sel[:, 0:1024], in_=sel_d[:, 0:1024]), writes=["sel"])
            p.dma("pool", "sel", lambda e: e.dma_start(out=sel[:, 1024:2048], in_=sel_d[:, 1024:2048]), writes=["sel"])
            g2 = MOD[:, l, 40:48]
            RB = [PS[0][:, 0:512], PS[0][:, 512:1024], PS[1][:, 0:512]]
            RBK = ["psR0", "psR1", "psR2"]
            CBP = PS[1][:, 512:1024]
            ACC = [PS[2][:, 0:512], PS[2][:, 512:1024], PS[3][:, 0:512], PS[3][:, 512:1024]]

            def load_expert(ex):
                sl = ex % 3
                wg, wu, wd = wslot[sl]
                wk = "w%d" % sl
                p.dma("pool", wk, lambda e: e.dma_start(out=wg[:], in_=wg_d[l, ex].rearrange("(c p) n -> p c n", p=128)), writes=[wk + "g"])
                p.dma("pool", wk, lambda e: e.dma_start(out=wu[:], in_=wu_d[l, ex].rearrange("(c p) n -> p c n", p=128)), writes=[wk + "u"])
                p.dma("pool", wk, lambda e: e.dma_start(out=wd[:], in_=wd_d[l, ex].rearrange("(c p) n -> p c n", p=128)), writes=[wk + "d"])
            st = {"step": 0, "rk": 0, "sgi": 0}

            def hbuf(b, j):
                if j == 0:
                    return hb0[b % 2], "h0_%d" % (b % 2)
                return hb1, "h1"

            def G(pr, b, j):
                ex = pr * 2 + j
                sl = ex % 3
                wg, wu, wd = wslot[sl]
                wk = "w%d" % sl
                if j == 0:
                    ab = a2[st["step"] % 2]
                    ak = "a2_%d" % (st["step"] % 2)
                    st["cur"] = (ab, ak)
                    st["step"] += 1
                    p.dma("sp", ak, lambda e: e.dma_start(out=ab[:].rearrange("p c t -> p (c t)"), in_=A2_d[b, :, :]), writes=[ak])
                ab, ak = st["cur"]
                hbb, hk = hbuf(b, j)
                p.pe(lambda e: e.matmul(CBP, sel[:, ex * 128:(ex + 1) * 128], combT[:, b * T:(b + 1) * T], start=True, stop=True),
                     reads=["sel", "combT"], writes=["psCB"])
                p.act(lambda e: e.activation(cb[:], CBP, AF.Identity), reads=["psCB"], writes=["cb"])
                for f2 in range(2):
                    pg = RB[st["rk"] % 3]
                    pgk = RBK[st["rk"] % 3]
                    st["rk"] += 1
                    pu = RB[st["rk"] % 3]
                    puk = RBK[st["rk"] % 3]
                    st["rk"] += 1

                    def gmm(e, pg=pg, f2=f2):
                        for k in range(8):
                            ins = e.matmul(pg, wg[:, k, f2 * 128:(f2 + 1) * 128], ab[:, k, :], start=(k == 0), stop=(k == 7))
                        return ins

                    def umm(e, pu=pu, f2=f2):
                        for k in range(8):
                            ins = e.matmul(pu, wu[:, k, f2 * 128:(f2 + 1) * 128], ab[:, k, :], start=(k == 0), stop=(k == 7))
                        return ins
                    p.pe(gmm, reads=[wk + "g", ak], writes=[pgk])
                    p.pe(umm, reads=[wk + "u", ak], writes=[puk])
                    sgb = sg[st["sgi"] % 2]
                    sgk = "sg0"
                    st["sgi"] += 1
                    p.act(lambda e, sgb=sgb, pg=pg: e.activation(sgb[:], pg, AF.Silu), reads=[pgk], writes=[sgk])
                    p.dve(lambda e, pu=pu: e.tensor_tensor(tt_[:], pu, cb[:], ALU.mult), reads=[puk, "cb"], writes=["tt"])
                    p.pool(lambda e, sgb=sgb, f2=f2: e.tensor_tensor(hbb[:, f2, :], tt_[:], sgb[:], ALU.mult),
                           reads=["tt", sgk], writes=[hk + "_%d" % f2])

            def DOWN(pr, b, half):
                hs = []
                for j in range(2):
                    ex = pr * 2 + j
                    wg, wu, wd = wslot[ex % 3]
                    hbb, hk = hbuf(b, j)
                    hs.append((wd, "w%dd" % (ex % 3), hbb, hk))
                for fi in range(4):
                    f = half * 4 + fi

                    def dmm(e, fi=fi, f=f):
                        for j in range(2):
                            wd, wdk, hbb, hk = hs[j]
                            for k in range(2):
                                ins = e.matmul(ACC[fi], wd[:, k, f * 128:(f + 1) * 128], hbb[:, k, :], start=(j == 0 and k == 0), stop=(j == 1 and k == 1))
                        return ins
                    p.pe(dmm, reads=[hs[0][1], hs[1][1], hs[0][3] + "_0", hs[0][3] + "_1", hs[1][3] + "_0", hs[1][3] + "_1"], writes=["psD%d" % fi])
                for fi in range(4):
                    f = half * 4 + fi
                    hsl = HT[:, f, b * T:(b + 1) * T]
                    p.dve(lambda e, fi=fi, f=f, hsl=hsl: e.scalar_tensor_tensor(hsl, ACC[fi], g2[:, f:f + 1], hsl, op0=ALU.mult, op1=ALU.add),
                          reads=["psD%d" % fi], writes=["HT"])

            load_expert(0)
            load_expert(1)
            for pr in range(8):
                if pr > 0:
                    load_expert(2 * pr + 1)
                G(pr, 0, 0)
                G(pr, 0, 1)
                if pr < 7:
                    load_expert(2 * pr + 2)
                for b in range(NB):
                    DOWN(pr, b, 0)
                    if b + 1 < NB:
                        G(pr, b + 1, 0)
                    DOWN(pr, b, 1)
                    if b + 1 < NB:
                        G(pr, b + 1, 1)
            ph.done()

        def store_phase():
            ph = Phase(nc)
            p = ph.p
            PS = ph.psum4()
            ob = [ph.sb([128, 4, D], F32) for _ in range(2)]
            for tt in range(32):
                pst = PS[tt % 2]
                pk = "psO%d" % (tt % 2)

                def tr(e, tt=tt, pst=pst):
                    for c in range(8):
                        ins = e.matmul(pst[:, c * 128:(c + 1) * 128], HT[:, c, tt * 128:(tt + 1) * 128], ident[:], start=True, stop=True)
                    return ins
                p.pe(tr, reads=["HT", "ident"], writes=[pk])
                g4 = tt // 4
                o = ob[g4 % 2]
                ok = "ob%d_%d" % (g4 % 2, tt % 4)
                if tt % 2 == 0:
                    p.act(lambda e, o=o, pst=pst, tt=tt: e.activation(o[:, tt % 4, :], pst[:, :], AF.Identity), reads=[pk], writes=[ok])
                else:
                    p.dve(lambda e, o=o, pst=pst, tt=tt: e.tensor_copy(o[:, tt % 4, :], pst[:, :]), reads=[pk], writes=[ok])
                if tt % 4 == 3:
                    p.dma("sp", "ost%d" % (g4 % 2), lambda e, o=o, g4=g4: e.dma_start(
                        out=out_d[g4 * 512:(g4 + 1) * 512, :].rearrange("(t p) n -> p t n", p=128), in_=o[:]),
                        reads=["ob%d_%d" % (g4 % 2, q) for q in range(4)], writes=["out"])
            ph.done()

        def qk_chain(p, PS, psA, kA, psB, kB, psC, T, gcol, gpcol, cosb, sinb, tq, dst, dstkey, rope=True):
            sqq, rsq, t1, t2 = tq
            p.act(lambda e: e.activation(sqq[:, 0:T], psA, AF.Square), reads=[kA], writes=["sqq"])
            p.pe(lambda e: e.matmul(psC[:, 0:T], onesblk_bf[:], sqq[:, 0:T], start=True, stop=True), reads=["sqq", "onesblk"], writes=["psC"])
            p.act(lambda e: e.activation(rsq[:, 0:T], psC[:, 0:T], AF.Sqrt, bias=EPSB[:, 0:1]), reads=["psC"], writes=["rsq0"])
            p.dve(lambda e: e.reciprocal(rsq[:, 0:T], rsq[:, 0:T]), reads=["rsq0"], writes=["rsq"])
            if rope:
                p.dve(lambda e: e.scalar_tensor_tensor(t1[:, 0:T], psA, gcol, cosb[:, 0:T], op0=ALU.mult, op1=ALU.mult),
                      reads=[kA, "cosb"], writes=["t1"])
                p.dve(lambda e: e.scalar_tensor_tensor(t2[:, 0:T], psB, gpcol, sinb[:, 0:T], op0=ALU.mult, op1=ALU.mult),
                      reads=[kB, "sinb"], writes=["t2"])
                p.pool(lambda e: e.tensor_tensor(t1[:, 0:T], t1[:, 0:T], t2[:, 0:T], ALU.add), reads=["t1", "t2"], writes=["t3"])
                p.dve(lambda e: e.tensor_tensor(dst, t1[:, 0:T], rsq[:, 0:T], ALU.mult), reads=["t3", "rsq"], writes=[dstkey])
            else:
                p.dve(lambda e: e.scalar_tensor_tensor(dst, psA, gcol, rsq[:, 0:T], op0=ALU.mult, op1=ALU.mult),
                      reads=[kA, "rsq"], writes=[dstkey])

        def layer0_mixer():
            ph = Phase(nc)
            p = ph.p
            PS = ph.psum4()
            T = 256
            wq = ph.sb([128, 8, 1792], BF16)
            wv = ph.sb([128, 8, 128], BF16)
            gains = ph.sb([128, 4], F32)
            sqb = [ph.sb([128, T], BF16) for _ in range(2)]
            rstd = ph.sb([128, T], F32)
            tmpb = [ph.sb([128, T], F32) for _ in range(2)]
            aTs = [ph.sb([128, 8, T], BF16) for _ in range(2)]
            cur = {"aT": aTs[1], "k": ["aT1_%d" % c for c in range(8)]}
            cosb = ph.sb([128, T], F32)
            sinb = ph.sb([128, T], F32)
            tq = (ph.sb([128, T], BF16), ph.sb([128, T], F32), ph.sb([128, T], F32), ph.sb([128, T], F32))
            qf = [ph.sb([128, T], BF16) for _ in range(2)]
            qf4 = ph.sb([128, 4, T], BF16)
            pst4 = ph.sb([128, 4, T], F32)
            vst = [ph.sb([128, 2, 193], BF16) for _ in range(2)]
            ctin = ph.sb([128, D], F32)
            CT = ph.sb([128, 8, 256], F32)
            p.dma("sp", "c1", lambda e: e.dma_start(out=gains[:], in_=gains_d[:, :]), writes=["gains"])
            for vi in range(2):
                p.dve(lambda e, vi=vi: e.memset(vst[vi][:], 0.0), writes=["vst%d" % vi])
                p.dve(lambda e, vi=vi: e.memset(vst[vi][:, :, 64:66], 1.0), reads=["vst%d" % vi], writes=["vst%d" % vi])
            p.dma("pool", "wq", lambda e: e.dma_start(out=wq[:], in_=wqkp_d.rearrange("(c p) n -> p c n", p=128)), writes=["wq"])
            p.dma("pool", "wq", lambda e: e.dma_start(out=wv[:], in_=wv_d.rearrange("(c p) n -> p c n", p=128)), writes=["wv"])
            B_ST = PS[0][:, 0:512]
            B_A = [PS[0][:, 512:1024], PS[1][:, 0:512]]
            B_B = [PS[1][:, 512:1024], PS[2][:, 0:512]]
            B_C = PS[2][:, 512:1024]
            B_M = [PS[3][:, 0:512], PS[3][:, 512:1024]]
            for tt in range(2):
                p.dma("sp", "ctin", lambda e, tt=tt: e.dma_start(out=ctin[:], in_=ctx_d[tt * 128:(tt + 1) * 128, :]), writes=["ctin"])
                for half in range(2):
                    bm = B_M[half]

                    def trc(e, half=half, bm=bm):
                        for c in range(4):
                            cc = half * 4 + c
                            ins = e.matmul(bm[:, c * 128:(c + 1) * 128], ctin[:, cc * 128:(cc + 1) * 128], ident[:], start=True, stop=True)
                        return ins
                    p.pe(trc, reads=["ctin", "ident"], writes=["psM%d" % half])
                    p.dve(lambda e, half=half, bm=bm, tt=tt: e.tensor_copy(CT[:, half * 4:half * 4 + 4, tt * 128:(tt + 1) * 128],
                                                                          bm.rearrange("p (c t) -> p c t", t=128)),
                          reads=["psM%d" % half], writes=["CT"])
            mcnt = [0]

            def proj(p, col0, bank, bkey, T=T):
                aT = cur["aT"]

                def mm(e):
                    for k in range(8):
                        ins = e.matmul(bank[:, 0:T], wq[:, k, col0:col0 + 128], aT[:, k, :], start=(k == 0), stop=(k == 7))
                    return ins
                p.pe(mm, reads=["wq"] + cur["k"], writes=[bkey])

            def vproj(p, tile0):
                i = mcnt[0] % 2
                mcnt[0] += 1
                bm = B_M[i]
                bk = "psM%d" % i
                vs_ = vst[i]
                aT = cur["aT"]

                def mm(e):
                    for t2 in range(2):
                        for k in range(8):
                            ins = e.matmul(bm[:, t2 * 128:(t2 + 1) * 128], aT[:, k, t2 * 128:(t2 + 1) * 128], wv[:, k, :], start=(k == 0), stop=(k == 7))
                    return ins
                p.pe(mm, reads=["wv"] + cur["k"], writes=[bk])
                p.dve(lambda e: e.tensor_copy(vs_[:, :, 0:64], bm[:, 0:256].rearrange("p (t n) -> p t n", n=128)[:, :, 0:64]),
                      reads=[bk], writes=["vst%da" % i])
                p.dve(lambda e: e.tensor_copy(vs_[:, :, 129:193], bm[:, 0:256].rearrange("p (t n) -> p t n", n=128)[:, :, 64:128]),
                      reads=[bk, "vst%da" % i], writes=["vst%d" % i])
                p.dma("sp", "vsst%d" % i, lambda e: e.dma_start(out=VS_d[:, tile0:tile0 + 2, :], in_=vs_[:]), reads=["vst%d" % i, "vst%da" % i], writes=["VSd"])

            def nrm(b):
                bi = b % 2
                norm_block(p, B_ST, "psST", b * T, T, GS[:, 0, :], MOD[:, 0, 0:8], sqb, rstd, tmpb,
                           lambda c: (aTs[bi][:, c, :], "aT%d_%d" % (bi, c)), "m")
            norm_block(p, B_ST, "psST", 0, 256, GSC, MODC, sqb, rstd, tmpb, lambda c: (aTs[1][:, c, :], "aT1_%d" % c), "m", src=CT, srckey="CT")
            nrm(0)
            proj(p, 512, B_A[0], "psA0")
            qk_chain(p, PS, B_A[0][:, 0:T], "psA0", None, None, B_C, T, gains[:, 2:3], None, None, None, tq, qf[0][:, 0:T], "qf0", rope=False)
            p.dma("sp", "qst0", lambda e: e.dma_start(out=KT_d[:, 0:256], in_=qf[0][:, 0:T]), reads=["qf0"], writes=["KTd"])
            vproj(p, 0)
            qi = 1
            for b in range(S // T):
                t0 = b * T
                p.dma("sp", "cs", lambda e, t0=t0: e.dma_start(out=cosb[:], in_=cos_d[:, t0:t0 + T]), writes=["cosb"])
                p.dma("sp", "cs", lambda e, t0=t0: e.dma_start(out=sinb[:], in_=sin_d[:, t0:t0 + T]), writes=["sinb"])
                cur["aT"] = aTs[b % 2]
                cur["k"] = ["aT%d_%d" % (b % 2, c) for c in range(8)]
                if b + 1 < S // T:
                    nrm(b + 1)
                for j in range(5):
                    i = qi % 2
                    qi += 1
                    col = j * 128 if j < 4 else 512
                    colp = 1152 + j * 128 if j < 4 else 1664
                    proj(p, col, B_A[i], "psA%d" % i)
                    proj(p, colp, B_B[i], "psB%d" % i)
                    gcol = gains[:, 0:1] if j < 4 else gains[:, 2:3]
                    gpcol = gains[:, 1:2] if j < 4 else gains[:, 3:4]
                    if j < 4:
                        qk_chain(p, PS, B_A[i][:, 0:T], "psA%d" % i, B_B[i][:, 0:T], "psB%d" % i, B_C, T, gcol, gpcol, cosb, sinb, tq,
                                 qf4[:, j, :], "qf4_%d" % j)
                        if j == 3:
                            p.dma("sp", "qst4", lambda e, t0=t0: e.dma_start(out=QT_d[:, :, t0:t0 + T].rearrange("c p t -> p c t"), in_=qf4[:]),
                                  reads=["qf4_%d" % q for q in range(4)], writes=["QTd"])
                    else:
                        qk_chain(p, PS, B_A[i][:, 0:T], "psA%d" % i, B_B[i][:, 0:T], "psB%d" % i, B_C, T, gcol, gpcol, cosb, sinb, tq,
                                 qf[i][:, 0:T], "qf%d" % i)
                        p.dma("sp", "qst%d" % i, lambda e, i=i, t0=t0: e.dma_start(out=KT_d[:, 256 + t0:256 + t0 + T], in_=qf[i][:, 0:T]),
                              reads=["qf%d" % i], writes=["KTd"])
                for g in range(4):
                    i = mcnt[0] % 2
                    mcnt[0] += 1
                    proj(p, 640 + g * 128, B_M[i], "psM%d" % i)
                    p.act(lambda e, i=i, g=g: e.activation(pst4[:, g, :], B_M[i][:, 0:T], AF.Identity), reads=["psM%d" % i], writes=["qpst4_%d" % g])
                    if g == 3:
                        p.dma("sp", "pst4", lambda e, t0=t0: e.dma_start(out=PT_d[:, :, t0:t0 + T].rearrange("c p t -> p c t"), in_=pst4[:]),
                              reads=["qpst4_%d" % q for q in range(4)], writes=["PTd"])
                vproj(p, 2 + 2 * b)
            ph.done()
            if stage == 20:
                return
            ph = Phase(nc)
            p = ph.p
            PS = ph.psum4()
            KT = ph.sb([128, 4352], BF16)
            VS = ph.sb([128, 34, 193], BF16)
            Qb = [ph.sb([128, 512], BF16) for _ in range(2)]
            Pb = [ph.sb([128, 1024], BF16) for _ in range(3)]
            rr = ph.sb([128, 512], F32)
            bcs = ph.sb([128, 512], F32)
            mixo = [ph.sb([128, 512], BF16) for _ in range(2)]
            p.dma("sp", "kt", lambda e: e.dma_start(out=KT[:], in_=KT_d[:, :]), writes=["KT"])
            p.dma("sp", "kt", lambda e: e.dma_start(out=VS[:], in_=VS_d[:, :, :]), writes=["VS"])
            SB_ = [PS[0], PS[1]]
            OA = PS[2][:, 0:512]
            OB = PS[2][:, 512:1024]
            BCA = PS[3][:, 0:512]
            BCB = PS[3][:, 512:1024]
            NKT = 24 if stage == 7 else 34
            units = [(j, qb) for j in range(4) for qb in range(8)]
            steps = [(u, kt) for u in range(len(units)) for kt in range(NKT)]
            qbuf = {}

            def load_q(u):
                j, qb = units[u]
                qt = Qb[u % 2]
                qk_ = "Qb%d" % (u % 2)
                p.dma("sp", qk_, lambda e: e.dma_start(out=qt[:], in_=QT_d[j, :, qb * 512:(qb + 1) * 512]), writes=[qk_])
                qbuf[u] = (qt, qk_)

            def S_(i):
                u, kt = steps[i]
                if kt == 0:
                    load_q(u)
                qt, qk_ = qbuf[u]
                sb_ = SB_[i % 2]
                sk = "psS%d" % (i % 2)

                def smm(e):
                    e.matmul(sb_[:, 0:512], KT[0:64, kt * 128:(kt + 1) * 128], qt[0:64, :], start=True, stop=True)
                    return e.matmul(sb_[:, 512:1024], KT[64:128, kt * 128:(kt + 1) * 128], qt[64:128, :], start=True, stop=True)
                p.pe(smm, reads=["KT", qk_], writes=[sk])

            def finalize(u):
                j, qb = units[u]
                p.dve(lambda e: e.reciprocal(rr[64:65, :], OA[64:65, :]), reads=["psOA"], writes=["rrA"])
                p.dve(lambda e: e.reciprocal(rr[0:1, :], OB[0:1, :]), reads=["psOB"], writes=["rrB"])
                p.pe(lambda e: e.matmul(BCA[0:64, :], ones_f[64:65, 0:64], rr[64:65, :], start=True, stop=True), reads=["rrA", "ones_f"], writes=["psBCA"])
                p.pe(lambda e: e.matmul(BCB[:, :], ones_f[0:1, :], rr[0:1, :], start=True, stop=True), reads=["rrB", "ones_f"], writes=["psBCB"])
                p.act(lambda e: e.activation(bcs[0:64, :], BCA[0:64, :], AF.Identity), reads=["psBCA"], writes=["bcsA"])
                p.act(lambda e: e.activation(bcs[64:128, :], BCB[64:128, :], AF.Identity), reads=["psBCB"], writes=["bcsB"])
                mo = mixo[u % 2]
                mk = "mixo%d" % (u % 2)
                p.dve(lambda e: e.tensor_tensor(mo[0:64, :], OA[0:64, :], bcs[0:64, :], ALU.mult), reads=["psOA", "bcsA"], writes=[mk + "a"])
                p.dve(lambda e: e.tensor_tensor(mo[64:128, :], OB[64:128, :], bcs[64:128, :], ALU.mult), reads=["psOB", "bcsB"], writes=[mk + "b"])
                p.dma("sp", "mst%d" % (u % 2), lambda e: e.dma_start(out=MIX_d[j, :, qb * 512:(qb + 1) * 512], in_=mo[:]),
                      reads=[mk + "a", mk + "b"], writes=["MIXd"])

            S_(0)
            for i in range(len(steps)):
                u, kt = steps[i]
                if i + 1 < len(steps):
                    S_(i + 1)
                sb_ = SB_[i % 2]
                sk = "psS%d" % (i % 2)
                pb = Pb[i % 3]
                pk = "P%d" % (i % 3)
                p.act(lambda e, pb=pb, sb_=sb_: e.activation(pb[:], sb_[:, :], AF.Exp, scale=0.125), reads=[sk], writes=[pk])

                def pv(e, pb=pb, kt=kt):
                    e.matmul(OA[0:65, :], VS[:, kt, 0:65], pb[:, 0:512], start=(kt == 0), stop=(kt == NKT - 1))
                    return e.matmul(OB[:, :], VS[:, kt, 65:193], pb[:, 512:1024], start=(kt == 0), stop=(kt == NKT - 1))
                p.pe(pv, reads=["VS", pk], writes=["psOA", "psOB"])
                if kt == NKT - 1:
                    finalize(u)
            ph.done()
            ph = Phase(nc)
            p = ph.p
            PS = ph.psum4()
            W = S + 16
            Pf = ph.sb([128, W], F32)
            sa = ph.sb([128, W], F32)
            sb2 = ph.sb([128, W], F32)
            dbf = ph.sb([128, S], BF16)
            pw = ph.sb([128, 512], BF16)
            psc = ph.sb([128, 4], F32)
            edg = ph.sb([128, 64], F32)
            et = ph.sb([128, 16], F32)
            po = [ph.sb([128, 512], BF16) for _ in range(2)]
            p.dma("pool", "pw", lambda e: e.dma_start(out=pw[:], in_=poolw_d[:, :]), writes=["pw"])
            p.dma("sp", "pc", lambda e: e.dma_start(out=psc[:], in_=poolsc_d[:, :]), writes=["psc"])
            p.dma("sp", "pc", lambda e: e.dma_start(out=edg[:], in_=pooledge_d[:, :]), writes=["edg"])
            p.dve(lambda e: e.memset(Pf[:, 0:8], 0.0), writes=["PfL"])
            p.dve(lambda e: e.memset(Pf[:, W - 8:W], 0.0), writes=["PfR"])
            oc = 0
            for g in range(4):
                w_ = 2 ** (g + 1)
                p.dma("sp", "pf", lambda e, g=g: e.dma_start(out=Pf[:, 8:8 + S], in_=PT_d[g, :, :]), writes=["Pf"])
                p.dve(lambda e: e.tensor_tensor(sa[:, 1:W], Pf[:, 0:W - 1], Pf[:, 1:W], ALU.add), reads=["Pf", "PfL", "PfR"], writes=["sa"])
                cur, ck = sa, "sa"
                oth, ok_ = sb2, "sb"
                lo, hi, sh_ = 1, W, 1
                for st in range(g):
                    nlo, nhi = lo + sh_, hi - sh_
                    eng = p.pool if st % 2 == 0 else p.dve
                    eng(lambda e, cur=cur, oth=oth, nlo=nlo, nhi=nhi, sh_=sh_: e.tensor_tensor(oth[:, nlo:nhi], cur[:, nlo - sh_:nhi - sh_], cur[:, nlo + sh_:nhi + sh_], ALU.add),
                        reads=[ck], writes=[ok_])
                    cur, ck, oth, ok_ = oth, ok_, cur, ck
                    lo, hi = nlo, nhi
                    sh_ *= 2
                assert lo <= 8 and hi >= 8 + S
                p.dve(lambda e, cur=cur, w_=w_: e.scalar_tensor_tensor(dbf[:, :], cur[:, 8:8 + S], 1.0 / w_, Pf[:, 8:8 + S], op0=ALU.mult, op1=ALU.subtract),
                      reads=[ck, "Pf"], writes=["dbf0"])
                p.dve(lambda e, cur=cur, g=g: e.tensor_tensor(et[:, 0:8], cur[:, 8:16], edg[:, g * 16:g * 16 + 8], ALU.mult), reads=[ck, "edg"], writes=["et0"])
                p.dve(lambda e, cur=cur, g=g: e.tensor_tensor(et[:, 8:16], cur[:, S:8 + S], edg[:, g * 16 + 8:g * 16 + 16], ALU.mult), reads=[ck, "edg", "et0"], writes=["et1"])
                p.dve(lambda e: e.tensor_tensor(dbf[:, 0:8], et[:, 0:8], Pf[:, 8:16], ALU.subtract), reads=["et1", "Pf", "dbf0"], writes=["dbf1"])
                p.dve(lambda e: e.tensor_tensor(dbf[:, S - 8:S], et[:, 8:16], Pf[:, S:8 + S], ALU.subtract), reads=["et1", "Pf", "dbf1"], writes=["dbf"])
                for b in range(8):
                    i = oc % 2
                    oc += 1
                    bank = PS[i][:, 0:512]
                    p.pe(lambda e, bank=bank, g=g, b=b: e.matmul(bank, pw[:, g * 128:(g + 1) * 128], dbf[:, b * 512:(b + 1) * 512], start=True, stop=True),
                         reads=["pw", "dbf"], writes=["psP%d" % i])
                    p.act(lambda e, bank=bank, i=i, g=g: e.activation(po[i][:], bank, AF.Identity, scale=psc[:, g:g + 1]), reads=["psP%d" % i, "psc"], writes=["po%d" % i])
                    p.dma("sp", "post%d" % i, lambda e, i=i, g=g, b=b: e.dma_start(out=MIX_d[4 + g, :, b * 512:(b + 1) * 512], in_=po[i][:]),
                          reads=["po%d" % i], writes=["MIXd"])
            ph.done()
            wout_phase(wout0_d, 0)

        def layer1_mixer():
            ph = Phase(nc)
            p = ph.p
            PS = ph.psum4()
            T = 256
            w1 = ph.sb([128, 8, 2560], BF16)
            sqb = [ph.sb([128, T], BF16) for _ in range(2)]
            rstd = ph.sb([128, T], F32)
            tmpb = [ph.sb([128, T], F32) for _ in range(2)]
            aTs = [ph.sb([128, 8, T], BF16) for _ in range(2)]
            cur = {"aT": aTs[0], "k": ["aT0_%d" % c for c in range(8)]}
            sggain = ph.sb([128, 512], F32)
            sgwT = ph.sb([128, 512], BF16)
            sgbb = ph.sb([128, 512], F32)
            usb = ph.sb([128, 4, T], F32)
            hxs = [ph.sb([128, T], F32)] * 2
            zs = hxs
            bgs = [ph.sb([128, T], F32)] * 2
            sqv = ph.sb([128, 512], F32)
            ssum = ph.sb([128, 4], F32)
            vt = sqv
            vn = ph.sb([128, 4, 128], BF16)
            st_ = ph.sb([128, 4, 128], F32)
            yc = [ph.sb([128, 4, T], BF16) for _ in range(2)]
            p.dma("pool", "w1", lambda e: e.dma_start(out=w1[:, :, 0:1280], in_=win1_d[:, 0:1280].rearrange("(c p) n -> p c n", p=128)), writes=["w1"])
            p.dma("pool", "w1", lambda e: e.dma_start(out=w1[:, :, 1280:2560], in_=win1_d[:, 1280:2560].rearrange("(c p) n -> p c n", p=128)), writes=["w1"])
            p.dma("pool", "w1", lambda e: e.dma_start(out=sgwT[:], in_=sgw_d[:, :]), writes=["sgwT"])
            p.dma("sp", "c2", lambda e: e.dma_start(out=sggain[:], in_=sggain_d[:, :]), writes=["sggain"])
            p.dma("sp", "c2", lambda e: e.dma_start(out=sgbb[:], in_=sgb_d[:, :]), writes=["sgbb"])
            B_ST = PS[0][:, 0:512]
            B_U = [PS[0][:, 512:1024], PS[1][:, 0:512]]
            B_H = [PS[1][:, 512:1024], PS[2][:, 0:512]]
            B_G = PS[2][:, 512:1024]
            B_V = PS[3][:, 0:512]
            B_S = PS[3][:, 512:1024]
            def proj(col0, tgt, bkey):
                aT = cur["aT"]

                def mm(e):
                    for k in range(8):
                        ins = e.matmul(tgt, w1[:, k, col0:col0 + 128], aT[:, k, :], start=(k == 0), stop=(k == 7))
                    return ins
                p.pe(mm, reads=["w1"] + cur["k"], writes=[bkey])

            def nrm(b):
                bi = b % 2
                norm_block(p, B_ST, "psST", b * T, T, GS[:, 2, :], MOD[:, 1, 0:8], sqb, rstd, tmpb,
                           lambda c: (aTs[bi][:, c, :], "aT%d_%d" % (bi, c)), "m")
            nrm(0)
            hi_ = 0
            for b in range(S // T):
                t0 = b * T
                cur["aT"] = aTs[b % 2]
                cur["k"] = ["aT%d_%d" % (b % 2, c) for c in range(8)]
                akeys = cur["k"]
                aT = cur["aT"]
                if b + 1 < S // T:
                    nrm(b + 1)
                for half in range(2):
                    for q in range(2):
                        g = half * 2 + q
                        proj(g * 128, B_U[half][:, q * T:(q + 1) * T], "psU%d" % half)
                    p.act(lambda e, half=half: e.activation(usb[:, half * 2:half * 2 + 2, :], B_U[half][:, 0:2 * T].rearrange("p (q t) -> p q t", t=T), AF.Identity),
                          reads=["psU%d" % half], writes=["usb%d" % half])
                for c in range(4):
                    i = hi_ % 2
                    hi_ += 1
                    proj(1024 + c * 128, B_H[i][:, 0:T], "psH%d" % i)
                    proj(2048 + c * 128, B_H[i][:, T:2 * T], "psH%d" % i)
                    p.act(lambda e, i=i: e.activation(hxs[i][:], B_H[i][:, 0:T], AF.Identity), reads=["psH%d" % i], writes=["hxs0"])
                    p.dve(lambda e, i=i: e.tensor_tensor(zs[i][:], B_H[i][:, T:2 * T], hxs[i][:], ALU.mult), reads=["psH%d" % i, "hxs0"], writes=["hxs0"])
                    p.dma("sp", "zst%d" % i, lambda e, i=i, c=c, t0=t0: e.dma_start(out=PT_d[c, :, t0:t0 + T], in_=zs[i][:]), reads=["hxs0"], writes=["PTd"])
                    proj(1536 + c * 128, B_G[:, 0:T], "psG")
                    p.act(lambda e, i=i: e.activation(bgs[i][:], B_G[:, 0:T], AF.Identity), reads=["psG"], writes=["bgs0"])
                    p.dma("sp", "bst%d" % i, lambda e, i=i, c=c, t0=t0: e.dma_start(out=BG_d[c, :, t0:t0 + T], in_=bgs[i][:]), reads=["bgs0"], writes=["BGd"])
                yb = yc[b % 2]
                yk = "yc%d" % (b % 2)
                for n in range(2):
                    def vmm(e, n=n, aT=aT):
                        for g in range(4):
                            for k in range(8):
                                ins = e.matmul(B_V[:, g * 128:(g + 1) * 128], aT[:, k, n * 128:(n + 1) * 128], w1[:, k, 512 + g * 128:512 + (g + 1) * 128],
                                               start=(k == 0), stop=(k == 7))
                        return ins
                    p.pe(vmm, reads=["w1"] + akeys, writes=["psV"])
                    p.act(lambda e: e.activation(sqv[:], B_V, AF.Square), reads=["psV"], writes=["sqv"])
                    p.dve(lambda e: e.tensor_reduce(ssum[:], sqv[:].rearrange("p (g c) -> p g c", c=128), AX.X, ALU.add), reads=["sqv"], writes=["ssum0"])
                    p.act(lambda e: e.activation(ssum[:], ssum[:], AF.Sqrt, bias=EPSB[:, 0:1], scale=1.0 / 128.0), reads=["ssum0"], writes=["ssum1"])
                    p.dve(lambda e: e.reciprocal(ssum[:], ssum[:]), reads=["ssum1"], writes=["ssum"])
                    p.dve(lambda e: e.tensor_tensor(vt[:].rearrange("p (g c) -> p g c", c=128), B_V.rearrange("p (g c) -> p g c", c=128), ssum[:].unsqueeze(2).to_broadcast([128, 4, 128]), ALU.mult),
                          reads=["psV", "ssum", "sqv"], writes=["sqv"])
                    p.pool(lambda e: e.tensor_tensor(vn[:].rearrange("p g c -> p (g c)"), vt[:], sggain[:], ALU.mult), reads=["sqv", "sggain"], writes=["vn"])

                    def smm(e):
                        for g in range(4):
                            ins = e.matmul(B_S[:, g * 128:(g + 1) * 128], vn[:, g, :], sgwT[:, g * 128:(g + 1) * 128], start=True, stop=True)
                        return ins
                    p.pe(smm, reads=["vn", "sgwT"], writes=["psS"])
                    p.dve(lambda e: e.tensor_tensor(st_[:], B_S.rearrange("p (g c) -> p g c", c=128), sgbb[:].rearrange("p (g c) -> p g c", c=128), ALU.add),
                          reads=["psS", "sgbb"], writes=["st"])
                    p.pool(lambda e, n=n, yb=yb: e.tensor_tensor(yb[:, :, n * 128:(n + 1) * 128], st_[:], usb[:, :, n * 128:(n + 1) * 128], ALU.mult),
                           reads=["st", "usb0", "usb1"], writes=[yk + "_%d" % n])
                p.dma("sp", "yst%d" % (b % 2), lambda e, yb=yb, t0=t0: e.dma_start(out=MIX_d[0:4, :, t0:t0 + T].rearrange("c p t -> p c t"), in_=yb[:]),
                      reads=[yk + "_0", yk + "_1"], writes=["MIXd"])
            ph.done()
            ph = Phase(nc)
            p = ph.p
            W = S + 2
            Z = ph.sb([128, W], F32)
            BGr = ph.sb([128, S], F32)
            t1 = ph.sb([128, S], F32)
            yo = ph.sb([128, S], BF16)
            cw = ph.sb([128, 12], F32)
            p.dma("sp", "cw", lambda e: e.dma_start(out=cw[:], in_=convw_d[:, :]), writes=["cw"])
            p.dve(lambda e: e.memset(Z[:, 0:1], 0.0), writes=["ZL"])
            p.dve(lambda e: e.memset(Z[:, W - 1:W], 0.0), writes=["ZR"])
            for c in range(4):
                p.dma("sp", "z", lambda e, c=c: e.dma_start(out=Z[:, 1:1 + S], in_=PT_d[c, :, :]), writes=["Z"])
                p.dma("sp", "bg", lambda e, c=c: e.dma_start(out=BGr[:], in_=BG_d[c, :, :]), writes=["BGr"])
                p.dve(lambda e, c=c: e.tensor_scalar(t1[:], Z[:, 1:1 + S], cw[:, c * 3 + 1:c * 3 + 2], None, op0=ALU.mult), reads=["Z", "cw"], writes=["t1a"])
                p.dve(lambda e, c=c: e.scalar_tensor_tensor(t1[:], Z[:, 0:S], cw[:, c * 3:c * 3 + 1], t1[:], op0=ALU.mult, op1=ALU.add),
                      reads=["Z", "ZL", "cw", "t1a"], writes=["t1b"])
                p.dve(lambda e, c=c: e.scalar_tensor_tensor(t1[:], Z[:, 2:2 + S], cw[:, c * 3 + 2:c * 3 + 3], t1[:], op0=ALU.mult, op1=ALU.add),
                      reads=["Z", "ZR", "cw", "t1b"], writes=["t1c"])
                p.pool(lambda e: e.tensor_tensor(yo[:], t1[:], BGr[:], ALU.mult), reads=["t1c", "BGr"], writes=["yo"])
                p.dma("sp", "yo", lambda e, c=c: e.dma_start(out=MIX_d[4 + c, :, :], in_=yo[:]), reads=["yo"], writes=["MIXd"])
            ph.done()
            wout_phase(wout1_d, 1)

        if tail_only:
            router_moe(1)
        else:
            if stage >= 1:
                layer0_mixer()
            if stage >= 2 and stage < 20:
                router_moe(0)
            if stage == 9:
                router_moe(1)
                layer1_mixer()
            if stage >= 3 and stage < 9 and stage != 5:
                layer1_mixer()
            if stage >= 4 and stage < 9 and stage != 6:
                router_moe(1)
        if stage == 12:
            ph = Phase(nc)
            big = ph.sb([128, 4096], F32)
            for i_ in range(4000):
                ph.p.dve(lambda e: e.memset(big[:], 1.0), writes=["big"])
            ph.done()
        if stage == 6:
            for _ in range(3):
                ph = Phase(nc)
                ph.p.dve(lambda e: e.memset(EPSB[:], EPS), writes=["epsb"])
                ph.done()
        store_phase()
    return nc


def _perm64():
    d = np.arange(64)
    return np.where((d % 32) < 16, d + 16, d - 16)


def prep_inputs(inputs):
    f = lambda a: np.ascontiguousarray(np.asarray(a, dtype=np.float32))
    I = {k: np.asarray(v) for k, v in inputs.items()}
    shared = {}
    shared["ident"] = np.eye(128, dtype=np.float32)
    shared["mod_w"] = f(I["mod_w"])
    shared["mod_bT"] = f(I["mod_b"].reshape(2, 48, 128).transpose(2, 0, 1).reshape(128, 96))
    ng = np.stack([I["norm1_g"], I["norm2_g"]], 0)
    shared["norm_g"] = f(ng.reshape(2, 2, 8, 128).transpose(3, 0, 1, 2).reshape(128, 32))
    w_in0 = I["even_w_in"][0]
    pi = _perm64()
    qcols, qpcols = [], []
    for j in range(4):
        for h in (j, j + 4):
            qcols.append(h * 64 + np.arange(64))
            qpcols.append(h * 64 + pi)
    qcols = np.concatenate(qcols)
    qpcols = np.concatenate(qpcols)
    kcols = 512 + np.arange(128)
    kpcols = 512 + np.concatenate([pi, 64 + pi])
    pcols = 768 + np.arange(512)
    allc = np.concatenate([qcols, kcols, pcols, qpcols, kpcols])
    shared["w_qkp"] = f(w_in0[:, allc])
    shared["w_v"] = f(w_in0[:, 640:768])
    qg = I["q_gain"][0]
    kg = I["k_gain"][0]
    d = np.arange(128) % 64
    shared["gains"] = f(np.stack([qg[d], qg[pi[d]], kg[d], kg[pi[d]]], 1))
    t = np.arange(S)
    row = (t // 64).astype(np.float32)
    col = (t % 64).astype(np.float32)
    inv = (10000.0 ** (-np.arange(0, 16, dtype=np.float32) * 2 / 32.0)).astype(np.float32)
    cos_t = np.zeros((128, S), np.float32)
    sin_t = np.zeros((128, S), np.float32)
    for pp in range(128):
        dd = pp % 64
        jj = dd % 16
        pos = row if dd < 32 else col
        ang = (pos * inv[jj]).astype(np.float32)
        sgn = -1.0 if (dd % 32) < 16 else 1.0
        cos_t[pp] = np.cos(ang)
        sin_t[pp] = sgn * np.sin(ang)
    shared["cos_t"] = cos_t
    shared["sin_t"] = sin_t
    shared["pool_wT"] = f(I["pool_w"][0].transpose(1, 0, 2).reshape(128, 512))
    shared["pool_sc"] = f(I["pool_scale"][0].reshape(4, 128).T)
    edge = np.zeros((128, 4, 16), np.float32)
    for g, w in enumerate((2, 4, 8, 16)):
        for jx in range(16):
            tpos = jx if jx < 8 else S - 16 + jx
            lo = max(tpos - w // 2, 0)
            hi = min(tpos + w - w // 2, S)
            edge[:, g, jx] = 1.0 / (hi - lo)
    shared["pool_edge"] = edge.reshape(128, 64)
    w_out0 = I["even_w_out"][0]
    rows = []
    for c in range(4):
        for h in (c, c + 4):
            rows.append(h * 64 + np.arange(64))
    rows.append(512 + np.arange(512))
    shared["w_out0"] = f(w_out0[np.concatenate(rows), :])
    shared["w_out1"] = f(I["odd_w_out"][0])
    shared["w_in1"] = f(I["odd_w_in"][0])
    shared["sg_gain_b"] = f(np.broadcast_to(I["sg_gain"][0].reshape(1, 512), (128, 512)))
    shared["sg_wT"] = f(I["sg_w"][0].transpose(2, 0, 1).reshape(128, 512))
    shared["sg_b_b"] = f(np.broadcast_to(I["sg_b"][0].reshape(1, 512), (128, 512)))
    shared["conv_wT"] = f(I["conv_w"][0][:, 0, :].reshape(3, 4, 128).transpose(2, 1, 0).reshape(128, 12))
    shared["rw"] = f(np.concatenate([I["router_g_w"], I["router_e_w"]], axis=2))
    rb = np.concatenate([I["router_g_b"], I["router_e_b"]], axis=1)
    shared["rb_b"] = f(np.broadcast_to(np.tile(rb[:, None, :], (1, 4, 1)).reshape(1, 160), (128, 160)))
    shared["w_gate"] = f(I["w_gate"])
    shared["w_up"] = f(I["w_up"])
    shared["w_down"] = f(I["w_down"])
    sel = np.zeros((32, 16, 128), np.float32)
    for ex in range(16):
        sel[ex, ex, :] = 1.0
        sel[16 + ex, ex, :] = 1.0
    shared["sel"] = sel.reshape(32, 2048)
    in_maps = []
    for b in range(NCORES):
        m = dict(shared)
        m["x"] = f(I["x"][b])
        m["ctx"] = f(I["ctx"][b])
        cv = np.stack([I["c"][b], I["c_ctx"]], 0)
        m["cvec"] = f(cv.reshape(2, 8, 128).transpose(2, 1, 0).reshape(128, 16))
        in_maps.append(m)
    return in_maps


_NC_CACHE = {}


def kernel(**inputs):
    in_maps = prep_inputs(inputs)
    if "nc" not in _NC_CACHE:
        _NC_CACHE["nc"] = (build(stage=3), build(tail_only=True))
    nc1, nc2 = _NC_CACHE["nc"]
    res = run_bass_kernel_spmd(nc1, in_maps, core_ids=list(range(NCORES)))
    for b in range(NCORES):
        in_maps[b]["x"] = np.ascontiguousarray(np.asarray(res.results[b]["out"], dtype=np.float32))
    res = run_bass_kernel_spmd(nc2, in_maps, core_ids=list(range(NCORES)))
    out = np.stack([np.asarray(r["out"]) for r in res.results], axis=0)
    return out.astype(np.float32)
```

```python
import numpy as np
from contextlib import ExitStack
import concourse.bass as bass
import concourse.mybir as mybir
from concourse.bass_utils import run_bass_kernel_spmd

F32 = mybir.dt.float32
BF16 = mybir.dt.bfloat16
AF = mybir.ActivationFunctionType
ALU = mybir.AluOpType
AX = mybir.AxisListType

ENGS = ["pe", "act", "dve", "pool", "sp"]
S = 4096
D = 1024
NCORES = 8
EPS = 1e-6
_CNT = [0]
_PHASE = [0]
_SEMPOOL = [[], []]
NSEM = 16


class Op:
    __slots__ = ("eng", "fn", "reads", "writes", "dma_key", "idx", "waits", "signal", "semval")

    def __init__(self, eng, fn, reads, writes, dma_key):
        self.eng = eng
        self.fn = fn
        self.reads = reads
        self.writes = writes
        self.dma_key = dma_key
        self.waits = []
        self.signal = False
        self.semval = 0


class Prog:
    def __init__(self, nc):
        self.nc = nc
        self.ops = []

    def op(self, eng, fn, reads=(), writes=(), dma_key=None):
        reads = tuple(reads)
        writes = tuple(writes)
        ex = tuple(r for r in reads if r.startswith("ps"))
        o = Op(eng, fn, reads, writes + ex, dma_key)
        o.idx = len(self.ops)
        self.ops.append(o)
        return o

    def pe(self, fn, reads=(), writes=()):
        return self.op("pe", fn, reads, writes)

    def act(self, fn, reads=(), writes=()):
        return self.op("act", fn, reads, writes)

    def dve(self, fn, reads=(), writes=()):
        return self.op("dve", fn, reads, writes)

    def pool(self, fn, reads=(), writes=()):
        return self.op("pool", fn, reads, writes)

    def dma(self, eng, key, fn, reads=(), writes=()):
        return self.op(eng, fn, reads, writes, dma_key=key)

    def finalize(self):
        ops = self.ops

        def tl(o):
            return ("dma", o.dma_key) if o.dma_key is not None else o.eng

        pos = {}
        cnt = {}
        for o in ops:
            t = tl(o)
            cnt[t] = cnt.get(t, 0) + 1
            pos[o.idx] = cnt[t]
        last_writer = {}
        readers = {}
        known = {e: {} for e in ENGS}
        done_clock = {}
        needed = set()
        latest_on_key = {}
        for o in ops:
            deps = set()
            raw = set()
            for r in o.reads:
                w = last_writer.get(r)
                if w is not None:
                    deps.add(w)
                    raw.add(w)
            for r in o.writes:
                w = last_writer.get(r)
                if w is not None:
                    deps.add(w)
                    if r.startswith("ps"):
                        raw.add(w)
                for rd in readers.get(r, ()):
                    deps.add(rd)
            req = {}
            for d in deps:
                if d == o.idx:
                    continue
                po = ops[d]
                t = tl(po)
                if po.dma_key is None and o.dma_key is None and po.eng == o.eng:
                    if o.eng == "pe":
                        continue
                    if d not in raw:
                        continue
                if t not in req or pos[d] > pos[req[t]]:
                    req[t] = d
            kn = known[o.eng]
            for t in list(req.keys()):
                if not isinstance(t, str):
                    req[t] = latest_on_key[t]
            waits = []
            for t, d in req.items():
                if kn.get(t, 0) >= pos[d]:
                    continue
                waits.append(d)
                needed.add(d)
                for t2, p2 in done_clock[d].items():
                    if kn.get(t2, 0) < p2:
                        kn[t2] = p2
            o.waits = waits
            dc = dict(kn)
            dc[tl(o)] = max(dc.get(tl(o), 0), pos[o.idx])
            done_clock[o.idx] = dc
            if o.dma_key is not None:
                latest_on_key[tl(o)] = o.idx
            for r in o.writes:
                last_writer[r] = o.idx
                readers[r] = []
            for r in o.reads:
                if r not in o.writes:
                    readers.setdefault(r, []).append(o.idx)
        semcnt = {}
        for o in ops:
            t = tl(o)
            if o.dma_key is not None:
                semcnt[t] = semcnt.get(t, 0) + 16
                o.signal = True
                o.semval = semcnt[t]
            elif o.idx in needed:
                semcnt[t] = semcnt.get(t, 0) + 1
                o.signal = True
                o.semval = semcnt[t]
        self.timelines = sorted(set(tl(o) for o in ops if o.signal), key=str)
        self._tl = tl
        return self

    def emit(self):
        nc = self.nc
        ops = self.ops
        tl = self._tl
        with ExitStack() as es:
            pidx = _PHASE[0] % 2
            _PHASE[0] += 1
            mypool = _SEMPOOL[pidx]
            other = _SEMPOOL[1 - pidx]
            assert len(self.timelines) <= len(mypool), len(self.timelines)
            sems = {}
            for i, t in enumerate(self.timelines):
                sems[t] = mypool[i]
            block = es.enter_context(nc.Block())
            by_eng = {e: [o for o in ops if o.eng == e] for e in ENGS}
            final_dma = {}
            for o in ops:
                if o.dma_key is not None:
                    final_dma[tl(o)] = o.semval

            def run(engname, eng):
                for o in by_eng[engname]:
                    ws = list(o.waits)
                    att = None
                    if ws and engname != "pe":
                        att = ws.pop()
                    for d in ws:
                        po = ops[d]
                        eng.wait_ge(sems[tl(po)], po.semval)
                    if att is not None:
                        rec = _Rec(eng)
                        ins = o.fn(rec)
                        po = ops[att]
                        rec.first._wait_ge(sems[tl(po)], po.semval)
                    else:
                        ins = o.fn(eng)
                    if o.signal:
                        ins.then_inc(sems[tl(o)], 16 if o.dma_key is not None else 1)
                if engname == "sp":
                    for t, v in final_dma.items():
                        eng.wait_ge(sems[t], v)
                    for sm in other:
                        eng.sem_clear(sm)

            @block.tensor
            def _(eng):
                run("pe", eng)

            @block.scalar
            def _(eng):
                run("act", eng)

            @block.vector
            def _(eng):
                run("dve", eng)

            @block.gpsimd
            def _(eng):
                run("pool", eng)

            @block.sync
            def _(eng):
                run("sp", eng)


class _Rec:
    def __init__(self, eng):
        self._eng = eng
        self.first = None

    def __getattr__(self, name):
        f = getattr(self._eng, name)

        def g(*a, **k):
            r = f(*a, **k)
            if self.first is None:
                self.first = r
            return r
        return g


class Phase:
    def __init__(self, nc):
        self.nc = nc
        self.es = ExitStack()
        self.p = Prog(nc)
        self._n = 0

    def sb(self, shape, dt):
        self._n += 1
        _CNT[0] += 1
        return self.es.enter_context(self.nc.sbuf_tensor("sb%d" % _CNT[0], list(shape), dt))

    def psum4(self):
        r = []
        for _ in range(4):
            _CNT[0] += 1
            r.append(self.es.enter_context(self.nc.psum_tensor("ps%d" % _CNT[0], [128, 1024], F32)))
        return r

    def done(self):
        self.p.finalize()
        self.p.emit()
        self.es.close()


def build(stage=4, tail_only=False):
    nc = bass.Bass("TRN2", target_bir_lowering=False)

    def din(name, shape, dt=F32):
        return nc.dram_tensor(name, list(shape), dt, kind="ExternalInput").ap()

    def dscr(name, shape, dt):
        return nc.dram_tensor(name, list(shape), dt, kind="Internal").ap()

    x_d = din("x", [S, D])
    ctx_d = din("ctx", [256, D])
    cvec_d = din("cvec", [128, 16])
    ident_d = din("ident", [128, 128])
    modw_d = din("mod_w", [2, D, 6144])
    modb_d = din("mod_bT", [128, 96])
    ng_d = din("norm_g", [128, 32])
    wqkp_d = din("w_qkp", [D, 1792])
    wv_d = din("w_v", [D, 128])
    gains_d = din("gains", [128, 4])
    cos_d = din("cos_t", [128, S])
    sin_d = din("sin_t", [128, S])
    poolw_d = din("pool_wT", [128, 512])
    poolsc_d = din("pool_sc", [128, 4])
    pooledge_d = din("pool_edge", [128, 64])
    wout0_d = din("w_out0", [D, D])
    wout1_d = din("w_out1", [D, D])
    win1_d = din("w_in1", [D, 2560])
    sggain_d = din("sg_gain_b", [128, 512])
    sgw_d = din("sg_wT", [128, 512])
    sgb_d = din("sg_b_b", [128, 512])
    convw_d = din("conv_wT", [128, 12])
    rw_d = din("rw", [2, D, 20])
    rb_d = din("rb_b", [128, 160])
    wg_d = din("w_gate", [2, 16, D, 256])
    wu_d = din("w_up", [2, 16, D, 256])
    wd_d = din("w_down", [2, 16, 256, D])
    sel_d = din("sel", [32, 2048])
    out_d = nc.dram_tensor("out", [S, D], F32, kind="ExternalOutput").ap()

    A2_d = dscr("A2s", [8, 128, 8 * 512], BF16)
    QT_d = dscr("QTs", [4, 128, S], BF16)
    MIX_d = dscr("MIXs", [8, 128, S], BF16)
    PT_d = dscr("PTs", [4, 128, S], F32)
    BG_d = dscr("BGs", [4, 128, S], F32)
    KT_d = dscr("KTs", [128, 4352], BF16)
    VS_d = dscr("VSs", [128, 34, 193], BF16)

    with ExitStack() as top:
        def psb(shape, dt):
            _CNT[0] += 1
            return top.enter_context(nc.sbuf_tensor("pt%d" % _CNT[0], list(shape), dt))

        _PHASE[0] = 0
        for pi_ in range(2):
            _SEMPOOL[pi_] = []
            for si_ in range(NSEM):
                _SEMPOOL[pi_].append(top.enter_context(nc.semaphore("sp%d_%d" % (pi_, si_))))
        HT = psb([128, 8, S], F32)
        ident = psb([128, 128], F32)
        ident_bf = psb([128, 128], BF16)
        ones_bf = psb([128, 128], BF16)
        onesblk_bf = psb([128, 128], BF16)
        ones_f = psb([128, 128], F32)
        MOD = psb([128, 2, 48], F32)
        MODC = psb([128, 16], F32)
        NG = psb([128, 32], F32)
        GS = psb([128, 4, 8], F32)
        GSC = psb([128, 8], F32)
        EPSB = psb([128, 1], F32)

        ph = Phase(nc)
        p = ph.p
        PS = ph.psum4()
        cvec = ph.sb([128, 16], F32)
        scv = ph.sb([128, 16], F32)
        modb = ph.sb([128, 96], F32)
        mrow = ph.sb([2, 6144], F32)
        mwbuf = [ph.sb([128, 8, 512], F32) for _ in range(2)]
        xin = [ph.sb([128, D], F32) for _ in range(2)]
        p.dma("sp", "c0", lambda e: e.dma_start(out=ident[:], in_=ident_d[:, :]), writes=["ident"])
        p.dma("sp", "c0", lambda e: e.dma_start(out=cvec[:], in_=cvec_d[:, :]), writes=["cvec"])
        p.dma("sp", "c0", lambda e: e.dma_start(out=modb[:], in_=modb_d[:, :]), writes=["modb"])
        p.dma("sp", "c0", lambda e: e.dma_start(out=NG[:], in_=ng_d[:, :]), writes=["NG"])
        p.dve(lambda e: e.tensor_copy(ident_bf[:], ident[:]), reads=["ident"], writes=["ident_bf"])
        p.dve(lambda e: e.memset(ones_bf[:], 1.0 / 1024.0), writes=["ones_bf"])
        p.dve(lambda e: e.memset(ones_f[:], 1.0), writes=["ones_f"])
        p.dve(lambda e: e.memset(EPSB[:], EPS), writes=["epsb"])
        p.dve(lambda e: e.memset(onesblk_bf[:], 0.0), writes=["onesblk0"])
        p.dve(lambda e: e.memset(onesblk_bf[0:64, 0:64], 1.0 / 64.0), reads=["onesblk0"], writes=["onesblk1"])
        p.dve(lambda e: e.memset(onesblk_bf[64:128, 64:128], 1.0 / 64.0), reads=["onesblk1"], writes=["onesblk"])
        p.act(lambda e: e.activation(scv[:], cvec[:], AF.Silu), reads=["cvec"], writes=["scv"])
        for l in ([1] if tail_only else [0, 1]):
            for fb in range(12):
                it = l * 12 + fb
                buf = mwbuf[it % 2]
                bk = "mw%d" % (it % 2)
                p.dma("sp", bk, lambda e, buf=buf, l=l, fb=fb: e.dma_start(
                    out=buf[:], in_=modw_d[l, :, fb * 512:(fb + 1) * 512].rearrange("(c p) n -> p c n", p=128)),
                    writes=[bk])
                pb = "psA%d" % (it % 2)
                pst = PS[0][:, (it % 2) * 512:(it % 2) * 512 + 512]

                def mm(e, buf=buf, pst=pst):
                    for c in range(8):
                        ins = e.matmul(pst[0:2, :], scv[:, c * 2:c * 2 + 2], buf[:, c, :], start=(c == 0), stop=(c == 7))
                    return ins
                p.pe(mm, reads=[bk, "scv"], writes=[pb])
                if it % 2 == 0:
                    p.act(lambda e, pst=pst, fb=fb: e.activation(mrow[0:2, fb * 512:(fb + 1) * 512], pst[0:2, :], AF.Identity),
                          reads=[pb], writes=["mrow_%d" % fb])
                else:
                    p.dve(lambda e, pst=pst, fb=fb: e.tensor_copy(mrow[0:2, fb * 512:(fb + 1) * 512], pst[0:2, :]),
                          reads=[pb], writes=["mrow_%d" % fb])

            def tr(e):
                for j in range(48):
                    ins = e.matmul(PS[1][:, j * 2:j * 2 + 2], mrow[0:2, j * 128:(j + 1) * 128], ident[0:2, 0:2], start=True, stop=True)
                return ins
            p.pe(tr, reads=["mrow_%d" % fb for fb in range(12)] + ["ident"], writes=["psB0"])
            p.dve(lambda e, l=l: e.tensor_tensor(MOD[:, l, :], PS[1][:, 0:96].rearrange("p (j t) -> p j t", t=2)[:, :, 0],
                                                modb[:, l * 48:(l + 1) * 48], ALU.add),
                  reads=["psB0", "modb"], writes=["MOD%d" % l])
            if l == 0:
                p.dve(lambda e: e.tensor_tensor(MODC[:], PS[1][:, 0:32].rearrange("p (j t) -> p j t", t=2)[:, :, 1],
                                                modb[:, 0:16], ALU.add),
                      reads=["psB0", "modb"], writes=["MODC"])
        for l in ([1] if tail_only else [0, 1]):
            for n in range(2):
                sc = MOD[:, l, (1 + 3 * n) * 8:(2 + 3 * n) * 8]
                p.dve(lambda e, l=l, n=n, sc=sc: e.scalar_tensor_tensor(GS[:, l * 2 + n, :], sc, 1.0, NG[:, n * 16 + l * 8:n * 16 + l * 8 + 8],
                                                                      op0=ALU.add, op1=ALU.mult),
                      reads=["MOD%d" % l, "NG"], writes=["GS%d%d" % (l, n)])
        if not tail_only:
            p.dve(lambda e: e.scalar_tensor_tensor(GSC[:], MODC[:, 8:16], 1.0, NG[:, 0:8], op0=ALU.add, op1=ALU.mult),
                  reads=["MODC", "NG"], writes=["GSC"])
        for tt in range(32):
            xb = xin[tt % 2]
            xk = "xin%d" % (tt % 2)
            p.dma("sp", xk, lambda e, xb=xb, tt=tt: e.dma_start(out=xb[:], in_=x_d[tt * 128:(tt + 1) * 128, :]), writes=[xk])
            pst = PS[2 + tt % 2]
            pk = "psX%d" % (tt % 2)

            def trx(e, xb=xb, pst=pst):
                for c in range(8):
                    ins = e.matmul(pst[:, c * 128:(c + 1) * 128], xb[:, c * 128:(c + 1) * 128], ident[:], start=True, stop=True)
                return ins
            p.pe(trx, reads=[xk, "ident"], writes=[pk])
            dst = HT[:, :, tt * 128:(tt + 1) * 128]
            src = pst[:, :].rearrange("p (c t) -> p c t", t=128)
            if tt % 2 == 0:
                p.act(lambda e, dst=dst, src=src: e.activation(dst, src, AF.Identity), reads=[pk], writes=["HT%d" % tt])
            else:
                p.dve(lambda e, dst=dst, src=src: e.tensor_copy(dst, src), reads=[pk], writes=["HT%d" % tt])
        ph.done()

        def norm_block(p, PSst, pskey, t0, T, gs, sh, sqb, rstd, tmpb, dst_fn, tag, src=None, srckey="HT"):
            srcT = HT if src is None else src
            for c in range(8):
                sq = sqb[c % 2]
                p.act(lambda e, sq=sq, c=c: e.activation(sq[:, 0:T], srcT[:, c, t0:t0 + T], AF.Square),
                      reads=[srckey], writes=["sq%s%d" % (tag, c % 2)])
                p.pe(lambda e, sq=sq, c=c: e.matmul(PSst[:, 0:T], ones_bf[:], sq[:, 0:T], start=(c == 0), stop=(c == 7)),
                     reads=["sq%s%d" % (tag, c % 2), "ones_bf"], writes=[pskey])
            p.act(lambda e: e.activation(rstd[:, 0:T], PSst[:, 0:T], AF.Sqrt, bias=EPSB[:, 0:1]), reads=[pskey], writes=["rstd0" + tag])
            p.dve(lambda e: e.reciprocal(rstd[:, 0:T], rstd[:, 0:T]), reads=["rstd0" + tag], writes=["rstd" + tag])
            for c in range(8):
                tb = tmpb[c % 2]
                p.dve(lambda e, tb=tb, c=c: e.scalar_tensor_tensor(tb[:, 0:T], srcT[:, c, t0:t0 + T], gs[:, c:c + 1], rstd[:, 0:T],
                                                                  op0=ALU.mult, op1=ALU.mult),
                      reads=[srckey, "rstd" + tag], writes=["tmp%s%d" % (tag, c % 2)])
                dst, dkey = dst_fn(c)
                p.act(lambda e, tb=tb, c=c, dst=dst: e.activation(dst, tb[:, 0:T], AF.Identity, bias=sh[:, c:c + 1]),
                      reads=["tmp%s%d" % (tag, c % 2)], writes=[dkey])

        def wout_phase(wout_d, l):
            ph = Phase(nc)
            p = ph.p
            PS = ph.psum4()
            w = ph.sb([128, 8, D], BF16)
            mixb = [ph.sb([128, 8, 512], BF16) for _ in range(2)]
            p.dma("pool", "w", lambda e: e.dma_start(out=w[:], in_=wout_d.rearrange("(c p) n -> p c n", p=128)), writes=["w"])
            for b in range(8):
                mb = mixb[b % 2]
                mk = "mix%d" % (b % 2)
                p.dma("sp", mk, lambda e, mb=mb, b=b: e.dma_start(out=mb[:], in_=MIX_d[:, :, b * 512:(b + 1) * 512].rearrange("c p t -> p c t")),
                      writes=[mk])
                for f in range(8):
                    pst = PS[f % 4][:, 0:512]
                    pk = "psY%d" % (f % 4)

                    def mm(e, mb=mb, f=f, pst=pst):
                        for k in range(8):
                            ins = e.matmul(pst, w[:, k, f * 128:(f + 1) * 128], mb[:, k, :], start=(k == 0), stop=(k == 7))
                        return ins
                    p.pe(mm, reads=["w", mk], writes=[pk])
                    hsl = HT[:, f, b * 512:(b + 1) * 512]
                    p.dve(lambda e, pst=pst, f=f, hsl=hsl: e.scalar_tensor_tensor(hsl, pst, MOD[:, l, 16 + f:17 + f], hsl, op0=ALU.mult, op1=ALU.add),
                          reads=[pk], writes=["HT"])
            ph.done()

        def router_moe(l):
            with ExitStack() as rstack:
                _CNT[0] += 1
                combT = rstack.enter_context(nc.sbuf_tensor("combT%d" % _CNT[0], [32, S], BF16))
                router_moe_inner(l, combT)

        def router_moe_inner(l, combT):
            ph = Phase(nc)
            p = ph.p
            PS = ph.psum4()
            sqb = [ph.sb([128, 512], BF16) for _ in range(2)]
            rstd = ph.sb([128, 512], F32)
            tmpb = [ph.sb([128, 512], F32) for _ in range(2)]
            a2f = ph.sb([128, 8, 512], F32)
            a2b = [ph.sb([128, 8, 512], BF16) for _ in range(2)]
            rw = ph.sb([128, 8, 20], F32)
            rbb = ph.sb([128, 80], F32)
            lgT = ph.sb([32, 512], F32)
            LG = ph.sb([128, 32, 20], F32)
            p.dma("sp", "rw", lambda e: e.dma_start(out=rw[:], in_=rw_d[l].rearrange("(c p) n -> p c n", p=128)), writes=["rw"])
            p.dma("sp", "rw", lambda e: e.dma_start(out=rbb[:], in_=rb_d[:, l * 80:(l + 1) * 80]), writes=["rbb"])
            gs = GS[:, l * 2 + 1, :]
            sh = MOD[:, l, 24:32]
            for b in range(8):
                af = a2f
                ab = a2b[b % 2]
                abk = "a2b%d" % (b % 2)
                norm_block(p, PS[0], "psS", b * 512, 512, gs, sh, sqb, rstd, tmpb,
                           lambda c, af=af: (af[:, c, :], "a2f_%d" % c), "n")
                akeys = ["a2f_%d" % c for c in range(8)]
                p.pool(lambda e, af=af, ab=ab: e.tensor_copy(ab[:], af[:]), reads=akeys, writes=[abk])
                p.dma("sp", "a2st%d" % (b % 2), lambda e, ab=ab, b=b: e.dma_start(
                    out=A2_d[b, :, :], in_=ab[:].rearrange("p c t -> p (c t)")), reads=[abk], writes=["A2"])

                def rmm(e, af=af):
                    for c in range(8):
                        ins = e.matmul(PS[1][0:20, 0:512], rw[:, c, :], af[:, c, :], start=(c == 0), stop=(c == 7))
                    return ins
                p.pe(rmm, reads=["rw"] + akeys, writes=["psR"])
                p.act(lambda e: e.activation(lgT[0:20, :], PS[1][0:20, 0:512], AF.Identity), reads=["psR"], writes=["lgT"])

                def rtr(e):
                    for tt in range(4):
                        ins = e.matmul(PS[2][:, tt * 32:tt * 32 + 20], lgT[0:20, tt * 128:(tt + 1) * 128], ident[0:20, 0:20], start=True, stop=True)
                    return ins
                p.pe(rtr, reads=["lgT", "ident"], writes=["psT"])
                p.dve(lambda e, b=b: e.tensor_tensor(LG[:, b * 4:(b + 1) * 4, :], PS[2][:, 0:128].rearrange("p (t n) -> p t n", n=32)[:, :, 0:20],
                                                    rbb[:].rearrange("p (t n) -> p t n", n=20), ALU.add),
                      reads=["psT", "rbb"], writes=["LG"])
            NT = 32
            RT = ph.sb([128, 2880], F32)
            _o = [0]

            def rt2():
                a = _o[0]
                _o[0] += NT
                return RT[:, a:a + NT]

            def rt3():
                a = _o[0]
                _o[0] += NT * 4
                return RT[:, a:a + NT * 4].rearrange("p (n g) -> p n g", g=4)

            def rt4():
                a = _o[0]
                _o[0] += NT * 16
                return RT[:, a:a + NT * 16].rearrange("p (n g i) -> p n g i", g=4, i=4)

            def rt16():
                a = _o[0]
                _o[0] += NT * 16
                return RT[:, a:a + NT * 16].rearrange("p (n k) -> p n k", k=16)
            gmax = rt2()
            ohg = rt3()
            gd = rt3()
            gsum = rt2()
            gp = rt2()
            t44 = rt4()
            esel = rt3()
            e1 = rt2()
            sel1 = rt3()
            em = rt3()
            e2 = rt2()
            sel2 = rt3()
            dd = rt2()
            w1 = rt2()
            w2 = rt2()
            ce = rt3()
            ce2 = rt3()
            comb = rt16()
            chf = rt16()
            assert _o[0] <= 2880
            chl = ph.sb([128, NT, 32], BF16)
            gl = LG[:, :, 0:4]
            el = LG[:, :, 4:20].rearrange("p n (g i) -> p n g i", i=4)

            def b3(ap2):
                return ap2.unsqueeze(2).to_broadcast([128, NT, 4])
            p.dve(lambda e: e.tensor_reduce(gmax[:], gl, AX.X, ALU.max), reads=["LG"], writes=["gmax"])
            p.dve(lambda e: e.tensor_tensor(ohg[:], gl, b3(gmax[:]), ALU.is_equal), reads=["LG", "gmax"], writes=["ohg"])
            p.dve(lambda e: e.tensor_tensor(gd[:], gl, b3(gmax[:]), ALU.subtract), reads=["LG", "gmax"], writes=["gd"])
            p.act(lambda e: e.activation(gd[:], gd[:], AF.Exp), reads=["gd"], writes=["ge"])
            p.dve(lambda e: e.tensor_reduce(gsum[:], gd[:], AX.X, ALU.add), reads=["ge"], writes=["gsum"])
            p.dve(lambda e: e.reciprocal(gp[:], gsum[:]), reads=["gsum"], writes=["gp"])
            p.dve(lambda e: e.tensor_tensor(t44[:], el, ohg[:].unsqueeze(3).to_broadcast([128, NT, 4, 4]), ALU.mult),
                  reads=["LG", "ohg"], writes=["t44"])
            p.dve(lambda e: e.tensor_reduce(esel[:], t44[:].rearrange("p n g i -> p n i g"), AX.X, ALU.add), reads=["t44"], writes=["esel"])
            p.dve(lambda e: e.tensor_reduce(e1[:], esel[:], AX.X, ALU.max), reads=["esel"], writes=["e1"])
            p.dve(lambda e: e.tensor_tensor(sel1[:], esel[:], b3(e1[:]), ALU.is_equal), reads=["esel", "e1"], writes=["sel1"])
            p.dve(lambda e: e.scalar_tensor_tensor(em[:], sel1[:], -1e30, esel[:], op0=ALU.mult, op1=ALU.add), reads=["sel1", "esel"], writes=["em"])
            p.dve(lambda e: e.tensor_reduce(e2[:], em[:], AX.X, ALU.max), reads=["em"], writes=["e2"])
            p.dve(lambda e: e.tensor_tensor(sel2[:], em[:], b3(e2[:]), ALU.is_equal), reads=["em", "e2"], writes=["sel2"])
            p.dve(lambda e: e.tensor_tensor(dd[:], e2[:], e1[:], ALU.subtract), reads=["e1", "e2"], writes=["dd"])
            p.act(lambda e: e.activation(dd[:], dd[:], AF.Exp), reads=["dd"], writes=["ex"])
            p.dve(lambda e: e.tensor_scalar(w1[:], dd[:], 1.0, None, op0=ALU.add), reads=["ex"], writes=["w1a"])
            p.dve(lambda e: e.reciprocal(w1[:], w1[:]), reads=["w1a"], writes=["w1"])
            p.dve(lambda e: e.tensor_tensor(w2[:], dd[:], w1[:], ALU.mult), reads=["ex", "w1"], writes=["w2a"])
            p.dve(lambda e: e.tensor_tensor(w1[:], w1[:], gp[:], ALU.mult), reads=["w1", "gp", "w2a"], writes=["wt1"])
            p.dve(lambda e: e.tensor_tensor(w2[:], w2[:], gp[:], ALU.mult), reads=["w2a", "gp"], writes=["wt2"])
            p.dve(lambda e: e.tensor_tensor(ce[:], sel1[:], b3(w1[:]), ALU.mult), reads=["sel1", "wt1"], writes=["ce"])
            p.dve(lambda e: e.tensor_tensor(ce2[:], sel2[:], b3(w2[:]), ALU.mult), reads=["sel2", "wt2"], writes=["ce2"])
            p.dve(lambda e: e.tensor_tensor(ce[:], ce[:], ce2[:], ALU.add), reads=["ce", "ce2"], writes=["cef"])
            p.dve(lambda e: e.tensor_tensor(comb[:].rearrange("p n (g i) -> p n g i", i=4),
                                            ohg[:].unsqueeze(3).to_broadcast([128, NT, 4, 4]),
                                            ce[:].unsqueeze(2).to_broadcast([128, NT, 4, 4]), ALU.mult),
                  reads=["ohg", "cef"], writes=["comb"])
            p.dve(lambda e: e.tensor_copy(chl[:, :, 0:16], comb[:]), reads=["comb"], writes=["chi"])
            p.dve(lambda e: e.tensor_copy(chf[:], chl[:, :, 0:16]), reads=["chi"], writes=["chf"])
            p.dve(lambda e: e.tensor_tensor(chf[:], comb[:], chf[:], ALU.subtract), reads=["comb", "chf"], writes=["clo"])
            p.dve(lambda e: e.tensor_copy(chl[:, :, 16:32], chf[:]), reads=["clo"], writes=["chl"])
            for q in range(8):
                pst = PS[3][:, (q % 2) * 512:(q % 2) * 512 + 512]
                pk = "psC%d" % (q % 2)

                def ctr(e, q=q, pst=pst):
                    for tt in range(4):
                        ins = e.matmul(pst[0:32, tt * 128:(tt + 1) * 128], chl[:, q * 4 + tt, :], ident_bf[:], start=True, stop=True)
                    return ins
                p.pe(ctr, reads=["chl", "chi", "ident_bf"], writes=[pk])
                p.act(lambda e, q=q, pst=pst: e.activation(combT[:, q * 512:(q + 1) * 512], pst[0:32, :], AF.Identity), reads=[pk], writes=["combT"])
            ph.done()
            if stage == 10 + l or (stage == 8 and l == 1):
                return
            ph = Phase(nc)
            p = ph.p
            PS = ph.psum4()
            T = 512
            NB = S // T
            wslot = [(ph.sb([128, 8, 256], BF16), ph.sb([128, 8, 256], BF16), ph.sb([128, 2, D], BF16)) for _ in range(3)]
            a2 = [ph.sb([128, 8, T], BF16) for _ in range(2)]
            sel = ph.sb([32, 2048], BF16)
            cb = ph.sb([128, T], F32)
            sg = [ph.sb([128, T], F32)] * 2
            tt_ = ph.sb([128, T], F32)
            hb0 = [ph.sb([128, 2, T], BF16) for _ in range(2)]
            hb1 = ph.sb([128, 2, T], BF16)
            p.dma("pool", "sel", lambda e: e.dma_start(out=sel[:, 0:1024], in_=sel_d[:, 0:1024]), writes=["sel"])
            p.dma("pool", "sel", lambda e: e.dma_start(out=sel[:, 1024:2048], in_=sel_d[:, 1024:2048]), writes=["sel"])
            g2 = MOD[:, l, 40:48]
            RB = [PS[0][:, 0:512], PS[0][:, 512:1024], PS[1][:, 0:512]]
            RBK = ["psR0", "psR1", "psR2"]
            CBP = PS[1][:, 512:1024]
            ACC = [PS[2][:, 0:512], PS[2][:, 512:1024], PS[3][:, 0:512], PS[3][:, 512:1024]]

            def load_expert(ex):
                sl = ex % 3
                wg, wu, wd = wslot[sl]
                wk = "w%d" % sl
                p.dma("pool", wk, lambda e: e.dma_start(out=wg[:], in_=wg_d[l, ex].rearrange("(c p) n -> p c n", p=128)), writes=[wk + "g"])
                p.dma("pool", wk, lambda e: e.dma_start(out=wu[:], in_=wu_d[l, ex].rearrange("(c p) n -> p c n", p=128)), writes=[wk + "u"])
                p.dma("pool", wk, lambda e: e.dma_start(out=wd[:], in_=wd_d[l, ex].rearrange("(c p) n -> p c n", p=128)), writes=[wk + "d"])
            st = {"step": 0, "rk": 0, "sgi": 0}

            def hbuf(b, j):
                if j == 0:
                    return hb0[b % 2], "h0_%d" % (b % 2)
                return hb1, "h1"

            def G(pr, b, j):
                ex = pr * 2 + j
                sl = ex % 3
                wg, wu, wd = wslot[sl]
                wk = "w%d" % sl
                if j == 0:
                    ab = a2[st["step"] % 2]
                    ak = "a2_%d" % (st["step"] % 2)
                    st["cur"] = (ab, ak)
                    st["step"] += 1
                    p.dma("sp", ak, lambda e: e.dma_start(out=ab[:].rearrange("p c t -> p (c t)"), in_=A2_d[b, :, :]), writes=[ak])
                ab, ak = st["cur"]
                hbb, hk = hbuf(b, j)
                p.pe(lambda e: e.matmul(CBP, sel[:, ex * 128:(ex + 1) * 128], combT[:, b * T:(b + 1) * T], start=True, stop=True),
                     reads=["sel", "combT"], writes=["psCB"])
                p.act(lambda e: e.activation(cb[:], CBP, AF.Identity), reads=["psCB"], writes=["cb"])
                for f2 in range(2):
                    pg = RB[st["rk"] % 3]
                    pgk = RBK[st["rk"] % 3]
                    st["rk"] += 1
                    pu = RB[st["rk"] % 3]
                    puk = RBK[st["rk"] % 3]
                    st["rk"] += 1

                    def gmm(e, pg=pg, f2=f2):
                        for k in range(8):
                            ins = e.matmul(pg, wg[:, k, f2 * 128:(f2 + 1) * 128], ab[:, k, :], start=(k == 0), stop=(k == 7))
                        return ins

                    def umm(e, pu=pu, f2=f2):
                        for k in range(8):
                            ins = e.matmul(pu, wu[:, k, f2 * 128:(f2 + 1) * 128], ab[:, k, :], start=(k == 0), stop=(k == 7))
                        return ins
                    p.pe(gmm, reads=[wk + "g", ak], writes=[pgk])
                    p.pe(umm, reads=[wk + "u", ak], writes=[puk])
                    sgb = sg[st["sgi"] % 2]
                    sgk = "sg0"
                    st["sgi"] += 1
                    p.act(lambda e, sgb=sgb, pg=pg: e.activation(sgb[:], pg, AF.Silu), reads=[pgk], writes=[sgk])
                    p.dve(lambda e, pu=pu: e.tensor_tensor(tt_[:], pu, cb[:], ALU.mult), reads=[puk, "cb"], writes=["tt"])
                    p.pool(lambda e, sgb=sgb, f2=f2: e.tensor_tensor(hbb[:, f2, :], tt_[:], sgb[:], ALU.mult),
                           reads=["tt", sgk], writes=[hk + "_%d" % f2])

            def DOWN(pr, b, half):
                hs = []
                for j in range(2):
                    ex = pr * 2 + j
                    wg, wu, wd = wslot[ex % 3]
                    hbb, hk = hbuf(b, j)
                    hs.append((wd, "w%dd" % (ex % 3), hbb, hk))
                for fi in range(4):
                    f = half * 4 + fi

                    def dmm(e, fi=fi, f=f):
                        for j in range(2):
                            wd, wdk, hbb, hk = hs[j]
                            for k in range(2):
                                ins = e.matmul(ACC[fi], wd[:, k, f * 128:(f + 1) * 128], hbb[:, k, :], start=(j == 0 and k == 0), stop=(j == 1 and k == 1))
                        return ins
                    p.pe(dmm, reads=[hs[0][1], hs[1][1], hs[0][3] + "_0", hs[0][3] + "_1", hs[1][3] + "_0", hs[1][3] + "_1"], writes=["psD%d" % fi])
                for fi in range(4):
                    f = half * 4 + fi
                    hsl = HT[:, f, b * T:(b + 1) * T]
                    p.dve(lambda e, fi=fi, f=f, hsl=hsl: e.scalar_tensor_tensor(hsl, ACC[fi], g2[:, f:f + 1], hsl, op0=ALU.mult, op1=ALU.add),
                          reads=["psD%d" % fi], writes=["HT"])

            load_expert(0)
            load_expert(1)
            for pr in range(8):
                if pr > 0:
                    load_expert(2 * pr + 1)
                G(pr, 0, 0)
                G(pr, 0, 1)
                if pr < 7:
                    load_expert(2 * pr + 2)
                for b in range(NB):
                    DOWN(pr, b, 0)
                    if b + 1 < NB:
                        G(pr, b + 1, 0)
                    DOWN(pr, b, 1)
                    if b + 1 < NB:
                        G(pr, b + 1, 1)
            ph.done()

        def store_phase():
            ph = Phase(nc)
            p = ph.p
            PS = ph.psum4()
            ob = [ph.sb([128, 4, D], F32) for _ in range(2)]
            for tt in range(32):
                pst = PS[tt % 2]
                pk = "psO%d" % (tt % 2)

                def tr(e, tt=tt, pst=pst):
                    for c in range(8):
                        ins = e.matmul(pst[:, c * 128:(c + 1) * 128], HT[:, c, tt * 128:(tt + 1) * 128], ident[:], start=True, stop=True)
                    return ins
                p.pe(tr, reads=["HT", "ident"], writes=[pk])
                g4 = tt // 4
                o = ob[g4 % 2]
                ok = "ob%d_%d" % (g4 % 2, tt % 4)
                if tt % 2 == 0:
                    p.act(lambda e, o=o, pst=pst, tt=tt: e.activation(o[:, tt % 4, :], pst[:, :], AF.Identity), reads=[pk], writes=[ok])
                else:
                    p.dve(lambda e, o=o, pst=pst, tt=tt: e.tensor_copy(o[:, tt % 4, :], pst[:, :]), reads=[pk], writes=[ok])
                if tt % 4 == 3:
                    p.dma("sp", "ost%d" % (g4 % 2), lambda e, o=o, g4=g4: e.dma_start(
                        out=out_d[g4 * 512:(g4 + 1) * 512, :].rearrange("(t p) n -> p t n", p=128), in_=o[:]),
                        reads=["ob%d_%d" % (g4 % 2, q) for q in range(4)], writes=["out"])
            ph.done()

        def qk_chain(p, PS, psA, kA, psB, kB, psC, T, gcol, gpcol, cosb, sinb, tq, dst, dstkey, rope=True):
            sqq, rsq, t1, t2 = tq
            p.act(lambda e: e.activation(sqq[:, 0:T], psA, AF.Square), reads=[kA], writes=["sqq"])
            p.pe(lambda e: e.matmul(psC[:, 0:T], onesblk_bf[:], sqq[:, 0:T], start=True, stop=True), reads=["sqq", "onesblk"], writes=["psC"])
            p.act(lambda e: e.activation(rsq[:, 0:T], psC[:, 0:T], AF.Sqrt, bias=EPSB[:, 0:1]), reads=["psC"], writes=["rsq0"])
            p.dve(lambda e: e.reciprocal(rsq[:, 0:T], rsq[:, 0:T]), reads=["rsq0"], writes=["rsq"])
            if rope:
                p.dve(lambda e: e.scalar_tensor_tensor(t1[:, 0:T], psA, gcol, cosb[:, 0:T], op0=ALU.mult, op1=ALU.mult),
                      reads=[kA, "cosb", "gains"], writes=["t1"])
                p.dve(lambda e: e.scalar_tensor_tensor(t2[:, 0:T], psB, gpcol, sinb[:, 0:T], op0=ALU.mult, op1=ALU.mult),
                      reads=[kB, "sinb", "gains"], writes=["t2"])
                p.pool(lambda e: e.tensor_tensor(t1[:, 0:T], t1[:, 0:T], t2[:, 0:T], ALU.add), reads=["t1", "t2"], writes=["t3"])
                p.dve(lambda e: e.tensor_tensor(dst, t1[:, 0:T], rsq[:, 0:T], ALU.mult), reads=["t3", "rsq"], writes=[dstkey])
            else:
                p.dve(lambda e: e.scalar_tensor_tensor(dst, psA, gcol, rsq[:, 0:T], op0=ALU.mult, op1=ALU.mult),
                      reads=[kA, "rsq", "gains"], writes=[dstkey])

        def layer0_mixer():
            ph = Phase(nc)
            p = ph.p
            PS = ph.psum4()
            T = 256
            wq = ph.sb([128, 8, 1792], BF16)
            wv = ph.sb([128, 8, 128], BF16)
            gains = ph.sb([128, 4], F32)
            sqb = [ph.sb([128, T], BF16) for _ in range(2)]
            rstd = ph.sb([128, T], F32)
            tmpb = [ph.sb([128, T], F32) for _ in range(2)]
            aTs = [ph.sb([128, 8, T], BF16) for _ in range(2)]
            cur = {"aT": aTs[1], "k": ["aT1_%d" % c for c in range(8)]}
            cosb = ph.sb([128, T], F32)
            sinb = ph.sb([128, T], F32)
            tq = (ph.sb([128, T], BF16), ph.sb([128, T], F32), ph.sb([128, T], F32), ph.sb([128, T], F32))
            qf = [ph.sb([128, T], BF16) for _ in range(2)]
            qf4 = ph.sb([128, 4, T], BF16)
            pst4 = ph.sb([128, 4, T], F32)
            vst = [ph.sb([128, 2, 193], BF16) for _ in range(2)]
            ctin = ph.sb([128, D], F32)
            CT = ph.sb([128, 8, 256], F32)
            p.dma("sp", "c1", lambda e: e.dma_start(out=gains[:], in_=gains_d[:, :]), writes=["gains"])
            for vi in range(2):
                p.dve(lambda e, vi=vi: e.memset(vst[vi][:], 0.0), writes=["vst%d" % vi])
                p.dve(lambda e, vi=vi: e.memset(vst[vi][:, :, 64:66], 1.0), reads=["vst%d" % vi], writes=["vst%d" % vi])
            p.dma("pool", "wq", lambda e: e.dma_start(out=wq[:], in_=wqkp_d.rearrange("(c p) n -> p c n", p=128)), writes=["wq"])
            p.dma("pool", "wq", lambda e: e.dma_start(out=wv[:], in_=wv_d.rearrange("(c p) n -> p c n", p=128)), writes=["wv"])
            B_ST = PS[0][:, 0:512]
            B_A = [PS[0][:, 512:1024], PS[1][:, 0:512]]
            B_B = [PS[1][:, 512:1024], PS[2][:, 0:512]]
            B_C = PS[2][:, 512:1024]
            B_M = [PS[3][:, 0:512], PS[3][:, 512:1024]]
            for tt in range(2):
                p.dma("sp", "ctin", lambda e, tt=tt: e.dma_start(out=ctin[:], in_=ctx_d[tt * 128:(tt + 1) * 128, :]), writes=["ctin"])
                for half in range(2):
                    bm = B_M[half]

                    def trc(e, half=half, bm=bm):
                        for c in range(4):
                            cc = half * 4 + c
                            ins = e.matmul(bm[:, c * 128:(c + 1) * 128], ctin[:, cc * 128:(cc + 1) * 128], ident[:], start=True, stop=True)
                        return ins
                    p.pe(trc, reads=["ctin", "ident"], writes=["psM%d" % half])
                    p.dve(lambda e, half=half, bm=bm, tt=tt: e.tensor_copy(CT[:, half * 4:half * 4 + 4, tt * 128:(tt + 1) * 128],
                                                                          bm.rearrange("p (c t) -> p c t", t=128)),
                          reads=["psM%d" % half], writes=["CT"])
            mcnt = [0]

            def proj(p, col0, bank, bkey, T=T):
                aT = cur["aT"]

                def mm(e):
                    for k in range(8):
                        ins = e.matmul(bank[:, 0:T], wq[:, k, col0:col0 + 128], aT[:, k, :], start=(k == 0), stop=(k == 7))
                    return ins
                p.pe(mm, reads=["wq"] + cur["k"], writes=[bkey])

            def vproj(p, tile0):
                i = mcnt[0] % 2
                mcnt[0] += 1
                bm = B_M[i]
                bk = "psM%d" % i
                vs_ = vst[i]
                aT = cur["aT"]

                def mm(e):
                    for t2 in range(2):
                        for k in range(8):
                            ins = e.matmul(bm[:, t2 * 128:(t2 + 1) * 128], aT[:, k, t2 * 128:(t2 + 1) * 128], wv[:, k, :], start=(k == 0), stop=(k == 7))
                    return ins
                p.pe(mm, reads=["wv"] + cur["k"], writes=[bk])
                p.dve(lambda e: e.tensor_copy(vs_[:, :, 0:64], bm[:, 0:256].rearrange("p (t n) -> p t n", n=128)[:, :, 0:64]),
                      reads=[bk], writes=["vst%da" % i])
                p.dve(lambda e: e.tensor_copy(vs_[:, :, 129:193], bm[:, 0:256].rearrange("p (t n) -> p t n", n=128)[:, :, 64:128]),
                      reads=[bk, "vst%da" % i], writes=["vst%d" % i])
                p.dma("sp", "vsst%d" % i, lambda e: e.dma_start(out=VS_d[:, tile0:tile0 + 2, :], in_=vs_[:]), reads=["vst%d" % i, "vst%da" % i], writes=["VSd"])

            def nrm(b):
                bi = b % 2
                norm_block(p, B_ST, "psST", b * T, T, GS[:, 0, :], MOD[:, 0, 0:8], sqb, rstd, tmpb,
                           lambda c: (aTs[bi][:, c, :], "aT%d_%d" % (bi, c)), "m")
            norm_block(p, B_ST, "psST", 0, 256, GSC, MODC, sqb, rstd, tmpb, lambda c: (aTs[1][:, c, :], "aT1_%d" % c), "m", src=CT, srckey="CT")
            nrm(0)
            proj(p, 512, B_A[0], "psA0")
            qk_chain(p, PS, B_A[0][:, 0:T], "psA0", None, None, B_C, T, gains[:, 2:3], None, None, None, tq, qf[0][:, 0:T], "qf0", rope=False)
            p.dma("sp", "qst0", lambda e: e.dma_start(out=KT_d[:, 0:256], in_=qf[0][:, 0:T]), reads=["qf0"], writes=["KTd"])
            vproj(p, 0)
            qi = 1
            for b in range(S // T):
                t0 = b * T
                p.dma("sp", "cs", lambda e, t0=t0: e.dma_start(out=cosb[:], in_=cos_d[:, t0:t0 + T]), writes=["cosb"])
                p.dma("sp", "cs", lambda e, t0=t0: e.dma_start(out=sinb[:], in_=sin_d[:, t0:t0 + T]), writes=["sinb"])
                cur["aT"] = aTs[b % 2]
                cur["k"] = ["aT%d_%d" % (b % 2, c) for c in range(8)]
                if b + 1 < S // T:
                    nrm(b + 1)
                for j in range(5):
                    i = qi % 2
                    qi += 1
                    col = j * 128 if j < 4 else 512
                    colp = 1152 + j * 128 if j < 4 else 1664
                    proj(p, col, B_A[i], "psA%d" % i)
                    proj(p, colp, B_B[i], "psB%d" % i)
                    gcol = gains[:, 0:1] if j < 4 else gains[:, 2:3]
                    gpcol = gains[:, 1:2] if j < 4 else gains[:, 3:4]
                    if j < 4:
                        qk_chain(p, PS, B_A[i][:, 0:T], "psA%d" % i, B_B[i][:, 0:T], "psB%d" % i, B_C, T, gcol, gpcol, cosb, sinb, tq,
                                 qf4[:, j, :], "qf4_%d" % j)
                        if j == 3:
                            p.dma("sp", "qst4", lambda e, t0=t0: e.dma_start(out=QT_d[:, :, t0:t0 + T].rearrange("c p t -> p c t"), in_=qf4[:]),
                                  reads=["qf4_%d" % q for q in range(4)], writes=["QTd"])
                    else:
                        qk_chain(p, PS, B_A[i][:, 0:T], "psA%d" % i, B_B[i][:, 0:T], "psB%d" % i, B_C, T, gcol, gpcol, cosb, sinb, tq,
                                 qf[i][:, 0:T], "qf%d" % i)
                        p.dma("sp", "qst%d" % i, lambda e, i=i, t0=t0: e.dma_start(out=KT_d[:, 256 + t0:256 + t0 + T], in_=qf[i][:, 0:T]),
                              reads=["qf%d" % i], writes=["KTd"])
                for g in range(4):
                    i = mcnt[0] % 2
                    mcnt[0] += 1
                    proj(p, 640 + g * 128, B_M[i], "psM%d" % i)
                    p.act(lambda e, i=i, g=g: e.activation(pst4[:, g, :], B_M[i][:, 0:T], AF.Identity), reads=["psM%d" % i], writes=["qpst4_%d" % g])
                    if g == 3:
                        p.dma("sp", "pst4", lambda e, t0=t0: e.dma_start(out=PT_d[:, :, t0:t0 + T].rearrange("c p t -> p c t"), in_=pst4[:]),
                              reads=["qpst4_%d" % q for q in range(4)], writes=["PTd"])
                vproj(p, 2 + 2 * b)
            ph.done()
            if stage == 20:
                return
            ph = Phase(nc)
            p = ph.p
            PS = ph.psum4()
            KT = ph.sb([128, 4352], BF16)
            VS = ph.sb([128, 34, 193], BF16)
            Qb = [ph.sb([128, 512], BF16) for _ in range(2)]
            Pb = [ph.sb([128, 1024], BF16) for _ in range(3)]
            rr = ph.sb([128, 512], F32)
            bcs = ph.sb([128, 512], F32)
            mixo = [ph.sb([128, 512], BF16) for _ in range(2)]
            p.dma("sp", "kt", lambda e: e.dma_start(out=KT[:], in_=KT_d[:, :]), writes=["KT"])
            p.dma("sp", "kt", lambda e: e.dma_start(out=VS[:], in_=VS_d[:, :, :]), writes=["VS"])
            SB_ = [PS[0], PS[1]]
            OA = PS[2][:, 0:512]
            OB = PS[2][:, 512:1024]
            BCA = PS[3][:, 0:512]
            BCB = PS[3][:, 512:1024]
            NKT = 24 if stage == 7 else 34
            units = [(j, qb) for j in range(4) for qb in range(8)]
            steps = [(u, kt) for u in range(len(units)) for kt in range(NKT)]
            qbuf = {}

            def load_q(u):
                j, qb = units[u]
                qt = Qb[u % 2]
                qk_ = "Qb%d" % (u % 2)
                p.dma("sp", qk_, lambda e: e.dma_start(out=qt[:], in_=QT_d[j, :, qb * 512:(qb + 1) * 512]), writes=[qk_])
                qbuf[u] = (qt, qk_)

            def S_(i):
                u, kt = steps[i]
                if kt == 0:
                    load_q(u)
                qt, qk_ = qbuf[u]
                sb_ = SB_[i % 2]
                sk = "psS%d" % (i % 2)

                def smm(e):
                    e.matmul(sb_[:, 0:512], KT[0:64, kt * 128:(kt + 1) * 128], qt[0:64, :], start=True, stop=True)
                    return e.matmul(sb_[:, 512:1024], KT[64:128, kt * 128:(kt + 1) * 128], qt[64:128, :], start=True, stop=True)
                p.pe(smm, reads=["KT", qk_], writes=[sk])

            def finalize(u):
                j, qb = units[u]
                p.dve(lambda e: e.reciprocal(rr[64:65, :], OA[64:65, :]), reads=["psOA"], writes=["rrA"])
                p.dve(lambda e: e.reciprocal(rr[0:1, :], OB[0:1, :]), reads=["psOB"], writes=["rrB"])
                p.pe(lambda e: e.matmul(BCA[0:64, :], ones_f[64:65, 0:64], rr[64:65, :], start=True, stop=True), reads=["rrA", "ones_f"], writes=["psBCA"])
                p.pe(lambda e: e.matmul(BCB[:, :], ones_f[0:1, :], rr[0:1, :], start=True, stop=True), reads=["rrB", "ones_f"], writes=["psBCB"])
                p.act(lambda e: e.activation(bcs[0:64, :], BCA[0:64, :], AF.Identity), reads=["psBCA"], writes=["bcsA"])
                p.act(lambda e: e.activation(bcs[64:128, :], BCB[64:128, :], AF.Identity), reads=["psBCB"], writes=["bcsB"])
                mo = mixo[u % 2]
                mk = "mixo%d" % (u % 2)
                p.dve(lambda e: e.tensor_tensor(mo[0:64, :], OA[0:64, :], bcs[0:64, :], ALU.mult), reads=["psOA", "bcsA"], writes=[mk + "a"])
                p.dve(lambda e: e.tensor_tensor(mo[64:128, :], OB[64:128, :], bcs[64:128, :], ALU.mult), reads=["psOB", "bcsB"], writes=[mk + "b"])
                p.dma("sp", "mst%d" % (u % 2), lambda e: e.dma_start(out=MIX_d[j, :, qb * 512:(qb + 1) * 512], in_=mo[:]),
                      reads=[mk + "a", mk + "b"], writes=["MIXd"])

            S_(0)
            for i in range(len(steps)):
                u, kt = steps[i]
                if i + 1 < len(steps):
                    S_(i + 1)
                sb_ = SB_[i % 2]
                sk = "psS%d" % (i % 2)
                pb = Pb[i % 3]
                pk = "P%d" % (i % 3)
                p.act(lambda e, pb=pb, sb_=sb_: e.activation(pb[:], sb_[:, :], AF.Exp, scale=0.125), reads=[sk], writes=[pk])

                def pv(e, pb=pb, kt=kt):
                    e.matmul(OA[0:65, :], VS[:, kt, 0:65], pb[:, 0:512], start=(kt == 0), stop=(kt == NKT - 1))
                    return e.matmul(OB[:, :], VS[:, kt, 65:193], pb[:, 512:1024], start=(kt == 0), stop=(kt == NKT - 1))
                p.pe(pv, reads=["VS", pk], writes=["psOA", "psOB"])
                if kt == NKT - 1:
                    finalize(u)
            ph.done()
            ph = Phase(nc)
            p = ph.p
            PS = ph.psum4()
            W = S + 16
            Pf = ph.sb([128, W], F32)
            sa = ph.sb([128, W], F32)
            sb2 = ph.sb([128, W], F32)
            dbf = ph.sb([128, S], BF16)
            pw = ph.sb([128, 512], BF16)
            psc = ph.sb([128, 4], F32)
            edg = ph.sb([128, 64], F32)
            et = ph.sb([128, 16], F32)
            po = [ph.sb([128, 512], BF16) for _ in range(2)]
            p.dma("pool", "pw", lambda e: e.dma_start(out=pw[:], in_=poolw_d[:, :]), writes=["pw"])
            p.dma("sp", "pc", lambda e: e.dma_start(out=psc[:], in_=poolsc_d[:, :]), writes=["psc"])
            p.dma("sp", "pc", lambda e: e.dma_start(out=edg[:], in_=pooledge_d[:, :]), writes=["edg"])
            p.dve(lambda e: e.memset(Pf[:, 0:8], 0.0), writes=["PfL"])
            p.dve(lambda e: e.memset(Pf[:, W - 8:W], 0.0), writes=["PfR"])
            oc = 0
            for g in range(4):
                w_ = 2 ** (g + 1)
                p.dma("sp", "pf", lambda e, g=g: e.dma_start(out=Pf[:, 8:8 + S], in_=PT_d[g, :, :]), writes=["Pf"])
                p.dve(lambda e: e.tensor_tensor(sa[:, 1:W], Pf[:, 0:W - 1], Pf[:, 1:W], ALU.add), reads=["Pf", "PfL", "PfR"], writes=["sa"])
                cur, ck = sa, "sa"
                oth, ok_ = sb2, "sb"
                lo, hi, sh_ = 1, W, 1
                for st in range(g):
                    nlo, nhi = lo + sh_, hi - sh_
                    eng = p.pool if st % 2 == 0 else p.dve
                    eng(lambda e, cur=cur, oth=oth, nlo=nlo, nhi=nhi, sh_=sh_: e.tensor_tensor(oth[:, nlo:nhi], cur[:, nlo - sh_:nhi - sh_], cur[:, nlo + sh_:nhi + sh_], ALU.add),
                        reads=[ck], writes=[ok_])
                    cur, ck, oth, ok_ = oth, ok_, cur, ck
                    lo, hi = nlo, nhi
                    sh_ *= 2
                assert lo <= 8 and hi >= 8 + S
                p.dve(lambda e, cur=cur, w_=w_: e.scalar_tensor_tensor(dbf[:, :], cur[:, 8:8 + S], 1.0 / w_, Pf[:, 8:8 + S], op0=ALU.mult, op1=ALU.subtract),
                      reads=[ck, "Pf"], writes=["dbf0"])
                p.dve(lambda e, cur=cur, g=g: e.tensor_tensor(et[:, 0:8], cur[:, 8:16], edg[:, g * 16:g * 16 + 8], ALU.mult), reads=[ck, "edg"], writes=["et0"])
                p.dve(lambda e, cur=cur, g=g: e.tensor_tensor(et[:, 8:16], cur[:, S:8 + S], edg[:, g * 16 + 8:g * 16 + 16], ALU.mult), reads=[ck, "edg", "et0"], writes=["et1"])
                p.dve(lambda e: e.tensor_tensor(dbf[:, 0:8], et[:, 0:8], Pf[:, 8:16], ALU.subtract), reads=["et1", "Pf", "dbf0"], writes=["dbf1"])
                p.dve(lambda e: e.tensor_tensor(dbf[:, S - 8:S], et[:, 8:16], Pf[:, S:8 + S], ALU.subtract), reads=["et1", "Pf", "dbf1"], writes=["dbf"])
                for b in range(8):
                    i = oc % 2
                    oc += 1
                    bank = PS[i][:, 0:512]
                    p.pe(lambda e, bank=bank, g=g, b=b: e.matmul(bank, pw[:, g * 128:(g + 1) * 128], dbf[:, b * 512:(b + 1) * 512], start=True, stop=True),
                         reads=["pw", "dbf"], writes=["psP%d" % i])
                    p.act(lambda e, bank=bank, i=i, g=g: e.activation(po[i][:], bank, AF.Identity, scale=psc[:, g:g + 1]), reads=["psP%d" % i, "psc"], writes=["po%d" % i])
                    p.dma("sp", "post%d" % i, lambda e, i=i, g=g, b=b: e.dma_start(out=MIX_d[4 + g, :, b * 512:(b + 1) * 512], in_=po[i][:]),
                          reads=["po%d" % i], writes=["MIXd"])
            ph.done()
            wout_phase(wout0_d, 0)

        def layer1_mixer():
            ph = Phase(nc)
            p = ph.p
            PS = ph.psum4()
            T = 256
            w1 = ph.sb([128, 8, 2560], BF16)
            sqb = [ph.sb([128, T], BF16) for _ in range(2)]
            rstd = ph.sb([128, T], F32)
            tmpb = [ph.sb([128, T], F32) for _ in range(2)]
            aTs = [ph.sb([128, 8, T], BF16) for _ in range(2)]
            cur = {"aT": aTs[0], "k": ["aT0_%d" % c for c in range(8)]}
            sggain = ph.sb([128, 512], F32)
            sgwT = ph.sb([128, 512], BF16)
            sgbb = ph.sb([128, 512], F32)
            usb = ph.sb([128, 4, T], F32)
            hxs = [ph.sb([128, T], F32)] * 2
            zs = hxs
            bgs = [ph.sb([128, T], F32)] * 2
            sqv = ph.sb([128, 512], F32)
            ssum = ph.sb([128, 4], F32)
            vt = sqv
            vn = ph.sb([128, 4, 128], BF16)
            st_ = ph.sb([128, 4, 128], F32)
            yc = [ph.sb([128, 4, T], BF16) for _ in range(2)]
            p.dma("pool", "w1", lambda e: e.dma_start(out=w1[:, :, 0:1280], in_=win1_d[:, 0:1280].rearrange("(c p) n -> p c n", p=128)), writes=["w1"])
            p.dma("pool", "w1", lambda e: e.dma_start(out=w1[:, :, 1280:2560], in_=win1_d[:, 1280:2560].rearrange("(c p) n -> p c n", p=128)), writes=["w1"])
            p.dma("pool", "w1", lambda e: e.dma_start(out=sgwT[:], in_=sgw_d[:, :]), writes=["sgwT"])
            p.dma("sp", "c2", lambda e: e.dma_start(out=sggain[:], in_=sggain_d[:, :]), writes=["sggain"])
            p.dma("sp", "c2", lambda e: e.dma_start(out=sgbb[:], in_=sgb_d[:, :]), writes=["sgbb"])
            B_ST = PS[0][:, 0:512]
            B_U = [PS[0][:, 512:1024], PS[1][:, 0:512]]
            B_H = [PS[1][:, 512:1024], PS[2][:, 0:512]]
            B_G = PS[2][:, 512:1024]
            B_V = PS[3][:, 0:512]
            B_S = PS[3][:, 512:1024]
            def proj(col0, tgt, bkey):
                aT = cur["aT"]

                def mm(e):
                    for k in range(8):
                        ins = e.matmul(tgt, w1[:, k, col0:col0 + 128], aT[:, k, :], start=(k == 0), stop=(k == 7))
                    return ins
                p.pe(mm, reads=["w1"] + cur["k"], writes=[bkey])

            def nrm(b):
                bi = b % 2
                norm_block(p, B_ST, "psST", b * T, T, GS[:, 2, :], MOD[:, 1, 0:8], sqb, rstd, tmpb,
                           lambda c: (aTs[bi][:, c, :], "aT%d_%d" % (bi, c)), "m")
            nrm(0)
            hi_ = 0
            for b in range(S // T):
                t0 = b * T
                cur["aT"] = aTs[b % 2]
                cur["k"] = ["aT%d_%d" % (b % 2, c) for c in range(8)]
                akeys = cur["k"]
                aT = cur["aT"]
                if b + 1 < S // T:
                    nrm(b + 1)
                for half in range(2):
                    for q in range(2):
                        g = half * 2 + q
                        proj(g * 128, B_U[half][:, q * T:(q + 1) * T], "psU%d" % half)
                    p.act(lambda e, half=half: e.activation(usb[:, half * 2:half * 2 + 2, :], B_U[half][:, 0:2 * T].rearrange("p (q t) -> p q t", t=T), AF.Identity),
                          reads=["psU%d" % half], writes=["usb%d" % half])
                for c in range(4):
                    i = hi_ % 2
                    hi_ += 1
                    proj(1024 + c * 128, B_H[i][:, 0:T], "psH%d" % i)
                    proj(2048 + c * 128, B_H[i][:, T:2 * T], "psH%d" % i)
                    p.act(lambda e, i=i: e.activation(hxs[i][:], B_H[i][:, 0:T], AF.Identity), reads=["psH%d" % i], writes=["hxs0"])
                    p.dve(lambda e, i=i: e.tensor_tensor(zs[i][:], B_H[i][:, T:2 * T], hxs[i][:], ALU.mult), reads=["psH%d" % i, "hxs0"], writes=["hxs0"])
                    p.dma("sp", "zst%d" % i, lambda e, i=i, c=c, t0=t0: e.dma_start(out=PT_d[c, :, t0:t0 + T], in_=zs[i][:]), reads=["hxs0"], writes=["PTd"])
                    proj(1536 + c * 128, B_G[:, 0:T], "psG")
                    p.act(lambda e, i=i: e.activation(bgs[i][:], B_G[:, 0:T], AF.Identity), reads=["psG"], writes=["bgs0"])
                    p.dma("sp", "bst%d" % i, lambda e, i=i, c=c, t0=t0: e.dma_start(out=BG_d[c, :, t0:t0 + T], in_=bgs[i][:]), reads=["bgs0"], writes=["BGd"])
                yb = yc[b % 2]
                yk = "yc%d" % (b % 2)
                for n in range(2):
                    def vmm(e, n=n, aT=aT):
                        for g in range(4):
                            for k in range(8):
                                ins = e.matmul(B_V[:, g * 128:(g + 1) * 128], aT[:, k, n * 128:(n + 1) * 128], w1[:, k, 512 + g * 128:512 + (g + 1) * 128],
                                               start=(k == 0), stop=(k == 7))
                        return ins
                    p.pe(vmm, reads=["w1"] + akeys, writes=["psV"])
                    p.act(lambda e: e.activation(sqv[:], B_V, AF.Square), reads=["psV"], writes=["sqv"])
                    p.dve(lambda e: e.tensor_reduce(ssum[:], sqv[:].rearrange("p (g c) -> p g c", c=128), AX.X, ALU.add), reads=["sqv"], writes=["ssum0"])
                    p.act(lambda e: e.activation(ssum[:], ssum[:], AF.Sqrt, bias=EPSB[:, 0:1], scale=1.0 / 128.0), reads=["ssum0"], writes=["ssum1"])
                    p.dve(lambda e: e.reciprocal(ssum[:], ssum[:]), reads=["ssum1"], writes=["ssum"])
                    p.dve(lambda e: e.tensor_tensor(vt[:].rearrange("p (g c) -> p g c", c=128), B_V.rearrange("p (g c) -> p g c", c=128), ssum[:].unsqueeze(2).to_broadcast([128, 4, 128]), ALU.mult),
                          reads=["psV", "ssum", "sqv"], writes=["sqv"])
                    p.pool(lambda e: e.tensor_tensor(vn[:].rearrange("p g c -> p (g c)"), vt[:], sggain[:], ALU.mult), reads=["sqv", "sggain"], writes=["vn"])

                    def smm(e):
                        for g in range(4):
                            ins = e.matmul(B_S[:, g * 128:(g + 1) * 128], vn[:, g, :], sgwT[:, g * 128:(g + 1) * 128], start=True, stop=True)
                        return ins
                    p.pe(smm, reads=["vn", "sgwT"], writes=["psS"])
                    p.dve(lambda e: e.tensor_tensor(st_[:], B_S.rearrange("p (g c) -> p g c", c=128), sgbb[:].rearrange("p (g c) -> p g c", c=128), ALU.add),
                          reads=["psS", "sgbb"], writes=["st"])
                    p.pool(lambda e, n=n, yb=yb: e.tensor_tensor(yb[:, :, n * 128:(n + 1) * 128], st_[:], usb[:, :, n * 128:(n + 1) * 128], ALU.mult),
                           reads=["st", "usb0", "usb1"], writes=[yk + "_%d" % n])
                p.dma("sp", "yst%d" % (b % 2), lambda e, yb=yb, t0=t0: e.dma_start(out=MIX_d[0:4, :, t0:t0 + T].rearrange("c p t -> p c t"), in_=yb[:]),
                      reads=[yk + "_0", yk + "_1"], writes=["MIXd"])
            ph.done()
            ph = Phase(nc)
            p = ph.p
            W = S + 2
            Z = ph.sb([128, W], F32)
            BGr = ph.sb([128, S], F32)
            t1 = ph.sb([128, S], F32)
            yo = ph.sb([128, S], BF16)
            cw = ph.sb([128, 12], F32)
            p.dma("sp", "cw", lambda e: e.dma_start(out=cw[:], in_=convw_d[:, :]), writes=["cw"])
            p.dve(lambda e: e.memset(Z[:, 0:1], 0.0), writes=["ZL"])
            p.dve(lambda e: e.memset(Z[:, W - 1:W], 0.0), writes=["ZR"])
            for c in range(4):
                p.dma("sp", "z", lambda e, c=c: e.dma_start(out=Z[:, 1:1 + S], in_=PT_d[c, :, :]), writes=["Z"])
                p.dma("sp", "bg", lambda e, c=c: e.dma_start(out=BGr[:], in_=BG_d[c, :, :]), writes=["BGr"])
                p.dve(lambda e, c=c: e.tensor_scalar(t1[:], Z[:, 1:1 + S], cw[:, c * 3 + 1:c * 3 + 2], None, op0=ALU.mult), reads=["Z", "cw"], writes=["t1a"])
                p.dve(lambda e, c=c: e.scalar_tensor_tensor(t1[:], Z[:, 0:S], cw[:, c * 3:c * 3 + 1], t1[:], op0=ALU.mult, op1=ALU.add),
                      reads=["Z", "ZL", "cw", "t1a"], writes=["t1b"])
                p.dve(lambda e, c=c: e.scalar_tensor_tensor(t1[:], Z[:, 2:2 + S], cw[:, c * 3 + 2:c * 3 + 3], t1[:], op0=ALU.mult, op1=ALU.add),
                      reads=["Z", "ZR", "cw", "t1b"], writes=["t1c"])
                p.pool(lambda e: e.tensor_tensor(yo[:], t1[:], BGr[:], ALU.mult), reads=["t1c", "BGr"], writes=["yo"])
                p.dma("sp", "yo", lambda e, c=c: e.dma_start(out=MIX_d[4 + c, :, :], in_=yo[:]), reads=["yo"], writes=["MIXd"])
            ph.done()
            wout_phase(wout1_d, 1)

        if tail_only:
            router_moe(1)
        else:
            if stage >= 1:
                layer0_mixer()
            if stage >= 2 and stage < 20:
                router_moe(0)
            if stage == 9:
                router_moe(1)
                layer1_mixer()
            if stage >= 3 and stage < 9 and stage != 5:
                layer1_mixer()
            if stage >= 4 and stage < 9 and stage != 6:
                router_moe(1)
        if stage == 12:
            ph = Phase(nc)
            big = ph.sb([128, 4096], F32)
            for i_ in range(4000):
                ph.p.dve(lambda e: e.memset(big[:], 1.0), writes=["big"])
            ph.done()
        if stage == 6:
            for _ in range(3):
                ph = Phase(nc)
                ph.p.dve(lambda e: e.memset(EPSB[:], EPS), writes=["epsb"])
                ph.done()
        store_phase()
    return nc


def _perm64():
    d = np.arange(64)
    return np.where((d % 32) < 16, d + 16, d - 16)


def prep_inputs(inputs):
    f = lambda a: np.ascontiguousarray(np.asarray(a, dtype=np.float32))
    I = {k: np.asarray(v) for k, v in inputs.items()}
    shared = {}
    shared["ident"] = np.eye(128, dtype=np.float32)
    shared["mod_w"] = f(I["mod_w"])
    shared["mod_bT"] = f(I["mod_b"].reshape(2, 48, 128).transpose(2, 0, 1).reshape(128, 96))
    ng = np.stack([I["norm1_g"], I["norm2_g"]], 0)
    shared["norm_g"] = f(ng.reshape(2, 2, 8, 128).transpose(3, 0, 1, 2).reshape(128, 32))
    w_in0 = I["even_w_in"][0]
    pi = _perm64()
    qcols, qpcols = [], []
    for j in range(4):
        for h in (j, j + 4):
            qcols.append(h * 64 + np.arange(64))
            qpcols.append(h * 64 + pi)
    qcols = np.concatenate(qcols)
    qpcols = np.concatenate(qpcols)
    kcols = 512 + np.arange(128)
    kpcols = 512 + np.concatenate([pi, 64 + pi])
    pcols = 768 + np.arange(512)
    allc = np.concatenate([qcols, kcols, pcols, qpcols, kpcols])
    shared["w_qkp"] = f(w_in0[:, allc])
    shared["w_v"] = f(w_in0[:, 640:768])
    qg = I["q_gain"][0]
    kg = I["k_gain"][0]
    d = np.arange(128) % 64
    shared["gains"] = f(np.stack([qg[d], qg[pi[d]], kg[d], kg[pi[d]]], 1))
    t = np.arange(S)
    row = (t // 64).astype(np.float32)
    col = (t % 64).astype(np.float32)
    inv = (10000.0 ** (-np.arange(0, 16, dtype=np.float32) * 2 / 32.0)).astype(np.float32)
    cos_t = np.zeros((128, S), np.float32)
    sin_t = np.zeros((128, S), np.float32)
    for pp in range(128):
        dd = pp % 64
        jj = dd % 16
        pos = row if dd < 32 else col
        ang = (pos * inv[jj]).astype(np.float32)
        sgn = -1.0 if (dd % 32) < 16 else 1.0
        cos_t[pp] = np.cos(ang)
        sin_t[pp] = sgn * np.sin(ang)
    shared["cos_t"] = cos_t
    shared["sin_t"] = sin_t
    shared["pool_wT"] = f(I["pool_w"][0].transpose(1, 0, 2).reshape(128, 512))
    shared["pool_sc"] = f(I["pool_scale"][0].reshape(4, 128).T)
    edge = np.zeros((128, 4, 16), np.float32)
    for g, w in enumerate((2, 4, 8, 16)):
        for jx in range(16):
            tpos = jx if jx < 8 else S - 16 + jx
            lo = max(tpos - w // 2, 0)
            hi = min(tpos + w - w // 2, S)
            edge[:, g, jx] = 1.0 / (hi - lo)
    shared["pool_edge"] = edge.reshape(128, 64)
    w_out0 = I["even_w_out"][0]
    rows = []
    for c in range(4):
        for h in (c, c + 4):
            rows.append(h * 64 + np.arange(64))
    rows.append(512 + np.arange(512))
    shared["w_out0"] = f(w_out0[np.concatenate(rows), :])
    shared["w_out1"] = f(I["odd_w_out"][0])
    shared["w_in1"] = f(I["odd_w_in"][0])
    shared["sg_gain_b"] = f(np.broadcast_to(I["sg_gain"][0].reshape(1, 512), (128, 512)))
    shared["sg_wT"] = f(I["sg_w"][0].transpose(2, 0, 1).reshape(128, 512))
    shared["sg_b_b"] = f(np.broadcast_to(I["sg_b"][0].reshape(1, 512), (128, 512)))
    shared["conv_wT"] = f(I["conv_w"][0][:, 0, :].reshape(3, 4, 128).transpose(2, 1, 0).reshape(128, 12))
    shared["rw"] = f(np.concatenate([I["router_g_w"], I["router_e_w"]], axis=2))
    rb = np.concatenate([I["router_g_b"], I["router_e_b"]], axis=1)
    shared["rb_b"] = f(np.broadcast_to(np.tile(rb[:, None, :], (1, 4, 1)).reshape(1, 160), (128, 160)))
    shared["w_gate"] = f(I["w_gate"])
    shared["w_up"] = f(I["w_up"])
    shared["w_down"] = f(I["w_down"])
    sel = np.zeros((32, 16, 128), np.float32)
    for ex in range(16):
        sel[ex, ex, :] = 1.0
        sel[16 + ex, ex, :] = 1.0
    shared["sel"] = sel.reshape(32, 2048)
    in_maps = []
    for b in range(NCORES):
        m = dict(shared)
        m["x"] = f(I["x"][b])
        m["ctx"] = f(I["ctx"][b])
        cv = np.stack([I["c"][b], I["c_ctx"]], 0)
        m["cvec"] = f(cv.reshape(2, 8, 128).transpose(2, 1, 0).reshape(128, 16))
        in_maps.append(m)
    return in_maps


_NC_CACHE = {}


def kernel(**inputs):
    in_maps = prep_inputs(inputs)
    if "nc" not in _NC_CACHE:
        _NC_CACHE["nc"] = (build(stage=3), build(tail_only=True))
    nc1, nc2 = _NC_CACHE["nc"]
    res = run_bass_kernel_spmd(nc1, in_maps, core_ids=list(range(NCORES)))
    for b in range(NCORES):
        in_maps[b]["x"] = np.ascontiguousarray(np.asarray(res.results[b]["out"], dtype=np.float32))
    res = run_bass_kernel_spmd(nc2, in_maps, core_ids=list(range(NCORES)))
    out = np.stack([np.asarray(r["out"]) for r in res.results], axis=0)
    return out.astype(np.float32)
```
